# Optimizing a Trainium2 kernel written in Bass

```python
import math
import jax
import jax.numpy as jnp
from jax import lax
import numpy as np

D_MODEL = 1024
BATCH = 4
SEQ = 8192
DEPTH = 1

CTX_LEN = 256
GRID_W = 64
CHUNK = 128
ROWS_PER_CHUNK = CHUNK // GRID_W
MIX_W = D_MODEL
S5_W = D_MODEL // 4
GM_W = MIX_W - S5_W
GM_HEAD_DIM = 128
GM_HEADS = GM_W // GM_HEAD_DIM
S5_GROUP = 16
S5_GROUPS = S5_W // S5_GROUP
S5_STATE = 64
IN_COLS = S5_W + 2 * GM_W
N_EXPERTS = 16
CAPACITY_FACTOR = 2
D_FF = 2816
ALPHA = (2.0 * DEPTH) ** 0.25
BETA = (8.0 * DEPTH) ** -0.25
LN_EPS = 1e-6

kernel_name = 'hybrid_gmlp_s5_ecmoe_prefix_dit_block'


def layer_norm(x, g=None, b=None):
    xf = x.astype(jnp.float32)
    mu = jnp.mean(xf, axis=-1, keepdims=True)
    var = jnp.mean(jnp.square(xf - mu), axis=-1, keepdims=True)
    y = (xf - mu) * lax.rsqrt(var + LN_EPS)
    if g is not None:
        y = y * g.astype(jnp.float32) + b.astype(jnp.float32)
    return y.astype(x.dtype)


def modulate(x, shift, scale):
    return layer_norm(x) * (1 + scale) + shift


def chunk_mlp(u, v, w_s, b_s, n_chunks):
    bn, length, _ = u.shape
    u = u.reshape(bn, n_chunks, CHUNK, GM_HEADS, GM_HEAD_DIM)
    v = layer_norm(v.reshape(bn, n_chunks, CHUNK, GM_HEADS, GM_HEAD_DIM))
    mixed = jnp.einsum('gqk,bnkgc->bnqgc', w_s, v) + jnp.swapaxes(b_s, 0, 1)[:, :, None]
    return (u * mixed).reshape(bn, length, GM_W)


def diag_scan(lam_bar, drive, s0=None):
    length = drive.shape[1]
    a = jnp.broadcast_to(lam_bar, (1, length) + lam_bar.shape)

    def combine(e1, e2):
        a1, b1 = e1
        a2, b2 = e2
        return a1 * a2, a2 * b1 + b2

    a_cum, s = lax.associative_scan(combine, (a, drive), axis=1)
    if s0 is not None:
        s = s + a_cum * s0[:, None]
    return s


def s5_direction(u_lat, u_ctx, a_re, a_im, log_step, b_re, b_im, c_re, c_im, need_ctx):
    f32 = jnp.float32
    lam = lax.complex(a_re.astype(f32), a_im.astype(f32))
    lam_bar = jnp.exp(lam * jnp.exp(log_step.astype(f32))[:, None])
    b_bar = ((lam_bar - 1.0) / lam)[:, :, None] * lax.complex(b_re.astype(f32), b_im.astype(f32))
    c_mat = lax.complex(c_re.astype(f32), c_im.astype(f32))

    def drive(u):
        return jnp.einsum('blgh,gph->blgp', u.astype(f32).astype(jnp.complex64), b_bar)

    def readout(s):
        return jnp.real(jnp.einsum('blgp,ghp->blgh', s, c_mat))

    s_ctx = diag_scan(lam_bar, drive(u_ctx))
    s_lat = diag_scan(lam_bar, drive(u_lat), s_ctx[:, -1])
    y_ctx = readout(s_ctx) if need_ctx else None
    return readout(s_lat), y_ctx


def s5_block(u_lat, u_ctx, a_re, a_im, log_step, b_re, b_im, c_re, c_im, d, w_glu, b_glu, need_ctx):
    dtype = u_lat.dtype

    def groups(u):
        return u.reshape(u.shape[0], u.shape[1], S5_GROUPS, S5_GROUP)

    def flip(t, rev):
        return jnp.flip(t, axis=1) if rev else t

    def glu(y):
        g = jax.nn.gelu(y.reshape(y.shape[0], y.shape[1], S5_W).astype(dtype))
        return g * jax.nn.sigmoid(g @ w_glu + b_glu)

    ug, ucg = groups(u_lat), groups(u_ctx)
    d32 = d.astype(jnp.float32)
    y = d32 * ug.astype(jnp.float32)
    yc = d32 * ucg.astype(jnp.float32) if need_ctx else None
    for direction in range(2):
        rev = direction == 1
        yd, ycd = s5_direction(flip(ug, rev), flip(ucg, rev), a_re[direction], a_im[direction],
                               log_step[direction], b_re[direction], b_im[direction],
                               c_re[direction], c_im[direction], need_ctx)
        y = y + flip(yd, rev)
        if need_ctx:
            yc = yc + flip(ycd, rev)
    out_ctx = glu(yc) if need_ctx else None
    return glu(y), out_ctx


def ec_moe(h, w_router, w_gate, w_up, w_down):
    bn, length, _ = h.shape
    cap = CAPACITY_FACTOR * length // N_EXPERTS
    aff = jax.nn.softmax((h @ w_router).astype(jnp.float32), axis=-1)
    gate, idx = lax.top_k(jnp.swapaxes(aff, 1, 2), cap)
    bidx = jnp.arange(bn)[:, None, None]
    xg = h[bidx, idx]
    hid = jax.nn.silu(jnp.einsum('becd,edf->becf', xg, w_gate)) * jnp.einsum('becd,edf->becf', xg, w_up)
    out = jnp.einsum('becf,efd->becd', hid, w_down) * gate[..., None].astype(h.dtype)
    return jnp.zeros_like(h).at[bidx, idx].add(out)


def hybrid_layer(x, xc, mod_lat, mod_ctx, n_chunks_lat, w_in, gm_ws, gm_bs, s5_a_re, s5_a_im, s5_log_step,
                 s5_b_re, s5_b_im, s5_c_re, s5_c_im, s5_d, s5_w_glu, s5_b_glu, w_out, ln1_g, ln1_b,
                 w_router, moe_w_gate, moe_w_up, moe_w_down, ln2_g, ln2_b, update_ctx):
    sh1, sc1, g1, sh2, sc2, g2 = jnp.split(mod_lat, 6, axis=-1)
    csh1, csc1, cg1, csh2, csc2, cg2 = jnp.split(mod_ctx, 6, axis=-1)
    z = modulate(x, sh1, sc1) @ w_in
    zc = modulate(xc, csh1, csc1) @ (w_in if update_ctx else w_in[:, :S5_W])
    s5_lat, s5_ctx = s5_block(z[..., :S5_W], zc[..., :S5_W], s5_a_re, s5_a_im, s5_log_step, s5_b_re, s5_b_im,
                              s5_c_re, s5_c_im, s5_d, s5_w_glu, s5_b_glu, update_ctx)
    gm_lat = chunk_mlp(jax.nn.gelu(z[..., S5_W:S5_W + GM_W]), jax.nn.gelu(z[..., S5_W + GM_W:]),
                       gm_ws, gm_bs, n_chunks_lat)
    mix = jnp.concatenate([s5_lat, gm_lat], axis=-1) @ w_out
    x_new = layer_norm(ALPHA * x + g1 * mix, ln1_g, ln1_b)
    moe = ec_moe(modulate(x_new, sh2, sc2), w_router, moe_w_gate, moe_w_up, moe_w_down)
    x_new = layer_norm(ALPHA * x_new + g2 * moe, ln2_g, ln2_b)
    if update_ctx:
        gm_ctx = chunk_mlp(jax.nn.gelu(zc[..., S5_W:S5_W + GM_W]), jax.nn.gelu(zc[..., S5_W + GM_W:]),
                           gm_ws, gm_bs, xc.shape[1] // CHUNK)
        mix_c = jnp.concatenate([s5_ctx, gm_ctx], axis=-1) @ w_out
        xc = layer_norm(ALPHA * xc + cg1 * mix_c, ln1_g, ln1_b)
        moe_c = ec_moe(modulate(xc, csh2, csc2), w_router, moe_w_gate, moe_w_up, moe_w_down)
        xc = layer_norm(ALPHA * xc + cg2 * moe_c, ln2_g, ln2_b)
    return x_new, xc


def setup_inputs(seed: int = 0) -> dict:
    key = jax.random.key(seed)
    ks = jax.random.split(key, 32)
    f32 = jnp.float32

    def nrm(k, shape, scale=1.0):
        return jax.random.normal(k, shape, f32) * scale

    L2 = (DEPTH, 2, S5_GROUPS)
    return {
        'x': nrm(ks[0], (BATCH, SEQ, D_MODEL)),
        'c': nrm(ks[1], (BATCH, D_MODEL)),
        'ctx': nrm(ks[2], (BATCH, CTX_LEN, D_MODEL)),
        'c_ctx': nrm(ks[3], (D_MODEL,)),
        'w_ada': nrm(ks[4], (DEPTH, D_MODEL, 6 * D_MODEL), D_MODEL ** -0.5),
        'b_ada': nrm(ks[5], (DEPTH, 6 * D_MODEL), 0.02),
        'w_in': nrm(ks[6], (DEPTH, D_MODEL, IN_COLS), D_MODEL ** -0.5),
        'gm_ws': nrm(ks[7], (DEPTH, GM_HEADS, CHUNK, CHUNK), CHUNK ** -0.5),
        'gm_bs': 1.0 + nrm(ks[8], (DEPTH, GM_HEADS, CHUNK), 0.02),
        's5_a_re': -0.5 + nrm(ks[9], L2 + (S5_STATE,), 0.01),
        's5_a_im': math.pi * jnp.arange(S5_STATE, dtype=f32) + nrm(ks[10], L2 + (S5_STATE,), 0.01),
        's5_log_step': jax.random.uniform(ks[11], L2, f32, math.log(1e-3), math.log(1e-1)),
        's5_b_re': nrm(ks[12], L2 + (S5_STATE, S5_GROUP), (2.0 * S5_GROUP) ** -0.5),
        's5_b_im': nrm(ks[13], L2 + (S5_STATE, S5_GROUP), (2.0 * S5_GROUP) ** -0.5),
        's5_c_re': nrm(ks[14], L2 + (S5_GROUP, S5_STATE), (2.0 * S5_STATE) ** -0.5),
        's5_c_im': nrm(ks[15], L2 + (S5_GROUP, S5_STATE), (2.0 * S5_STATE) ** -0.5),
        's5_d': nrm(ks[16], (DEPTH, S5_GROUPS, S5_GROUP)),
        's5_w_glu': nrm(ks[17], (DEPTH, S5_W, S5_W), S5_W ** -0.5),
        's5_b_glu': nrm(ks[18], (DEPTH, S5_W), 0.02),
        'w_out': nrm(ks[19], (DEPTH, MIX_W, D_MODEL), BETA * MIX_W ** -0.5),
        'ln1_g': 1.0 + nrm(ks[20], (DEPTH, D_MODEL), 0.02),
        'ln1_b': nrm(ks[21], (DEPTH, D_MODEL), 0.02),
        'w_router': nrm(ks[22], (DEPTH, D_MODEL, N_EXPERTS), D_MODEL ** -0.5),
        'moe_w_gate': nrm(ks[23], (DEPTH, N_EXPERTS, D_MODEL, D_FF), D_MODEL ** -0.5),
        'moe_w_up': nrm(ks[24], (DEPTH, N_EXPERTS, D_MODEL, D_FF), D_MODEL ** -0.5),
        'moe_w_down': nrm(ks[25], (DEPTH, N_EXPERTS, D_FF, D_MODEL), BETA * D_FF ** -0.5),
        'ln2_g': 1.0 + nrm(ks[26], (DEPTH, D_MODEL), 0.02),
        'ln2_b': nrm(ks[27], (DEPTH, D_MODEL), 0.02),
    }


def reference(x, c, ctx, c_ctx, w_ada, b_ada, w_in, gm_ws, gm_bs, s5_a_re, s5_a_im, s5_log_step,
              s5_b_re, s5_b_im, s5_c_re, s5_c_im, s5_d, s5_w_glu, s5_b_glu, w_out, ln1_g, ln1_b,
              w_router, moe_w_gate, moe_w_up, moe_w_down, ln2_g, ln2_b):
    rows = x.shape[1] // GRID_W
    n_chunks_lat = rows // ROWS_PER_CHUNK
    xc = ctx
    for l in range(DEPTH):
        mod_lat = (jax.nn.silu(c) @ w_ada[l] + b_ada[l])[:, None, :]
        mod_ctx = (jax.nn.silu(c_ctx) @ w_ada[l] + b_ada[l])[None, None, :]
        x, xc = hybrid_layer(x, xc, mod_lat, mod_ctx, n_chunks_lat, w_in[l], gm_ws[l], gm_bs[l],
                             s5_a_re[l], s5_a_im[l], s5_log_step[l], s5_b_re[l], s5_b_im[l],
                             s5_c_re[l], s5_c_im[l], s5_d[l], s5_w_glu[l], s5_b_glu[l], w_out[l],
                             ln1_g[l], ln1_b[l], w_router[l], moe_w_gate[l], moe_w_up[l],
                             moe_w_down[l], ln2_g[l], ln2_b[l], l < DEPTH - 1)
    return x
```

```python
import contextlib
import math
import numpy as np
import ml_dtypes
import concourse.bass as bass
import concourse.mybir as mybir
from concourse.bass_utils import run_bass_kernel_spmd

F32 = mybir.dt.float32
BF16 = mybir.dt.bfloat16
I32 = mybir.dt.int32
AF = mybir.ActivationFunctionType
ALU = mybir.AluOpType
AX = mybir.AxisListType

D = 1024
SEQ = 8192
CTX = 256
NT = SEQ // 128
NTC = CTX // 128
TOT = SEQ + CTX
DFF = 2816
NFT = DFF // 128
NE = 8
CAP = 1024
ALPHA = 2.0 ** 0.25
EPS = 1e-6
TWO_PI = 6.283185
SEG = 1024

ENGS = ("sync", "scalar", "vector", "gpsimd", "tensor")
DMA_K = 8


class Prog:
    def __init__(self, nc, stack):
        self.nc = nc
        self.eng = {"sync": nc.sync, "scalar": nc.scalar, "vector": nc.vector,
                    "gpsimd": nc.gpsimd, "tensor": nc.tensor}
        self.sems = {}
        for e in ENGS:
            self.sems["c" + e] = stack.enter_context(nc.semaphore("c" + e))
        for e in ("sync", "scalar", "gpsimd"):
            for i in range(DMA_K):
                n = "d%s%d" % (e, i)
                self.sems[n] = stack.enter_context(nc.semaphore(n))
        self.ccnt = {e: 0 for e in ENGS}
        self.dcnt = {e: 0 for e in ENGS}
        self.lastw = {}
        self.readers = {}
        self.known = {e: {} for e in ENGS}
        self.latest = {}

    def _need(self, eng, tok):
        if tok is None:
            return
        name, val = tok
        if name == "c" + eng and eng in ("tensor", "sync"):
            return
        if self.known[eng].get(name, 0) >= val:
            return
        self.known[eng][name] = val
        self.eng[eng].wait_ge(self.sems[name], val)

    def op(self, eng, fn, reads=(), writes=(), dma=False, sem_inc=None):
        for k in reads:
            self._need(eng, self.lastw.get(k))
        for k in writes:
            self._need(eng, self.lastw.get(k))
            for t in self.readers.get(k, ()):
                self._need(eng, t)
        if dma:
            i = self.dcnt[eng]
            self.dcnt[eng] += 1
            name = "d%s%d" % (eng, i % DMA_K)
            inc = 16 if sem_inc is None else sem_inc
            val = self.latest.get(name, 0) + inc
            if i >= DMA_K:
                self._need(eng, (name, self.latest.get(name, 0)))
        else:
            self.ccnt[eng] += 1
            name = "c" + eng
            val = self.ccnt[eng]
            inc = 1
        tok = (name, val)
        ins = fn(self.eng[eng])
        ins.then_inc(self.sems[name], inc)
        self.latest[name] = val
        for k in writes:
            self.lastw[k] = tok
            self.readers[k] = []
        for k in reads:
            if k not in writes:
                self.readers.setdefault(k, []).append(tok)
        return tok

    def barrier(self):
        toks = list(self.latest.items())
        for e in ENGS:
            for t in toks:
                self._need(e, t)
        self.lastw = {}
        self.readers = {}


def build(debug=0):
    nc = bass.Bass("TRN2", target_bir_lowering=False)

    def din(name, shape, dt=F32):
        return nc.dram_tensor(name, list(shape), dt, kind="ExternalInput").ap()

    def dscr(name, shape, dt=F32):
        return nc.dram_tensor(name, list(shape), dt).ap()

    xs = din("xs", [TOT, D])
    cT = din("cT", [128, 8, 2])
    w_ada = din("w_ada", [D, 6 * D])
    bada_fm = din("bada_fm", [128, 16])
    bada_row = din("bada_row", [1, 4 * D])
    w_in_s5 = din("w_in_s5", [D, 512])
    w_in_u = din("w_in_u", [D, 768])
    w_in_v = din("w_in_v", [D, 768])
    gm_wsT = din("gm_wsT", [128, 6, 128])
    gm_bs_row = din("gm_bs_row", [1, 768])
    are_row = din("are_row", [1, 2048])
    aim_row = din("aim_row", [1, 2048])
    ls_row = din("ls_row", [1, 2048])
    are_pp = din("are_pp", [128, 16])
    aim_pp = din("aim_pp", [128, 16])
    ls_pp = din("ls_pp", [128, 16])
    WBre_raw = din("WBre_raw", [128, 2048])
    WBim_raw = din("WBim_raw", [128, 2048])
    WCre_raw = din("WCre_raw", [128, 2048])
    WCim_raw = din("WCim_raw", [128, 2048])
    dpp = din("dpp", [128, 4])
    w_glu_pad = din("w_glu_pad", [512, 512])
    b_glu_pp = din("b_glu_pp", [128, 4])
    w_out_pad = din("w_out_pad", [1280, D])
    ln1_g = din("ln1_g", [1, D])
    ln1_b = din("ln1_b", [1, D])
    ln2_g = din("ln2_g", [1, D])
    ln2_b = din("ln2_b", [1, D])
    w_router = din("w_router", [D, 16])
    if debug in (0, 3):
        wg = din("wg", [NE, D, DFF])
        wu = din("wu", [NE, D, DFF])
        wd = din("wd", [NE, DFF, D])
    ident_bf_d = din("ident_bf", [128, 128], BF16)
    ident_f_d = din("ident_f", [128, 128])
    tri_d = din("tri", [128, 128], BF16)
    ones_d = din("ones", [128, 128], BF16)
    iota_row_d = din("iota_row", [1, 1024])
    slot_pp_d = din("slot_pp", [128, 8])
    half_sel = din("half_sel", [128, 16])
    tokidx_d = din("tokidx", [128, NT // 2], I32)

    out_d = nc.dram_tensor("out", [SEQ // 2, D], F32, kind="ExternalOutput").ap()
    okind = {"kind": "ExternalOutput"} if debug else {}
    xnew_d = nc.dram_tensor("xnew_s", [SEQ, D], F32, **okind).ap()
    hrow_d = nc.dram_tensor("hrow_s", [SEQ, D], BF16, **okind).ap()
    catT_d = dscr("catT_s", [10, 128, SEQ], BF16)
    yf_d = dscr("yf_s", [4, 128, SEQ], F32)
    aff_d = nc.dram_tensor("aff_s", [SEQ, 16], F32, **okind).ap()
    if debug:
        dbg_idx = nc.dram_tensor("dbg_idx", [128, NE * 8], I32, kind="ExternalOutput").ap()
        dbg_gate = nc.dram_tensor("dbg_gate", [128, NE * 8], F32, kind="ExternalOutput").ap()
    cin_d = dscr("cin_s", [NE * NT, 128], F32)
    moe_d = nc.dram_tensor("moe_s", [SEQ, D], F32, **({"kind": "ExternalOutput"} if debug == 3 else {})).ap()
    moe_r = dscr("moe_r", [SEQ, D], F32)

    with contextlib.ExitStack() as gst:
        P = Prog(nc, gst)

        _uid = [0]

        def sb(st, name, shape, dt=F32):
            _uid[0] += 1
            return st.enter_context(nc.sbuf_tensor("%s_%d" % (name, _uid[0]), list(shape), dt))

        def ps(st, name, shape, dt=F32):
            _uid[0] += 1
            return st.enter_context(nc.psum_tensor("%s_%d" % (name, _uid[0]), list(shape), dt))

        def V(fn, r=(), w=()):
            return P.op("vector", fn, r, w)

        def A(fn, r=(), w=()):
            return P.op("scalar", fn, r, w)

        def G(fn, r=(), w=()):
            return P.op("gpsimd", fn, r, w)

        def T(fn, r=(), w=()):
            return P.op("tensor", fn, r, w)

        def DS(fn, r=(), w=()):
            return P.op("sync", fn, r, w, dma=True)

        def DG(fn, r=(), w=()):
            return P.op("gpsimd", fn, r, w, dma=True)

        def bc(ap_, shape):
            return ap_.to_broadcast(list(shape))

        ident_bf = sb(gst, "ident_bf_t", [128, 128], BF16)
        ident_f = sb(gst, "ident_f_t", [128, 128])
        ones_bf = sb(gst, "ones_t", [128, 128], BF16)
        tri_bf = sb(gst, "tri_t", [128, 128], BF16)
        iota_r = sb(gst, "iota_r", [128, 1024])
        modfm = sb(gst, "modfm", [128, 2, 8, 2])
        modrow = sb(gst, "modrow", [128, 4, D])
        lnrow = sb(gst, "lnrow", [128, 4, D])
        DS(lambda e: e.dma_start(out=ident_bf[:], in_=ident_bf_d), w=["ident_bf"])
        DS(lambda e: e.dma_start(out=ident_f[:], in_=ident_f_d), w=["ident_f"])
        DS(lambda e: e.dma_start(out=ones_bf[:], in_=ones_d), w=["ones"])
        DS(lambda e: e.dma_start(out=tri_bf[:], in_=tri_d), w=["tri"])
        DS(lambda e: e.dma_start(out=iota_r[:], in_=bc(iota_row_d, [128, 1024])), w=["iota_r"])
        for i, a in enumerate((ln1_g, ln1_b, ln2_g, ln2_b)):
            DS(lambda e, i=i, a=a: e.dma_start(out=lnrow[:, i, :], in_=bc(a, [128, D])), w=["lnrow"])

        with contextlib.ExitStack() as st:
            ct = sb(st, "ct", [128, 8, 2])
            sc = sb(st, "sc", [128, 8, 2])
            scb = sb(st, "scb", [128, 8, 128])
            wa = sb(st, "wa", [128, 8, D])
            bfm = sb(st, "bfm", [128, 16])
            brow = sb(st, "brow", [128, 4 * D])
            pfm = ps(st, "pfm", [128, 8, 2])
            prow = ps(st, "prow", [128, D])
            DS(lambda e: e.dma_start(out=ct[:], in_=cT), w=["ct"])
            DS(lambda e: e.dma_start(out=bfm[:], in_=bada_fm), w=["bfm"])
            DS(lambda e: e.dma_start(out=brow[:], in_=bc(bada_row, [128, 4 * D])), w=["brow"])
            A(lambda e: e.activation(out=sc[:], in_=ct[:], func=AF.Silu), ["ct"], ["sc"])
            V(lambda e: e.tensor_copy(out=scb[:], in_=bc(sc[:, :, 0:1], [128, 8, 128])), ["sc"], ["scb"])
            wav = w_ada.rearrange("(kt p) f -> p kt f", p=128)
            for ch in range(6):
                DS(lambda e, ch=ch: e.dma_start(out=wa[:], in_=wav[:, :, ch * D:(ch + 1) * D]), w=["wa"])
                if ch < 2:
                    for ft in range(8):
                        for kt in range(8):
                            T(lambda e, ft=ft, kt=kt: e.matmul(pfm[:, ft, :], lhsT=wa[:, kt, ft * 128:(ft + 1) * 128],
                                                               rhs=sc[:, kt, :], start=(kt == 0), stop=(kt == 7)),
                              ["wa", "sc"], ["pfm"])
                    V(lambda e, ch=ch: e.tensor_tensor(out=modfm[:, ch, :, :], in0=pfm[:],
                                                       in1=bc(bfm[:, ch * 8:(ch + 1) * 8].unsqueeze(2), [128, 8, 2]),
                                                       op=ALU.add), ["pfm", "bfm"], ["modfm"])
                else:
                    for nh in range(2):
                        for kt in range(8):
                            T(lambda e, nh=nh, kt=kt: e.matmul(prow[:, nh * 512:(nh + 1) * 512], lhsT=scb[:, kt, :],
                                                               rhs=wa[:, kt, nh * 512:(nh + 1) * 512],
                                                               start=(kt == 0), stop=(kt == 7)),
                              ["wa", "scb"], ["prow"])
                    V(lambda e, ch=ch: e.tensor_tensor(out=modrow[:, ch - 2, :], in0=prow[:],
                                                       in1=brow[:, (ch - 2) * D:(ch - 1) * D], op=ALU.add),
                      ["prow", "brow"], ["modrow"])
            V(lambda e: e.tensor_scalar_add(out=modfm[:, 1, :, :], in0=modfm[:, 1, :, :], scalar1=1.0), ["modfm"], ["modfm"])
            V(lambda e: e.tensor_scalar_add(out=modrow[:, 2, :], in0=modrow[:, 2, :], scalar1=1.0), ["modrow"], ["modrow"])
            P.barrier()

        with contextlib.ExitStack() as mst:
            U = sb(mst, "U", [128, 4, TOT], BF16)
            wst_ = contextlib.ExitStack()
            wS = sb(wst_, "wS", [128, 8, 512], BF16)
            wU = sb(wst_, "wU", [128, 8, 768], BF16)
            wV = sb(wst_, "wV", [128, 8, 768], BF16)
            wsT = sb(wst_, "wsT", [128, 6, 128], BF16)
            bsrow = sb(wst_, "bsrow", [128, 768])
            with contextlib.ExitStack() as st:
                stg = sb(st, "stg", [128, 8, 768])
                for (src, dst, n, nm) in ((w_in_s5, wS, 512, "wS"), (w_in_u, wU, 768, "wU"), (w_in_v, wV, 768, "wV")):
                    DS(lambda e, src=src, n=n: e.dma_start(out=stg[:, :, 0:n], in_=src.rearrange("(kt p) f -> p kt f", p=128)),
                       w=["stg"])
                    V(lambda e, dst=dst, n=n: e.tensor_copy(out=dst[:], in_=stg[:, :, 0:n]), ["stg"], [nm])
                DS(lambda e: e.dma_start(out=stg[:, 0:6, 0:128], in_=gm_wsT), w=["stg"])
                V(lambda e: e.tensor_copy(out=wsT[:], in_=stg[:, 0:6, 0:128]), ["stg"], ["wsT"])
                DS(lambda e: e.dma_start(out=bsrow[:], in_=bc(gm_bs_row, [128, 768])), w=["bsrow"])
                P.barrier()

            with contextlib.ExitStack() as st:
                xt = [sb(st, "xt%d" % i, [128, D]) for i in range(2)]
                xn = sb(st, "xn", [128, D], BF16)
                xmT = sb(st, "xmT", [128, 8, 128], BF16)
                stats = sb(st, "stats", [128, 2, 6])
                mv = sb(st, "mv", [128, 2])
                rstd = sb(st, "rstd", [128, 1])
                uT = sb(st, "uT", [128, 6, 128])
                vv = sb(st, "vv", [128, 6, 128])
                vc = sb(st, "vc", [128, 6, 128])
                vln = sb(st, "vln", [128, 6, 128], BF16)
                st6 = sb(st, "st6", [128, 6, 6])
                mv6 = sb(st, "mv6", [128, 6, 2])
                rs6 = sb(st, "rs6", [128, 6])
                gmt = sb(st, "gmt", [128, 6, 128])
                gmb = [sb(st, "gmb%d" % i, [128, 6, 128], BF16) for i in range(2)]
                pT = ps(st, "pT", [128, D], BF16)
                pS = ps(st, "pS", [128, 4, 128])
                pU = ps(st, "pU", [128, 8, 128])
                pV = ps(st, "pV", [128, D])
                pM = ps(st, "pM", [128, 8, 128])
                for t in range(NTC + NT):
                    lat = t >= NTC
                    col = 0 if lat else 1
                    x_ = xt[t % 2]
                    xk = "xt%d" % (t % 2)
                    DS(lambda e, t=t, x_=x_: e.dma_start(out=x_[:], in_=xs[t * 128:(t + 1) * 128, :]), w=[xk])
                    for c in range(2):
                        V(lambda e, c=c, x_=x_: e.bn_stats(out=stats[:, c, :], in_=x_[:, c * 512:(c + 1) * 512]), [xk], ["stats"])
                    V(lambda e: e.bn_aggr(out=mv[:], in_=stats[:].rearrange("p a b -> p (a b)")), ["stats"], ["mv"])
                    V(lambda e: e.tensor_scalar_add(out=rstd[:], in0=mv[:, 1:2], scalar1=EPS), ["mv"], ["rstd"])
                    A(lambda e: e.activation(out=rstd[:], in_=rstd[:], func=AF.Sqrt), ["rstd"], ["rstd"])
                    V(lambda e: e.reciprocal(out=rstd[:], in_=rstd[:]), ["rstd"], ["rstd"])
                    V(lambda e, x_=x_: e.tensor_scalar(out=xn[:], in0=x_[:], scalar1=mv[:, 0:1], scalar2=rstd[:, 0:1],
                                                       op0=ALU.subtract, op1=ALU.mult), [xk, "mv", "rstd"], ["xn"])
                    for kt in range(8):
                        T(lambda e, kt=kt: e.transpose(out=pT[:, kt * 128:(kt + 1) * 128], in_=xn[:, kt * 128:(kt + 1) * 128],
                                                       identity=ident_bf[:]), ["xn", "ident_bf"], ["pT"])
                    for kt in range(8):
                        A(lambda e, kt=kt, col=col: e.activation(out=xmT[:, kt, :], in_=pT[:, kt * 128:(kt + 1) * 128],
                                                                 func=AF.Identity, scale=modfm[:, 1, kt, col:col + 1],
                                                                 bias=modfm[:, 0, kt, col:col + 1]),
                          ["pT", "modfm"], ["xmT"])
                    for ct_ in range(4):
                        for kt in range(8):
                            T(lambda e, ct_=ct_, kt=kt: e.matmul(pS[:, ct_, :], lhsT=wS[:, kt, ct_ * 128:(ct_ + 1) * 128],
                                                                 rhs=xmT[:, kt, :], start=(kt == 0), stop=(kt == 7)),
                              ["wS", "xmT"], ["pS"])
                    V(lambda e, t=t: e.tensor_copy(out=U[:, :, t * 128:(t + 1) * 128], in_=pS[:]), ["pS"], ["U"])
                    if not lat:
                        continue
                    tl = t - NTC
                    for ct_ in range(6):
                        for kt in range(8):
                            T(lambda e, ct_=ct_, kt=kt: e.matmul(pU[:, ct_, :], lhsT=wU[:, kt, ct_ * 128:(ct_ + 1) * 128],
                                                                 rhs=xmT[:, kt, :], start=(kt == 0), stop=(kt == 7)),
                              ["wU", "xmT"], ["pU"])
                    A(lambda e: e.activation(out=uT[:], in_=pU[:, 0:6, :], func=AF.Gelu_apprx_tanh), ["pU"], ["uT"])
                    for (c0, c1) in ((0, 512), (512, 768)):
                        for kt in range(8):
                            T(lambda e, c0=c0, c1=c1, kt=kt: e.matmul(pV[:, c0:c1], lhsT=xmT[:, kt, :], rhs=wV[:, kt, c0:c1],
                                                                      start=(kt == 0), stop=(kt == 7)),
                              ["wV", "xmT"], ["pV"])
                    A(lambda e: e.activation(out=vv[:].rearrange("p a b -> p (a b)"), in_=pV[:, 0:768], func=AF.Gelu_apprx_tanh),
                      ["pV"], ["vv"])
                    for g in range(6):
                        V(lambda e, g=g: e.bn_stats(out=st6[:, g, :], in_=vv[:, g, :]), ["vv"], ["st6"])
                    for g in range(6):
                        V(lambda e, g=g: e.bn_aggr(out=mv6[:, g, :], in_=st6[:, g, :]), ["st6"], ["mv6"])
                    V(lambda e: e.tensor_scalar_add(out=rs6[:], in0=mv6[:, :, 1], scalar1=EPS), ["mv6"], ["rs6"])
                    A(lambda e: e.activation(out=rs6[:], in_=rs6[:], func=AF.Sqrt), ["rs6"], ["rs6"])
                    V(lambda e: e.reciprocal(out=rs6[:], in_=rs6[:]), ["rs6"], ["rs6"])
                    V(lambda e: e.tensor_tensor(out=vc[:], in0=vv[:], in1=bc(mv6[:, :, 0:1], [128, 6, 128]), op=ALU.subtract),
                      ["vv", "mv6"], ["vc"])
                    V(lambda e: e.tensor_tensor(out=vln[:], in0=vc[:], in1=bc(rs6[:].unsqueeze(2), [128, 6, 128]), op=ALU.mult),
                      ["vc", "rs6"], ["vln"])
                    for g in range(6):
                        T(lambda e, g=g: e.matmul(pM[:, g, :], lhsT=vln[:, g, :], rhs=wsT[:, g, :], start=True, stop=True),
                          ["vln", "wsT"], ["pM"])
                    V(lambda e: e.tensor_tensor(out=gmt[:], in0=pM[:, 0:6, :], in1=bsrow[:].rearrange("p (a b) -> p a b", a=6),
                                                op=ALU.add), ["pM", "bsrow"], ["gmt"])
                    gb = gmb[tl % 2]
                    gk = "gmb%d" % (tl % 2)
                    V(lambda e, gb=gb: e.tensor_tensor(out=gb[:], in0=gmt[:], in1=uT[:], op=ALU.mult), ["gmt", "uT"], [gk])
                    DS(lambda e, gb=gb, tl=tl: e.dma_start(out=catT_d[4:10, :, tl * 128:(tl + 1) * 128].rearrange("a p t -> p a t"),
                                                           in_=gb[:]), [gk], ["catT_gm"])
                P.barrier()
            wst_.close()

            with contextlib.ExitStack() as st:
                WB = sb(st, "WB", [128, 2, 2048], BF16)
                WC = sb(st, "WC", [128, 2, 2048], BF16)
                rho_pp = sb(st, "rho_pp", [128, 16])
                f_pp = sb(st, "f_pp", [128, 16])
                dsc = sb(st, "dsc", [128, 4])
                bglu = sb(st, "bglu", [128, 4])
                wglu = sb(st, "wglu", [128, 4, 512], BF16)
                for dr_ in range(2):
                  csl = slice(dr_ * 1024, (dr_ + 1) * 1024)
                  with contextlib.ExitStack() as s2:
                        r = {n: sb(s2, "r_" + n, [128, 1024]) for n in
                             ("are", "aim", "stp", "rho", "f", "y", "y2", "sn", "cs", "x", "yv", "den", "cr", "ci", "t1", "t2", "bre", "bim")}
                        ri_ = sb(s2, "r_int", [128, 1024], I32)
                        DS(lambda e: e.dma_start(out=r["are"][:], in_=bc(are_row[:, csl], [128, 1024])), w=["are"])
                        DS(lambda e: e.dma_start(out=r["aim"][:], in_=bc(aim_row[:, csl], [128, 1024])), w=["aim"])
                        DS(lambda e: e.dma_start(out=r["stp"][:], in_=bc(ls_row[:, csl], [128, 1024])), w=["stp"])
                        DS(lambda e: e.dma_start(out=r["bre"][:], in_=WBre_raw[:, csl]), w=["bre"])
                        DS(lambda e: e.dma_start(out=r["bim"][:], in_=WBim_raw[:, csl]), w=["bim"])

                        def vt(o, a, b, op):
                            V(lambda e: e.tensor_tensor(out=r[o][:], in0=r[a][:], in1=r[b][:], op=op), [a, b], [o])

                        def frac(o, i):
                            V(lambda e: e.tensor_copy(out=ri_[:], in_=r[i][:]), [i], ["rint"])
                            V(lambda e: e.tensor_copy(out=r["t1"][:], in_=ri_[:]), ["rint"], ["t1"])
                            vt(o, i, "t1", ALU.subtract)

                        A(lambda e: e.activation(out=r["stp"][:], in_=r["stp"][:], func=AF.Exp), ["stp"], ["stp"])
                        vt("rho", "are", "stp", ALU.mult)
                        A(lambda e: e.activation(out=r["rho"][:], in_=r["rho"][:], func=AF.Exp), ["rho"], ["rho"])
                        vt("f", "aim", "stp", ALU.mult)
                        V(lambda e: e.tensor_scalar_mul(out=r["f"][:], in0=r["f"][:], scalar1=1.0 / (2 * math.pi)), ["f"], ["f"])
                        frac("y", "f")
                        A(lambda e: e.activation(out=r["sn"][:], in_=r["y"][:], func=AF.Sin, scale=TWO_PI), ["y"], ["sn"])
                        V(lambda e: e.tensor_scalar_add(out=r["y2"][:], in0=r["y"][:], scalar1=0.25), ["y"], ["y2"])
                        frac("y2", "y2")
                        A(lambda e: e.activation(out=r["cs"][:], in_=r["y2"][:], func=AF.Sin, scale=TWO_PI), ["y2"], ["cs"])
                        vt("x", "rho", "cs", ALU.mult)
                        V(lambda e: e.tensor_scalar_add(out=r["x"][:], in0=r["x"][:], scalar1=-1.0), ["x"], ["x"])
                        vt("yv", "rho", "sn", ALU.mult)
                        vt("den", "are", "are", ALU.mult)
                        vt("t2", "aim", "aim", ALU.mult)
                        vt("den", "den", "t2", ALU.add)
                        V(lambda e: e.reciprocal(out=r["den"][:], in_=r["den"][:]), ["den"], ["den"])
                        vt("cr", "x", "are", ALU.mult)
                        vt("t2", "yv", "aim", ALU.mult)
                        vt("cr", "cr", "t2", ALU.add)
                        vt("cr", "cr", "den", ALU.mult)
                        vt("ci", "yv", "are", ALU.mult)
                        vt("t2", "x", "aim", ALU.mult)
                        vt("ci", "ci", "t2", ALU.subtract)
                        vt("ci", "ci", "den", ALU.mult)
                        vt("t1", "cr", "bre", ALU.mult)
                        vt("t2", "ci", "bim", ALU.mult)
                        V(lambda e: e.tensor_tensor(out=WB[:, 0, csl], in0=r["t1"][:], in1=r["t2"][:], op=ALU.subtract), ["t1", "t2"], ["WB"])
                        vt("t1", "cr", "bim", ALU.mult)
                        vt("t2", "ci", "bre", ALU.mult)
                        V(lambda e: e.tensor_tensor(out=WB[:, 1, csl], in0=r["t1"][:], in1=r["t2"][:], op=ALU.add), ["t1", "t2"], ["WB"])
                        DS(lambda e: e.dma_start(out=r["bre"][:], in_=WCre_raw[:, csl]), w=["bre"])
                        DS(lambda e: e.dma_start(out=r["bim"][:], in_=WCim_raw[:, csl]), w=["bim"])
                        V(lambda e: e.tensor_copy(out=WC[:, 0, csl], in_=r["bre"][:]), ["bre"], ["WC"])
                        V(lambda e: e.tensor_scalar_mul(out=WC[:, 1, csl], in0=r["bim"][:], scalar1=-1.0), ["bim"], ["WC"])

                        P.barrier()
                with contextlib.ExitStack() as s2:
                    pa = sb(s2, "pa", [128, 16])
                    pb = sb(s2, "pb", [128, 16])
                    pc = sb(s2, "pc", [128, 16])
                    DS(lambda e: e.dma_start(out=pa[:], in_=are_pp), w=["pa"])
                    DS(lambda e: e.dma_start(out=pb[:], in_=aim_pp), w=["pb"])
                    DS(lambda e: e.dma_start(out=pc[:], in_=ls_pp), w=["pc"])
                    A(lambda e: e.activation(out=pc[:], in_=pc[:], func=AF.Exp), ["pc"], ["pc"])
                    V(lambda e: e.tensor_tensor(out=rho_pp[:], in0=pa[:], in1=pc[:], op=ALU.mult), ["pa", "pc"], ["rho_pp"])
                    A(lambda e: e.activation(out=rho_pp[:], in_=rho_pp[:], func=AF.Exp), ["rho_pp"], ["rho_pp"])
                    V(lambda e: e.tensor_tensor(out=f_pp[:], in0=pb[:], in1=pc[:], op=ALU.mult), ["pb", "pc"], ["f_pp"])
                    V(lambda e: e.tensor_scalar_mul(out=f_pp[:], in0=f_pp[:], scalar1=1.0 / (2 * math.pi)), ["f_pp"], ["f_pp"])
                    DS(lambda e: e.dma_start(out=dsc[:], in_=dpp), w=["dsc"])
                    DS(lambda e: e.dma_start(out=bglu[:], in_=b_glu_pp), w=["bglu"])
                    gst_ = sb(s2, "gst_", [128, 4, 512])
                    DS(lambda e: e.dma_start(out=gst_[:], in_=w_glu_pad.rearrange("(kt p) f -> p kt f", p=128)), w=["gst_"])
                    V(lambda e: e.tensor_copy(out=wglu[:], in_=gst_[:]), ["gst_"], ["wglu"])
                    P.barrier()

                L = SEG
                arg = sb(st, "arg", [128, L])
                argi = sb(st, "argi", [128, L], I32)
                argf = sb(st, "argf", [128, L])
                cs = sb(st, "cs", [128, L])
                sn = sb(st, "sn", [128, L])
                dre = sb(st, "dre", [128, L])
                dim_ = sb(st, "dim", [128, L])
                tA = sb(st, "tA", [128, L])
                tB = sb(st, "tB", [128, L])
                zre = sb(st, "zre", [128, L])
                zim = sb(st, "zim", [128, L])
                Sre = sb(st, "Sre", [128, L], BF16)
                Sim = sb(st, "Sim", [128, L], BF16)
                zst = sb(st, "zst", [128, 16, 2])
                ysb = sb(st, "ysb", [128, L])
                yfl = sb(st, "yfl", [128, L])
                gg = sb(st, "gg", [128, 4, L], BF16)
                sg = sb(st, "sg", [128, L])
                s5o = sb(st, "s5o", [128, L], BF16)
                pBr = ps(st, "pBr", [128, L])
                pBi = ps(st, "pBi", [128, L])
                pY = ps(st, "pY", [128, L])
                pG = ps(st, "pG", [128, L])
                V(lambda e: e.memset(zst[:], 0.0), w=["zst"])

                def s5_col(dr, gp, u_lo, n, t0, rev, readout):
                    ci_ = dr * 8 + gp
                    kt = gp // 2
                    r0 = 64 * (gp % 2)
                    wcol = slice(ci_ * 128, (ci_ + 1) * 128)
                    for c0 in range(0, n, 512):
                        c1 = min(n, c0 + 512)
                        for ri, pB in ((0, pBr), (1, pBi)):
                            T(lambda e, ri=ri, pB=pB, c0=c0, c1=c1: e.matmul(pB[:, c0:c1], lhsT=WB[r0:r0 + 64, ri, wcol],
                                                                           rhs=U[r0:r0 + 64, kt, u_lo + c0:u_lo + c1],
                                                                           start=True, stop=True),
                              ["WB", "U"], ["pBr" if ri == 0 else "pBi"])
                    G(lambda e: e.tensor_scalar(out=arg[:, 0:n], in0=iota_r[:, 0:n], scalar1=float(t0), scalar2=f_pp[:, ci_:ci_ + 1],
                                                op0=ALU.add, op1=ALU.mult), ["iota_r", "f_pp"], ["arg"])
                    G(lambda e: e.tensor_copy(out=argi[:, 0:n], in_=arg[:, 0:n]), ["arg"], ["argi"])
                    G(lambda e: e.tensor_copy(out=argf[:, 0:n], in_=argi[:, 0:n]), ["argi"], ["argf"])
                    G(lambda e: e.tensor_tensor(out=arg[:, 0:n], in0=arg[:, 0:n], in1=argf[:, 0:n], op=ALU.subtract), ["arg", "argf"], ["arg"])
                    A(lambda e: e.activation(out=sn[:, 0:n], in_=arg[:, 0:n], func=AF.Sin, scale=TWO_PI), ["arg"], ["sn"])
                    G(lambda e: e.tensor_scalar_add(out=arg[:, 0:n], in0=arg[:, 0:n], scalar1=0.25), ["arg"], ["arg"])
                    G(lambda e: e.tensor_copy(out=argi[:, 0:n], in_=arg[:, 0:n]), ["arg"], ["argi"])
                    G(lambda e: e.tensor_copy(out=argf[:, 0:n], in_=argi[:, 0:n]), ["argi"], ["argf"])
                    G(lambda e: e.tensor_tensor(out=arg[:, 0:n], in0=arg[:, 0:n], in1=argf[:, 0:n], op=ALU.subtract), ["arg", "argf"], ["arg"])
                    A(lambda e: e.activation(out=cs[:, 0:n], in_=arg[:, 0:n], func=AF.Sin, scale=TWO_PI), ["arg"], ["cs"])

                    def tv(ap_):
                        return ap_[:, n - 1::-1] if rev and n > 0 else ap_[:, 0:n]

                    def tvn(t_):
                        if not rev:
                            return t_[:, 0:n]
                        return t_[:, 0:n][:, ::-1]

                    V(lambda e: e.tensor_tensor(out=tA[:, 0:n], in0=pBr[:, 0:n], in1=tvn(cs), op=ALU.mult), ["pBr", "cs"], ["tA"])
                    V(lambda e: e.tensor_tensor(out=tB[:, 0:n], in0=pBi[:, 0:n], in1=tvn(sn), op=ALU.mult), ["pBi", "sn"], ["tB"])
                    V(lambda e: e.tensor_tensor(out=dre[:, 0:n], in0=tA[:, 0:n], in1=tB[:, 0:n], op=ALU.add), ["tA", "tB"], ["dre"])
                    V(lambda e: e.tensor_tensor(out=tA[:, 0:n], in0=pBi[:, 0:n], in1=tvn(cs), op=ALU.mult), ["pBi", "cs"], ["tA"])
                    V(lambda e: e.tensor_tensor(out=tB[:, 0:n], in0=pBr[:, 0:n], in1=tvn(sn), op=ALU.mult), ["pBr", "sn"], ["tB"])
                    V(lambda e: e.tensor_tensor(out=dim_[:, 0:n], in0=tA[:, 0:n], in1=tB[:, 0:n], op=ALU.subtract), ["tA", "tB"], ["dim"])
                    for (src, dst, ri) in ((dre, zre, 0), (dim_, zim, 1)):
                        V(lambda e, src=src, dst=dst, ri=ri: e.tensor_tensor_scan(
                            out=tvn(dst), data0=bc(rho_pp[:, ci_:ci_ + 1], [128, n]), data1=tvn(src),
                            initial=zst[:, ci_, ri:ri + 1], op0=ALU.mult, op1=ALU.add),
                          ["dre" if ri == 0 else "dim", "rho_pp", "zst"], ["zre" if ri == 0 else "zim"])
                    last = 0 if rev else n - 1
                    V(lambda e: e.tensor_copy(out=zst[:, ci_, 0:1], in_=zre[:, last:last + 1]), ["zre"], ["zst"])
                    V(lambda e: e.tensor_copy(out=zst[:, ci_, 1:2], in_=zim[:, last:last + 1]), ["zim"], ["zst"])
                    if not readout:
                        return
                    V(lambda e: e.tensor_tensor(out=tA[:, 0:n], in0=zre[:, 0:n], in1=tvn(cs), op=ALU.mult), ["zre", "cs"], ["tA"])
                    V(lambda e: e.tensor_tensor(out=tB[:, 0:n], in0=zim[:, 0:n], in1=tvn(sn), op=ALU.mult), ["zim", "sn"], ["tB"])
                    V(lambda e: e.tensor_tensor(out=Sre[:, 0:n], in0=tA[:, 0:n], in1=tB[:, 0:n], op=ALU.subtract), ["tA", "tB"], ["Sre"])
                    V(lambda e: e.tensor_tensor(out=tA[:, 0:n], in0=zre[:, 0:n], in1=tvn(sn), op=ALU.mult), ["zre", "sn"], ["tA"])
                    V(lambda e: e.tensor_tensor(out=tB[:, 0:n], in0=zim[:, 0:n], in1=tvn(cs), op=ALU.mult), ["zim", "cs"], ["tB"])
                    V(lambda e: e.tensor_tensor(out=Sim[:, 0:n], in0=tA[:, 0:n], in1=tB[:, 0:n], op=ALU.add), ["tA", "tB"], ["Sim"])
                    first = (gp % 2 == 0)
                    for c0 in range(0, n, 512):
                        for ri, S_ in ((0, Sre), (1, Sim)):
                            T(lambda e, ri=ri, S_=S_, c0=c0: e.matmul(pY[:, c0:c0 + 512], lhsT=WC[:, ri, wcol], rhs=S_[:, c0:c0 + 512],
                                                                     start=(first and ri == 0), stop=((not first) and ri == 1)),
                              ["WC", "Sre" if ri == 0 else "Sim"], ["pY"])

                for gp in range(8):
                    s5_col(0, gp, 0, CTX, 0, False, False)
                for sgi in range(SEQ // L):
                    for kt in range(4):
                        for g2 in range(2):
                            s5_col(0, kt * 2 + g2, CTX + sgi * L, L, CTX + sgi * L, False, True)
                        V(lambda e: e.tensor_copy(out=ysb[:], in_=pY[:]), ["pY"], ["ysb"])
                        DS(lambda e, kt=kt, sgi=sgi: e.dma_start(out=yf_d[kt, :, sgi * L:(sgi + 1) * L], in_=ysb[:]), ["ysb"], ["yf"])
                for gp in range(8):
                    s5_col(1, gp, 0, CTX, 0, True, False)
                for sb_i in range(SEQ // L):
                    sgi = SEQ // L - 1 - sb_i
                    for kt in range(4):
                        for g2 in range(2):
                            s5_col(1, kt * 2 + g2, CTX + sgi * L, L, CTX + sb_i * L, True, True)
                        DS(lambda e, kt=kt, sgi=sgi: e.dma_start(out=yfl[:], in_=yf_d[kt, :, sgi * L:(sgi + 1) * L]), ["yf"], ["yfl"])
                        V(lambda e: e.tensor_tensor(out=ysb[:], in0=pY[:], in1=yfl[:], op=ALU.add), ["pY", "yfl"], ["ysb"])
                        V(lambda e, kt=kt, sgi=sgi: e.scalar_tensor_tensor(out=ysb[:], in0=U[:, kt, CTX + sgi * L:CTX + (sgi + 1) * L],
                                                                           scalar=dsc[:, kt:kt + 1], in1=ysb[:],
                                                                           op0=ALU.mult, op1=ALU.add), ["U", "dsc", "ysb"], ["ysb"])
                        A(lambda e, kt=kt: e.activation(out=gg[:, kt, :], in_=ysb[:], func=AF.Gelu_apprx_tanh), ["ysb"], ["gg"])
                    for mt in range(4):
                        for c0 in range(0, L, 512):
                            for kt in range(4):
                                T(lambda e, mt=mt, c0=c0, kt=kt: e.matmul(pG[:, c0:c0 + 512], lhsT=wglu[:, kt, mt * 128:(mt + 1) * 128],
                                                                         rhs=gg[:, kt, c0:c0 + 512], start=(kt == 0), stop=(kt == 3)),
                                  ["wglu", "gg"], ["pG"])
                        A(lambda e, mt=mt: e.activation(out=sg[:], in_=pG[:], func=AF.Sigmoid, bias=bglu[:, mt:mt + 1]), ["pG", "bglu"], ["sg"])
                        V(lambda e, mt=mt: e.tensor_tensor(out=s5o[:], in0=sg[:], in1=gg[:, mt, :], op=ALU.mult), ["sg", "gg"], ["s5o"])
                        DS(lambda e, mt=mt, sgi=sgi: e.dma_start(out=catT_d[mt, :, sgi * L:(sgi + 1) * L], in_=s5o[:]), ["s5o"], ["catT_s5"])
                P.barrier()
        P.barrier()

        aff = sb(gst, "aff", [128, NT, 16])
        with contextlib.ExitStack() as st:
            wo = sb(st, "wo", [128, 10, D], BF16)
            wr = sb(st, "wr", [128, 8, 16], BF16)
            with contextlib.ExitStack() as s2:
                stg = sb(s2, "stg3", [128, 10, D])
                DS(lambda e: e.dma_start(out=stg[:], in_=w_out_pad.rearrange("(kt p) f -> p kt f", p=128)), w=["stg3"])
                V(lambda e: e.tensor_copy(out=wo[:], in_=stg[:]), ["stg3"], ["wo"])
                DS(lambda e: e.dma_start(out=stg[:, 0:8, 0:16], in_=w_router.rearrange("(kt p) f -> p kt f", p=128)), w=["stg3"])
                V(lambda e: e.tensor_copy(out=wr[:], in_=stg[:, 0:8, 0:16]), ["stg3"], ["wr"])
                P.barrier()
            cat = [sb(st, "cat%d" % i, [128, 10, 128], BF16) for i in range(2)]
            xt3 = [sb(st, "x3_%d" % i, [128, D]) for i in range(2)]
            tm = sb(st, "tm", [128, D])
            xr = sb(st, "xr", [128, D])
            xnw = sb(st, "xnw", [128, D])
            hh = sb(st, "hh", [128, D])
            hb = sb(st, "hb", [128, D], BF16)
            hT = sb(st, "hT", [128, 8, 128], BF16)
            stats = sb(st, "stats3", [128, 2, 6])
            mv = sb(st, "mv3", [128, 2])
            rstd = sb(st, "rstd3", [128, 1])
            lg = sb(st, "lg", [128, 16])
            mx = sb(st, "mx", [128, 1])
            sm = sb(st, "sm", [128, 1])
            pMx = ps(st, "pMx", [128, D])
            pT3 = ps(st, "pT3", [128, D], BF16)
            pL = ps(st, "pL", [128, 16])

            def lnorm(src, sk, dst, dk):
                for c in range(2):
                    V(lambda e, c=c: e.bn_stats(out=stats[:, c, :], in_=src[:, c * 512:(c + 1) * 512]), [sk], ["stats3"])
                V(lambda e: e.bn_aggr(out=mv[:], in_=stats[:].rearrange("p a b -> p (a b)")), ["stats3"], ["mv3"])
                V(lambda e: e.tensor_scalar_add(out=rstd[:], in0=mv[:, 1:2], scalar1=EPS), ["mv3"], ["rstd3"])
                A(lambda e: e.activation(out=rstd[:], in_=rstd[:], func=AF.Sqrt), ["rstd3"], ["rstd3"])
                V(lambda e: e.reciprocal(out=rstd[:], in_=rstd[:]), ["rstd3"], ["rstd3"])
                V(lambda e: e.tensor_scalar(out=dst[:], in0=src[:], scalar1=mv[:, 0:1], scalar2=rstd[:, 0:1],
                                            op0=ALU.subtract, op1=ALU.mult), [sk, "mv3", "rstd3"], [dk])

            for t in range(NT):
                c_ = cat[t % 2]
                ck = "cat%d" % (t % 2)
                x_ = xt3[t % 2]
                xk = "x3_%d" % (t % 2)
                DS(lambda e, t=t, c_=c_: e.dma_start(out=c_[:], in_=catT_d[:, :, t * 128:(t + 1) * 128].rearrange("a p t -> p a t")), w=[ck])
                DS(lambda e, t=t, x_=x_: e.dma_start(out=x_[:], in_=xs[CTX + t * 128:CTX + (t + 1) * 128, :]), w=[xk])
                for nh in range(2):
                    for kt in range(10):
                        T(lambda e, nh=nh, kt=kt, c_=c_: e.matmul(pMx[:, nh * 512:(nh + 1) * 512], lhsT=c_[:, kt, :],
                                                                  rhs=wo[:, kt, nh * 512:(nh + 1) * 512], start=(kt == 0), stop=(kt == 9)),
                          [ck, "wo"], ["pMx"])
                V(lambda e: e.tensor_tensor(out=tm[:], in0=pMx[:], in1=modrow[:, 0, :], op=ALU.mult), ["pMx", "modrow"], ["tm"])
                V(lambda e, x_=x_: e.scalar_tensor_tensor(out=xr[:], in0=x_[:], scalar=ALPHA, in1=tm[:], op0=ALU.mult, op1=ALU.add),
                  [xk, "tm"], ["xr"])
                lnorm(xr, "xr", tm, "tm")
                V(lambda e: e.tensor_tensor(out=tm[:], in0=tm[:], in1=lnrow[:, 0, :], op=ALU.mult), ["tm", "lnrow"], ["tm"])
                V(lambda e: e.tensor_tensor(out=xnw[:], in0=tm[:], in1=lnrow[:, 1, :], op=ALU.add), ["tm", "lnrow"], ["xnw"])
                DS(lambda e, t=t: e.dma_start(out=xnew_d[t * 128:(t + 1) * 128, :], in_=xnw[:]), ["xnw"], ["xnew_d"])
                lnorm(xnw, "xnw", hh, "hh")
                V(lambda e: e.tensor_tensor(out=hh[:], in0=hh[:], in1=modrow[:, 2, :], op=ALU.mult), ["hh", "modrow"], ["hh"])
                V(lambda e: e.tensor_tensor(out=hb[:], in0=hh[:], in1=modrow[:, 1, :], op=ALU.add), ["hh", "modrow"], ["hb"])
                DS(lambda e, t=t: e.dma_start(out=hrow_d[t * 128:(t + 1) * 128, :], in_=hb[:]), ["hb"], ["hrow_d"])
                for kt in range(8):
                    T(lambda e, kt=kt: e.transpose(out=pT3[:, kt * 128:(kt + 1) * 128], in_=hb[:, kt * 128:(kt + 1) * 128],
                                                   identity=ident_bf[:]), ["hb", "ident_bf"], ["pT3"])
                A(lambda e: e.activation(out=hT[:].rearrange("p a b -> p (a b)"), in_=pT3[:], func=AF.Identity), ["pT3"], ["hT"])
                for kt in range(8):
                    T(lambda e, kt=kt: e.matmul(pL[:], lhsT=hT[:, kt, :], rhs=wr[:, kt, :], start=(kt == 0), stop=(kt == 7)),
                      ["hT", "wr"], ["pL"])
                V(lambda e: e.tensor_reduce(out=mx[:], in_=pL[:], axis=AX.X, op=ALU.max), ["pL"], ["mx"])
                V(lambda e: e.tensor_scalar_mul(out=mx[:], in0=mx[:], scalar1=-1.0), ["mx"], ["mx"])
                A(lambda e: e.activation(out=lg[:], in_=pL[:], func=AF.Exp, bias=mx[:, 0:1]), ["pL", "mx"], ["lg"])
                V(lambda e: e.tensor_reduce(out=sm[:], in_=lg[:], axis=AX.X, op=ALU.add), ["lg"], ["sm"])
                V(lambda e: e.reciprocal(out=sm[:], in_=sm[:]), ["sm"], ["sm"])
                V(lambda e, t=t: e.tensor_scalar_mul(out=aff[:, t, :], in0=lg[:], scalar1=sm[:, 0:1]), ["lg", "sm"], ["aff"])
                DS(lambda e, t=t: e.dma_start(out=aff_d[t * 128:(t + 1) * 128, :], in_=aff[:, t, :]), ["aff"], ["aff_d"])
            P.barrier()

        if debug == 1:
            return nc

        idx_all = sb(gst, "idx_all", [128, NE, 8], I32)
        gate_all = sb(gst, "gate_all", [128, NE, 8])
        with contextlib.ExitStack() as st:
            hs = sb(st, "hs", [128, 16])
            am = sb(st, "am", [128, NT, NE])
            lo = sb(st, "lo", [128, NE])
            hi = sb(st, "hi", [128, NE])
            mid = sb(st, "mid", [128, NE])
            cmp_ = sb(st, "cmp", [128, NT, NE])
            cnt = sb(st, "cnt", [128, NE])
            cntb = sb(st, "cntb", [128, NE], BF16)
            ge = sb(st, "ge", [128, NE])
            dl = sb(st, "dl", [128, NE])
            pC = ps(st, "pC", [128, NE])
            DS(lambda e: e.dma_start(out=hs[:], in_=half_sel), w=["hs"])
            V(lambda e: e.tensor_tensor(out=am[:], in0=aff[:, :, 0:8], in1=bc(hs[:, 0:8].unsqueeze(1), [128, NT, NE]), op=ALU.mult),
              ["aff", "hs"], ["am"])
            V(lambda e: e.tensor_tensor(out=cmp_[:], in0=aff[:, :, 8:16], in1=bc(hs[:, 8:16].unsqueeze(1), [128, NT, NE]), op=ALU.mult),
              ["aff", "hs"], ["cmp"])
            V(lambda e: e.tensor_tensor(out=am[:], in0=am[:], in1=cmp_[:], op=ALU.add), ["am", "cmp"], ["am"])
            V(lambda e: e.memset(lo[:], 0.0), w=["lo"])
            V(lambda e: e.memset(hi[:], 1.0), w=["hi"])
            for it in range(30):
                V(lambda e: e.tensor_tensor(out=mid[:], in0=lo[:], in1=hi[:], op=ALU.add), ["lo", "hi"], ["mid"])
                V(lambda e: e.tensor_scalar_mul(out=mid[:], in0=mid[:], scalar1=0.5), ["mid"], ["mid"])
                V(lambda e: e.tensor_tensor(out=cmp_[:], in0=am[:], in1=bc(mid[:].unsqueeze(1), [128, NT, NE]), op=ALU.is_ge),
                  ["am", "mid"], ["cmp"])
                V(lambda e: e.tensor_reduce(out=cnt[:], in_=cmp_[:].rearrange("p t e -> p e t"), axis=AX.X, op=ALU.add), ["cmp"], ["cnt"])
                V(lambda e: e.tensor_copy(out=cntb[:], in_=cnt[:]), ["cnt"], ["cntb"])
                T(lambda e: e.matmul(pC[:], lhsT=ones_bf[:], rhs=cntb[:], start=True, stop=True), ["ones", "cntb"], ["pC"])
                V(lambda e: e.tensor_single_scalar(out=ge[:], in_=pC[:], scalar=float(CAP), op=ALU.is_ge), ["pC"], ["ge"])
                V(lambda e: e.tensor_tensor(out=dl[:], in0=mid[:], in1=lo[:], op=ALU.subtract), ["mid", "lo"], ["dl"])
                V(lambda e: e.tensor_tensor(out=dl[:], in0=dl[:], in1=ge[:], op=ALU.mult), ["dl", "ge"], ["dl"])
                V(lambda e: e.tensor_tensor(out=lo[:], in0=lo[:], in1=dl[:], op=ALU.add), ["lo", "dl"], ["lo"])
                V(lambda e: e.tensor_tensor(out=dl[:], in0=hi[:], in1=mid[:], op=ALU.subtract), ["hi", "mid"], ["dl"])
                V(lambda e: e.tensor_tensor(out=dl[:], in0=dl[:], in1=ge[:], op=ALU.mult), ["dl", "ge"], ["dl"])
                V(lambda e: e.tensor_tensor(out=hi[:], in0=mid[:], in1=dl[:], op=ALU.add), ["mid", "dl"], ["hi"])
            mk = sb(st, "mk", [128, NE, NT], BF16)
            cin = sb(st, "cin", [128, NE * NT])
            tot = sb(st, "tot", [128, NE, NT])
            cend = sb(st, "cend", [128, NE, NT])
            pP = ps(st, "pP", [128, NE * NT])
            pTt = ps(st, "pTt", [128, NE * NT])
            V(lambda e: e.tensor_tensor(out=mk[:], in0=am[:].rearrange("p t e -> p e t"), in1=bc(lo[:].unsqueeze(2), [128, NE, NT]),
                                        op=ALU.is_ge), ["am", "lo"], ["mk"])
            T(lambda e: e.matmul(pP[:], lhsT=tri_bf[:], rhs=mk[:].rearrange("p e t -> p (e t)"), start=True, stop=True), ["tri", "mk"], ["pP"])
            T(lambda e: e.matmul(pTt[:], lhsT=ones_bf[:], rhs=mk[:].rearrange("p e t -> p (e t)"), start=True, stop=True), ["ones", "mk"], ["pTt"])
            V(lambda e: e.tensor_copy(out=cin[:], in_=pP[:]), ["pP"], ["cin"])
            V(lambda e: e.tensor_copy(out=tot[:].rearrange("p e t -> p (e t)"), in_=pTt[:]), ["pTt"], ["tot"])
            V(lambda e: e.tensor_copy(out=cend[:], in_=tot[:]), ["tot"], ["cend"])
            sh = 1
            tmpc = sb(st, "tmpc", [128, NE, NT])
            while sh < NT:
                V(lambda e: e.tensor_copy(out=tmpc[:], in_=cend[:]), ["cend"], ["tmpc"])
                V(lambda e, sh=sh: e.tensor_tensor(out=cend[:, :, sh:NT], in0=tmpc[:, :, sh:NT], in1=tmpc[:, :, 0:NT - sh], op=ALU.add),
                  ["tmpc"], ["cend"])
                sh *= 2
            cinT = sb(st, "cinT", [128, 4, 128])
            pX = ps(st, "pX", [128, 4, 128])
            for a in range(4):
                T(lambda e, a=a: e.transpose(out=pX[:, a, :], in_=cin[:, a * 128:(a + 1) * 128], identity=ident_f[:]), ["cin", "ident_f"], ["pX"])
            V(lambda e: e.tensor_copy(out=cinT[:], in_=pX[:]), ["pX"], ["cinT"])
            DS(lambda e: e.dma_start(out=cin_d.rearrange("(a p) t -> p a t", p=128), in_=cinT[:]), ["cinT"], ["cin_d"])
            spp = sb(st, "spp", [128, 8])
            DS(lambda e: e.dma_start(out=spp[:], in_=slot_pp_d), w=["spp"])
            le = sb(st, "le", [128, NE, 8, NT])
            tl_ = sb(st, "tl_", [128, NE, 8])
            cst = sb(st, "cst", [128, NE, 8])
            rr = sb(st, "rr", [128, NE, 8])
            rowi = sb(st, "rowi", [128, NE, 8], I32)
            rowf = sb(st, "rowf", [128, NE, 8])
            for e_ in range(NE):
                V(lambda e, e_=e_: e.tensor_tensor(out=le[:, e_, :, :], in0=bc(cend[:, e_, :].unsqueeze(1), [128, 8, NT]),
                                                   in1=bc(spp[:].unsqueeze(2), [128, 8, NT]), op=ALU.is_le), ["cend", "spp"], ["le"])
            V(lambda e: e.tensor_reduce(out=tl_[:].rearrange("p e j -> p (e j)"), in_=le[:].rearrange("p e j t -> p (e j) t"),
                                        axis=AX.X, op=ALU.add), ["le"], ["tl_"])
            for e_ in range(NE):
                V(lambda e, e_=e_: e.tensor_tensor(out=le[:, e_, :, :], in0=le[:, e_, :, :], in1=bc(tot[:, e_, :].unsqueeze(1), [128, 8, NT]),
                                                   op=ALU.mult), ["le", "tot"], ["le"])
            V(lambda e: e.tensor_reduce(out=cst[:].rearrange("p e j -> p (e j)"), in_=le[:].rearrange("p e j t -> p (e j) t"),
                                        axis=AX.X, op=ALU.add), ["le"], ["cst"])
            V(lambda e: e.tensor_scalar_min(out=tl_[:], in0=tl_[:], scalar1=float(NT - 1)), ["tl_"], ["tl_"])
            V(lambda e: e.tensor_tensor(out=rr[:], in0=bc(spp[:].unsqueeze(1), [128, NE, 8]), in1=cst[:], op=ALU.subtract), ["spp", "cst"], ["rr"])
            for e_ in range(NE):
                V(lambda e, e_=e_: e.tensor_scalar_add(out=rowf[:, e_, :], in0=tl_[:, e_, :], scalar1=float(e_ * NT)), ["tl_"], ["rowf"])
            V(lambda e: e.tensor_copy(out=rowi[:], in_=rowf[:]), ["rowf"], ["rowi"])
            crow = sb(st, "crow", [128, 128])
            cle = sb(st, "cle", [128, 128])
            tloc = sb(st, "tloc", [128, NE, 8])
            arow = sb(st, "arow", [128, 16])
            idf = sb(st, "idf", [128, NE, 8])
            V(lambda e: e.memset(tloc[:], 0.0), w=["tloc"])
            for e_ in range(NE):
                for j in range(8):
                    DG(lambda e, e_=e_, j=j: e.indirect_dma_start(out=crow[:], out_offset=None, in_=cin_d,
                                                                 in_offset=bass.IndirectOffsetOnAxis(ap=rowi[:, e_, j:j + 1], axis=0)),
                       ["cin_d", "rowi"], ["crow"])
                    V(lambda e, e_=e_, j=j: e.tensor_scalar(out=cle[:], in0=crow[:], scalar1=rr[:, e_, j:j + 1], scalar2=0.0,
                                                            op0=ALU.is_le, op1=ALU.add, accum_out=tloc[:, e_, j:j + 1]),
                      ["crow", "rr"], ["cle", "tloc"])
            V(lambda e: e.tensor_scalar_min(out=tloc[:], in0=tloc[:], scalar1=127.0), ["tloc"], ["tloc"])
            V(lambda e: e.scalar_tensor_tensor(out=idf[:], in0=tl_[:], scalar=128.0, in1=tloc[:], op0=ALU.mult, op1=ALU.add),
              ["tl_", "tloc"], ["idf"])
            V(lambda e: e.tensor_copy(out=idx_all[:], in_=idf[:]), ["idf"], ["idx_all"])
            for e_ in range(NE):
                for j in range(8):
                    DG(lambda e, e_=e_, j=j: e.indirect_dma_start(out=arow[:], out_offset=None, in_=aff_d,
                                                                 in_offset=bass.IndirectOffsetOnAxis(ap=idx_all[:, e_, j:j + 1], axis=0)),
                       ["aff_d", "idx_all"], ["arow"])
                    V(lambda e, e_=e_, j=j: e.tensor_tensor(out=cle[:, 0:16], in0=arow[:], in1=hs[:], op=ALU.mult), ["arow", "hs"], ["cle"])
                    V(lambda e, e_=e_, j=j: e.tensor_tensor(out=gate_all[:, e_, j:j + 1], in0=cle[:, e_:e_ + 1], in1=cle[:, 8 + e_:9 + e_],
                                                            op=ALU.add), ["cle"], ["gate_all"])
            P.barrier()

        if debug:
            DS(lambda e: e.dma_start(out=dbg_idx, in_=idx_all[:].rearrange("p a b -> p (a b)")), ["idx_all"], ["dbg_idx"])
            DS(lambda e: e.dma_start(out=dbg_gate, in_=gate_all[:].rearrange("p a b -> p (a b)")), ["gate_all"], ["dbg_gate"])
            P.barrier()
        if debug == 2:
            return nc

        zt = sb(gst, "zt", [128, D])
        V(lambda e: e.memset(zt[:], 0.0), w=["zt"])
        for t in range(NT):
            DS(lambda e, t=t: e.dma_start(out=moe_d[t * 128:(t + 1) * 128, :], in_=zt[:]), ["zt"], ["moe_d"])
        P.barrier()
        with contextlib.ExitStack() as st:
            xg = sb(st, "xg", [128, D], BF16)
            xgT = sb(st, "xgT", [128, 8, CAP], BF16)
            wdn = sb(st, "wdn", [128, NFT, D], BF16)
            hid = sb(st, "hid", [128, NFT, CAP], BF16)
            stg = [sb(st, "wstg%d" % i, [128, 8, 256]) for i in range(2)]
            wgb = sb(st, "wgb", [128, 8, 128], BF16)
            wub = sb(st, "wub", [128, 8, 128], BF16)
            stgd = sb(st, "stgd", [128, D])
            sl = sb(st, "sl", [128, CAP])
            ob = sb(st, "ob", [128, D])
            pTg = ps(st, "pTg", [128, D], BF16)
            pGt = ps(st, "pGt", [128, CAP])
            pUp = ps(st, "pUp", [128, CAP])
            pO = ps(st, "pO", [128, D])
            for e_ in range(NE):
                for j in range(8):
                    DG(lambda e, e_=e_, j=j: e.indirect_dma_start(out=xg[:], out_offset=None, in_=hrow_d,
                                                                 in_offset=bass.IndirectOffsetOnAxis(ap=idx_all[:, e_, j:j + 1], axis=0)),
                       ["hrow_d", "idx_all"], ["xg"])
                    for kt in range(8):
                        T(lambda e, kt=kt: e.transpose(out=pTg[:, kt * 128:(kt + 1) * 128], in_=xg[:, kt * 128:(kt + 1) * 128],
                                                       identity=ident_bf[:]), ["xg", "ident_bf"], ["pTg"])
                    V(lambda e, j=j: e.tensor_copy(out=xgT[:, :, j * 128:(j + 1) * 128], in_=pTg[:].rearrange("p (a b) -> p a b", a=8)),
                      ["pTg"], ["xgT"])
                for ft in range(NFT):
                    DS(lambda e, e_=e_, ft=ft: e.dma_start(out=stgd[:], in_=wd[e_, ft * 128:(ft + 1) * 128, :]), w=["stgd"])
                    G(lambda e, ft=ft: e.tensor_copy(out=wdn[:, ft, :], in_=stgd[:]), ["stgd"], ["wdn"])
                for ft in range(NFT):
                    s_ = stg[ft % 2]
                    sk = "wstg%d" % (ft % 2)
                    DS(lambda e, e_=e_, ft=ft, s_=s_: e.dma_start(out=s_[:, :, 0:128],
                                                                  in_=wg[e_].rearrange("(kt p) f -> p kt f", p=128)[:, :, ft * 128:(ft + 1) * 128]),
                       w=[sk])
                    DS(lambda e, e_=e_, ft=ft, s_=s_: e.dma_start(out=s_[:, :, 128:256],
                                                                  in_=wu[e_].rearrange("(kt p) f -> p kt f", p=128)[:, :, ft * 128:(ft + 1) * 128]),
                       w=[sk])
                    A(lambda e, s_=s_: e.activation(out=wgb[:], in_=s_[:, :, 0:128], func=AF.Identity), [sk], ["wgb"])
                    A(lambda e, s_=s_: e.activation(out=wub[:], in_=s_[:, :, 128:256], func=AF.Identity), [sk], ["wub"])
                    for c0 in (0, 512):
                        for kt in range(8):
                            T(lambda e, kt=kt, c0=c0: e.matmul(pGt[:, c0:c0 + 512], lhsT=wgb[:, kt, :], rhs=xgT[:, kt, c0:c0 + 512],
                                                               start=(kt == 0), stop=(kt == 7)), ["wgb", "xgT"], ["pGt"])
                        for kt in range(8):
                            T(lambda e, kt=kt, c0=c0: e.matmul(pUp[:, c0:c0 + 512], lhsT=wub[:, kt, :], rhs=xgT[:, kt, c0:c0 + 512],
                                                               start=(kt == 0), stop=(kt == 7)), ["wub", "xgT"], ["pUp"])
                    A(lambda e: e.activation(out=sl[:], in_=pGt[:], func=AF.Silu), ["pGt"], ["sl"])
                    V(lambda e, ft=ft: e.tensor_tensor(out=hid[:, ft, :], in0=sl[:], in1=pUp[:], op=ALU.mult), ["sl", "pUp"], ["hid"])
                for j in range(8):
                    for nh in range(2):
                        for ft in range(NFT):
                            T(lambda e, j=j, nh=nh, ft=ft: e.matmul(pO[:, nh * 512:(nh + 1) * 512], lhsT=hid[:, ft, j * 128:(j + 1) * 128],
                                                                    rhs=wdn[:, ft, nh * 512:(nh + 1) * 512], start=(ft == 0), stop=(ft == NFT - 1)),
                              ["hid", "wdn"], ["pO"])
                    V(lambda e, e_=e_, j=j: e.tensor_scalar_mul(out=ob[:], in0=pO[:], scalar1=gate_all[:, e_, j:j + 1]), ["pO", "gate_all"], ["ob"])
                    DG(lambda e, e_=e_, j=j: e.indirect_dma_start(out=moe_d, out_offset=bass.IndirectOffsetOnAxis(ap=idx_all[:, e_, j:j + 1], axis=0),
                                                                 in_=ob[:], in_offset=None, compute_op=ALU.add, oob_is_err=True),
                       ["ob", "idx_all", "moe_d"], ["moe_d"])
            P.barrier()

        if debug == 3:
            return nc

        ccsem = gst.enter_context(nc.semaphore("ccsem"))
        CCH = 16
        crow_ = SEQ // CCH
        for cc_ in range(CCH):
            nc.gpsimd.collective_compute("AllReduce", ALU.add, replica_groups=[[0, 1], [2, 3], [4, 5], [6, 7]],
                                         ins=[moe_d[cc_ * crow_:(cc_ + 1) * crow_, :]],
                                         outs=[moe_r[cc_ * crow_:(cc_ + 1) * crow_, :]]).then_inc(ccsem)
            nc.gpsimd.wait_ge(ccsem, cc_ + 1)
        G(lambda e: e.memset(zt[:, 0:8], 0.0), w=["zt"])
        P.barrier()
        with contextlib.ExitStack() as st:
            mt_ = [sb(st, "m6_%d" % i, [128, D]) for i in range(2)]
            xq = [sb(st, "x6_%d" % i, [128, D]) for i in range(2)]
            tm = sb(st, "tm6", [128, D])
            xr = sb(st, "xr6", [128, D])
            oo = [sb(st, "o6_%d" % i, [128, D]) for i in range(2)]
            stats = sb(st, "stats6", [128, 2, 6])
            mv = sb(st, "mv6_", [128, 2])
            rstd = sb(st, "rstd6", [128, 1])
            tki = sb(st, "tki", [128, NT // 2], I32)
            DS(lambda e: e.dma_start(out=tki[:], in_=tokidx_d), w=["tki"])
            for t in range(NT // 2):
                m_ = mt_[t % 2]
                mk_ = "m6_%d" % (t % 2)
                x_ = xq[t % 2]
                xk = "x6_%d" % (t % 2)
                o_ = oo[t % 2]
                ok = "o6_%d" % (t % 2)
                DG(lambda e, t=t, m_=m_: e.indirect_dma_start(out=m_[:], out_offset=None, in_=moe_r,
                                                             in_offset=bass.IndirectOffsetOnAxis(ap=tki[:, t:t + 1], axis=0)),
                   ["moe_r", "tki"], [mk_])
                DG(lambda e, t=t, x_=x_: e.indirect_dma_start(out=x_[:], out_offset=None, in_=xnew_d,
                                                             in_offset=bass.IndirectOffsetOnAxis(ap=tki[:, t:t + 1], axis=0)),
                   ["xnew_d", "tki"], [xk])
                V(lambda e, m_=m_: e.tensor_tensor(out=tm[:], in0=m_[:], in1=modrow[:, 3, :], op=ALU.mult), [mk_, "modrow"], ["tm6"])
                V(lambda e, x_=x_: e.scalar_tensor_tensor(out=xr[:], in0=x_[:], scalar=ALPHA, in1=tm[:], op0=ALU.mult, op1=ALU.add),
                  [xk, "tm6"], ["xr6"])
                for c in range(2):
                    V(lambda e, c=c: e.bn_stats(out=stats[:, c, :], in_=xr[:, c * 512:(c + 1) * 512]), ["xr6"], ["stats6"])
                V(lambda e: e.bn_aggr(out=mv[:], in_=stats[:].rearrange("p a b -> p (a b)")), ["stats6"], ["mv6_"])
                V(lambda e: e.tensor_scalar_add(out=rstd[:], in0=mv[:, 1:2], scalar1=EPS), ["mv6_"], ["rstd6"])
                A(lambda e: e.activation(out=rstd[:], in_=rstd[:], func=AF.Sqrt), ["rstd6"], ["rstd6"])
                V(lambda e: e.reciprocal(out=rstd[:], in_=rstd[:]), ["rstd6"], ["rstd6"])
                V(lambda e: e.tensor_scalar(out=tm[:], in0=xr[:], scalar1=mv[:, 0:1], scalar2=rstd[:, 0:1],
                                            op0=ALU.subtract, op1=ALU.mult), ["xr6", "mv6_", "rstd6"], ["tm6"])
                V(lambda e: e.tensor_tensor(out=tm[:], in0=tm[:], in1=lnrow[:, 2, :], op=ALU.mult), ["tm6", "lnrow"], ["tm6"])
                V(lambda e, o_=o_: e.tensor_tensor(out=o_[:], in0=tm[:], in1=lnrow[:, 3, :], op=ALU.add), ["tm6", "lnrow"], [ok])
                DS(lambda e, t=t, o_=o_: e.dma_start(out=out_d[t * 128:(t + 1) * 128, :], in_=o_[:]), [ok], ["out_d"])
            P.barrier()
    return nc


def _prep(inputs):
    f32 = np.float32
    g = {k: np.asarray(v) for k, v in inputs.items()}
    bf = ml_dtypes.bfloat16
    com = {}
    com["w_ada"] = np.ascontiguousarray(g["w_ada"][0], f32)
    ba = g["b_ada"][0]
    com["bada_fm"] = np.ascontiguousarray(ba[:2048].reshape(16, 128).T, f32)
    com["bada_row"] = np.ascontiguousarray(ba[2048:].reshape(1, 4096), f32)
    w_in = g["w_in"][0]
    ws5 = np.zeros((D, 4, 4, 32), f32)
    ws5[:, :, :, :16] = w_in[:, :256].reshape(D, 4, 4, 16)
    com["w_in_s5"] = ws5.reshape(D, 512)
    com["w_in_u"] = np.ascontiguousarray(w_in[:, 256:1024], f32)
    com["w_in_v"] = np.ascontiguousarray(w_in[:, 1024:1792], f32)
    com["gm_wsT"] = np.ascontiguousarray(g["gm_ws"][0].transpose(2, 0, 1), f32)
    com["gm_bs_row"] = np.ascontiguousarray(g["gm_bs"][0].reshape(1, 768), f32)
    are = g["s5_a_re"][0]; aim = g["s5_a_im"][0]; ls = g["s5_log_step"][0]
    com["are_row"] = np.ascontiguousarray(are.reshape(1, 2048), f32)
    com["aim_row"] = np.ascontiguousarray(aim.reshape(1, 2048), f32)
    com["ls_row"] = np.ascontiguousarray(np.repeat(ls.reshape(32), 64).reshape(1, 2048), f32)

    def pp(a):
        return np.ascontiguousarray(a.reshape(2, 8, 2, 64).transpose(2, 3, 0, 1).reshape(128, 16), f32)
    com["are_pp"] = pp(are)
    com["aim_pp"] = pp(aim)
    com["ls_pp"] = pp(np.repeat(ls[:, :, None], 64, axis=2))
    for nm, src in (("WBre_raw", g["s5_b_re"][0]), ("WBim_raw", g["s5_b_im"][0])):
        w = np.zeros((128, 2, 8, 128), f32)
        for gp in range(8):
            for g2 in range(2):
                gi = 2 * gp + g2
                r0 = 64 * (gp % 2) + 32 * g2
                w[r0:r0 + 16, :, gp, 64 * g2:64 * g2 + 64] = src[:, gi].transpose(2, 0, 1)
        com[nm] = w.reshape(128, 2048)
    for nm, src in (("WCre_raw", g["s5_c_re"][0]), ("WCim_raw", g["s5_c_im"][0])):
        w = np.zeros((128, 2, 8, 128), f32)
        for gp in range(8):
            for g2 in range(2):
                gi = 2 * gp + g2
                c0 = 64 * (gp % 2) + 32 * g2
                w[64 * g2:64 * g2 + 64, :, gp, c0:c0 + 16] = src[:, gi].transpose(2, 0, 1)
        com[nm] = w.reshape(128, 2048)

    def padpp(v):
        o = np.zeros((4, 4, 32), f32)
        o[:, :, :16] = v.reshape(4, 4, 16)
        return np.ascontiguousarray(o.reshape(4, 128).T, f32)
    com["dpp"] = padpp(g["s5_d"][0])
    com["b_glu_pp"] = padpp(g["s5_b_glu"][0].reshape(16, 16))
    wgl = np.zeros((4, 4, 32, 4, 4, 32), f32)
    wgl[:, :, :16, :, :, :16] = g["s5_w_glu"][0].reshape(4, 4, 16, 4, 4, 16)
    com["w_glu_pad"] = wgl.reshape(512, 512)
    wo = np.zeros((1280, D), f32)
    wo5 = np.zeros((4, 4, 32, D), f32)
    wo5[:, :, :16, :] = g["w_out"][0][:256].reshape(4, 4, 16, D)
    wo[:512] = wo5.reshape(512, D)
    wo[512:] = g["w_out"][0][256:]
    com["w_out_pad"] = wo
    for k in ("ln1_g", "ln1_b", "ln2_g", "ln2_b"):
        com[k] = np.ascontiguousarray(g[k][0].reshape(1, D), f32)
    com["w_router"] = np.ascontiguousarray(g["w_router"][0], f32)
    com["ident_bf"] = np.eye(128, dtype=f32).astype(bf)
    com["ident_f"] = np.eye(128, dtype=f32)
    com["tri"] = np.triu(np.ones((128, 128), f32)).astype(bf)
    com["ones"] = np.ones((128, 128), f32).astype(bf)
    com["iota_row"] = np.arange(1024, dtype=f32).reshape(1, 1024)
    com["slot_pp"] = (np.arange(128, dtype=f32)[:, None] + 128.0 * np.arange(8, dtype=f32)[None, :]).astype(f32)
    maps = []
    for c in range(8):
        b, half = c // 2, c % 2
        m = dict(com)
        m["xs"] = np.ascontiguousarray(np.concatenate([g["ctx"][b], g["x"][b]], axis=0), f32)
        cv = np.stack([g["c"][b], g["c_ctx"]], axis=1)
        m["cT"] = np.ascontiguousarray(cv.reshape(8, 128, 2).transpose(1, 0, 2), f32)
        es = slice(8 * half, 8 * half + 8)
        m["wg"] = np.ascontiguousarray(g["moe_w_gate"][0][es], f32)
        m["wu"] = np.ascontiguousarray(g["moe_w_up"][0][es], f32)
        m["wd"] = np.ascontiguousarray(g["moe_w_down"][0][es], f32)
        hs = np.zeros((128, 16), f32)
        hs[:, es] = 1.0
        m["half_sel"] = hs
        m["tokidx"] = (half * 4096 + np.arange(32, dtype=np.int32)[None, :] * 128 + np.arange(128, dtype=np.int32)[:, None]).astype(np.int32)
        maps.append(m)
    return maps


def kernel(**inputs):
    maps = _prep(inputs)
    nc = build()
    res = run_bass_kernel_spmd(nc, maps, core_ids=list(range(8)))
    out = np.zeros((4, SEQ, D), np.float32)
    for c in range(8):
        b, half = c // 2, c % 2
        out[b, half * 4096:(half + 1) * 4096] = res.results[c]["out"]
    return out
```

```python
import contextlib
import math
import numpy as np
import ml_dtypes
import concourse.bass as bass
import concourse.mybir as mybir
from concourse.bass_utils import run_bass_kernel_spmd

F32 = mybir.dt.float32
BF16 = mybir.dt.bfloat16
I32 = mybir.dt.int32
AF = mybir.ActivationFunctionType
ALU = mybir.AluOpType
AX = mybir.AxisListType

D = 1024
SEQ = 8192
CTX = 256
NT = SEQ // 128
NTC = CTX // 128
TOT = SEQ + CTX
DFF = 2816
NFT = DFF // 128
NE = 8
CAP = 1024
ALPHA = 2.0 ** 0.25
EPS = 1e-6
TWO_PI = 6.283185
SEG = 1024

ENGS = ("sync", "scalar", "vector", "gpsimd", "tensor")
DMA_K = 8


class Prog:
    def __init__(self, nc, stack):
        self.nc = nc
        self.eng = {"sync": nc.sync, "scalar": nc.scalar, "vector": nc.vector,
                    "gpsimd": nc.gpsimd, "tensor": nc.tensor}
        self.sems = {}
        for e in ENGS:
            self.sems["c" + e] = stack.enter_context(nc.semaphore("c" + e))
        for e in ("sync", "scalar", "gpsimd"):
            for i in range(DMA_K):
                n = "d%s%d" % (e, i)
                self.sems[n] = stack.enter_context(nc.semaphore(n))
        self.ccnt = {e: 0 for e in ENGS}
        self.dcnt = {e: 0 for e in ENGS}
        self.lastw = {}
        self.readers = {}
        self.known = {e: {} for e in ENGS}
        self.latest = {}

    def _need(self, eng, tok):
        if tok is None:
            return
        name, val = tok
        if name == "c" + eng and eng in ("tensor", "sync"):
            return
        if self.known[eng].get(name, 0) >= val:
            return
        self.known[eng][name] = val
        self.eng[eng].wait_ge(self.sems[name], val)

    def op(self, eng, fn, reads=(), writes=(), dma=False, sem_inc=None):
        for k in reads:
            self._need(eng, self.lastw.get(k))
        for k in writes:
            self._need(eng, self.lastw.get(k))
            for t in self.readers.get(k, ()):
                self._need(eng, t)
        if dma:
            i = self.dcnt[eng]
            self.dcnt[eng] += 1
            name = "d%s%d" % (eng, i % DMA_K)
            inc = 16 if sem_inc is None else sem_inc
            val = self.latest.get(name, 0) + inc
            if i >= DMA_K:
                self._need(eng, (name, self.latest.get(name, 0)))
        else:
            self.ccnt[eng] += 1
            name = "c" + eng
            val = self.ccnt[eng]
            inc = 1
        tok = (name, val)
        ins = fn(self.eng[eng])
        ins.then_inc(self.sems[name], inc)
        self.latest[name] = val
        for k in writes:
            self.lastw[k] = tok
            self.readers[k] = []
        for k in reads:
            if k not in writes:
                self.readers.setdefault(k, []).append(tok)
        return tok

    def barrier(self):
        toks = list(self.latest.items())
        for e in ENGS:
            for t in toks:
                self._need(e, t)
        self.lastw = {}
        self.readers = {}


def build(debug=0):
    nc = bass.Bass("TRN2", target_bir_lowering=False)

    def din(name, shape, dt=F32):
        return nc.dram_tensor(name, list(shape), dt, kind="ExternalInput").ap()

    def dscr(name, shape, dt=F32):
        return nc.dram_tensor(name, list(shape), dt).ap()

    xs = din("xs", [TOT, D])
    cT = din("cT", [128, 8, 2])
    w_ada = din("w_ada", [D, 6 * D])
    bada_fm = din("bada_fm", [128, 16])
    bada_row = din("bada_row", [1, 4 * D])
    w_in_s5 = din("w_in_s5", [D, 512])
    w_in_u = din("w_in_u", [D, 768])
    w_in_v = din("w_in_v", [D, 768])
    gm_wsT = din("gm_wsT", [128, 6, 128])
    gm_bs_row = din("gm_bs_row", [1, 768])
    are_row = din("are_row", [1, 2048])
    aim_row = din("aim_row", [1, 2048])
    ls_row = din("ls_row", [1, 2048])
    are_pp = din("are_pp", [128, 16])
    aim_pp = din("aim_pp", [128, 16])
    ls_pp = din("ls_pp", [128, 16])
    WBre_raw = din("WBre_raw", [128, 2048])
    WBim_raw = din("WBim_raw", [128, 2048])
    WCre_raw = din("WCre_raw", [128, 2048])
    WCim_raw = din("WCim_raw", [128, 2048])
    dpp = din("dpp", [128, 4])
    w_glu_pad = din("w_glu_pad", [512, 512])
    b_glu_pp = din("b_glu_pp", [128, 4])
    w_out_pad = din("w_out_pad", [1280, D])
    ln1_g = din("ln1_g", [1, D])
    ln1_b = din("ln1_b", [1, D])
    ln2_g = din("ln2_g", [1, D])
    ln2_b = din("ln2_b", [1, D])
    w_router = din("w_router", [D, 16])
    if debug in (0, 3):
        wg = din("wg", [NE, D, DFF])
        wu = din("wu", [NE, D, DFF])
        wd = din("wd", [NE, DFF, D])
    ident_bf_d = din("ident_bf", [128, 128], BF16)
    ident_f_d = din("ident_f", [128, 128])
    tri_d = din("tri", [128, 128], BF16)
    ones_d = din("ones", [128, 128], BF16)
    iota_row_d = din("iota_row", [1, 1024])
    slot_pp_d = din("slot_pp", [128, 8])
    half_sel = din("half_sel", [128, 16])
    tokidx_d = din("tokidx", [128, NT // 2], I32)

    out_d = nc.dram_tensor("out", [SEQ // 2, D], F32, kind="ExternalOutput").ap()
    okind = {"kind": "ExternalOutput"} if debug else {}
    xnew_d = nc.dram_tensor("xnew_s", [SEQ, D], F32, **okind).ap()
    hrow_d = nc.dram_tensor("hrow_s", [SEQ, D], BF16, **okind).ap()
    catT_d = dscr("catT_s", [10, 128, SEQ], BF16)
    yf_d = dscr("yf_s", [4, 128, SEQ], F32)
    aff_d = nc.dram_tensor("aff_s", [SEQ, 16], F32, **okind).ap()
    if debug:
        dbg_idx = nc.dram_tensor("dbg_idx", [128, NE * 8], I32, kind="ExternalOutput").ap()
        dbg_gate = nc.dram_tensor("dbg_gate", [128, NE * 8], F32, kind="ExternalOutput").ap()
    cin_d = dscr("cin_s", [NE * NT, 128], F32)
    moe_d = nc.dram_tensor("moe_s", [SEQ, D], F32, **({"kind": "ExternalOutput"} if debug == 3 else {})).ap()
    moe_r = dscr("moe_r", [SEQ, D], F32)

    with contextlib.ExitStack() as gst:
        P = Prog(nc, gst)

        _uid = [0]

        def sb(st, name, shape, dt=F32):
            _uid[0] += 1
            return st.enter_context(nc.sbuf_tensor("%s_%d" % (name, _uid[0]), list(shape), dt))

        def ps(st, name, shape, dt=F32):
            _uid[0] += 1
            return st.enter_context(nc.psum_tensor("%s_%d" % (name, _uid[0]), list(shape), dt))

        def V(fn, r=(), w=()):
            return P.op("vector", fn, r, w)

        def A(fn, r=(), w=()):
            return P.op("scalar", fn, r, w)

        def G(fn, r=(), w=()):
            return P.op("gpsimd", fn, r, w)

        def T(fn, r=(), w=()):
            return P.op("tensor", fn, r, w)

        def DS(fn, r=(), w=()):
            return P.op("sync", fn, r, w, dma=True)

        def DG(fn, r=(), w=()):
            return P.op("gpsimd", fn, r, w, dma=True)

        def bc(ap_, shape):
            return ap_.to_broadcast(list(shape))

        ident_bf = sb(gst, "ident_bf_t", [128, 128], BF16)
        ident_f = sb(gst, "ident_f_t", [128, 128])
        ones_bf = sb(gst, "ones_t", [128, 128], BF16)
        tri_bf = sb(gst, "tri_t", [128, 128], BF16)
        modfm = sb(gst, "modfm", [128, 2, 8, 2])
        fin3 = sb(gst, "fin3", [128, 3, D])
        idx_all = sb(gst, "idx_all", [128, NE, 8], I32)
        gate_all = sb(gst, "gate_all", [128, NE, 8])
        early = contextlib.ExitStack()
        iota_r = sb(early, "iota_r", [128, 1024])
        modrow = sb(early, "modrow", [128, 4, D])
        lnrow = sb(early, "lnrow", [128, 4, D])
        aff = sb(early, "aff", [128, NT, 16])
        DS(lambda e: e.dma_start(out=ident_bf[:], in_=ident_bf_d), w=["ident_bf"])
        DS(lambda e: e.dma_start(out=ident_f[:], in_=ident_f_d), w=["ident_f"])
        DS(lambda e: e.dma_start(out=ones_bf[:], in_=ones_d), w=["ones"])
        DS(lambda e: e.dma_start(out=tri_bf[:], in_=tri_d), w=["tri"])
        DS(lambda e: e.dma_start(out=iota_r[:], in_=bc(iota_row_d, [128, 1024])), w=["iota_r"])
        for i, a in enumerate((ln1_g, ln1_b, ln2_g, ln2_b)):
            DS(lambda e, i=i, a=a: e.dma_start(out=lnrow[:, i, :], in_=bc(a, [128, D])), w=["lnrow"])

        with contextlib.ExitStack() as st:
            ct = sb(st, "ct", [128, 8, 2])
            sc = sb(st, "sc", [128, 8, 2])
            scb = sb(st, "scb", [128, 8, 128])
            wa = sb(st, "wa", [128, 8, D])
            bfm = sb(st, "bfm", [128, 16])
            brow = sb(st, "brow", [128, 4 * D])
            pfm = ps(st, "pfm", [128, 8, 2])
            prow = ps(st, "prow", [128, D])
            DS(lambda e: e.dma_start(out=ct[:], in_=cT), w=["ct"])
            DS(lambda e: e.dma_start(out=bfm[:], in_=bada_fm), w=["bfm"])
            DS(lambda e: e.dma_start(out=brow[:], in_=bc(bada_row, [128, 4 * D])), w=["brow"])
            A(lambda e: e.activation(out=sc[:], in_=ct[:], func=AF.Silu), ["ct"], ["sc"])
            V(lambda e: e.tensor_copy(out=scb[:], in_=bc(sc[:, :, 0:1], [128, 8, 128])), ["sc"], ["scb"])
            wav = w_ada.rearrange("(kt p) f -> p kt f", p=128)
            for ch in range(6):
                DS(lambda e, ch=ch: e.dma_start(out=wa[:], in_=wav[:, :, ch * D:(ch + 1) * D]), w=["wa"])
                if ch < 2:
                    for ft in range(8):
                        for kt in range(8):
                            T(lambda e, ft=ft, kt=kt: e.matmul(pfm[:, ft, :], lhsT=wa[:, kt, ft * 128:(ft + 1) * 128],
                                                               rhs=sc[:, kt, :], start=(kt == 0), stop=(kt == 7)),
                              ["wa", "sc"], ["pfm"])
                    V(lambda e, ch=ch: e.tensor_tensor(out=modfm[:, ch, :, :], in0=pfm[:],
                                                       in1=bc(bfm[:, ch * 8:(ch + 1) * 8].unsqueeze(2), [128, 8, 2]),
                                                       op=ALU.add), ["pfm", "bfm"], ["modfm"])
                else:
                    for nh in range(2):
                        for kt in range(8):
                            T(lambda e, nh=nh, kt=kt: e.matmul(prow[:, nh * 512:(nh + 1) * 512], lhsT=scb[:, kt, :],
                                                               rhs=wa[:, kt, nh * 512:(nh + 1) * 512],
                                                               start=(kt == 0), stop=(kt == 7)),
                              ["wa", "scb"], ["prow"])
                    V(lambda e, ch=ch: e.tensor_tensor(out=modrow[:, ch - 2, :], in0=prow[:],
                                                       in1=brow[:, (ch - 2) * D:(ch - 1) * D], op=ALU.add),
                      ["prow", "brow"], ["modrow"])
            V(lambda e: e.tensor_scalar_add(out=modfm[:, 1, :, :], in0=modfm[:, 1, :, :], scalar1=1.0), ["modfm"], ["modfm"])
            V(lambda e: e.tensor_scalar_add(out=modrow[:, 2, :], in0=modrow[:, 2, :], scalar1=1.0), ["modrow"], ["modrow"])
            V(lambda e: e.tensor_copy(out=fin3[:, 0, :], in_=modrow[:, 3, :]), ["modrow"], ["fin3"])
            V(lambda e: e.tensor_copy(out=fin3[:, 1:3, :], in_=lnrow[:, 2:4, :]), ["lnrow"], ["fin3"])
            P.barrier()

        with contextlib.ExitStack() as mst:
            U = sb(mst, "U", [128, 4, TOT], BF16)
            wst_ = contextlib.ExitStack()
            wS = sb(wst_, "wS", [128, 8, 512], BF16)
            wU = sb(wst_, "wU", [128, 8, 768], BF16)
            wV = sb(wst_, "wV", [128, 8, 768], BF16)
            wsT = sb(wst_, "wsT", [128, 6, 128], BF16)
            bsrow = sb(wst_, "bsrow", [128, 768])
            with contextlib.ExitStack() as st:
                stg = sb(st, "stg", [128, 8, 768])
                for (src, dst, n, nm) in ((w_in_s5, wS, 512, "wS"), (w_in_u, wU, 768, "wU"), (w_in_v, wV, 768, "wV")):
                    DS(lambda e, src=src, n=n: e.dma_start(out=stg[:, :, 0:n], in_=src.rearrange("(kt p) f -> p kt f", p=128)),
                       w=["stg"])
                    V(lambda e, dst=dst, n=n: e.tensor_copy(out=dst[:], in_=stg[:, :, 0:n]), ["stg"], [nm])
                DS(lambda e: e.dma_start(out=stg[:, 0:6, 0:128], in_=gm_wsT), w=["stg"])
                V(lambda e: e.tensor_copy(out=wsT[:], in_=stg[:, 0:6, 0:128]), ["stg"], ["wsT"])
                DS(lambda e: e.dma_start(out=bsrow[:], in_=bc(gm_bs_row, [128, 768])), w=["bsrow"])
                P.barrier()

            with contextlib.ExitStack() as st:
                xt = [sb(st, "xt%d" % i, [128, D]) for i in range(2)]
                xn = sb(st, "xn", [128, D], BF16)
                xmT = sb(st, "xmT", [128, 8, 128], BF16)
                stats = sb(st, "stats", [128, 2, 6])
                mv = sb(st, "mv", [128, 2])
                rstd = sb(st, "rstd", [128, 1])
                uT = sb(st, "uT", [128, 6, 128])
                vv = sb(st, "vv", [128, 6, 128])
                vc = sb(st, "vc", [128, 6, 128])
                vln = sb(st, "vln", [128, 6, 128], BF16)
                st6 = sb(st, "st6", [128, 6, 6])
                mv6 = sb(st, "mv6", [128, 6, 2])
                rs6 = sb(st, "rs6", [128, 6])
                gmt = sb(st, "gmt", [128, 6, 128])
                gmb = [sb(st, "gmb%d" % i, [128, 6, 128], BF16) for i in range(2)]
                pT = ps(st, "pT", [128, D], BF16)
                pS = ps(st, "pS", [128, 4, 128])
                pU = ps(st, "pU", [128, 8, 128])
                pV = ps(st, "pV", [128, D])
                pM = ps(st, "pM", [128, 8, 128])
                for t in range(NTC + NT):
                    lat = t >= NTC
                    col = 0 if lat else 1
                    x_ = xt[t % 2]
                    xk = "xt%d" % (t % 2)
                    DS(lambda e, t=t, x_=x_: e.dma_start(out=x_[:], in_=xs[t * 128:(t + 1) * 128, :]), w=[xk])
                    for c in range(2):
                        V(lambda e, c=c, x_=x_: e.bn_stats(out=stats[:, c, :], in_=x_[:, c * 512:(c + 1) * 512]), [xk], ["stats"])
                    V(lambda e: e.bn_aggr(out=mv[:], in_=stats[:].rearrange("p a b -> p (a b)")), ["stats"], ["mv"])
                    V(lambda e: e.tensor_scalar_add(out=rstd[:], in0=mv[:, 1:2], scalar1=EPS), ["mv"], ["rstd"])
                    A(lambda e: e.activation(out=rstd[:], in_=rstd[:], func=AF.Sqrt), ["rstd"], ["rstd"])
                    V(lambda e: e.reciprocal(out=rstd[:], in_=rstd[:]), ["rstd"], ["rstd"])
                    V(lambda e, x_=x_: e.tensor_scalar(out=xn[:], in0=x_[:], scalar1=mv[:, 0:1], scalar2=rstd[:, 0:1],
                                                       op0=ALU.subtract, op1=ALU.mult), [xk, "mv", "rstd"], ["xn"])
                    for kt in range(8):
                        T(lambda e, kt=kt: e.transpose(out=pT[:, kt * 128:(kt + 1) * 128], in_=xn[:, kt * 128:(kt + 1) * 128],
                                                       identity=ident_bf[:]), ["xn", "ident_bf"], ["pT"])
                    for kt in range(8):
                        A(lambda e, kt=kt, col=col: e.activation(out=xmT[:, kt, :], in_=pT[:, kt * 128:(kt + 1) * 128],
                                                                 func=AF.Identity, scale=modfm[:, 1, kt, col:col + 1],
                                                                 bias=modfm[:, 0, kt, col:col + 1]),
                          ["pT", "modfm"], ["xmT"])
                    for ct_ in range(4):
                        for kt in range(8):
                            T(lambda e, ct_=ct_, kt=kt: e.matmul(pS[:, ct_, :], lhsT=wS[:, kt, ct_ * 128:(ct_ + 1) * 128],
                                                                 rhs=xmT[:, kt, :], start=(kt == 0), stop=(kt == 7)),
                              ["wS", "xmT"], ["pS"])
                    V(lambda e, t=t: e.tensor_copy(out=U[:, :, t * 128:(t + 1) * 128], in_=pS[:]), ["pS"], ["U"])
                    if not lat:
                        continue
                    tl = t - NTC
                    for ct_ in range(6):
                        for kt in range(8):
                            T(lambda e, ct_=ct_, kt=kt: e.matmul(pU[:, ct_, :], lhsT=wU[:, kt, ct_ * 128:(ct_ + 1) * 128],
                                                                 rhs=xmT[:, kt, :], start=(kt == 0), stop=(kt == 7)),
                              ["wU", "xmT"], ["pU"])
                    A(lambda e: e.activation(out=uT[:], in_=pU[:, 0:6, :], func=AF.Gelu_apprx_tanh), ["pU"], ["uT"])
                    for (c0, c1) in ((0, 512), (512, 768)):
                        for kt in range(8):
                            T(lambda e, c0=c0, c1=c1, kt=kt: e.matmul(pV[:, c0:c1], lhsT=xmT[:, kt, :], rhs=wV[:, kt, c0:c1],
                                                                      start=(kt == 0), stop=(kt == 7)),
                              ["wV", "xmT"], ["pV"])
                    A(lambda e: e.activation(out=vv[:].rearrange("p a b -> p (a b)"), in_=pV[:, 0:768], func=AF.Gelu_apprx_tanh),
                      ["pV"], ["vv"])
                    for g in range(6):
                        V(lambda e, g=g: e.bn_stats(out=st6[:, g, :], in_=vv[:, g, :]), ["vv"], ["st6"])
                    for g in range(6):
                        V(lambda e, g=g: e.bn_aggr(out=mv6[:, g, :], in_=st6[:, g, :]), ["st6"], ["mv6"])
                    V(lambda e: e.tensor_scalar_add(out=rs6[:], in0=mv6[:, :, 1], scalar1=EPS), ["mv6"], ["rs6"])
                    A(lambda e: e.activation(out=rs6[:], in_=rs6[:], func=AF.Sqrt), ["rs6"], ["rs6"])
                    V(lambda e: e.reciprocal(out=rs6[:], in_=rs6[:]), ["rs6"], ["rs6"])
                    V(lambda e: e.tensor_tensor(out=vc[:], in0=vv[:], in1=bc(mv6[:, :, 0:1], [128, 6, 128]), op=ALU.subtract),
                      ["vv", "mv6"], ["vc"])
                    V(lambda e: e.tensor_tensor(out=vln[:], in0=vc[:], in1=bc(rs6[:].unsqueeze(2), [128, 6, 128]), op=ALU.mult),
                      ["vc", "rs6"], ["vln"])
                    for g in range(6):
                        T(lambda e, g=g: e.matmul(pM[:, g, :], lhsT=vln[:, g, :], rhs=wsT[:, g, :], start=True, stop=True),
                          ["vln", "wsT"], ["pM"])
                    V(lambda e: e.tensor_tensor(out=gmt[:], in0=pM[:, 0:6, :], in1=bsrow[:].rearrange("p (a b) -> p a b", a=6),
                                                op=ALU.add), ["pM", "bsrow"], ["gmt"])
                    gb = gmb[tl % 2]
                    gk = "gmb%d" % (tl % 2)
                    V(lambda e, gb=gb: e.tensor_tensor(out=gb[:], in0=gmt[:], in1=uT[:], op=ALU.mult), ["gmt", "uT"], [gk])
                    DS(lambda e, gb=gb, tl=tl: e.dma_start(out=catT_d[4:10, :, tl * 128:(tl + 1) * 128].rearrange("a p t -> p a t"),
                                                           in_=gb[:]), [gk], ["catT_gm"])
                P.barrier()
            wst_.close()

            with contextlib.ExitStack() as st:
                WB = sb(st, "WB", [128, 2, 2048], BF16)
                WC = sb(st, "WC", [128, 2, 2048], BF16)
                rho_pp = sb(st, "rho_pp", [128, 16])
                f_pp = sb(st, "f_pp", [128, 16])
                dsc = sb(st, "dsc", [128, 4])
                bglu = sb(st, "bglu", [128, 4])
                wglu = sb(st, "wglu", [128, 4, 512], BF16)
                for dr_ in range(4):
                  csl = slice(dr_ * 512, (dr_ + 1) * 512)
                  with contextlib.ExitStack() as s2:
                        r = {n: sb(s2, "r_" + n, [128, 512]) for n in
                             ("are", "aim", "stp", "rho", "f", "y", "y2", "sn", "cs", "x", "yv", "den", "cr", "ci", "t1", "t2", "bre", "bim")}
                        ri_ = sb(s2, "r_int", [128, 512], I32)
                        DS(lambda e: e.dma_start(out=r["are"][:], in_=bc(are_row[:, csl], [128, 512])), w=["are"])
                        DS(lambda e: e.dma_start(out=r["aim"][:], in_=bc(aim_row[:, csl], [128, 512])), w=["aim"])
                        DS(lambda e: e.dma_start(out=r["stp"][:], in_=bc(ls_row[:, csl], [128, 512])), w=["stp"])
                        DS(lambda e: e.dma_start(out=r["bre"][:], in_=WBre_raw[:, csl]), w=["bre"])
                        DS(lambda e: e.dma_start(out=r["bim"][:], in_=WBim_raw[:, csl]), w=["bim"])

                        def vt(o, a, b, op):
                            V(lambda e: e.tensor_tensor(out=r[o][:], in0=r[a][:], in1=r[b][:], op=op), [a, b], [o])

                        def frac(o, i):
                            V(lambda e: e.tensor_copy(out=ri_[:], in_=r[i][:]), [i], ["rint"])
                            V(lambda e: e.tensor_copy(out=r["t1"][:], in_=ri_[:]), ["rint"], ["t1"])
                            vt(o, i, "t1", ALU.subtract)

                        A(lambda e: e.activation(out=r["stp"][:], in_=r["stp"][:], func=AF.Exp), ["stp"], ["stp"])
                        vt("rho", "are", "stp", ALU.mult)
                        A(lambda e: e.activation(out=r["rho"][:], in_=r["rho"][:], func=AF.Exp), ["rho"], ["rho"])
                        vt("f", "aim", "stp", ALU.mult)
                        V(lambda e: e.tensor_scalar_mul(out=r["f"][:], in0=r["f"][:], scalar1=1.0 / (2 * math.pi)), ["f"], ["f"])
                        frac("y", "f")
                        A(lambda e: e.activation(out=r["sn"][:], in_=r["y"][:], func=AF.Sin, scale=TWO_PI), ["y"], ["sn"])
                        V(lambda e: e.tensor_scalar_add(out=r["y2"][:], in0=r["y"][:], scalar1=0.25), ["y"], ["y2"])
                        frac("y2", "y2")
                        A(lambda e: e.activation(out=r["cs"][:], in_=r["y2"][:], func=AF.Sin, scale=TWO_PI), ["y2"], ["cs"])
                        vt("x", "rho", "cs", ALU.mult)
                        V(lambda e: e.tensor_scalar_add(out=r["x"][:], in0=r["x"][:], scalar1=-1.0), ["x"], ["x"])
                        vt("yv", "rho", "sn", ALU.mult)
                        vt("den", "are", "are", ALU.mult)
                        vt("t2", "aim", "aim", ALU.mult)
                        vt("den", "den", "t2", ALU.add)
                        V(lambda e: e.reciprocal(out=r["den"][:], in_=r["den"][:]), ["den"], ["den"])
                        vt("cr", "x", "are", ALU.mult)
                        vt("t2", "yv", "aim", ALU.mult)
                        vt("cr", "cr", "t2", ALU.add)
                        vt("cr", "cr", "den", ALU.mult)
                        vt("ci", "yv", "are", ALU.mult)
                        vt("t2", "x", "aim", ALU.mult)
                        vt("ci", "ci", "t2", ALU.subtract)
                        vt("ci", "ci", "den", ALU.mult)
                        vt("t1", "cr", "bre", ALU.mult)
                        vt("t2", "ci", "bim", ALU.mult)
                        V(lambda e: e.tensor_tensor(out=WB[:, 0, csl], in0=r["t1"][:], in1=r["t2"][:], op=ALU.subtract), ["t1", "t2"], ["WB"])
                        vt("t1", "cr", "bim", ALU.mult)
                        vt("t2", "ci", "bre", ALU.mult)
                        V(lambda e: e.tensor_tensor(out=WB[:, 1, csl], in0=r["t1"][:], in1=r["t2"][:], op=ALU.add), ["t1", "t2"], ["WB"])
                        DS(lambda e: e.dma_start(out=r["bre"][:], in_=WCre_raw[:, csl]), w=["bre"])
                        DS(lambda e: e.dma_start(out=r["bim"][:], in_=WCim_raw[:, csl]), w=["bim"])
                        V(lambda e: e.tensor_copy(out=WC[:, 0, csl], in_=r["bre"][:]), ["bre"], ["WC"])
                        V(lambda e: e.tensor_scalar_mul(out=WC[:, 1, csl], in0=r["bim"][:], scalar1=-1.0), ["bim"], ["WC"])

                        P.barrier()
                with contextlib.ExitStack() as s2:
                    pa = sb(s2, "pa", [128, 16])
                    pb = sb(s2, "pb", [128, 16])
                    pc = sb(s2, "pc", [128, 16])
                    DS(lambda e: e.dma_start(out=pa[:], in_=are_pp), w=["pa"])
                    DS(lambda e: e.dma_start(out=pb[:], in_=aim_pp), w=["pb"])
                    DS(lambda e: e.dma_start(out=pc[:], in_=ls_pp), w=["pc"])
                    A(lambda e: e.activation(out=pc[:], in_=pc[:], func=AF.Exp), ["pc"], ["pc"])
                    V(lambda e: e.tensor_tensor(out=rho_pp[:], in0=pa[:], in1=pc[:], op=ALU.mult), ["pa", "pc"], ["rho_pp"])
                    A(lambda e: e.activation(out=rho_pp[:], in_=rho_pp[:], func=AF.Exp), ["rho_pp"], ["rho_pp"])
                    V(lambda e: e.tensor_tensor(out=f_pp[:], in0=pb[:], in1=pc[:], op=ALU.mult), ["pb", "pc"], ["f_pp"])
                    V(lambda e: e.tensor_scalar_mul(out=f_pp[:], in0=f_pp[:], scalar1=1.0 / (2 * math.pi)), ["f_pp"], ["f_pp"])
                    DS(lambda e: e.dma_start(out=dsc[:], in_=dpp), w=["dsc"])
                    DS(lambda e: e.dma_start(out=bglu[:], in_=b_glu_pp), w=["bglu"])
                    gst_ = sb(s2, "gst_", [128, 4, 512])
                    DS(lambda e: e.dma_start(out=gst_[:], in_=w_glu_pad.rearrange("(kt p) f -> p kt f", p=128)), w=["gst_"])
                    V(lambda e: e.tensor_copy(out=wglu[:], in_=gst_[:]), ["gst_"], ["wglu"])
                    P.barrier()

                L = SEG
                H = 512
                arg1 = sb(st, "arg", [128, L])
                arg = [arg1, arg1]
                argi1 = sb(st, "argi", [128, L], I32)
                argi = [argi1, argi1]
                yv1 = sb(st, "yv", [128, L])
                yv = [yv1, yv1]
                s2 = arg
                sq = arg
                cs = [sb(st, "cs%d" % i, [128, L], BF16) for i in range(2)]
                sn = [sb(st, "sn%d" % i, [128, L], BF16) for i in range(2)]
                bre = [sb(st, "bre%d" % i, [128, L], BF16) for i in range(2)]
                bim = [sb(st, "bim%d" % i, [128, L], BF16) for i in range(2)]
                dre = sb(st, "dre", [128, L], BF16)
                dim_ = sb(st, "dim", [128, L], BF16)
                tA = sb(st, "tA", [128, L], BF16)
                tB = sb(st, "tB", [128, L], BF16)
                zre = sb(st, "zre", [128, L], BF16)
                zim = sb(st, "zim", [128, L], BF16)
                Sre1 = sb(st, "Sre", [128, L], BF16)
                Sim1 = sb(st, "Sim", [128, L], BF16)
                Sre = [Sre1, Sre1]
                Sim = [Sim1, Sim1]
                zst = sb(st, "zst", [128, 16, 2])
                ysb1 = sb(st, "ysb", [128, L])
                ysb = [ysb1, ysb1]
                yfl1 = sb(st, "yfl", [128, L])
                yfl = [yfl1, yfl1]
                gg = sb(st, "gg", [128, 4, L], BF16)
                sg1 = sb(st, "sg", [128, L])
                sg = [sg1, sg1]
                s5o1 = sb(st, "s5o", [128, L], BF16)
                s5o = [s5o1, s5o1]
                pB = [[ps(st, "pB%d%d" % (h_, ri), [128, H]) for ri in range(2)] for h_ in range(2)]
                pY = ps(st, "pY", [128, L])
                pG = ps(st, "pG", [128, L])
                V(lambda e: e.memset(zst[:], 0.0), w=["zst"])
                colctr = [0]

                def s5_col(dr, gp, u_lo, n, t0, rev, readout):
                    ci_ = dr * 8 + gp
                    kt = gp // 2
                    r0 = 64 * (gp % 2)
                    wcol = slice(ci_ * 128, (ci_ + 1) * 128)
                    cb = colctr[0] % 2
                    colctr[0] += 1
                    arg_, argi_, yv_, s2_, sq_, cs_, sn_ = arg[cb], argi[cb], yv[cb], s2[cb], sq[cb], cs[cb], sn[cb]
                    bre_, bim_, Sre_, Sim_ = bre[cb], bim[cb], Sre[cb], Sim[cb]
                    k = lambda nm: nm if nm in ("arg", "yv", "Sre", "Sim") else "%s%d" % (nm, cb)
                    G(lambda e: e.tensor_scalar(out=arg_[:, 0:n], in0=iota_r[:, 0:n], scalar1=float(t0), scalar2=f_pp[:, ci_:ci_ + 1],
                                                op0=ALU.add, op1=ALU.mult), ["iota_r", "f_pp"], [k("arg")])
                    G(lambda e: e.tensor_copy(out=argi_[:, 0:n], in_=arg_[:, 0:n]), [k("arg")], ["argi"])
                    G(lambda e: e.tensor_tensor(out=yv_[:, 0:n], in0=arg_[:, 0:n], in1=argi_[:, 0:n], op=ALU.subtract),
                      [k("arg"), "argi"], [k("yv")])
                    A(lambda e: e.activation(out=sn_[:, 0:n], in_=yv_[:, 0:n], func=AF.Sin, scale=TWO_PI), [k("yv")], [k("sn")])
                    A(lambda e: e.activation(out=s2_[:, 0:n], in_=yv_[:, 0:n], func=AF.Sin, scale=TWO_PI / 2), [k("yv")], [k("arg")])
                    A(lambda e: e.activation(out=sq_[:, 0:n], in_=s2_[:, 0:n], func=AF.Square), [k("arg")], [k("arg")])
                    G(lambda e: e.tensor_scalar(out=cs_[:, 0:n], in0=sq_[:, 0:n], scalar1=-2.0, scalar2=1.0, op0=ALU.mult, op1=ALU.add),
                      [k("arg")], [k("cs")])
                    for hi_, c0 in enumerate(range(0, n, H)):
                        c1 = min(n, c0 + H)
                        for ri, dst, dk in ((0, bre_, k("bre")), (1, bim_, k("bim"))):
                            pb_ = pB[hi_][ri]
                            pk = "pB%d%d" % (hi_, ri)
                            T(lambda e, ri=ri, pb_=pb_, c0=c0, c1=c1: e.matmul(pb_[:, 0:c1 - c0], lhsT=WB[r0:r0 + 64, ri, wcol],
                                                                             rhs=U[r0:r0 + 64, kt, u_lo + c0:u_lo + c1],
                                                                             start=True, stop=True), ["WB", "U"], [pk])
                            A(lambda e, pb_=pb_, dst=dst, c0=c0, c1=c1: e.activation(out=dst[:, c0:c1], in_=pb_[:, 0:c1 - c0], func=AF.Identity),
                              [pk], [dk])

                    def tvn(t_):
                        if not rev:
                            return t_[:, 0:n]
                        return t_[:, 0:n][:, ::-1]

                    def vtt(o, ok, a_, ak, b_, bk, op):
                        V(lambda e: e.tensor_tensor(out=o, in0=a_, in1=b_, op=op), [ak, bk], [ok])

                    vtt(tA[:, 0:n], "tA", bre_[:, 0:n], k("bre"), tvn(cs_), k("cs"), ALU.mult)
                    vtt(tB[:, 0:n], "tB", bim_[:, 0:n], k("bim"), tvn(sn_), k("sn"), ALU.mult)
                    vtt(dre[:, 0:n], "dre", tA[:, 0:n], "tA", tB[:, 0:n], "tB", ALU.add)
                    vtt(tA[:, 0:n], "tA", bim_[:, 0:n], k("bim"), tvn(cs_), k("cs"), ALU.mult)
                    vtt(tB[:, 0:n], "tB", bre_[:, 0:n], k("bre"), tvn(sn_), k("sn"), ALU.mult)
                    vtt(dim_[:, 0:n], "dim", tA[:, 0:n], "tA", tB[:, 0:n], "tB", ALU.subtract)
                    for (src, dst, ri) in ((dre, zre, 0), (dim_, zim, 1)):
                        V(lambda e, src=src, dst=dst, ri=ri: e.tensor_tensor_scan(
                            out=tvn(dst), data0=bc(rho_pp[:, ci_:ci_ + 1], [128, n]), data1=tvn(src),
                            initial=zst[:, ci_, ri:ri + 1], op0=ALU.mult, op1=ALU.add),
                          ["dre" if ri == 0 else "dim", "rho_pp", "zst"], ["zre" if ri == 0 else "zim"])
                    last = 0 if rev else n - 1
                    V(lambda e: e.tensor_copy(out=zst[:, ci_, 0:1], in_=zre[:, last:last + 1]), ["zre"], ["zst"])
                    V(lambda e: e.tensor_copy(out=zst[:, ci_, 1:2], in_=zim[:, last:last + 1]), ["zim"], ["zst"])
                    if not readout:
                        return
                    vtt(tA[:, 0:n], "tA", zre[:, 0:n], "zre", tvn(cs_), k("cs"), ALU.mult)
                    vtt(tB[:, 0:n], "tB", zim[:, 0:n], "zim", tvn(sn_), k("sn"), ALU.mult)
                    vtt(Sre_[:, 0:n], k("Sre"), tA[:, 0:n], "tA", tB[:, 0:n], "tB", ALU.subtract)
                    vtt(tA[:, 0:n], "tA", zre[:, 0:n], "zre", tvn(sn_), k("sn"), ALU.mult)
                    vtt(tB[:, 0:n], "tB", zim[:, 0:n], "zim", tvn(cs_), k("cs"), ALU.mult)
                    vtt(Sim_[:, 0:n], k("Sim"), tA[:, 0:n], "tA", tB[:, 0:n], "tB", ALU.add)
                    first = (gp % 2 == 0)
                    for c0 in range(0, n, 512):
                        for ri, S_, sk in ((0, Sre_, k("Sre")), (1, Sim_, k("Sim"))):
                            T(lambda e, ri=ri, S_=S_, c0=c0: e.matmul(pY[:, c0:c0 + 512], lhsT=WC[:, ri, wcol], rhs=S_[:, c0:c0 + 512],
                                                                     start=(first and ri == 0), stop=((not first) and ri == 1)),
                              ["WC", sk], ["pY"])

                for gp in range(8):
                    s5_col(0, gp, 0, CTX, 0, False, False)
                cnt_ = 0
                for sgi in range(SEQ // L):
                    for kt in range(4):
                        for g2 in range(2):
                            s5_col(0, kt * 2 + g2, CTX + sgi * L, L, CTX + sgi * L, False, True)
                        yb = ysb[cnt_ % 2]
                        yk = "ysb"
                        cnt_ += 1
                        A(lambda e, yb=yb: e.activation(out=yb[:], in_=pY[:], func=AF.Identity), ["pY"], [yk])
                        DS(lambda e, kt=kt, sgi=sgi, yb=yb: e.dma_start(out=yf_d[kt, :, sgi * L:(sgi + 1) * L], in_=yb[:]), [yk], ["yf"])
                for gp in range(8):
                    s5_col(1, gp, 0, CTX, 0, True, False)
                for sb_i in range(SEQ // L):
                    sgi = SEQ // L - 1 - sb_i
                    for kt in range(4):
                        yl = yfl[cnt_ % 2]
                        ylk = "yfl"
                        yb = ysb[cnt_ % 2]
                        yk = "ysb"
                        cnt_ += 1
                        DS(lambda e, kt=kt, sgi=sgi, yl=yl: e.dma_start(out=yl[:], in_=yf_d[kt, :, sgi * L:(sgi + 1) * L]), ["yf"], [ylk])
                        for g2 in range(2):
                            s5_col(1, kt * 2 + g2, CTX + sgi * L, L, CTX + sb_i * L, True, True)
                        V(lambda e, yb=yb, yl=yl: e.tensor_tensor(out=yb[:], in0=pY[:], in1=yl[:], op=ALU.add), ["pY", ylk], [yk])
                        V(lambda e, kt=kt, sgi=sgi, yb=yb: e.scalar_tensor_tensor(out=yb[:], in0=U[:, kt, CTX + sgi * L:CTX + (sgi + 1) * L],
                                                                                  scalar=dsc[:, kt:kt + 1], in1=yb[:],
                                                                                  op0=ALU.mult, op1=ALU.add), ["U", "dsc", yk], [yk])
                        A(lambda e, kt=kt, yb=yb: e.activation(out=gg[:, kt, :], in_=yb[:], func=AF.Gelu_apprx_tanh), [yk], ["gg"])
                    for mt in range(4):
                        sg_ = sg[mt % 2]
                        sgk = "sg"
                        so_ = s5o[mt % 2]
                        sok = "s5o"
                        for c0 in range(0, L, 512):
                            for kt in range(4):
                                T(lambda e, mt=mt, c0=c0, kt=kt: e.matmul(pG[:, c0:c0 + 512], lhsT=wglu[:, kt, mt * 128:(mt + 1) * 128],
                                                                         rhs=gg[:, kt, c0:c0 + 512], start=(kt == 0), stop=(kt == 3)),
                                  ["wglu", "gg"], ["pG"])
                        A(lambda e, mt=mt, sg_=sg_: e.activation(out=sg_[:], in_=pG[:], func=AF.Sigmoid, bias=bglu[:, mt:mt + 1]), ["pG", "bglu"], [sgk])
                        G(lambda e, mt=mt, sg_=sg_, so_=so_: e.tensor_tensor(out=so_[:], in0=sg_[:], in1=gg[:, mt, :], op=ALU.mult), [sgk, "gg"], [sok])
                        DS(lambda e, mt=mt, sgi=sgi, so_=so_: e.dma_start(out=catT_d[mt, :, sgi * L:(sgi + 1) * L], in_=so_[:]), [sok], ["catT_s5"])
                P.barrier()
        P.barrier()

        with contextlib.ExitStack() as st:
            wo = sb(st, "wo", [128, 10, D], BF16)
            wr = sb(st, "wr", [128, 8, 16], BF16)
            with contextlib.ExitStack() as s2:
                stg = sb(s2, "stg3", [128, 10, D])
                DS(lambda e: e.dma_start(out=stg[:], in_=w_out_pad.rearrange("(kt p) f -> p kt f", p=128)), w=["stg3"])
                V(lambda e: e.tensor_copy(out=wo[:], in_=stg[:]), ["stg3"], ["wo"])
                DS(lambda e: e.dma_start(out=stg[:, 0:8, 0:16], in_=w_router.rearrange("(kt p) f -> p kt f", p=128)), w=["stg3"])
                V(lambda e: e.tensor_copy(out=wr[:], in_=stg[:, 0:8, 0:16]), ["stg3"], ["wr"])
                P.barrier()
            cat = [sb(st, "cat%d" % i, [128, 10, 128], BF16) for i in range(2)]
            xt3 = [sb(st, "x3_%d" % i, [128, D]) for i in range(2)]
            tmA = [sb(st, "tm%d" % i, [128, D]) for i in range(2)]
            xrA = [sb(st, "xr%d" % i, [128, D]) for i in range(2)]
            xnwA = [sb(st, "xnw%d" % i, [128, D]) for i in range(2)]
            hhA = [sb(st, "hh%d" % i, [128, D]) for i in range(2)]
            hbA = [sb(st, "hb%d" % i, [128, D], BF16) for i in range(2)]
            hTA = [sb(st, "hT%d" % i, [128, 8, 128], BF16) for i in range(2)]
            statsA = [sb(st, "stats3_%d" % i, [128, 2, 6]) for i in range(4)]
            mvA = [sb(st, "mv3_%d" % i, [128, 2]) for i in range(4)]
            rstdA = [sb(st, "rstd3_%d" % i, [128, 1]) for i in range(4)]
            lgA = [sb(st, "lg%d" % i, [128, 16]) for i in range(2)]
            mxA = [sb(st, "mx%d" % i, [128, 1]) for i in range(2)]
            smA = [sb(st, "sm%d" % i, [128, 1]) for i in range(2)]
            pMxA = [ps(st, "pMx%d" % i, [128, D]) for i in range(2)]
            pT3A = [ps(st, "pT3%d" % i, [128, D], BF16) for i in range(2)]
            pLA = [ps(st, "pL%d" % i, [128, 16]) for i in range(2)]

            def lnorm(src, sk, dst, dk, si):
                stats, mv, rstd = statsA[si], mvA[si], rstdA[si]
                k1, k2, k3 = "stats3_%d" % si, "mv3_%d" % si, "rstd3_%d" % si
                for c in range(2):
                    V(lambda e, c=c: e.bn_stats(out=stats[:, c, :], in_=src[:, c * 512:(c + 1) * 512]), [sk], [k1])
                V(lambda e: e.bn_aggr(out=mv[:], in_=stats[:].rearrange("p a b -> p (a b)")), [k1], [k2])
                V(lambda e: e.tensor_scalar_add(out=rstd[:], in0=mv[:, 1:2], scalar1=EPS), [k2], [k3])
                A(lambda e: e.activation(out=rstd[:], in_=rstd[:], func=AF.Sqrt), [k3], [k3])
                V(lambda e: e.reciprocal(out=rstd[:], in_=rstd[:]), [k3], [k3])
                V(lambda e: e.tensor_scalar(out=dst[:], in0=src[:], scalar1=mv[:, 0:1], scalar2=rstd[:, 0:1],
                                            op0=ALU.subtract, op1=ALU.mult), [sk, k2, k3], [dk])

            for t in range(NT):
                i2 = t % 2
                c_, ck = cat[i2], "cat%d" % i2
                x_, xk = xt3[i2], "x3_%d" % i2
                tm, tmk = tmA[i2], "tm%d" % i2
                xr, xrk = xrA[i2], "xr%d" % i2
                xnw, xnk = xnwA[i2], "xnw%d" % i2
                hh, hhk = hhA[i2], "hh%d" % i2
                hb, hbk = hbA[i2], "hb%d" % i2
                hT, hTk = hTA[i2], "hT%d" % i2
                lg, lgk = lgA[i2], "lg%d" % i2
                mx, mxk = mxA[i2], "mx%d" % i2
                sm, smk = smA[i2], "sm%d" % i2
                pMx, pMk = pMxA[i2], "pMx%d" % i2
                pT3, pTk = pT3A[i2], "pT3%d" % i2
                pL, pLk = pLA[i2], "pL%d" % i2
                DS(lambda e, t=t, c_=c_: e.dma_start(out=c_[:], in_=catT_d[:, :, t * 128:(t + 1) * 128].rearrange("a p t -> p a t")), w=[ck])
                DS(lambda e, t=t, x_=x_: e.dma_start(out=x_[:], in_=xs[CTX + t * 128:CTX + (t + 1) * 128, :]), w=[xk])
                for nh in range(2):
                    for kt in range(10):
                        T(lambda e, nh=nh, kt=kt, c_=c_, pMx=pMx: e.matmul(pMx[:, nh * 512:(nh + 1) * 512], lhsT=c_[:, kt, :],
                                                                           rhs=wo[:, kt, nh * 512:(nh + 1) * 512], start=(kt == 0), stop=(kt == 9)),
                          [ck, "wo"], [pMk])
                V(lambda e, tm=tm, pMx=pMx: e.tensor_tensor(out=tm[:], in0=pMx[:], in1=modrow[:, 0, :], op=ALU.mult), [pMk, "modrow"], [tmk])
                V(lambda e, x_=x_, xr=xr, tm=tm: e.scalar_tensor_tensor(out=xr[:], in0=x_[:], scalar=ALPHA, in1=tm[:], op0=ALU.mult, op1=ALU.add),
                  [xk, tmk], [xrk])
                lnorm(xr, xrk, tm, tmk, 2 * i2)
                G(lambda e, tm=tm: e.tensor_tensor(out=tm[:], in0=tm[:], in1=lnrow[:, 0, :], op=ALU.mult), [tmk, "lnrow"], [tmk])
                G(lambda e, tm=tm, xnw=xnw: e.tensor_tensor(out=xnw[:], in0=tm[:], in1=lnrow[:, 1, :], op=ALU.add), [tmk, "lnrow"], [xnk])
                DS(lambda e, t=t, xnw=xnw: e.dma_start(out=xnew_d[t * 128:(t + 1) * 128, :], in_=xnw[:]), [xnk], ["xnew_d"])
                lnorm(xnw, xnk, hh, hhk, 2 * i2 + 1)
                V(lambda e, hh=hh: e.tensor_tensor(out=hh[:], in0=hh[:], in1=modrow[:, 2, :], op=ALU.mult), [hhk, "modrow"], [hhk])
                V(lambda e, hh=hh, hb=hb: e.tensor_tensor(out=hb[:], in0=hh[:], in1=modrow[:, 1, :], op=ALU.add), [hhk, "modrow"], [hbk])
                DS(lambda e, t=t, hb=hb: e.dma_start(out=hrow_d[t * 128:(t + 1) * 128, :], in_=hb[:]), [hbk], ["hrow_d"])
                for kt in range(8):
                    T(lambda e, kt=kt, hb=hb, pT3=pT3: e.transpose(out=pT3[:, kt * 128:(kt + 1) * 128], in_=hb[:, kt * 128:(kt + 1) * 128],
                                                                   identity=ident_bf[:]), [hbk, "ident_bf"], [pTk])
                A(lambda e, hT=hT, pT3=pT3: e.activation(out=hT[:].rearrange("p a b -> p (a b)"), in_=pT3[:], func=AF.Identity), [pTk], [hTk])
                for kt in range(8):
                    T(lambda e, kt=kt, hT=hT, pL=pL: e.matmul(pL[:], lhsT=hT[:, kt, :], rhs=wr[:, kt, :], start=(kt == 0), stop=(kt == 7)),
                      [hTk, "wr"], [pLk])
                V(lambda e, mx=mx, pL=pL: e.tensor_reduce(out=mx[:], in_=pL[:], axis=AX.X, op=ALU.max), [pLk], [mxk])
                V(lambda e, mx=mx: e.tensor_scalar_mul(out=mx[:], in0=mx[:], scalar1=-1.0), [mxk], [mxk])
                A(lambda e, lg=lg, pL=pL, mx=mx: e.activation(out=lg[:], in_=pL[:], func=AF.Exp, bias=mx[:, 0:1]), [pLk, mxk], [lgk])
                V(lambda e, sm=sm, lg=lg: e.tensor_reduce(out=sm[:], in_=lg[:], axis=AX.X, op=ALU.add), [lgk], [smk])
                V(lambda e, sm=sm: e.reciprocal(out=sm[:], in_=sm[:]), [smk], [smk])
                V(lambda e, t=t, lg=lg, sm=sm: e.tensor_scalar_mul(out=aff[:, t, :], in0=lg[:], scalar1=sm[:, 0:1]), [lgk, smk], ["aff%d" % t])
                DS(lambda e, t=t: e.dma_start(out=aff_d[t * 128:(t + 1) * 128, :], in_=aff[:, t, :]), ["aff%d" % t], ["aff_d"])
            P.barrier()

        if debug == 1:
            return nc

        with contextlib.ExitStack() as st:
            hs = sb(st, "hs", [128, 16])
            am = sb(st, "am", [128, NT, NE])
            lo = sb(st, "lo", [128, NE])
            hi = sb(st, "hi", [128, NE])
            mid = sb(st, "mid", [128, NE])
            cmp_ = sb(st, "cmp", [128, NT, NE])
            cnt = sb(st, "cnt", [128, NE])
            cntb = sb(st, "cntb", [128, NE], BF16)
            ge = sb(st, "ge", [128, NE])
            dl = sb(st, "dl", [128, NE])
            pC = ps(st, "pC", [128, NE])
            DS(lambda e: e.dma_start(out=hs[:], in_=half_sel), w=["hs"])
            V(lambda e: e.tensor_tensor(out=am[:], in0=aff[:, :, 0:8], in1=bc(hs[:, 0:8].unsqueeze(1), [128, NT, NE]), op=ALU.mult),
              ["aff", "hs"], ["am"])
            V(lambda e: e.tensor_tensor(out=cmp_[:], in0=aff[:, :, 8:16], in1=bc(hs[:, 8:16].unsqueeze(1), [128, NT, NE]), op=ALU.mult),
              ["aff", "hs"], ["cmp"])
            V(lambda e: e.tensor_tensor(out=am[:], in0=am[:], in1=cmp_[:], op=ALU.add), ["am", "cmp"], ["am"])
            V(lambda e: e.memset(lo[:], 0.0), w=["lo"])
            V(lambda e: e.memset(hi[:], 1.0), w=["hi"])
            for it in range(30):
                V(lambda e: e.tensor_tensor(out=mid[:], in0=lo[:], in1=hi[:], op=ALU.add), ["lo", "hi"], ["mid"])
                V(lambda e: e.tensor_scalar_mul(out=mid[:], in0=mid[:], scalar1=0.5), ["mid"], ["mid"])
                V(lambda e: e.tensor_tensor(out=cmp_[:], in0=am[:], in1=bc(mid[:].unsqueeze(1), [128, NT, NE]), op=ALU.is_ge),
                  ["am", "mid"], ["cmp"])
                V(lambda e: e.tensor_reduce(out=cnt[:], in_=cmp_[:].rearrange("p t e -> p e t"), axis=AX.X, op=ALU.add), ["cmp"], ["cnt"])
                V(lambda e: e.tensor_copy(out=cntb[:], in_=cnt[:]), ["cnt"], ["cntb"])
                T(lambda e: e.matmul(pC[:], lhsT=ones_bf[:], rhs=cntb[:], start=True, stop=True), ["ones", "cntb"], ["pC"])
                V(lambda e: e.tensor_single_scalar(out=ge[:], in_=pC[:], scalar=float(CAP), op=ALU.is_ge), ["pC"], ["ge"])
                V(lambda e: e.tensor_tensor(out=dl[:], in0=mid[:], in1=lo[:], op=ALU.subtract), ["mid", "lo"], ["dl"])
                V(lambda e: e.tensor_tensor(out=dl[:], in0=dl[:], in1=ge[:], op=ALU.mult), ["dl", "ge"], ["dl"])
                V(lambda e: e.tensor_tensor(out=lo[:], in0=lo[:], in1=dl[:], op=ALU.add), ["lo", "dl"], ["lo"])
                V(lambda e: e.tensor_tensor(out=dl[:], in0=hi[:], in1=mid[:], op=ALU.subtract), ["hi", "mid"], ["dl"])
                V(lambda e: e.tensor_tensor(out=dl[:], in0=dl[:], in1=ge[:], op=ALU.mult), ["dl", "ge"], ["dl"])
                V(lambda e: e.tensor_tensor(out=hi[:], in0=mid[:], in1=dl[:], op=ALU.add), ["mid", "dl"], ["hi"])
            mk = sb(st, "mk", [128, NE, NT], BF16)
            cin = sb(st, "cin", [128, NE * NT])
            tot = sb(st, "tot", [128, NE, NT])
            cend = sb(st, "cend", [128, NE, NT])
            pP = ps(st, "pP", [128, NE * NT])
            pTt = ps(st, "pTt", [128, NE * NT])
            V(lambda e: e.tensor_tensor(out=mk[:], in0=am[:].rearrange("p t e -> p e t"), in1=bc(lo[:].unsqueeze(2), [128, NE, NT]),
                                        op=ALU.is_ge), ["am", "lo"], ["mk"])
            T(lambda e: e.matmul(pP[:], lhsT=tri_bf[:], rhs=mk[:].rearrange("p e t -> p (e t)"), start=True, stop=True), ["tri", "mk"], ["pP"])
            T(lambda e: e.matmul(pTt[:], lhsT=ones_bf[:], rhs=mk[:].rearrange("p e t -> p (e t)"), start=True, stop=True), ["ones", "mk"], ["pTt"])
            V(lambda e: e.tensor_copy(out=cin[:], in_=pP[:]), ["pP"], ["cin"])
            V(lambda e: e.tensor_copy(out=tot[:].rearrange("p e t -> p (e t)"), in_=pTt[:]), ["pTt"], ["tot"])
            V(lambda e: e.tensor_copy(out=cend[:], in_=tot[:]), ["tot"], ["cend"])
            sh = 1
            tmpc = sb(st, "tmpc", [128, NE, NT])
            while sh < NT:
                V(lambda e: e.tensor_copy(out=tmpc[:], in_=cend[:]), ["cend"], ["tmpc"])
                V(lambda e, sh=sh: e.tensor_tensor(out=cend[:, :, sh:NT], in0=tmpc[:, :, sh:NT], in1=tmpc[:, :, 0:NT - sh], op=ALU.add),
                  ["tmpc"], ["cend"])
                sh *= 2
            cinT = sb(st, "cinT", [128, 4, 128])
            pX = ps(st, "pX", [128, 4, 128])
            for a in range(4):
                T(lambda e, a=a: e.transpose(out=pX[:, a, :], in_=cin[:, a * 128:(a + 1) * 128], identity=ident_f[:]), ["cin", "ident_f"], ["pX"])
            V(lambda e: e.tensor_copy(out=cinT[:], in_=pX[:]), ["pX"], ["cinT"])
            DS(lambda e: e.dma_start(out=cin_d.rearrange("(a p) t -> p a t", p=128), in_=cinT[:]), ["cinT"], ["cin_d"])
            spp = sb(st, "spp", [128, 8])
            DS(lambda e: e.dma_start(out=spp[:], in_=slot_pp_d), w=["spp"])
            le = sb(st, "le", [128, NE, 8, NT])
            tl_ = sb(st, "tl_", [128, NE, 8])
            cst = sb(st, "cst", [128, NE, 8])
            rr = sb(st, "rr", [128, NE, 8])
            rowi = sb(st, "rowi", [128, NE, 8], I32)
            rowf = sb(st, "rowf", [128, NE, 8])
            for e_ in range(NE):
                V(lambda e, e_=e_: e.tensor_tensor(out=le[:, e_, :, :], in0=bc(cend[:, e_, :].unsqueeze(1), [128, 8, NT]),
                                                   in1=bc(spp[:].unsqueeze(2), [128, 8, NT]), op=ALU.is_le), ["cend", "spp"], ["le"])
            V(lambda e: e.tensor_reduce(out=tl_[:].rearrange("p e j -> p (e j)"), in_=le[:].rearrange("p e j t -> p (e j) t"),
                                        axis=AX.X, op=ALU.add), ["le"], ["tl_"])
            for e_ in range(NE):
                V(lambda e, e_=e_: e.tensor_tensor(out=le[:, e_, :, :], in0=le[:, e_, :, :], in1=bc(tot[:, e_, :].unsqueeze(1), [128, 8, NT]),
                                                   op=ALU.mult), ["le", "tot"], ["le"])
            V(lambda e: e.tensor_reduce(out=cst[:].rearrange("p e j -> p (e j)"), in_=le[:].rearrange("p e j t -> p (e j) t"),
                                        axis=AX.X, op=ALU.add), ["le"], ["cst"])
            V(lambda e: e.tensor_scalar_min(out=tl_[:], in0=tl_[:], scalar1=float(NT - 1)), ["tl_"], ["tl_"])
            V(lambda e: e.tensor_tensor(out=rr[:], in0=bc(spp[:].unsqueeze(1), [128, NE, 8]), in1=cst[:], op=ALU.subtract), ["spp", "cst"], ["rr"])
            for e_ in range(NE):
                V(lambda e, e_=e_: e.tensor_scalar_add(out=rowf[:, e_, :], in0=tl_[:, e_, :], scalar1=float(e_ * NT)), ["tl_"], ["rowf"])
            V(lambda e: e.tensor_copy(out=rowi[:], in_=rowf[:]), ["rowf"], ["rowi"])
            crow = sb(st, "crow", [128, 128])
            cle = sb(st, "cle", [128, 128])
            tloc = sb(st, "tloc", [128, NE, 8])
            arow = sb(st, "arow", [128, 16])
            idf = sb(st, "idf", [128, NE, 8])
            V(lambda e: e.memset(tloc[:], 0.0), w=["tloc"])
            for e_ in range(NE):
                for j in range(8):
                    DG(lambda e, e_=e_, j=j: e.indirect_dma_start(out=crow[:], out_offset=None, in_=cin_d,
                                                                 in_offset=bass.IndirectOffsetOnAxis(ap=rowi[:, e_, j:j + 1], axis=0)),
                       ["cin_d", "rowi"], ["crow"])
                    V(lambda e, e_=e_, j=j: e.tensor_scalar(out=cle[:], in0=crow[:], scalar1=rr[:, e_, j:j + 1], scalar2=0.0,
                                                            op0=ALU.is_le, op1=ALU.add, accum_out=tloc[:, e_, j:j + 1]),
                      ["crow", "rr"], ["cle", "tloc"])
            V(lambda e: e.tensor_scalar_min(out=tloc[:], in0=tloc[:], scalar1=127.0), ["tloc"], ["tloc"])
            V(lambda e: e.scalar_tensor_tensor(out=idf[:], in0=tl_[:], scalar=128.0, in1=tloc[:], op0=ALU.mult, op1=ALU.add),
              ["tl_", "tloc"], ["idf"])
            V(lambda e: e.tensor_copy(out=idx_all[:], in_=idf[:]), ["idf"], ["idx_all"])
            for e_ in range(NE):
                for j in range(8):
                    DG(lambda e, e_=e_, j=j: e.indirect_dma_start(out=arow[:], out_offset=None, in_=aff_d,
                                                                 in_offset=bass.IndirectOffsetOnAxis(ap=idx_all[:, e_, j:j + 1], axis=0)),
                       ["aff_d", "idx_all"], ["arow"])
                    V(lambda e, e_=e_, j=j: e.tensor_tensor(out=cle[:, 0:16], in0=arow[:], in1=hs[:], op=ALU.mult), ["arow", "hs"], ["cle"])
                    V(lambda e, e_=e_, j=j: e.tensor_tensor(out=gate_all[:, e_, j:j + 1], in0=cle[:, e_:e_ + 1], in1=cle[:, 8 + e_:9 + e_],
                                                            op=ALU.add), ["cle"], ["gate_all"])
            P.barrier()

        if debug:
            DS(lambda e: e.dma_start(out=dbg_idx, in_=idx_all[:].rearrange("p a b -> p (a b)")), ["idx_all"], ["dbg_idx"])
            DS(lambda e: e.dma_start(out=dbg_gate, in_=gate_all[:].rearrange("p a b -> p (a b)")), ["gate_all"], ["dbg_gate"])
            P.barrier()
        if debug == 2:
            return nc

        early.close()
        with contextlib.ExitStack() as st:
            zt = sb(st, "zt", [128, D])
            V(lambda e: e.memset(zt[:], 0.0), w=["zt"])
            for t in range(NT):
                DS(lambda e, t=t: e.dma_start(out=moe_d[t * 128:(t + 1) * 128, :], in_=zt[:]), ["zt"], ["moe_d"])
            P.barrier()
        FB = 256
        NFB = DFF // FB
        with contextlib.ExitStack() as st:
            xgA = [sb(st, "xg%d" % i, [128, D], BF16) for i in range(2)]
            xgT = sb(st, "xgT", [128, 8, CAP], BF16)
            wdn = sb(st, "wdn", [128, NFT, D], BF16)
            hid = sb(st, "hid", [128, NFT, CAP], BF16)
            stg = [sb(st, "wstg%d" % i, [128, 2, 8, FB]) for i in range(2)]
            wgbA = [sb(st, "wgb%d" % i, [128, 8, FB], BF16) for i in range(2)]
            wubA = [sb(st, "wub%d" % i, [128, 8, FB], BF16) for i in range(2)]
            stgdA = [sb(st, "stgd%d" % i, [128, D]) for i in range(2)]
            slA = [sb(st, "sl%d" % i, [128, 512]) for i in range(2)]
            obA = [sb(st, "ob%d" % i, [128, D]) for i in range(2)]
            pTg = ps(st, "pTg", [128, D], BF16)
            pGU = [[ps(st, "pGU%d%d" % (h_, m_), [128, 512]) for m_ in range(2)] for h_ in range(2)]
            pO = ps(st, "pO", [128, D])
            gcnt = 0
            ocnt = 0
            for e_ in range(NE):
                for j in range(8):
                    xg, xgk = xgA[gcnt % 2], "xg%d" % (gcnt % 2)
                    gcnt += 1
                    DG(lambda e, e_=e_, j=j, xg=xg: e.indirect_dma_start(out=xg[:], out_offset=None, in_=hrow_d,
                                                                        in_offset=bass.IndirectOffsetOnAxis(ap=idx_all[:, e_, j:j + 1], axis=0)),
                       ["hrow_d", "idx_all"], [xgk])
                    for kt in range(8):
                        T(lambda e, kt=kt, xg=xg: e.transpose(out=pTg[:, kt * 128:(kt + 1) * 128], in_=xg[:, kt * 128:(kt + 1) * 128],
                                                              identity=ident_bf[:]), [xgk, "ident_bf"], ["pTg"])
                    V(lambda e, j=j: e.tensor_copy(out=xgT[:, :, j * 128:(j + 1) * 128], in_=pTg[:].rearrange("p (a b) -> p a b", a=8)),
                      ["pTg"], ["xgT"])
                for fb in range(NFB):
                    s_, sk = stg[fb % 2], "wstg%d" % (fb % 2)
                    wgb, wgk = wgbA[fb % 2], "wgb%d" % (fb % 2)
                    wub, wuk = wubA[fb % 2], "wub%d" % (fb % 2)
                    DS(lambda e, e_=e_, fb=fb, s_=s_: e.dma_start(out=s_[:, 0, :, :],
                                                                  in_=wg[e_].rearrange("(kt p) f -> p kt f", p=128)[:, :, fb * FB:(fb + 1) * FB]),
                       w=[sk + "g"])
                    DS(lambda e, e_=e_, fb=fb, s_=s_: e.dma_start(out=s_[:, 1, :, :],
                                                                  in_=wu[e_].rearrange("(kt p) f -> p kt f", p=128)[:, :, fb * FB:(fb + 1) * FB]),
                       w=[sk + "u"])
                    A(lambda e, s_=s_, wgb=wgb: e.activation(out=wgb[:], in_=s_[:, 0, :, :], func=AF.Identity), [sk + "g"], [wgk])
                    G(lambda e, s_=s_, wub=wub: e.tensor_copy(out=wub[:], in_=s_[:, 1, :, :]), [sk + "u"], [wuk])
                    if fb < 2 * 0 + NFB:
                        for q_ in range(2):
                            ft = fb * 2 + q_
                            sd_, sdk = stgdA[ft % 2], "stgd%d" % (ft % 2)
                            DS(lambda e, e_=e_, ft=ft, sd_=sd_: e.dma_start(out=sd_[:], in_=wd[e_, ft * 128:(ft + 1) * 128, :]), w=[sdk])
                            G(lambda e, ft=ft, sd_=sd_: e.tensor_copy(out=wdn[:, ft, :], in_=sd_[:]), [sdk], ["wdn"])
                    for fl in range(FB // 128):
                        ft = fb * (FB // 128) + fl
                        for h_ in range(2):
                            c0 = h_ * 512
                            pg, pu = pGU[h_][0], pGU[h_][1]
                            pgk, puk = "pGU%d0" % h_, "pGU%d1" % h_
                            sl, slk = slA[h_], "sl%d" % h_
                            for kt in range(8):
                                T(lambda e, kt=kt, c0=c0, fl=fl, pg=pg, wgb=wgb: e.matmul(pg[:], lhsT=wgb[:, kt, fl * 128:(fl + 1) * 128],
                                                                                        rhs=xgT[:, kt, c0:c0 + 512], start=(kt == 0), stop=(kt == 7)),
                                  [wgk, "xgT"], [pgk])
                            for kt in range(8):
                                T(lambda e, kt=kt, c0=c0, fl=fl, pu=pu, wub=wub: e.matmul(pu[:], lhsT=wub[:, kt, fl * 128:(fl + 1) * 128],
                                                                                        rhs=xgT[:, kt, c0:c0 + 512], start=(kt == 0), stop=(kt == 7)),
                                  [wuk, "xgT"], [puk])
                            A(lambda e, sl=sl, pg=pg: e.activation(out=sl[:], in_=pg[:], func=AF.Silu), [pgk], [slk])
                            V(lambda e, ft=ft, c0=c0, sl=sl, pu=pu: e.tensor_tensor(out=hid[:, ft, c0:c0 + 512], in0=sl[:], in1=pu[:], op=ALU.mult),
                              [slk, puk], ["hid"])
                for j in range(8):
                    ob, obk = obA[ocnt % 2], "ob%d" % (ocnt % 2)
                    ocnt += 1
                    for nh in range(2):
                        for ft in range(NFT):
                            T(lambda e, j=j, nh=nh, ft=ft: e.matmul(pO[:, nh * 512:(nh + 1) * 512], lhsT=hid[:, ft, j * 128:(j + 1) * 128],
                                                                    rhs=wdn[:, ft, nh * 512:(nh + 1) * 512], start=(ft == 0), stop=(ft == NFT - 1)),
                              ["hid", "wdn"], ["pO"])
                    V(lambda e, e_=e_, j=j, ob=ob: e.tensor_scalar_mul(out=ob[:], in0=pO[:], scalar1=gate_all[:, e_, j:j + 1]), ["pO", "gate_all"], [obk])
                    DG(lambda e, e_=e_, j=j, ob=ob: e.indirect_dma_start(out=moe_d, out_offset=bass.IndirectOffsetOnAxis(ap=idx_all[:, e_, j:j + 1], axis=0),
                                                                        in_=ob[:], in_offset=None, compute_op=ALU.add, oob_is_err=True),
                       [obk, "idx_all", "moe_d"], ["moe_d"])
            P.barrier()

        if debug == 3:
            return nc

        ccsem = gst.enter_context(nc.semaphore("ccsem"))
        CCH = 16
        crow_ = SEQ // CCH
        for cc_ in range(CCH):
            nc.gpsimd.collective_compute("AllReduce", ALU.add, replica_groups=[[0, 1], [2, 3], [4, 5], [6, 7]],
                                         ins=[moe_d[cc_ * crow_:(cc_ + 1) * crow_, :]],
                                         outs=[moe_r[cc_ * crow_:(cc_ + 1) * crow_, :]]).then_inc(ccsem)
            nc.gpsimd.wait_ge(ccsem, cc_ + 1)
        G(lambda e: e.memset(gate_all[:, 0, 0:1], 0.0), w=["gate_all"])
        P.barrier()
        with contextlib.ExitStack() as st:
            mt_ = [sb(st, "m6_%d" % i, [128, D]) for i in range(2)]
            xq = [sb(st, "x6_%d" % i, [128, D]) for i in range(2)]
            tm = sb(st, "tm6", [128, D])
            xr = sb(st, "xr6", [128, D])
            oo = [sb(st, "o6_%d" % i, [128, D]) for i in range(2)]
            stats = sb(st, "stats6", [128, 2, 6])
            mv = sb(st, "mv6_", [128, 2])
            rstd = sb(st, "rstd6", [128, 1])
            tki = sb(st, "tki", [128, NT // 2], I32)
            DS(lambda e: e.dma_start(out=tki[:], in_=tokidx_d), w=["tki"])
            for t in range(NT // 2):
                m_ = mt_[t % 2]
                mk_ = "m6_%d" % (t % 2)
                x_ = xq[t % 2]
                xk = "x6_%d" % (t % 2)
                o_ = oo[t % 2]
                ok = "o6_%d" % (t % 2)
                DG(lambda e, t=t, m_=m_: e.indirect_dma_start(out=m_[:], out_offset=None, in_=moe_r,
                                                             in_offset=bass.IndirectOffsetOnAxis(ap=tki[:, t:t + 1], axis=0)),
                   ["moe_r", "tki"], [mk_])
                DG(lambda e, t=t, x_=x_: e.indirect_dma_start(out=x_[:], out_offset=None, in_=xnew_d,
                                                             in_offset=bass.IndirectOffsetOnAxis(ap=tki[:, t:t + 1], axis=0)),
                   ["xnew_d", "tki"], [xk])
                V(lambda e, m_=m_: e.tensor_tensor(out=tm[:], in0=m_[:], in1=fin3[:, 0, :], op=ALU.mult), [mk_, "fin3"], ["tm6"])
                V(lambda e, x_=x_: e.scalar_tensor_tensor(out=xr[:], in0=x_[:], scalar=ALPHA, in1=tm[:], op0=ALU.mult, op1=ALU.add),
                  [xk, "tm6"], ["xr6"])
                for c in range(2):
                    V(lambda e, c=c: e.bn_stats(out=stats[:, c, :], in_=xr[:, c * 512:(c + 1) * 512]), ["xr6"], ["stats6"])
                V(lambda e: e.bn_aggr(out=mv[:], in_=stats[:].rearrange("p a b -> p (a b)")), ["stats6"], ["mv6_"])
                V(lambda e: e.tensor_scalar_add(out=rstd[:], in0=mv[:, 1:2], scalar1=EPS), ["mv6_"], ["rstd6"])
                A(lambda e: e.activation(out=rstd[:], in_=rstd[:], func=AF.Sqrt), ["rstd6"], ["rstd6"])
                V(lambda e: e.reciprocal(out=rstd[:], in_=rstd[:]), ["rstd6"], ["rstd6"])
                V(lambda e: e.tensor_scalar(out=tm[:], in0=xr[:], scalar1=mv[:, 0:1], scalar2=rstd[:, 0:1],
                                            op0=ALU.subtract, op1=ALU.mult), ["xr6", "mv6_", "rstd6"], ["tm6"])
                V(lambda e: e.tensor_tensor(out=tm[:], in0=tm[:], in1=fin3[:, 1, :], op=ALU.mult), ["tm6", "fin3"], ["tm6"])
                V(lambda e, o_=o_: e.tensor_tensor(out=o_[:], in0=tm[:], in1=fin3[:, 2, :], op=ALU.add), ["tm6", "fin3"], [ok])
                DS(lambda e, t=t, o_=o_: e.dma_start(out=out_d[t * 128:(t + 1) * 128, :], in_=o_[:]), [ok], ["out_d"])
            P.barrier()
    return nc


def _prep(inputs):
    f32 = np.float32
    g = {k: np.asarray(v) for k, v in inputs.items()}
    bf = ml_dtypes.bfloat16
    com = {}
    com["w_ada"] = np.ascontiguousarray(g["w_ada"][0], f32)
    ba = g["b_ada"][0]
    com["bada_fm"] = np.ascontiguousarray(ba[:2048].reshape(16, 128).T, f32)
    com["bada_row"] = np.ascontiguousarray(ba[2048:].reshape(1, 4096), f32)
    w_in = g["w_in"][0]
    ws5 = np.zeros((D, 4, 4, 32), f32)
    ws5[:, :, :, :16] = w_in[:, :256].reshape(D, 4, 4, 16)
    com["w_in_s5"] = ws5.reshape(D, 512)
    com["w_in_u"] = np.ascontiguousarray(w_in[:, 256:1024], f32)
    com["w_in_v"] = np.ascontiguousarray(w_in[:, 1024:1792], f32)
    com["gm_wsT"] = np.ascontiguousarray(g["gm_ws"][0].transpose(2, 0, 1), f32)
    com["gm_bs_row"] = np.ascontiguousarray(g["gm_bs"][0].reshape(1, 768), f32)
    are = g["s5_a_re"][0]; aim = g["s5_a_im"][0]; ls = g["s5_log_step"][0]
    com["are_row"] = np.ascontiguousarray(are.reshape(1, 2048), f32)
    com["aim_row"] = np.ascontiguousarray(aim.reshape(1, 2048), f32)
    com["ls_row"] = np.ascontiguousarray(np.repeat(ls.reshape(32), 64).reshape(1, 2048), f32)

    def pp(a):
        return np.ascontiguousarray(a.reshape(2, 8, 2, 64).transpose(2, 3, 0, 1).reshape(128, 16), f32)
    com["are_pp"] = pp(are)
    com["aim_pp"] = pp(aim)
    com["ls_pp"] = pp(np.repeat(ls[:, :, None], 64, axis=2))
    for nm, src in (("WBre_raw", g["s5_b_re"][0]), ("WBim_raw", g["s5_b_im"][0])):
        w = np.zeros((128, 2, 8, 128), f32)
        for gp in range(8):
            for g2 in range(2):
                gi = 2 * gp + g2
                r0 = 64 * (gp % 2) + 32 * g2
                w[r0:r0 + 16, :, gp, 64 * g2:64 * g2 + 64] = src[:, gi].transpose(2, 0, 1)
        com[nm] = w.reshape(128, 2048)
    for nm, src in (("WCre_raw", g["s5_c_re"][0]), ("WCim_raw", g["s5_c_im"][0])):
        w = np.zeros((128, 2, 8, 128), f32)
        for gp in range(8):
            for g2 in range(2):
                gi = 2 * gp + g2
                c0 = 64 * (gp % 2) + 32 * g2
                w[64 * g2:64 * g2 + 64, :, gp, c0:c0 + 16] = src[:, gi].transpose(2, 0, 1)
        com[nm] = w.reshape(128, 2048)

    def padpp(v):
        o = np.zeros((4, 4, 32), f32)
        o[:, :, :16] = v.reshape(4, 4, 16)
        return np.ascontiguousarray(o.reshape(4, 128).T, f32)
    com["dpp"] = padpp(g["s5_d"][0])
    com["b_glu_pp"] = padpp(g["s5_b_glu"][0].reshape(16, 16))
    wgl = np.zeros((4, 4, 32, 4, 4, 32), f32)
    wgl[:, :, :16, :, :, :16] = g["s5_w_glu"][0].reshape(4, 4, 16, 4, 4, 16)
    com["w_glu_pad"] = wgl.reshape(512, 512)
    wo = np.zeros((1280, D), f32)
    wo5 = np.zeros((4, 4, 32, D), f32)
    wo5[:, :, :16, :] = g["w_out"][0][:256].reshape(4, 4, 16, D)
    wo[:512] = wo5.reshape(512, D)
    wo[512:] = g["w_out"][0][256:]
    com["w_out_pad"] = wo
    for k in ("ln1_g", "ln1_b", "ln2_g", "ln2_b"):
        com[k] = np.ascontiguousarray(g[k][0].reshape(1, D), f32)
    com["w_router"] = np.ascontiguousarray(g["w_router"][0], f32)
    com["ident_bf"] = np.eye(128, dtype=f32).astype(bf)
    com["ident_f"] = np.eye(128, dtype=f32)
    com["tri"] = np.triu(np.ones((128, 128), f32)).astype(bf)
    com["ones"] = np.ones((128, 128), f32).astype(bf)
    com["iota_row"] = np.arange(1024, dtype=f32).reshape(1, 1024)
    com["slot_pp"] = (np.arange(128, dtype=f32)[:, None] + 128.0 * np.arange(8, dtype=f32)[None, :]).astype(f32)
    maps = []
    for c in range(8):
        b, half = c // 2, c % 2
        m = dict(com)
        m["xs"] = np.ascontiguousarray(np.concatenate([g["ctx"][b], g["x"][b]], axis=0), f32)
        cv = np.stack([g["c"][b], g["c_ctx"]], axis=1)
        m["cT"] = np.ascontiguousarray(cv.reshape(8, 128, 2).transpose(1, 0, 2), f32)
        es = slice(8 * half, 8 * half + 8)
        m["wg"] = np.ascontiguousarray(g["moe_w_gate"][0][es], f32)
        m["wu"] = np.ascontiguousarray(g["moe_w_up"][0][es], f32)
        m["wd"] = np.ascontiguousarray(g["moe_w_down"][0][es], f32)
        hs = np.zeros((128, 16), f32)
        hs[:, es] = 1.0
        m["half_sel"] = hs
        m["tokidx"] = (half * 4096 + np.arange(32, dtype=np.int32)[None, :] * 128 + np.arange(128, dtype=np.int32)[:, None]).astype(np.int32)
        maps.append(m)
    return maps


def kernel(**inputs):
    maps = _prep(inputs)
    nc = build()
    res = run_bass_kernel_spmd(nc, maps, core_ids=list(range(8)))
    out = np.zeros((4, SEQ, D), np.float32)
    for c in range(8):
        b, half = c // 2, c % 2
        out[b, half * 4096:(half + 1) * 4096] = res.results[c]["out"]
    return out
```

```python
import contextlib
import math
import numpy as np
import ml_dtypes
import concourse.bass as bass
import concourse.mybir as mybir
from concourse.bass_utils import run_bass_kernel_spmd

F32 = mybir.dt.float32
BF16 = mybir.dt.bfloat16
I32 = mybir.dt.int32
AF = mybir.ActivationFunctionType
ALU = mybir.AluOpType
AX = mybir.AxisListType

D = 1024
SEQ = 8192
CTX = 256
NT = SEQ // 128
NTC = CTX // 128
TOT = SEQ + CTX
DFF = 2816
NFT = DFF // 128
NE = 8
CAP = 1024
ALPHA = 2.0 ** 0.25
EPS = 1e-6
TWO_PI = 6.283185
SEG = 1024

ENGS = ("sync", "scalar", "vector", "gpsimd", "tensor")
DMA_K = 8


class Prog:
    def __init__(self, nc, stack):
        self.nc = nc
        self.eng = {"sync": nc.sync, "scalar": nc.scalar, "vector": nc.vector,
                    "gpsimd": nc.gpsimd, "tensor": nc.tensor}
        self.sems = {}
        for e in ENGS:
            self.sems["c" + e] = stack.enter_context(nc.semaphore("c" + e))
        for e in ("sync", "scalar", "gpsimd"):
            for i in range(DMA_K):
                n = "d%s%d" % (e, i)
                self.sems[n] = stack.enter_context(nc.semaphore(n))
        self.ccnt = {e: 0 for e in ENGS}
        self.dcnt = {e: 0 for e in ENGS}
        self.lastw = {}
        self.readers = {}
        self.known = {e: {} for e in ENGS}
        self.latest = {}

    def _need(self, eng, tok):
        if tok is None:
            return
        name, val = tok
        if name == "c" + eng and eng in ("tensor", "sync"):
            return
        if self.known[eng].get(name, 0) >= val:
            return
        self.known[eng][name] = val
        self.eng[eng].wait_ge(self.sems[name], val)

    def op(self, eng, fn, reads=(), writes=(), dma=False, sem_inc=None):
        for k in reads:
            self._need(eng, self.lastw.get(k))
        for k in writes:
            self._need(eng, self.lastw.get(k))
            for t in self.readers.get(k, ()):
                self._need(eng, t)
        if dma:
            i = self.dcnt[eng]
            self.dcnt[eng] += 1
            name = "d%s%d" % (eng, i % DMA_K)
            inc = 16 if sem_inc is None else sem_inc
            val = self.latest.get(name, 0) + inc
            if i >= DMA_K:
                self._need(eng, (name, self.latest.get(name, 0)))
        else:
            self.ccnt[eng] += 1
            name = "c" + eng
            val = self.ccnt[eng]
            inc = 1
        tok = (name, val)
        ins = fn(self.eng[eng])
        ins.then_inc(self.sems[name], inc)
        self.latest[name] = val
        for k in writes:
            self.lastw[k] = tok
            self.readers[k] = []
        for k in reads:
            if k not in writes:
                self.readers.setdefault(k, []).append(tok)
        return tok

    def barrier(self):
        toks = list(self.latest.items())
        for e in ENGS:
            for t in toks:
                self._need(e, t)
        self.lastw = {}
        self.readers = {}


def build(debug=0):
    nc = bass.Bass("TRN2", target_bir_lowering=False)

    def din(name, shape, dt=F32):
        return nc.dram_tensor(name, list(shape), dt, kind="ExternalInput").ap()

    def dscr(name, shape, dt=F32):
        return nc.dram_tensor(name, list(shape), dt).ap()

    xs = din("xs", [TOT, D])
    cT = din("cT", [128, 8, 2])
    w_ada = din("w_ada", [D, 6 * D])
    bada_fm = din("bada_fm", [128, 16])
    bada_row = din("bada_row", [1, 4 * D])
    w_in_s5 = din("w_in_s5", [D, 512])
    w_in_u = din("w_in_u", [D, 768])
    w_in_v = din("w_in_v", [D, 768])
    gm_wsT = din("gm_wsT", [128, 6, 128])
    gm_bs_row = din("gm_bs_row", [1, 768])
    are_row = din("are_row", [1, 2048])
    aim_row = din("aim_row", [1, 2048])
    ls_row = din("ls_row", [1, 2048])
    are_pp = din("are_pp", [128, 16])
    aim_pp = din("aim_pp", [128, 16])
    ls_pp = din("ls_pp", [128, 16])
    WBre_raw = din("WBre_raw", [128, 2048])
    WBim_raw = din("WBim_raw", [128, 2048])
    WCre_raw = din("WCre_raw", [128, 2048])
    WCim_raw = din("WCim_raw", [128, 2048])
    dpp = din("dpp", [128, 4])
    w_glu_pad = din("w_glu_pad", [512, 512])
    b_glu_pp = din("b_glu_pp", [128, 4])
    w_out_pad = din("w_out_pad", [1280, D])
    ln1_g = din("ln1_g", [1, D])
    ln1_b = din("ln1_b", [1, D])
    ln2_g = din("ln2_g", [1, D])
    ln2_b = din("ln2_b", [1, D])
    w_router = din("w_router", [D, 16])
    if debug in (0, 3):
        wg = din("wg", [NE, D, DFF])
        wu = din("wu", [NE, D, DFF])
        wd = din("wd", [NE, DFF, D])
    ident_bf_d = din("ident_bf", [128, 128], BF16)
    ident_f_d = din("ident_f", [128, 128])
    tri_d = din("tri", [128, 128], BF16)
    ones_d = din("ones", [128, 128], BF16)
    iota_row_d = din("iota_row", [1, 1024])
    slot_pp_d = din("slot_pp", [128, 8])
    half_sel = din("half_sel", [128, 16])
    tokidx_d = din("tokidx", [128, NT // 2], I32)

    out_d = nc.dram_tensor("out", [SEQ // 2, D], F32, kind="ExternalOutput").ap()
    okind = {"kind": "ExternalOutput"} if debug else {}
    xnew_d = nc.dram_tensor("xnew_s", [SEQ, D], F32, **okind).ap()
    hrow_d = nc.dram_tensor("hrow_s", [SEQ, D], BF16, **okind).ap()
    catT_d = dscr("catT_s", [10, 128, SEQ], BF16)
    yf_d = dscr("yf_s", [4, 128, SEQ], F32)
    aff_d = nc.dram_tensor("aff_s", [SEQ, 16], F32, **okind).ap()
    if debug:
        dbg_idx = nc.dram_tensor("dbg_idx", [128, NE * 8], I32, kind="ExternalOutput").ap()
        dbg_gate = nc.dram_tensor("dbg_gate", [128, NE * 8], F32, kind="ExternalOutput").ap()
    cin_d = dscr("cin_s", [NE * NT, 128], F32)
    moe_d = nc.dram_tensor("moe_s", [SEQ, D], F32, **({"kind": "ExternalOutput"} if debug == 3 else {})).ap()
    moe_r = dscr("moe_r", [SEQ, D], F32)

    with contextlib.ExitStack() as gst:
        P = Prog(nc, gst)

        _uid = [0]

        def sb(st, name, shape, dt=F32):
            _uid[0] += 1
            return st.enter_context(nc.sbuf_tensor("%s_%d" % (name, _uid[0]), list(shape), dt))

        def ps(st, name, shape, dt=F32):
            _uid[0] += 1
            return st.enter_context(nc.psum_tensor("%s_%d" % (name, _uid[0]), list(shape), dt))

        def V(fn, r=(), w=()):
            return P.op("vector", fn, r, w)

        def A(fn, r=(), w=()):
            return P.op("scalar", fn, r, w)

        def G(fn, r=(), w=()):
            return P.op("gpsimd", fn, r, w)

        def T(fn, r=(), w=()):
            return P.op("tensor", fn, r, w)

        def DS(fn, r=(), w=()):
            return P.op("sync", fn, r, w, dma=True)

        def DG(fn, r=(), w=()):
            return P.op("gpsimd", fn, r, w, dma=True)

        def bc(ap_, shape):
            return ap_.to_broadcast(list(shape))

        ident_bf = sb(gst, "ident_bf_t", [128, 128], BF16)
        ident_f = sb(gst, "ident_f_t", [128, 128])
        ones_bf = sb(gst, "ones_t", [128, 128], BF16)
        tri_bf = sb(gst, "tri_t", [128, 128], BF16)
        modfm = sb(gst, "modfm", [128, 2, 8, 2])
        fin3 = sb(gst, "fin3", [128, 3, D])
        idx_all = sb(gst, "idx_all", [128, NE, 8], I32)
        gate_all = sb(gst, "gate_all", [128, NE, 8])
        early = contextlib.ExitStack()
        iota_r = sb(early, "iota_r", [128, 1024])
        modrow = sb(early, "modrow", [128, 4, D])
        lnrow = sb(early, "lnrow", [128, 4, D])
        aff = sb(early, "aff", [128, NT, 16])
        DS(lambda e: e.dma_start(out=ident_bf[:], in_=ident_bf_d), w=["ident_bf"])
        DS(lambda e: e.dma_start(out=ident_f[:], in_=ident_f_d), w=["ident_f"])
        DS(lambda e: e.dma_start(out=ones_bf[:], in_=ones_d), w=["ones"])
        DS(lambda e: e.dma_start(out=tri_bf[:], in_=tri_d), w=["tri"])
        DS(lambda e: e.dma_start(out=iota_r[:], in_=bc(iota_row_d, [128, 1024])), w=["iota_r"])
        for i, a in enumerate((ln1_g, ln1_b, ln2_g, ln2_b)):
            DS(lambda e, i=i, a=a: e.dma_start(out=lnrow[:, i, :], in_=bc(a, [128, D])), w=["lnrow"])

        with contextlib.ExitStack() as st:
            ct = sb(st, "ct", [128, 8, 2])
            sc = sb(st, "sc", [128, 8, 2])
            scb = sb(st, "scb", [128, 8, 128])
            wa = sb(st, "wa", [128, 8, D])
            bfm = sb(st, "bfm", [128, 16])
            brow = sb(st, "brow", [128, 4 * D])
            pfm = ps(st, "pfm", [128, 8, 2])
            prow = ps(st, "prow", [128, D])
            DS(lambda e: e.dma_start(out=ct[:], in_=cT), w=["ct"])
            DS(lambda e: e.dma_start(out=bfm[:], in_=bada_fm), w=["bfm"])
            DS(lambda e: e.dma_start(out=brow[:], in_=bc(bada_row, [128, 4 * D])), w=["brow"])
            A(lambda e: e.activation(out=sc[:], in_=ct[:], func=AF.Silu), ["ct"], ["sc"])
            V(lambda e: e.tensor_copy(out=scb[:], in_=bc(sc[:, :, 0:1], [128, 8, 128])), ["sc"], ["scb"])
            wav = w_ada.rearrange("(kt p) f -> p kt f", p=128)
            for ch in range(6):
                DS(lambda e, ch=ch: e.dma_start(out=wa[:], in_=wav[:, :, ch * D:(ch + 1) * D]), w=["wa"])
                if ch < 2:
                    for ft in range(8):
                        for kt in range(8):
                            T(lambda e, ft=ft, kt=kt: e.matmul(pfm[:, ft, :], lhsT=wa[:, kt, ft * 128:(ft + 1) * 128],
                                                               rhs=sc[:, kt, :], start=(kt == 0), stop=(kt == 7)),
                              ["wa", "sc"], ["pfm"])
                    V(lambda e, ch=ch: e.tensor_tensor(out=modfm[:, ch, :, :], in0=pfm[:],
                                                       in1=bc(bfm[:, ch * 8:(ch + 1) * 8].unsqueeze(2), [128, 8, 2]),
                                                       op=ALU.add), ["pfm", "bfm"], ["modfm"])
                else:
                    for nh in range(2):
                        for kt in range(8):
                            T(lambda e, nh=nh, kt=kt: e.matmul(prow[:, nh * 512:(nh + 1) * 512], lhsT=scb[:, kt, :],
                                                               rhs=wa[:, kt, nh * 512:(nh + 1) * 512],
                                                               start=(kt == 0), stop=(kt == 7)),
                              ["wa", "scb"], ["prow"])
                    V(lambda e, ch=ch: e.tensor_tensor(out=modrow[:, ch - 2, :], in0=prow[:],
                                                       in1=brow[:, (ch - 2) * D:(ch - 1) * D], op=ALU.add),
                      ["prow", "brow"], ["modrow"])
            V(lambda e: e.tensor_scalar_add(out=modfm[:, 1, :, :], in0=modfm[:, 1, :, :], scalar1=1.0), ["modfm"], ["modfm"])
            V(lambda e: e.tensor_scalar_add(out=modrow[:, 2, :], in0=modrow[:, 2, :], scalar1=1.0), ["modrow"], ["modrow"])
            V(lambda e: e.tensor_copy(out=fin3[:, 0, :], in_=modrow[:, 3, :]), ["modrow"], ["fin3"])
            V(lambda e: e.tensor_copy(out=fin3[:, 1:3, :], in_=lnrow[:, 2:4, :]), ["lnrow"], ["fin3"])
            P.barrier()

        with contextlib.ExitStack() as mst:
            U = sb(mst, "U", [128, 4, TOT], BF16)
            wst_ = contextlib.ExitStack()
            wS = sb(wst_, "wS", [128, 8, 512], BF16)
            wU = sb(wst_, "wU", [128, 8, 768], BF16)
            wV = sb(wst_, "wV", [128, 8, 768], BF16)
            wsT = sb(wst_, "wsT", [128, 6, 128], BF16)
            bsrow = sb(wst_, "bsrow", [128, 768])
            with contextlib.ExitStack() as st:
                stg = sb(st, "stg", [128, 8, 768])
                for (src, dst, n, nm) in ((w_in_s5, wS, 512, "wS"), (w_in_u, wU, 768, "wU"), (w_in_v, wV, 768, "wV")):
                    DS(lambda e, src=src, n=n: e.dma_start(out=stg[:, :, 0:n], in_=src.rearrange("(kt p) f -> p kt f", p=128)),
                       w=["stg"])
                    V(lambda e, dst=dst, n=n: e.tensor_copy(out=dst[:], in_=stg[:, :, 0:n]), ["stg"], [nm])
                DS(lambda e: e.dma_start(out=stg[:, 0:6, 0:128], in_=gm_wsT), w=["stg"])
                V(lambda e: e.tensor_copy(out=wsT[:], in_=stg[:, 0:6, 0:128]), ["stg"], ["wsT"])
                DS(lambda e: e.dma_start(out=bsrow[:], in_=bc(gm_bs_row, [128, 768])), w=["bsrow"])
                P.barrier()

            with contextlib.ExitStack() as st:
                xt = [sb(st, "xt%d" % i, [128, D]) for i in range(2)]
                xnA = [sb(st, "xn%d" % i, [128, D], BF16) for i in range(2)]
                xmTA = [sb(st, "xmT%d" % i, [128, 8, 128], BF16) for i in range(2)]
                statsA = [sb(st, "stats%d" % i, [128, 2, 6]) for i in range(2)]
                mvA = [sb(st, "mv%d" % i, [128, 2]) for i in range(2)]
                rstdA = [sb(st, "rstd%d" % i, [128, 1]) for i in range(2)]
                uTA = [sb(st, "uT%d" % i, [128, 6, 128]) for i in range(2)]
                vvA = [sb(st, "vv%d" % i, [128, 6, 128]) for i in range(2)]
                vcA = [sb(st, "vc%d" % i, [128, 6, 128]) for i in range(2)]
                vlnA = [sb(st, "vln%d" % i, [128, 6, 128], BF16) for i in range(2)]
                st6A = [sb(st, "st6%d" % i, [128, 6, 6]) for i in range(2)]
                mv6A = [sb(st, "mv6%d" % i, [128, 6, 2]) for i in range(2)]
                rs6A = [sb(st, "rs6%d" % i, [128, 6]) for i in range(2)]
                gmtA = [sb(st, "gmt%d" % i, [128, 6, 128]) for i in range(2)]
                gmb = [sb(st, "gmb%d" % i, [128, 6, 128], BF16) for i in range(2)]
                usb = [sb(st, "usb%d" % i, [128, 4, 128], BF16) for i in range(2)]
                pT = ps(st, "pT", [128, D], BF16)
                pS = ps(st, "pS", [128, 4, 128])
                pU = ps(st, "pU", [128, 8, 128])
                pV = ps(st, "pV", [128, D])
                pM = ps(st, "pM", [128, 8, 128])

                def stage_a(t):
                    i2 = t % 2
                    lat = t >= NTC
                    col = 0 if lat else 1
                    x_, xk = xt[i2], "xt%d" % i2
                    xn, xnk = xnA[i2], "xn%d" % i2
                    xmT, xmk = xmTA[i2], "xmT%d" % i2
                    stats, sk_ = statsA[i2], "stats%d" % i2
                    mv, mvk = mvA[i2], "mv%d" % i2
                    rstd, rk = rstdA[i2], "rstd%d" % i2
                    DS(lambda e: e.dma_start(out=x_[:], in_=xs[t * 128:(t + 1) * 128, :]), w=[xk])
                    for c in range(2):
                        V(lambda e, c=c: e.bn_stats(out=stats[:, c, :], in_=x_[:, c * 512:(c + 1) * 512]), [xk], [sk_])
                    V(lambda e: e.bn_aggr(out=mv[:], in_=stats[:].rearrange("p a b -> p (a b)")), [sk_], [mvk])
                    V(lambda e: e.tensor_scalar_add(out=rstd[:], in0=mv[:, 1:2], scalar1=EPS), [mvk], [rk])
                    A(lambda e: e.activation(out=rstd[:], in_=rstd[:], func=AF.Sqrt), [rk], [rk])
                    V(lambda e: e.reciprocal(out=rstd[:], in_=rstd[:]), [rk], [rk])
                    V(lambda e: e.tensor_scalar(out=xn[:], in0=x_[:], scalar1=mv[:, 0:1], scalar2=rstd[:, 0:1],
                                                op0=ALU.subtract, op1=ALU.mult), [xk, mvk, rk], [xnk])
                    for kt in range(8):
                        T(lambda e, kt=kt: e.transpose(out=pT[:, kt * 128:(kt + 1) * 128], in_=xn[:, kt * 128:(kt + 1) * 128],
                                                       identity=ident_bf[:]), [xnk, "ident_bf"], ["pT"])
                    for kt in range(8):
                        A(lambda e, kt=kt: e.activation(out=xmT[:, kt, :], in_=pT[:, kt * 128:(kt + 1) * 128],
                                                        func=AF.Identity, scale=modfm[:, 1, kt, col:col + 1],
                                                        bias=modfm[:, 0, kt, col:col + 1]),
                          ["pT", "modfm"], [xmk])

                def stage_b(t):
                    i2 = t % 2
                    lat = t >= NTC
                    xmT, xmk = xmTA[i2], "xmT%d" % i2
                    for ct_ in range(4):
                        for kt in range(8):
                            T(lambda e, ct_=ct_, kt=kt: e.matmul(pS[:, ct_, :], lhsT=wS[:, kt, ct_ * 128:(ct_ + 1) * 128],
                                                                 rhs=xmT[:, kt, :], start=(kt == 0), stop=(kt == 7)),
                              ["wS", xmk], ["pS"])
                    V(lambda e: e.tensor_copy(out=U[:, :, t * 128:(t + 1) * 128], in_=pS[:]), ["pS"], ["U"])
                    if not lat:
                        return
                    tl = t - NTC
                    uT, uk = uTA[i2], "uT%d" % i2
                    vv, vk = vvA[i2], "vv%d" % i2
                    vc, vck = vcA[i2], "vc%d" % i2
                    vln, vlk = vlnA[i2], "vln%d" % i2
                    st6, s6k = st6A[i2], "st6%d" % i2
                    mv6, m6k = mv6A[i2], "mv6%d" % i2
                    rs6, r6k = rs6A[i2], "rs6%d" % i2
                    gmt, gtk = gmtA[i2], "gmt%d" % i2
                    for ct_ in range(6):
                        for kt in range(8):
                            T(lambda e, ct_=ct_, kt=kt: e.matmul(pU[:, ct_, :], lhsT=wU[:, kt, ct_ * 128:(ct_ + 1) * 128],
                                                                 rhs=xmT[:, kt, :], start=(kt == 0), stop=(kt == 7)),
                              ["wU", xmk], ["pU"])
                    A(lambda e: e.activation(out=uT[:], in_=pU[:, 0:6, :], func=AF.Gelu_apprx_tanh), ["pU"], [uk])
                    for (c0, c1) in ((0, 512), (512, 768)):
                        for kt in range(8):
                            T(lambda e, c0=c0, c1=c1, kt=kt: e.matmul(pV[:, c0:c1], lhsT=xmT[:, kt, :], rhs=wV[:, kt, c0:c1],
                                                                      start=(kt == 0), stop=(kt == 7)),
                              ["wV", xmk], ["pV"])
                    A(lambda e: e.activation(out=vv[:].rearrange("p a b -> p (a b)"), in_=pV[:, 0:768], func=AF.Gelu_apprx_tanh),
                      ["pV"], [vk])
                    for g in range(6):
                        V(lambda e, g=g: e.bn_stats(out=st6[:, g, :], in_=vv[:, g, :]), [vk], [s6k])
                    for g in range(6):
                        V(lambda e, g=g: e.bn_aggr(out=mv6[:, g, :], in_=st6[:, g, :]), [s6k], [m6k])
                    V(lambda e: e.tensor_scalar_add(out=rs6[:], in0=mv6[:, :, 1], scalar1=EPS), [m6k], [r6k])
                    A(lambda e: e.activation(out=rs6[:], in_=rs6[:], func=AF.Sqrt), [r6k], [r6k])
                    V(lambda e: e.reciprocal(out=rs6[:], in_=rs6[:]), [r6k], [r6k])
                    V(lambda e: e.tensor_tensor(out=vc[:], in0=vv[:], in1=bc(mv6[:, :, 0:1], [128, 6, 128]), op=ALU.subtract),
                      [vk, m6k], [vck])
                    V(lambda e: e.tensor_tensor(out=vln[:], in0=vc[:], in1=bc(rs6[:].unsqueeze(2), [128, 6, 128]), op=ALU.mult),
                      [vck, r6k], [vlk])
                    for g in range(6):
                        T(lambda e, g=g: e.matmul(pM[:, g, :], lhsT=vln[:, g, :], rhs=wsT[:, g, :], start=True, stop=True),
                          [vlk, "wsT"], ["pM"])
                    G(lambda e: e.tensor_tensor(out=gmt[:], in0=bsrow[:].rearrange("p (a b) -> p a b", a=6), in1=bsrow[:].rearrange("p (a b) -> p a b", a=6),
                                                op=ALU.bypass), ["bsrow"], [gtk]) if False else None
                    V(lambda e: e.tensor_tensor(out=gmt[:], in0=pM[:, 0:6, :], in1=bsrow[:].rearrange("p (a b) -> p a b", a=6),
                                                op=ALU.add), ["pM", "bsrow"], [gtk])
                    gb = gmb[tl % 2]
                    gk = "gmb%d" % (tl % 2)
                    G(lambda e: e.tensor_tensor(out=gb[:], in0=gmt[:], in1=uT[:], op=ALU.mult), [gtk, uk], [gk])
                    DS(lambda e: e.dma_start(out=catT_d[4:10, :, tl * 128:(tl + 1) * 128].rearrange("a p t -> p a t"),
                                             in_=gb[:]), [gk], ["catT_gm"])

                stage_a(0)
                for t in range(NTC + NT):
                    if t + 1 < NTC + NT:
                        stage_a(t + 1)
                    stage_b(t)
                P.barrier()
            wst_.close()

            with contextlib.ExitStack() as st:
                WB = sb(st, "WB", [128, 2, 2048], BF16)
                WC = sb(st, "WC", [128, 2, 2048], BF16)
                rho_pp = sb(st, "rho_pp", [128, 16])
                f_pp = sb(st, "f_pp", [128, 16])
                dsc = sb(st, "dsc", [128, 4])
                bglu = sb(st, "bglu", [128, 4])
                wglu = sb(st, "wglu", [128, 4, 512], BF16)
                for dr_ in range(4):
                  csl = slice(dr_ * 512, (dr_ + 1) * 512)
                  with contextlib.ExitStack() as s2:
                        r = {n: sb(s2, "r_" + n, [128, 512]) for n in
                             ("are", "aim", "stp", "rho", "f", "y", "y2", "sn", "cs", "x", "yv", "den", "cr", "ci", "t1", "t2", "bre", "bim")}
                        ri_ = sb(s2, "r_int", [128, 512], I32)
                        DS(lambda e: e.dma_start(out=r["are"][:], in_=bc(are_row[:, csl], [128, 512])), w=["are"])
                        DS(lambda e: e.dma_start(out=r["aim"][:], in_=bc(aim_row[:, csl], [128, 512])), w=["aim"])
                        DS(lambda e: e.dma_start(out=r["stp"][:], in_=bc(ls_row[:, csl], [128, 512])), w=["stp"])
                        DS(lambda e: e.dma_start(out=r["bre"][:], in_=WBre_raw[:, csl]), w=["bre"])
                        DS(lambda e: e.dma_start(out=r["bim"][:], in_=WBim_raw[:, csl]), w=["bim"])

                        def vt(o, a, b, op):
                            V(lambda e: e.tensor_tensor(out=r[o][:], in0=r[a][:], in1=r[b][:], op=op), [a, b], [o])

                        def frac(o, i):
                            V(lambda e: e.tensor_copy(out=ri_[:], in_=r[i][:]), [i], ["rint"])
                            V(lambda e: e.tensor_copy(out=r["t1"][:], in_=ri_[:]), ["rint"], ["t1"])
                            vt(o, i, "t1", ALU.subtract)

                        A(lambda e: e.activation(out=r["stp"][:], in_=r["stp"][:], func=AF.Exp), ["stp"], ["stp"])
                        vt("rho", "are", "stp", ALU.mult)
                        A(lambda e: e.activation(out=r["rho"][:], in_=r["rho"][:], func=AF.Exp), ["rho"], ["rho"])
                        vt("f", "aim", "stp", ALU.mult)
                        V(lambda e: e.tensor_scalar_mul(out=r["f"][:], in0=r["f"][:], scalar1=1.0 / (2 * math.pi)), ["f"], ["f"])
                        frac("y", "f")
                        A(lambda e: e.activation(out=r["sn"][:], in_=r["y"][:], func=AF.Sin, scale=TWO_PI), ["y"], ["sn"])
                        V(lambda e: e.tensor_scalar_add(out=r["y2"][:], in0=r["y"][:], scalar1=0.25), ["y"], ["y2"])
                        frac("y2", "y2")
                        A(lambda e: e.activation(out=r["cs"][:], in_=r["y2"][:], func=AF.Sin, scale=TWO_PI), ["y2"], ["cs"])
                        vt("x", "rho", "cs", ALU.mult)
                        V(lambda e: e.tensor_scalar_add(out=r["x"][:], in0=r["x"][:], scalar1=-1.0), ["x"], ["x"])
                        vt("yv", "rho", "sn", ALU.mult)
                        vt("den", "are", "are", ALU.mult)
                        vt("t2", "aim", "aim", ALU.mult)
                        vt("den", "den", "t2", ALU.add)
                        V(lambda e: e.reciprocal(out=r["den"][:], in_=r["den"][:]), ["den"], ["den"])
                        vt("cr", "x", "are", ALU.mult)
                        vt("t2", "yv", "aim", ALU.mult)
                        vt("cr", "cr", "t2", ALU.add)
                        vt("cr", "cr", "den", ALU.mult)
                        vt("ci", "yv", "are", ALU.mult)
                        vt("t2", "x", "aim", ALU.mult)
                        vt("ci", "ci", "t2", ALU.subtract)
                        vt("ci", "ci", "den", ALU.mult)
                        vt("t1", "cr", "bre", ALU.mult)
                        vt("t2", "ci", "bim", ALU.mult)
                        V(lambda e: e.tensor_tensor(out=WB[:, 0, csl], in0=r["t1"][:], in1=r["t2"][:], op=ALU.subtract), ["t1", "t2"], ["WB"])
                        vt("t1", "cr", "bim", ALU.mult)
                        vt("t2", "ci", "bre", ALU.mult)
                        V(lambda e: e.tensor_tensor(out=WB[:, 1, csl], in0=r["t1"][:], in1=r["t2"][:], op=ALU.add), ["t1", "t2"], ["WB"])
                        DS(lambda e: e.dma_start(out=r["bre"][:], in_=WCre_raw[:, csl]), w=["bre"])
                        DS(lambda e: e.dma_start(out=r["bim"][:], in_=WCim_raw[:, csl]), w=["bim"])
                        V(lambda e: e.tensor_copy(out=WC[:, 0, csl], in_=r["bre"][:]), ["bre"], ["WC"])
                        V(lambda e: e.tensor_scalar_mul(out=WC[:, 1, csl], in0=r["bim"][:], scalar1=-1.0), ["bim"], ["WC"])

                        P.barrier()
                with contextlib.ExitStack() as s2:
                    pa = sb(s2, "pa", [128, 16])
                    pb = sb(s2, "pb", [128, 16])
                    pc = sb(s2, "pc", [128, 16])
                    DS(lambda e: e.dma_start(out=pa[:], in_=are_pp), w=["pa"])
                    DS(lambda e: e.dma_start(out=pb[:], in_=aim_pp), w=["pb"])
                    DS(lambda e: e.dma_start(out=pc[:], in_=ls_pp), w=["pc"])
                    A(lambda e: e.activation(out=pc[:], in_=pc[:], func=AF.Exp), ["pc"], ["pc"])
                    V(lambda e: e.tensor_tensor(out=rho_pp[:], in0=pa[:], in1=pc[:], op=ALU.mult), ["pa", "pc"], ["rho_pp"])
                    A(lambda e: e.activation(out=rho_pp[:], in_=rho_pp[:], func=AF.Exp), ["rho_pp"], ["rho_pp"])
                    V(lambda e: e.tensor_tensor(out=f_pp[:], in0=pb[:], in1=pc[:], op=ALU.mult), ["pb", "pc"], ["f_pp"])
                    V(lambda e: e.tensor_scalar_mul(out=f_pp[:], in0=f_pp[:], scalar1=1.0 / (2 * math.pi)), ["f_pp"], ["f_pp"])
                    DS(lambda e: e.dma_start(out=dsc[:], in_=dpp), w=["dsc"])
                    DS(lambda e: e.dma_start(out=bglu[:], in_=b_glu_pp), w=["bglu"])
                    gst_ = sb(s2, "gst_", [128, 4, 512])
                    DS(lambda e: e.dma_start(out=gst_[:], in_=w_glu_pad.rearrange("(kt p) f -> p kt f", p=128)), w=["gst_"])
                    V(lambda e: e.tensor_copy(out=wglu[:], in_=gst_[:]), ["gst_"], ["wglu"])
                    P.barrier()

                L = SEG
                H = 512
                arg1 = sb(st, "arg", [128, L])
                arg = [arg1, arg1]
                argi1 = sb(st, "argi", [128, L], I32)
                argi = [argi1, argi1]
                yv1 = sb(st, "yv", [128, L])
                yv = [yv1, yv1]
                s2 = arg
                sq = arg
                cs = [sb(st, "cs%d" % i, [128, L], BF16) for i in range(2)]
                sn = [sb(st, "sn%d" % i, [128, L], BF16) for i in range(2)]
                bre = [sb(st, "bre%d" % i, [128, L], BF16) for i in range(2)]
                bim = [sb(st, "bim%d" % i, [128, L], BF16) for i in range(2)]
                dre = sb(st, "dre", [128, L], BF16)
                dim_ = sb(st, "dim", [128, L], BF16)
                tA = sb(st, "tA", [128, L], BF16)
                tB = sb(st, "tB", [128, L], BF16)
                zre = sb(st, "zre", [128, L], BF16)
                zim = sb(st, "zim", [128, L], BF16)
                Sre1 = sb(st, "Sre", [128, L], BF16)
                Sim1 = sb(st, "Sim", [128, L], BF16)
                Sre = [Sre1, Sre1]
                Sim = [Sim1, Sim1]
                zst = sb(st, "zst", [128, 16, 2])
                ysb1 = sb(st, "ysb", [128, L])
                ysb = [ysb1, ysb1]
                yfl1 = sb(st, "yfl", [128, L])
                yfl = [yfl1, yfl1]
                gg = sb(st, "gg", [128, 4, L], BF16)
                sg1 = sb(st, "sg", [128, L])
                sg = [sg1, sg1]
                s5o1 = sb(st, "s5o", [128, L], BF16)
                s5o = [s5o1, s5o1]
                pB = [[ps(st, "pB%d%d" % (h_, ri), [128, H]) for ri in range(2)] for h_ in range(2)]
                pY = ps(st, "pY", [128, L])
                pG = ps(st, "pG", [128, L])
                V(lambda e: e.memset(zst[:], 0.0), w=["zst"])
                colctr = [0]

                plan = []
                for gp in range(8):
                    plan.append((0, gp, CTX, 0))
                for sgi in range(SEQ // L):
                    for kt in range(4):
                        for g2 in range(2):
                            plan.append((0, kt * 2 + g2, L, CTX + sgi * L))
                for gp in range(8):
                    plan.append((1, gp, CTX, 0))
                for sb_i in range(SEQ // L):
                    for kt in range(4):
                        for g2 in range(2):
                            plan.append((1, kt * 2 + g2, L, CTX + sb_i * L))

                def s5_tables(idx):
                    dr, gp, n, t0 = plan[idx]
                    ci_ = dr * 8 + gp
                    cb = idx % 2
                    arg_, argi_, yv_, s2_, sq_, cs_, sn_ = arg[cb], argi[cb], yv[cb], s2[cb], sq[cb], cs[cb], sn[cb]
                    k = lambda nm: nm if nm in ("arg", "yv", "Sre", "Sim") else "%s%d" % (nm, cb)
                    G(lambda e: e.tensor_scalar(out=arg_[:, 0:n], in0=iota_r[:, 0:n], scalar1=float(t0), scalar2=f_pp[:, ci_:ci_ + 1],
                                                op0=ALU.add, op1=ALU.mult), ["iota_r", "f_pp"], [k("arg")])
                    G(lambda e: e.tensor_copy(out=argi_[:, 0:n], in_=arg_[:, 0:n]), [k("arg")], ["argi"])
                    G(lambda e: e.tensor_tensor(out=yv_[:, 0:n], in0=arg_[:, 0:n], in1=argi_[:, 0:n], op=ALU.subtract),
                      [k("arg"), "argi"], [k("yv")])
                    A(lambda e: e.activation(out=sn_[:, 0:n], in_=yv_[:, 0:n], func=AF.Sin, scale=TWO_PI), [k("yv")], [k("sn")])
                    A(lambda e: e.activation(out=s2_[:, 0:n], in_=yv_[:, 0:n], func=AF.Sin, scale=TWO_PI / 2), [k("yv")], [k("arg")])
                    A(lambda e: e.activation(out=sq_[:, 0:n], in_=s2_[:, 0:n], func=AF.Square), [k("arg")], [k("arg")])
                    G(lambda e: e.tensor_scalar(out=cs_[:, 0:n], in0=sq_[:, 0:n], scalar1=-2.0, scalar2=1.0, op0=ALU.mult, op1=ALU.add),
                      [k("arg")], [k("cs")])

                def s5_col(dr, gp, u_lo, n, t0, rev, readout):
                    idx = colctr[0]
                    colctr[0] += 1
                    assert plan[idx] == (dr, gp, n, t0), (plan[idx], dr, gp, n, t0)
                    if idx == 0:
                        s5_tables(0)
                    if idx + 1 < len(plan):
                        s5_tables(idx + 1)
                    ci_ = dr * 8 + gp
                    kt = gp // 2
                    r0 = 64 * (gp % 2)
                    wcol = slice(ci_ * 128, (ci_ + 1) * 128)
                    cb = idx % 2
                    cs_, sn_ = cs[cb], sn[cb]
                    bre_, bim_, Sre_, Sim_ = bre[cb], bim[cb], Sre[cb], Sim[cb]
                    k = lambda nm: nm if nm in ("arg", "yv", "Sre", "Sim") else "%s%d" % (nm, cb)
                    for hi_, c0 in enumerate(range(0, n, H)):
                        c1 = min(n, c0 + H)
                        for ri, dst, dk in ((0, bre_, k("bre")), (1, bim_, k("bim"))):
                            pb_ = pB[hi_][ri]
                            pk = "pB%d%d" % (hi_, ri)
                            T(lambda e, ri=ri, pb_=pb_, c0=c0, c1=c1: e.matmul(pb_[:, 0:c1 - c0], lhsT=WB[r0:r0 + 64, ri, wcol],
                                                                             rhs=U[r0:r0 + 64, kt, u_lo + c0:u_lo + c1],
                                                                             start=True, stop=True), ["WB", "U"], [pk])
                            A(lambda e, pb_=pb_, dst=dst, c0=c0, c1=c1: e.activation(out=dst[:, c0:c1], in_=pb_[:, 0:c1 - c0], func=AF.Identity),
                              [pk], [dk])

                    def tvn(t_):
                        if not rev:
                            return t_[:, 0:n]
                        return t_[:, 0:n][:, ::-1]

                    def vtt(o, ok, a_, ak, b_, bk, op):
                        V(lambda e: e.tensor_tensor(out=o, in0=a_, in1=b_, op=op), [ak, bk], [ok])

                    vtt(tA[:, 0:n], "tA", bre_[:, 0:n], k("bre"), tvn(cs_), k("cs"), ALU.mult)
                    vtt(tB[:, 0:n], "tB", bim_[:, 0:n], k("bim"), tvn(sn_), k("sn"), ALU.mult)
                    vtt(dre[:, 0:n], "dre", tA[:, 0:n], "tA", tB[:, 0:n], "tB", ALU.add)
                    vtt(tA[:, 0:n], "tA", bim_[:, 0:n], k("bim"), tvn(cs_), k("cs"), ALU.mult)
                    vtt(tB[:, 0:n], "tB", bre_[:, 0:n], k("bre"), tvn(sn_), k("sn"), ALU.mult)
                    vtt(dim_[:, 0:n], "dim", tA[:, 0:n], "tA", tB[:, 0:n], "tB", ALU.subtract)
                    for (src, dst, ri) in ((dre, zre, 0), (dim_, zim, 1)):
                        V(lambda e, src=src, dst=dst, ri=ri: e.tensor_tensor_scan(
                            out=tvn(dst), data0=bc(rho_pp[:, ci_:ci_ + 1], [128, n]), data1=tvn(src),
                            initial=zst[:, ci_, ri:ri + 1], op0=ALU.mult, op1=ALU.add),
                          ["dre" if ri == 0 else "dim", "rho_pp", "zst"], ["zre" if ri == 0 else "zim"])
                    last = 0 if rev else n - 1
                    V(lambda e: e.tensor_copy(out=zst[:, ci_, 0:1], in_=zre[:, last:last + 1]), ["zre"], ["zst"])
                    V(lambda e: e.tensor_copy(out=zst[:, ci_, 1:2], in_=zim[:, last:last + 1]), ["zim"], ["zst"])
                    if not readout:
                        return
                    vtt(tA[:, 0:n], "tA", zre[:, 0:n], "zre", tvn(cs_), k("cs"), ALU.mult)
                    vtt(tB[:, 0:n], "tB", zim[:, 0:n], "zim", tvn(sn_), k("sn"), ALU.mult)
                    vtt(Sre_[:, 0:n], k("Sre"), tA[:, 0:n], "tA", tB[:, 0:n], "tB", ALU.subtract)
                    G(lambda e: e.tensor_tensor(out=bre_[:, 0:n], in0=zre[:, 0:n], in1=tvn(sn_), op=ALU.mult), ["zre", k("sn")], [k("bre")])
                    G(lambda e: e.tensor_tensor(out=bim_[:, 0:n], in0=zim[:, 0:n], in1=tvn(cs_), op=ALU.mult), ["zim", k("cs")], [k("bim")])
                    G(lambda e: e.tensor_tensor(out=Sim_[:, 0:n], in0=bre_[:, 0:n], in1=bim_[:, 0:n], op=ALU.add), [k("bre"), k("bim")], [k("Sim")])
                    first = (gp % 2 == 0)
                    for c0 in range(0, n, 512):
                        for ri, S_, sk in ((0, Sre_, k("Sre")), (1, Sim_, k("Sim"))):
                            T(lambda e, ri=ri, S_=S_, c0=c0: e.matmul(pY[:, c0:c0 + 512], lhsT=WC[:, ri, wcol], rhs=S_[:, c0:c0 + 512],
                                                                     start=(first and ri == 0), stop=((not first) and ri == 1)),
                              ["WC", sk], ["pY"])

                for gp in range(8):
                    s5_col(0, gp, 0, CTX, 0, False, False)
                cnt_ = 0
                for sgi in range(SEQ // L):
                    for kt in range(4):
                        for g2 in range(2):
                            s5_col(0, kt * 2 + g2, CTX + sgi * L, L, CTX + sgi * L, False, True)
                        yb = ysb[cnt_ % 2]
                        yk = "ysb"
                        cnt_ += 1
                        A(lambda e, yb=yb: e.activation(out=yb[:], in_=pY[:], func=AF.Identity), ["pY"], [yk])
                        DS(lambda e, kt=kt, sgi=sgi, yb=yb: e.dma_start(out=yf_d[kt, :, sgi * L:(sgi + 1) * L], in_=yb[:]), [yk], ["yf"])
                for gp in range(8):
                    s5_col(1, gp, 0, CTX, 0, True, False)
                for sb_i in range(SEQ // L):
                    sgi = SEQ // L - 1 - sb_i
                    for kt in range(4):
                        yl = yfl[cnt_ % 2]
                        ylk = "yfl"
                        yb = ysb[cnt_ % 2]
                        yk = "ysb"
                        cnt_ += 1
                        DS(lambda e, kt=kt, sgi=sgi, yl=yl: e.dma_start(out=yl[:], in_=yf_d[kt, :, sgi * L:(sgi + 1) * L]), ["yf"], [ylk])
                        for g2 in range(2):
                            s5_col(1, kt * 2 + g2, CTX + sgi * L, L, CTX + sb_i * L, True, True)
                        V(lambda e, yb=yb, yl=yl: e.tensor_tensor(out=yb[:], in0=pY[:], in1=yl[:], op=ALU.add), ["pY", ylk], [yk])
                        V(lambda e, kt=kt, sgi=sgi, yb=yb: e.scalar_tensor_tensor(out=yb[:], in0=U[:, kt, CTX + sgi * L:CTX + (sgi + 1) * L],
                                                                                  scalar=dsc[:, kt:kt + 1], in1=yb[:],
                                                                                  op0=ALU.mult, op1=ALU.add), ["U", "dsc", yk], [yk])
                        A(lambda e, kt=kt, yb=yb: e.activation(out=gg[:, kt, :], in_=yb[:], func=AF.Gelu_apprx_tanh), [yk], ["gg"])
                    for mt in range(4):
                        sg_ = sg[mt % 2]
                        sgk = "sg"
                        so_ = s5o[mt % 2]
                        sok = "s5o"
                        for c0 in range(0, L, 512):
                            for kt in range(4):
                                T(lambda e, mt=mt, c0=c0, kt=kt: e.matmul(pG[:, c0:c0 + 512], lhsT=wglu[:, kt, mt * 128:(mt + 1) * 128],
                                                                         rhs=gg[:, kt, c0:c0 + 512], start=(kt == 0), stop=(kt == 3)),
                                  ["wglu", "gg"], ["pG"])
                        A(lambda e, mt=mt, sg_=sg_: e.activation(out=sg_[:], in_=pG[:], func=AF.Sigmoid, bias=bglu[:, mt:mt + 1]), ["pG", "bglu"], [sgk])
                        G(lambda e, mt=mt, sg_=sg_, so_=so_: e.tensor_tensor(out=so_[:], in0=sg_[:], in1=gg[:, mt, :], op=ALU.mult), [sgk, "gg"], [sok])
                        DS(lambda e, mt=mt, sgi=sgi, so_=so_: e.dma_start(out=catT_d[mt, :, sgi * L:(sgi + 1) * L], in_=so_[:]), [sok], ["catT_s5"])
                P.barrier()
        P.barrier()

        with contextlib.ExitStack() as st:
            wo = sb(st, "wo", [128, 10, D], BF16)
            wr = sb(st, "wr", [128, 8, 16], BF16)
            with contextlib.ExitStack() as s2:
                stg = sb(s2, "stg3", [128, 10, D])
                DS(lambda e: e.dma_start(out=stg[:], in_=w_out_pad.rearrange("(kt p) f -> p kt f", p=128)), w=["stg3"])
                V(lambda e: e.tensor_copy(out=wo[:], in_=stg[:]), ["stg3"], ["wo"])
                DS(lambda e: e.dma_start(out=stg[:, 0:8, 0:16], in_=w_router.rearrange("(kt p) f -> p kt f", p=128)), w=["stg3"])
                V(lambda e: e.tensor_copy(out=wr[:], in_=stg[:, 0:8, 0:16]), ["stg3"], ["wr"])
                P.barrier()
            cat = [sb(st, "cat%d" % i, [128, 10, 128], BF16) for i in range(2)]
            xt3 = [sb(st, "x3_%d" % i, [128, D]) for i in range(2)]
            tmA = [sb(st, "tm%d" % i, [128, D]) for i in range(2)]
            xrA = [sb(st, "xr%d" % i, [128, D]) for i in range(2)]
            xnwA = [sb(st, "xnw%d" % i, [128, D]) for i in range(2)]
            hhA = [sb(st, "hh%d" % i, [128, D]) for i in range(2)]
            hbA = [sb(st, "hb%d" % i, [128, D], BF16) for i in range(2)]
            hTA = [sb(st, "hT%d" % i, [128, 8, 128], BF16) for i in range(2)]
            statsA = [sb(st, "stats3_%d" % i, [128, 2, 6]) for i in range(4)]
            mvA = [sb(st, "mv3_%d" % i, [128, 2]) for i in range(4)]
            rstdA = [sb(st, "rstd3_%d" % i, [128, 1]) for i in range(4)]
            lgA = [sb(st, "lg%d" % i, [128, 16]) for i in range(2)]
            mxA = [sb(st, "mx%d" % i, [128, 1]) for i in range(2)]
            smA = [sb(st, "sm%d" % i, [128, 1]) for i in range(2)]
            pMxA = [ps(st, "pMx%d" % i, [128, D]) for i in range(2)]
            pT3A = [ps(st, "pT3%d" % i, [128, D], BF16) for i in range(2)]
            pLA = [ps(st, "pL%d" % i, [128, 16]) for i in range(2)]

            def lnorm(src, sk, dst, dk, si):
                stats, mv, rstd = statsA[si], mvA[si], rstdA[si]
                k1, k2, k3 = "stats3_%d" % si, "mv3_%d" % si, "rstd3_%d" % si
                for c in range(2):
                    V(lambda e, c=c: e.bn_stats(out=stats[:, c, :], in_=src[:, c * 512:(c + 1) * 512]), [sk], [k1])
                V(lambda e: e.bn_aggr(out=mv[:], in_=stats[:].rearrange("p a b -> p (a b)")), [k1], [k2])
                V(lambda e: e.tensor_scalar_add(out=rstd[:], in0=mv[:, 1:2], scalar1=EPS), [k2], [k3])
                A(lambda e: e.activation(out=rstd[:], in_=rstd[:], func=AF.Sqrt), [k3], [k3])
                V(lambda e: e.reciprocal(out=rstd[:], in_=rstd[:]), [k3], [k3])
                V(lambda e: e.tensor_scalar(out=dst[:], in0=src[:], scalar1=mv[:, 0:1], scalar2=rstd[:, 0:1],
                                            op0=ALU.subtract, op1=ALU.mult), [sk, k2, k3], [dk])

            def p3_loads(t):
                i2 = t % 2
                c_, ck = cat[i2], "cat%d" % i2
                x_, xk = xt3[i2], "x3_%d" % i2
                DS(lambda e: e.dma_start(out=c_[:], in_=catT_d[:, :, t * 128:(t + 1) * 128].rearrange("a p t -> p a t")), w=[ck])
                DS(lambda e: e.dma_start(out=x_[:], in_=xs[CTX + t * 128:CTX + (t + 1) * 128, :]), w=[xk])

            for t in range(NT):
                i2 = t % 2
                c_, ck = cat[i2], "cat%d" % i2
                x_, xk = xt3[i2], "x3_%d" % i2
                tm, tmk = tmA[i2], "tm%d" % i2
                xr, xrk = xrA[i2], "xr%d" % i2
                xnw, xnk = xnwA[i2], "xnw%d" % i2
                hh, hhk = hhA[i2], "hh%d" % i2
                hb, hbk = hbA[i2], "hb%d" % i2
                hT, hTk = hTA[i2], "hT%d" % i2
                lg, lgk = lgA[i2], "lg%d" % i2
                mx, mxk = mxA[i2], "mx%d" % i2
                sm, smk = smA[i2], "sm%d" % i2
                pMx, pMk = pMxA[i2], "pMx%d" % i2
                pT3, pTk = pT3A[i2], "pT3%d" % i2
                pL, pLk = pLA[i2], "pL%d" % i2
                if t == 0:
                    p3_loads(0)
                if t + 1 < NT:
                    p3_loads(t + 1)
                for nh in range(2):
                    for kt in range(10):
                        T(lambda e, nh=nh, kt=kt, c_=c_, pMx=pMx: e.matmul(pMx[:, nh * 512:(nh + 1) * 512], lhsT=c_[:, kt, :],
                                                                           rhs=wo[:, kt, nh * 512:(nh + 1) * 512], start=(kt == 0), stop=(kt == 9)),
                          [ck, "wo"], [pMk])
                V(lambda e, tm=tm, pMx=pMx: e.tensor_tensor(out=tm[:], in0=pMx[:], in1=modrow[:, 0, :], op=ALU.mult), [pMk, "modrow"], [tmk])
                V(lambda e, x_=x_, xr=xr, tm=tm: e.scalar_tensor_tensor(out=xr[:], in0=x_[:], scalar=ALPHA, in1=tm[:], op0=ALU.mult, op1=ALU.add),
                  [xk, tmk], [xrk])
                lnorm(xr, xrk, tm, tmk, 2 * i2)
                G(lambda e, tm=tm: e.tensor_tensor(out=tm[:], in0=tm[:], in1=lnrow[:, 0, :], op=ALU.mult), [tmk, "lnrow"], [tmk])
                G(lambda e, tm=tm, xnw=xnw: e.tensor_tensor(out=xnw[:], in0=tm[:], in1=lnrow[:, 1, :], op=ALU.add), [tmk, "lnrow"], [xnk])
                DS(lambda e, t=t, xnw=xnw: e.dma_start(out=xnew_d[t * 128:(t + 1) * 128, :], in_=xnw[:]), [xnk], ["xnew_d"])
                lnorm(xnw, xnk, hh, hhk, 2 * i2 + 1)
                V(lambda e, hh=hh: e.tensor_tensor(out=hh[:], in0=hh[:], in1=modrow[:, 2, :], op=ALU.mult), [hhk, "modrow"], [hhk])
                V(lambda e, hh=hh, hb=hb: e.tensor_tensor(out=hb[:], in0=hh[:], in1=modrow[:, 1, :], op=ALU.add), [hhk, "modrow"], [hbk])
                DS(lambda e, t=t, hb=hb: e.dma_start(out=hrow_d[t * 128:(t + 1) * 128, :], in_=hb[:]), [hbk], ["hrow_d"])
                for kt in range(8):
                    T(lambda e, kt=kt, hb=hb, pT3=pT3: e.transpose(out=pT3[:, kt * 128:(kt + 1) * 128], in_=hb[:, kt * 128:(kt + 1) * 128],
                                                                   identity=ident_bf[:]), [hbk, "ident_bf"], [pTk])
                A(lambda e, hT=hT, pT3=pT3: e.activation(out=hT[:].rearrange("p a b -> p (a b)"), in_=pT3[:], func=AF.Identity), [pTk], [hTk])
                for kt in range(8):
                    T(lambda e, kt=kt, hT=hT, pL=pL: e.matmul(pL[:], lhsT=hT[:, kt, :], rhs=wr[:, kt, :], start=(kt == 0), stop=(kt == 7)),
                      [hTk, "wr"], [pLk])
                V(lambda e, mx=mx, pL=pL: e.tensor_reduce(out=mx[:], in_=pL[:], axis=AX.X, op=ALU.max), [pLk], [mxk])
                V(lambda e, mx=mx: e.tensor_scalar_mul(out=mx[:], in0=mx[:], scalar1=-1.0), [mxk], [mxk])
                A(lambda e, lg=lg, pL=pL, mx=mx: e.activation(out=lg[:], in_=pL[:], func=AF.Exp, bias=mx[:, 0:1]), [pLk, mxk], [lgk])
                V(lambda e, sm=sm, lg=lg: e.tensor_reduce(out=sm[:], in_=lg[:], axis=AX.X, op=ALU.add), [lgk], [smk])
                V(lambda e, sm=sm: e.reciprocal(out=sm[:], in_=sm[:]), [smk], [smk])
                V(lambda e, t=t, lg=lg, sm=sm: e.tensor_scalar_mul(out=aff[:, t, :], in0=lg[:], scalar1=sm[:, 0:1]), [lgk, smk], ["aff%d" % t])
                DS(lambda e, t=t: e.dma_start(out=aff_d[t * 128:(t + 1) * 128, :], in_=aff[:, t, :]), ["aff%d" % t], ["aff_d"])
            P.barrier()

        if debug == 1:
            early.close()
            return nc

        with contextlib.ExitStack() as st:
            hs = sb(st, "hs", [128, 16])
            am = sb(st, "am", [128, NT, NE])
            lo = sb(st, "lo", [128, NE])
            hi = sb(st, "hi", [128, NE])
            mid = sb(st, "mid", [128, NE])
            cmp_ = sb(st, "cmp", [128, NT, NE])
            cnt = sb(st, "cnt", [128, NE])
            cntb = sb(st, "cntb", [128, NE], BF16)
            ge = sb(st, "ge", [128, NE])
            dl = sb(st, "dl", [128, NE])
            pC = ps(st, "pC", [128, NE])
            DS(lambda e: e.dma_start(out=hs[:], in_=half_sel), w=["hs"])
            V(lambda e: e.tensor_tensor(out=am[:], in0=aff[:, :, 0:8], in1=bc(hs[:, 0:8].unsqueeze(1), [128, NT, NE]), op=ALU.mult),
              ["aff", "hs"], ["am"])
            V(lambda e: e.tensor_tensor(out=cmp_[:], in0=aff[:, :, 8:16], in1=bc(hs[:, 8:16].unsqueeze(1), [128, NT, NE]), op=ALU.mult),
              ["aff", "hs"], ["cmp"])
            V(lambda e: e.tensor_tensor(out=am[:], in0=am[:], in1=cmp_[:], op=ALU.add), ["am", "cmp"], ["am"])
            V(lambda e: e.memset(lo[:], 0.0), w=["lo"])
            V(lambda e: e.memset(hi[:], 1.0), w=["hi"])
            for it in range(30):
                V(lambda e: e.tensor_tensor(out=mid[:], in0=lo[:], in1=hi[:], op=ALU.add), ["lo", "hi"], ["mid"])
                V(lambda e: e.tensor_scalar_mul(out=mid[:], in0=mid[:], scalar1=0.5), ["mid"], ["mid"])
                V(lambda e: e.tensor_tensor(out=cmp_[:], in0=am[:], in1=bc(mid[:].unsqueeze(1), [128, NT, NE]), op=ALU.is_ge),
                  ["am", "mid"], ["cmp"])
                V(lambda e: e.tensor_reduce(out=cnt[:], in_=cmp_[:].rearrange("p t e -> p e t"), axis=AX.X, op=ALU.add), ["cmp"], ["cnt"])
                V(lambda e: e.tensor_copy(out=cntb[:], in_=cnt[:]), ["cnt"], ["cntb"])
                T(lambda e: e.matmul(pC[:], lhsT=ones_bf[:], rhs=cntb[:], start=True, stop=True), ["ones", "cntb"], ["pC"])
                V(lambda e: e.tensor_single_scalar(out=ge[:], in_=pC[:], scalar=float(CAP), op=ALU.is_ge), ["pC"], ["ge"])
                V(lambda e: e.tensor_tensor(out=dl[:], in0=mid[:], in1=lo[:], op=ALU.subtract), ["mid", "lo"], ["dl"])
                V(lambda e: e.tensor_tensor(out=dl[:], in0=dl[:], in1=ge[:], op=ALU.mult), ["dl", "ge"], ["dl"])
                V(lambda e: e.tensor_tensor(out=lo[:], in0=lo[:], in1=dl[:], op=ALU.add), ["lo", "dl"], ["lo"])
                V(lambda e: e.tensor_tensor(out=dl[:], in0=hi[:], in1=mid[:], op=ALU.subtract), ["hi", "mid"], ["dl"])
                V(lambda e: e.tensor_tensor(out=dl[:], in0=dl[:], in1=ge[:], op=ALU.mult), ["dl", "ge"], ["dl"])
                V(lambda e: e.tensor_tensor(out=hi[:], in0=mid[:], in1=dl[:], op=ALU.add), ["mid", "dl"], ["hi"])
            mk = sb(st, "mk", [128, NE, NT], BF16)
            cin = sb(st, "cin", [128, NE * NT])
            tot = sb(st, "tot", [128, NE, NT])
            cend = sb(st, "cend", [128, NE, NT])
            pP = ps(st, "pP", [128, NE * NT])
            pTt = ps(st, "pTt", [128, NE * NT])
            V(lambda e: e.tensor_tensor(out=mk[:], in0=am[:].rearrange("p t e -> p e t"), in1=bc(lo[:].unsqueeze(2), [128, NE, NT]),
                                        op=ALU.is_ge), ["am", "lo"], ["mk"])
            T(lambda e: e.matmul(pP[:], lhsT=tri_bf[:], rhs=mk[:].rearrange("p e t -> p (e t)"), start=True, stop=True), ["tri", "mk"], ["pP"])
            T(lambda e: e.matmul(pTt[:], lhsT=ones_bf[:], rhs=mk[:].rearrange("p e t -> p (e t)"), start=True, stop=True), ["ones", "mk"], ["pTt"])
            V(lambda e: e.tensor_copy(out=cin[:], in_=pP[:]), ["pP"], ["cin"])
            V(lambda e: e.tensor_copy(out=tot[:].rearrange("p e t -> p (e t)"), in_=pTt[:]), ["pTt"], ["tot"])
            V(lambda e: e.tensor_copy(out=cend[:], in_=tot[:]), ["tot"], ["cend"])
            sh = 1
            tmpc = sb(st, "tmpc", [128, NE, NT])
            while sh < NT:
                V(lambda e: e.tensor_copy(out=tmpc[:], in_=cend[:]), ["cend"], ["tmpc"])
                V(lambda e, sh=sh: e.tensor_tensor(out=cend[:, :, sh:NT], in0=tmpc[:, :, sh:NT], in1=tmpc[:, :, 0:NT - sh], op=ALU.add),
                  ["tmpc"], ["cend"])
                sh *= 2
            cinT = sb(st, "cinT", [128, 4, 128])
            pX = ps(st, "pX", [128, 4, 128])
            for a in range(4):
                T(lambda e, a=a: e.transpose(out=pX[:, a, :], in_=cin[:, a * 128:(a + 1) * 128], identity=ident_f[:]), ["cin", "ident_f"], ["pX"])
            V(lambda e: e.tensor_copy(out=cinT[:], in_=pX[:]), ["pX"], ["cinT"])
            DS(lambda e: e.dma_start(out=cin_d.rearrange("(a p) t -> p a t", p=128), in_=cinT[:]), ["cinT"], ["cin_d"])
            spp = sb(st, "spp", [128, 8])
            DS(lambda e: e.dma_start(out=spp[:], in_=slot_pp_d), w=["spp"])
            le = sb(st, "le", [128, NE, 8, NT])
            tl_ = sb(st, "tl_", [128, NE, 8])
            cst = sb(st, "cst", [128, NE, 8])
            rr = sb(st, "rr", [128, NE, 8])
            rowi = sb(st, "rowi", [128, NE, 8], I32)
            rowf = sb(st, "rowf", [128, NE, 8])
            for e_ in range(NE):
                V(lambda e, e_=e_: e.tensor_tensor(out=le[:, e_, :, :], in0=bc(cend[:, e_, :].unsqueeze(1), [128, 8, NT]),
                                                   in1=bc(spp[:].unsqueeze(2), [128, 8, NT]), op=ALU.is_le), ["cend", "spp"], ["le"])
            V(lambda e: e.tensor_reduce(out=tl_[:].rearrange("p e j -> p (e j)"), in_=le[:].rearrange("p e j t -> p (e j) t"),
                                        axis=AX.X, op=ALU.add), ["le"], ["tl_"])
            for e_ in range(NE):
                V(lambda e, e_=e_: e.tensor_tensor(out=le[:, e_, :, :], in0=le[:, e_, :, :], in1=bc(tot[:, e_, :].unsqueeze(1), [128, 8, NT]),
                                                   op=ALU.mult), ["le", "tot"], ["le"])
            V(lambda e: e.tensor_reduce(out=cst[:].rearrange("p e j -> p (e j)"), in_=le[:].rearrange("p e j t -> p (e j) t"),
                                        axis=AX.X, op=ALU.add), ["le"], ["cst"])
            V(lambda e: e.tensor_scalar_min(out=tl_[:], in0=tl_[:], scalar1=float(NT - 1)), ["tl_"], ["tl_"])
            V(lambda e: e.tensor_tensor(out=rr[:], in0=bc(spp[:].unsqueeze(1), [128, NE, 8]), in1=cst[:], op=ALU.subtract), ["spp", "cst"], ["rr"])
            for e_ in range(NE):
                V(lambda e, e_=e_: e.tensor_scalar_add(out=rowf[:, e_, :], in0=tl_[:, e_, :], scalar1=float(e_ * NT)), ["tl_"], ["rowf"])
            V(lambda e: e.tensor_copy(out=rowi[:], in_=rowf[:]), ["rowf"], ["rowi"])
            crow = sb(st, "crow", [128, 128])
            cle = sb(st, "cle", [128, 128])
            tloc = sb(st, "tloc", [128, NE, 8])
            arow = sb(st, "arow", [128, 16])
            idf = sb(st, "idf", [128, NE, 8])
            V(lambda e: e.memset(tloc[:], 0.0), w=["tloc"])
            for e_ in range(NE):
                for j in range(8):
                    DG(lambda e, e_=e_, j=j: e.indirect_dma_start(out=crow[:], out_offset=None, in_=cin_d,
                                                                 in_offset=bass.IndirectOffsetOnAxis(ap=rowi[:, e_, j:j + 1], axis=0)),
                       ["cin_d", "rowi"], ["crow"])
                    V(lambda e, e_=e_, j=j: e.tensor_scalar(out=cle[:], in0=crow[:], scalar1=rr[:, e_, j:j + 1], scalar2=0.0,
                                                            op0=ALU.is_le, op1=ALU.add, accum_out=tloc[:, e_, j:j + 1]),
                      ["crow", "rr"], ["cle", "tloc"])
            V(lambda e: e.tensor_scalar_min(out=tloc[:], in0=tloc[:], scalar1=127.0), ["tloc"], ["tloc"])
            V(lambda e: e.scalar_tensor_tensor(out=idf[:], in0=tl_[:], scalar=128.0, in1=tloc[:], op0=ALU.mult, op1=ALU.add),
              ["tl_", "tloc"], ["idf"])
            V(lambda e: e.tensor_copy(out=idx_all[:], in_=idf[:]), ["idf"], ["idx_all"])
            for e_ in range(NE):
                for j in range(8):
                    DG(lambda e, e_=e_, j=j: e.indirect_dma_start(out=arow[:], out_offset=None, in_=aff_d,
                                                                 in_offset=bass.IndirectOffsetOnAxis(ap=idx_all[:, e_, j:j + 1], axis=0)),
                       ["aff_d", "idx_all"], ["arow"])
                    V(lambda e, e_=e_, j=j: e.tensor_tensor(out=cle[:, 0:16], in0=arow[:], in1=hs[:], op=ALU.mult), ["arow", "hs"], ["cle"])
                    V(lambda e, e_=e_, j=j: e.tensor_tensor(out=gate_all[:, e_, j:j + 1], in0=cle[:, e_:e_ + 1], in1=cle[:, 8 + e_:9 + e_],
                                                            op=ALU.add), ["cle"], ["gate_all"])
            P.barrier()

        if debug:
            DS(lambda e: e.dma_start(out=dbg_idx, in_=idx_all[:].rearrange("p a b -> p (a b)")), ["idx_all"], ["dbg_idx"])
            DS(lambda e: e.dma_start(out=dbg_gate, in_=gate_all[:].rearrange("p a b -> p (a b)")), ["gate_all"], ["dbg_gate"])
            P.barrier()
        if debug == 2:
            early.close()
            return nc

        early.close()
        with contextlib.ExitStack() as st:
            zt = sb(st, "zt", [128, D])
            V(lambda e: e.memset(zt[:], 0.0), w=["zt"])
            for t in range(NT):
                DS(lambda e, t=t: e.dma_start(out=moe_d[t * 128:(t + 1) * 128, :], in_=zt[:]), ["zt"], ["moe_d"])
            P.barrier()
        FB = 256
        NFB = DFF // FB
        with contextlib.ExitStack() as st:
            xgA = [sb(st, "xg%d" % i, [128, D], BF16) for i in range(2)]
            xgT = sb(st, "xgT", [128, 8, CAP], BF16)
            wdn = sb(st, "wdn", [128, NFT, D], BF16)
            hid = sb(st, "hid", [128, NFT, CAP], BF16)
            stg = [sb(st, "wstg%d" % i, [128, 2, 8, FB]) for i in range(2)]
            wgbA = [sb(st, "wgb%d" % i, [128, 8, FB], BF16) for i in range(2)]
            wubA = [sb(st, "wub%d" % i, [128, 8, FB], BF16) for i in range(2)]
            stgdA = [sb(st, "stgd%d" % i, [128, D]) for i in range(2)]
            slA = [sb(st, "sl%d" % i, [128, 512]) for i in range(2)]
            obA = [sb(st, "ob%d" % i, [128, D]) for i in range(2)]
            pTg = ps(st, "pTg", [128, D], BF16)
            pGU = [[ps(st, "pGU%d%d" % (h_, m_), [128, 512]) for m_ in range(2)] for h_ in range(2)]
            pO = ps(st, "pO", [128, D])
            gcnt = 0
            ocnt = 0
            for e_ in range(NE):
                for j in range(8):
                    xg, xgk = xgA[gcnt % 2], "xg%d" % (gcnt % 2)
                    gcnt += 1
                    DG(lambda e, e_=e_, j=j, xg=xg: e.indirect_dma_start(out=xg[:], out_offset=None, in_=hrow_d,
                                                                        in_offset=bass.IndirectOffsetOnAxis(ap=idx_all[:, e_, j:j + 1], axis=0)),
                       ["hrow_d", "idx_all"], [xgk])
                    for kt in range(8):
                        T(lambda e, kt=kt, xg=xg: e.transpose(out=pTg[:, kt * 128:(kt + 1) * 128], in_=xg[:, kt * 128:(kt + 1) * 128],
                                                              identity=ident_bf[:]), [xgk, "ident_bf"], ["pTg"])
                    V(lambda e, j=j: e.tensor_copy(out=xgT[:, :, j * 128:(j + 1) * 128], in_=pTg[:].rearrange("p (a b) -> p a b", a=8)),
                      ["pTg"], ["xgT"])
                for fb in range(NFB):
                    s_, sk = stg[fb % 2], "wstg%d" % (fb % 2)
                    wgb, wgk = wgbA[fb % 2], "wgb%d" % (fb % 2)
                    wub, wuk = wubA[fb % 2], "wub%d" % (fb % 2)
                    DS(lambda e, e_=e_, fb=fb, s_=s_: e.dma_start(out=s_[:, 0, :, :],
                                                                  in_=wg[e_].rearrange("(kt p) f -> p kt f", p=128)[:, :, fb * FB:(fb + 1) * FB]),
                       w=[sk + "g"])
                    DS(lambda e, e_=e_, fb=fb, s_=s_: e.dma_start(out=s_[:, 1, :, :],
                                                                  in_=wu[e_].rearrange("(kt p) f -> p kt f", p=128)[:, :, fb * FB:(fb + 1) * FB]),
                       w=[sk + "u"])
                    A(lambda e, s_=s_, wgb=wgb: e.activation(out=wgb[:], in_=s_[:, 0, :, :], func=AF.Identity), [sk + "g"], [wgk])
                    G(lambda e, s_=s_, wub=wub: e.tensor_copy(out=wub[:], in_=s_[:, 1, :, :]), [sk + "u"], [wuk])
                    if fb < 2 * 0 + NFB:
                        for q_ in range(2):
                            ft = fb * 2 + q_
                            sd_, sdk = stgdA[ft % 2], "stgd%d" % (ft % 2)
                            DS(lambda e, e_=e_, ft=ft, sd_=sd_: e.dma_start(out=sd_[:], in_=wd[e_, ft * 128:(ft + 1) * 128, :]), w=[sdk])
                            G(lambda e, ft=ft, sd_=sd_: e.tensor_copy(out=wdn[:, ft, :], in_=sd_[:]), [sdk], ["wdn"])
                    for fl in range(FB // 128):
                        ft = fb * (FB // 128) + fl
                        for h_ in range(2):
                            c0 = h_ * 512
                            pg, pu = pGU[h_][0], pGU[h_][1]
                            pgk, puk = "pGU%d0" % h_, "pGU%d1" % h_
                            sl, slk = slA[h_], "sl%d" % h_
                            for kt in range(8):
                                T(lambda e, kt=kt, c0=c0, fl=fl, pg=pg, wgb=wgb: e.matmul(pg[:], lhsT=wgb[:, kt, fl * 128:(fl + 1) * 128],
                                                                                        rhs=xgT[:, kt, c0:c0 + 512], start=(kt == 0), stop=(kt == 7)),
                                  [wgk, "xgT"], [pgk])
                            for kt in range(8):
                                T(lambda e, kt=kt, c0=c0, fl=fl, pu=pu, wub=wub: e.matmul(pu[:], lhsT=wub[:, kt, fl * 128:(fl + 1) * 128],
                                                                                        rhs=xgT[:, kt, c0:c0 + 512], start=(kt == 0), stop=(kt == 7)),
                                  [wuk, "xgT"], [puk])
                            A(lambda e, sl=sl, pg=pg: e.activation(out=sl[:], in_=pg[:], func=AF.Silu), [pgk], [slk])
                            V(lambda e, ft=ft, c0=c0, sl=sl, pu=pu: e.tensor_tensor(out=hid[:, ft, c0:c0 + 512], in0=sl[:], in1=pu[:], op=ALU.mult),
                              [slk, puk], ["hid"])
                for j in range(8):
                    ob, obk = obA[ocnt % 2], "ob%d" % (ocnt % 2)
                    ocnt += 1
                    for nh in range(2):
                        for ft in range(NFT):
                            T(lambda e, j=j, nh=nh, ft=ft: e.matmul(pO[:, nh * 512:(nh + 1) * 512], lhsT=hid[:, ft, j * 128:(j + 1) * 128],
                                                                    rhs=wdn[:, ft, nh * 512:(nh + 1) * 512], start=(ft == 0), stop=(ft == NFT - 1)),
                              ["hid", "wdn"], ["pO"])
                    V(lambda e, e_=e_, j=j, ob=ob: e.tensor_scalar_mul(out=ob[:], in0=pO[:], scalar1=gate_all[:, e_, j:j + 1]), ["pO", "gate_all"], [obk])
                    DG(lambda e, e_=e_, j=j, ob=ob: e.indirect_dma_start(out=moe_d, out_offset=bass.IndirectOffsetOnAxis(ap=idx_all[:, e_, j:j + 1], axis=0),
                                                                        in_=ob[:], in_offset=None, compute_op=ALU.add, oob_is_err=True),
                       [obk, "idx_all", "moe_d"], ["moe_d"])
            P.barrier()

        if debug == 3:
            return nc

        ccsem = gst.enter_context(nc.semaphore("ccsem"))
        CCH = 16
        crow_ = SEQ // CCH
        for cc_ in range(CCH):
            nc.gpsimd.collective_compute("AllReduce", ALU.add, replica_groups=[[0, 1], [2, 3], [4, 5], [6, 7]],
                                         ins=[moe_d[cc_ * crow_:(cc_ + 1) * crow_, :]],
                                         outs=[moe_r[cc_ * crow_:(cc_ + 1) * crow_, :]]).then_inc(ccsem)
            nc.gpsimd.wait_ge(ccsem, cc_ + 1)
        G(lambda e: e.memset(gate_all[:, 0, 0:1], 0.0), w=["gate_all"])
        P.barrier()
        with contextlib.ExitStack() as st:
            mt_ = [sb(st, "m6_%d" % i, [128, D]) for i in range(2)]
            xq = [sb(st, "x6_%d" % i, [128, D]) for i in range(2)]
            tm = sb(st, "tm6", [128, D])
            xr = sb(st, "xr6", [128, D])
            oo = [sb(st, "o6_%d" % i, [128, D]) for i in range(2)]
            stats = sb(st, "stats6", [128, 2, 6])
            mv = sb(st, "mv6_", [128, 2])
            rstd = sb(st, "rstd6", [128, 1])
            tki = sb(st, "tki", [128, NT // 2], I32)
            DS(lambda e: e.dma_start(out=tki[:], in_=tokidx_d), w=["tki"])
            for t in range(NT // 2):
                m_ = mt_[t % 2]
                mk_ = "m6_%d" % (t % 2)
                x_ = xq[t % 2]
                xk = "x6_%d" % (t % 2)
                o_ = oo[t % 2]
                ok = "o6_%d" % (t % 2)
                DG(lambda e, t=t, m_=m_: e.indirect_dma_start(out=m_[:], out_offset=None, in_=moe_r,
                                                             in_offset=bass.IndirectOffsetOnAxis(ap=tki[:, t:t + 1], axis=0)),
                   ["moe_r", "tki"], [mk_])
                DG(lambda e, t=t, x_=x_: e.indirect_dma_start(out=x_[:], out_offset=None, in_=xnew_d,
                                                             in_offset=bass.IndirectOffsetOnAxis(ap=tki[:, t:t + 1], axis=0)),
                   ["xnew_d", "tki"], [xk])
                V(lambda e, m_=m_: e.tensor_tensor(out=tm[:], in0=m_[:], in1=fin3[:, 0, :], op=ALU.mult), [mk_, "fin3"], ["tm6"])
                V(lambda e, x_=x_: e.scalar_tensor_tensor(out=xr[:], in0=x_[:], scalar=ALPHA, in1=tm[:], op0=ALU.mult, op1=ALU.add),
                  [xk, "tm6"], ["xr6"])
                for c in range(2):
                    V(lambda e, c=c: e.bn_stats(out=stats[:, c, :], in_=xr[:, c * 512:(c + 1) * 512]), ["xr6"], ["stats6"])
                V(lambda e: e.bn_aggr(out=mv[:], in_=stats[:].rearrange("p a b -> p (a b)")), ["stats6"], ["mv6_"])
                V(lambda e: e.tensor_scalar_add(out=rstd[:], in0=mv[:, 1:2], scalar1=EPS), ["mv6_"], ["rstd6"])
                A(lambda e: e.activation(out=rstd[:], in_=rstd[:], func=AF.Sqrt), ["rstd6"], ["rstd6"])
                V(lambda e: e.reciprocal(out=rstd[:], in_=rstd[:]), ["rstd6"], ["rstd6"])
                V(lambda e: e.tensor_scalar(out=tm[:], in0=xr[:], scalar1=mv[:, 0:1], scalar2=rstd[:, 0:1],
                                            op0=ALU.subtract, op1=ALU.mult), ["xr6", "mv6_", "rstd6"], ["tm6"])
                V(lambda e: e.tensor_tensor(out=tm[:], in0=tm[:], in1=fin3[:, 1, :], op=ALU.mult), ["tm6", "fin3"], ["tm6"])
                V(lambda e, o_=o_: e.tensor_tensor(out=o_[:], in0=tm[:], in1=fin3[:, 2, :], op=ALU.add), ["tm6", "fin3"], [ok])
                DS(lambda e, t=t, o_=o_: e.dma_start(out=out_d[t * 128:(t + 1) * 128, :], in_=o_[:]), [ok], ["out_d"])
            P.barrier()
    return nc


def _prep(inputs):
    f32 = np.float32
    g = {k: np.asarray(v) for k, v in inputs.items()}
    bf = ml_dtypes.bfloat16
    com = {}
    com["w_ada"] = np.ascontiguousarray(g["w_ada"][0], f32)
    ba = g["b_ada"][0]
    com["bada_fm"] = np.ascontiguousarray(ba[:2048].reshape(16, 128).T, f32)
    com["bada_row"] = np.ascontiguousarray(ba[2048:].reshape(1, 4096), f32)
    w_in = g["w_in"][0]
    ws5 = np.zeros((D, 4, 4, 32), f32)
    ws5[:, :, :, :16] = w_in[:, :256].reshape(D, 4, 4, 16)
    com["w_in_s5"] = ws5.reshape(D, 512)
    com["w_in_u"] = np.ascontiguousarray(w_in[:, 256:1024], f32)
    com["w_in_v"] = np.ascontiguousarray(w_in[:, 1024:1792], f32)
    com["gm_wsT"] = np.ascontiguousarray(g["gm_ws"][0].transpose(2, 0, 1), f32)
    com["gm_bs_row"] = np.ascontiguousarray(g["gm_bs"][0].reshape(1, 768), f32)
    are = g["s5_a_re"][0]; aim = g["s5_a_im"][0]; ls = g["s5_log_step"][0]
    com["are_row"] = np.ascontiguousarray(are.reshape(1, 2048), f32)
    com["aim_row"] = np.ascontiguousarray(aim.reshape(1, 2048), f32)
    com["ls_row"] = np.ascontiguousarray(np.repeat(ls.reshape(32), 64).reshape(1, 2048), f32)

    def pp(a):
        return np.ascontiguousarray(a.reshape(2, 8, 2, 64).transpose(2, 3, 0, 1).reshape(128, 16), f32)
    com["are_pp"] = pp(are)
    com["aim_pp"] = pp(aim)
    com["ls_pp"] = pp(np.repeat(ls[:, :, None], 64, axis=2))
    for nm, src in (("WBre_raw", g["s5_b_re"][0]), ("WBim_raw", g["s5_b_im"][0])):
        w = np.zeros((128, 2, 8, 128), f32)
        for gp in range(8):
            for g2 in range(2):
                gi = 2 * gp + g2
                r0 = 64 * (gp % 2) + 32 * g2
                w[r0:r0 + 16, :, gp, 64 * g2:64 * g2 + 64] = src[:, gi].transpose(2, 0, 1)
        com[nm] = w.reshape(128, 2048)
    for nm, src in (("WCre_raw", g["s5_c_re"][0]), ("WCim_raw", g["s5_c_im"][0])):
        w = np.zeros((128, 2, 8, 128), f32)
        for gp in range(8):
            for g2 in range(2):
                gi = 2 * gp + g2
                c0 = 64 * (gp % 2) + 32 * g2
                w[64 * g2:64 * g2 + 64, :, gp, c0:c0 + 16] = src[:, gi].transpose(2, 0, 1)
        com[nm] = w.reshape(128, 2048)

    def padpp(v):
        o = np.zeros((4, 4, 32), f32)
        o[:, :, :16] = v.reshape(4, 4, 16)
        return np.ascontiguousarray(o.reshape(4, 128).T, f32)
    com["dpp"] = padpp(g["s5_d"][0])
    com["b_glu_pp"] = padpp(g["s5_b_glu"][0].reshape(16, 16))
    wgl = np.zeros((4, 4, 32, 4, 4, 32), f32)
    wgl[:, :, :16, :, :, :16] = g["s5_w_glu"][0].reshape(4, 4, 16, 4, 4, 16)
    com["w_glu_pad"] = wgl.reshape(512, 512)
    wo = np.zeros((1280, D), f32)
    wo5 = np.zeros((4, 4, 32, D), f32)
    wo5[:, :, :16, :] = g["w_out"][0][:256].reshape(4, 4, 16, D)
    wo[:512] = wo5.reshape(512, D)
    wo[512:] = g["w_out"][0][256:]
    com["w_out_pad"] = wo
    for k in ("ln1_g", "ln1_b", "ln2_g", "ln2_b"):
        com[k] = np.ascontiguousarray(g[k][0].reshape(1, D), f32)
    com["w_router"] = np.ascontiguousarray(g["w_router"][0], f32)
    com["ident_bf"] = np.eye(128, dtype=f32).astype(bf)
    com["ident_f"] = np.eye(128, dtype=f32)
    com["tri"] = np.triu(np.ones((128, 128), f32)).astype(bf)
    com["ones"] = np.ones((128, 128), f32).astype(bf)
    com["iota_row"] = np.arange(1024, dtype=f32).reshape(1, 1024)
    com["slot_pp"] = (np.arange(128, dtype=f32)[:, None] + 128.0 * np.arange(8, dtype=f32)[None, :]).astype(f32)
    maps = []
    for c in range(8):
        b, half = c // 2, c % 2
        m = dict(com)
        m["xs"] = np.ascontiguousarray(np.concatenate([g["ctx"][b], g["x"][b]], axis=0), f32)
        cv = np.stack([g["c"][b], g["c_ctx"]], axis=1)
        m["cT"] = np.ascontiguousarray(cv.reshape(8, 128, 2).transpose(1, 0, 2), f32)
        es = slice(8 * half, 8 * half + 8)
        m["wg"] = np.ascontiguousarray(g["moe_w_gate"][0][es], f32)
        m["wu"] = np.ascontiguousarray(g["moe_w_up"][0][es], f32)
        m["wd"] = np.ascontiguousarray(g["moe_w_down"][0][es], f32)
        hs = np.zeros((128, 16), f32)
        hs[:, es] = 1.0
        m["half_sel"] = hs
        m["tokidx"] = (half * 4096 + np.arange(32, dtype=np.int32)[None, :] * 128 + np.arange(128, dtype=np.int32)[:, None]).astype(np.int32)
        maps.append(m)
    return maps


def kernel(**inputs):
    maps = _prep(inputs)
    nc = build()
    res = run_bass_kernel_spmd(nc, maps, core_ids=list(range(8)))
    out = np.zeros((4, SEQ, D), np.float32)
    for c in range(8):
        b, half = c // 2, c % 2
        out[b, half * 4096:(half + 1) * 4096] = res.results[c]["out"]
    return out
```

```python
import contextlib
import math
import numpy as np
import ml_dtypes
import concourse.bass as bass
import concourse.mybir as mybir
from concourse.bass_utils import run_bass_kernel_spmd

F32 = mybir.dt.float32
BF16 = mybir.dt.bfloat16
I32 = mybir.dt.int32
AF = mybir.ActivationFunctionType
ALU = mybir.AluOpType
AX = mybir.AxisListType

D = 1024
SEQ = 8192
CTX = 256
NT = SEQ // 128
NTC = CTX // 128
TOT = SEQ + CTX
DFF = 2816
NFT = DFF // 128
NE = 8
CAP = 1024
ALPHA = 2.0 ** 0.25
EPS = 1e-6
TWO_PI = 6.283185
SEG = 1024

ENGS = ("sync", "scalar", "vector", "gpsimd", "tensor")
DMA_K = 8


class Prog:
    def __init__(self, nc, stack):
        self.nc = nc
        self.eng = {"sync": nc.sync, "scalar": nc.scalar, "vector": nc.vector,
                    "gpsimd": nc.gpsimd, "tensor": nc.tensor}
        self.sems = {}
        for e in ENGS:
            self.sems["c" + e] = stack.enter_context(nc.semaphore("c" + e))
        for e in ("sync", "scalar", "gpsimd"):
            for i in range(DMA_K):
                n = "d%s%d" % (e, i)
                self.sems[n] = stack.enter_context(nc.semaphore(n))
        self.ccnt = {e: 0 for e in ENGS}
        self.dcnt = {e: 0 for e in ENGS}
        self.lastw = {}
        self.readers = {}
        self.known = {e: {} for e in ENGS}
        self.latest = {}

    def _need(self, eng, tok):
        if tok is None:
            return
        name, val = tok
        if name == "c" + eng and eng in ("tensor", "sync"):
            return
        if self.known[eng].get(name, 0) >= val:
            return
        self.known[eng][name] = val
        self.eng[eng].wait_ge(self.sems[name], val)

    def op(self, eng, fn, reads=(), writes=(), dma=False, sem_inc=None):
        for k in reads:
            self._need(eng, self.lastw.get(k))
        for k in writes:
            self._need(eng, self.lastw.get(k))
            for t in self.readers.get(k, ()):
                self._need(eng, t)
        if dma:
            i = self.dcnt[eng]
            self.dcnt[eng] += 1
            name = "d%s%d" % (eng, i % DMA_K)
            inc = 16 if sem_inc is None else sem_inc
            val = self.latest.get(name, 0) + inc
            if i >= DMA_K:
                self._need(eng, (name, self.latest.get(name, 0)))
        else:
            self.ccnt[eng] += 1
            name = "c" + eng
            val = self.ccnt[eng]
            inc = 1
        tok = (name, val)
        ins = fn(self.eng[eng])
        ins.then_inc(self.sems[name], inc)
        self.latest[name] = val
        for k in writes:
            self.lastw[k] = tok
            self.readers[k] = []
        for k in reads:
            if k not in writes:
                self.readers.setdefault(k, []).append(tok)
        return tok

    def barrier(self):
        toks = list(self.latest.items())
        for e in ENGS:
            for t in toks:
                self._need(e, t)
        self.lastw = {}
        self.readers = {}


def build(debug=0):
    nc = bass.Bass("TRN2", target_bir_lowering=False)

    def din(name, shape, dt=F32):
        return nc.dram_tensor(name, list(shape), dt, kind="ExternalInput").ap()

    def dscr(name, shape, dt=F32):
        return nc.dram_tensor(name, list(shape), dt).ap()

    xs = din("xs", [TOT, D])
    cT = din("cT", [128, 8, 2])
    w_ada = din("w_ada", [D, 6 * D])
    bada_fm = din("bada_fm", [128, 16])
    bada_row = din("bada_row", [1, 4 * D])
    w_in_s5 = din("w_in_s5", [D, 512])
    w_in_u = din("w_in_u", [D, 768])
    w_in_v = din("w_in_v", [D, 768])
    gm_wsT = din("gm_wsT", [128, 6, 128])
    gm_bs_row = din("gm_bs_row", [1, 768])
    are_row = din("are_row", [1, 2048])
    aim_row = din("aim_row", [1, 2048])
    ls_row = din("ls_row", [1, 2048])
    are_pp = din("are_pp", [128, 16])
    aim_pp = din("aim_pp", [128, 16])
    ls_pp = din("ls_pp", [128, 16])
    WBre_raw = din("WBre_raw", [128, 2048])
    WBim_raw = din("WBim_raw", [128, 2048])
    WCre_raw = din("WCre_raw", [128, 2048])
    WCim_raw = din("WCim_raw", [128, 2048])
    dpp = din("dpp", [128, 4])
    w_glu_pad = din("w_glu_pad", [512, 512])
    b_glu_pp = din("b_glu_pp", [128, 4])
    w_out_pad = din("w_out_pad", [1280, D])
    ln1_g = din("ln1_g", [1, D])
    ln1_b = din("ln1_b", [1, D])
    ln2_g = din("ln2_g", [1, D])
    ln2_b = din("ln2_b", [1, D])
    w_router = din("w_router", [D, 16])
    if debug in (0, 3):
        wg = din("wg", [NE, D, DFF])
        wu = din("wu", [NE, D, DFF])
        wd = din("wd", [NE, DFF, D])
    ident_bf_d = din("ident_bf", [128, 128], BF16)
    ident_f_d = din("ident_f", [128, 128])
    tri_d = din("tri", [128, 128], BF16)
    ones_d = din("ones", [128, 128], BF16)
    iota_row_d = din("iota_row", [1, 1024])
    slot_pp_d = din("slot_pp", [128, 8])
    half_sel = din("half_sel", [128, 16])
    tokidx_d = din("tokidx", [128, NT // 2], I32)

    out_d = nc.dram_tensor("out", [SEQ // 2, D], F32, kind="ExternalOutput").ap()
    okind = {"kind": "ExternalOutput"} if debug else {}
    xnew_d = nc.dram_tensor("xnew_s", [SEQ, D], F32, **okind).ap()
    hrow_d = nc.dram_tensor("hrow_s", [SEQ, D], BF16, **okind).ap()
    catT_d = dscr("catT_s", [10, 128, SEQ], BF16)
    yf_d = dscr("yf_s", [4, 128, SEQ], F32)
    aff_d = nc.dram_tensor("aff_s", [SEQ, 16], F32, **okind).ap()
    if debug:
        dbg_idx = nc.dram_tensor("dbg_idx", [128, NE * 8], I32, kind="ExternalOutput").ap()
        dbg_gate = nc.dram_tensor("dbg_gate", [128, NE * 8], F32, kind="ExternalOutput").ap()
    cin_d = dscr("cin_s", [NE * NT, 128], F32)
    moe_d = nc.dram_tensor("moe_s", [SEQ, D], F32, **({"kind": "ExternalOutput"} if debug == 3 else {})).ap()
    moe_r = dscr("moe_r", [SEQ, D], F32)

    with contextlib.ExitStack() as gst:
        P = Prog(nc, gst)

        _uid = [0]

        def sb(st, name, shape, dt=F32):
            _uid[0] += 1
            return st.enter_context(nc.sbuf_tensor("%s_%d" % (name, _uid[0]), list(shape), dt))

        def ps(st, name, shape, dt=F32):
            _uid[0] += 1
            return st.enter_context(nc.psum_tensor("%s_%d" % (name, _uid[0]), list(shape), dt))

        REC = [None]

        def _op(eng, fn, r, w, dma=False):
            if REC[0] is not None:
                REC[0].append((eng, fn, tuple(r), tuple(w), dma))
                return None
            return P.op(eng, fn, r, w, dma=dma)

        def record(f, *args):
            REC[0] = []
            f(*args)
            ops, REC[0] = REC[0], None
            return ops

        def emit_zip(*lists):
            lists = [l for l in lists if l]
            pos = [0] * len(lists)
            total = sum(len(l) for l in lists)
            for _ in range(total):
                bi, bv = -1, 2.0
                for i, l in enumerate(lists):
                    if pos[i] < len(l):
                        v = pos[i] / len(l)
                        if v < bv:
                            bi, bv = i, v
                eng, fn, r, w, dma = lists[bi][pos[bi]]
                pos[bi] += 1
                P.op(eng, fn, r, w, dma=dma)

        def V(fn, r=(), w=()):
            return _op("vector", fn, r, w)

        def A(fn, r=(), w=()):
            return _op("scalar", fn, r, w)

        def G(fn, r=(), w=()):
            return _op("gpsimd", fn, r, w)

        def T(fn, r=(), w=()):
            return _op("tensor", fn, r, w)

        def DS(fn, r=(), w=()):
            return _op("sync", fn, r, w, dma=True)

        def DG(fn, r=(), w=()):
            return _op("gpsimd", fn, r, w, dma=True)

        def bc(ap_, shape):
            return ap_.to_broadcast(list(shape))

        ident_bf = sb(gst, "ident_bf_t", [128, 128], BF16)
        ident_f = sb(gst, "ident_f_t", [128, 128])
        ones_bf = sb(gst, "ones_t", [128, 128], BF16)
        tri_bf = sb(gst, "tri_t", [128, 128], BF16)
        modfm = sb(gst, "modfm", [128, 2, 8, 2])
        fin3 = sb(gst, "fin3", [128, 3, D])
        idx_all = sb(gst, "idx_all", [128, NE, 8], I32)
        gate_all = sb(gst, "gate_all", [128, NE, 8])
        early = contextlib.ExitStack()
        iota_r = sb(early, "iota_r", [128, 1024])
        modrow = sb(early, "modrow", [128, 4, D])
        lnrow = sb(early, "lnrow", [128, 4, D])
        aff = sb(early, "aff", [128, NT, 16])
        DS(lambda e: e.dma_start(out=ident_bf[:], in_=ident_bf_d), w=["ident_bf"])
        DS(lambda e: e.dma_start(out=ident_f[:], in_=ident_f_d), w=["ident_f"])
        DS(lambda e: e.dma_start(out=ones_bf[:], in_=ones_d), w=["ones"])
        DS(lambda e: e.dma_start(out=tri_bf[:], in_=tri_d), w=["tri"])
        DS(lambda e: e.dma_start(out=iota_r[:], in_=bc(iota_row_d, [128, 1024])), w=["iota_r"])
        for i, a in enumerate((ln1_g, ln1_b, ln2_g, ln2_b)):
            DS(lambda e, i=i, a=a: e.dma_start(out=lnrow[:, i, :], in_=bc(a, [128, D])), w=["lnrow"])

        with contextlib.ExitStack() as st:
            ct = sb(st, "ct", [128, 8, 2])
            sc = sb(st, "sc", [128, 8, 2])
            scb = sb(st, "scb", [128, 8, 128])
            wa = sb(st, "wa", [128, 8, D])
            bfm = sb(st, "bfm", [128, 16])
            brow = sb(st, "brow", [128, 4 * D])
            pfm = ps(st, "pfm", [128, 8, 2])
            prow = ps(st, "prow", [128, D])
            DS(lambda e: e.dma_start(out=ct[:], in_=cT), w=["ct"])
            DS(lambda e: e.dma_start(out=bfm[:], in_=bada_fm), w=["bfm"])
            DS(lambda e: e.dma_start(out=brow[:], in_=bc(bada_row, [128, 4 * D])), w=["brow"])
            A(lambda e: e.activation(out=sc[:], in_=ct[:], func=AF.Silu), ["ct"], ["sc"])
            V(lambda e: e.tensor_copy(out=scb[:], in_=bc(sc[:, :, 0:1], [128, 8, 128])), ["sc"], ["scb"])
            wav = w_ada.rearrange("(kt p) f -> p kt f", p=128)
            for ch in range(6):
                DS(lambda e, ch=ch: e.dma_start(out=wa[:], in_=wav[:, :, ch * D:(ch + 1) * D]), w=["wa"])
                if ch < 2:
                    for ft in range(8):
                        for kt in range(8):
                            T(lambda e, ft=ft, kt=kt: e.matmul(pfm[:, ft, :], lhsT=wa[:, kt, ft * 128:(ft + 1) * 128],
                                                               rhs=sc[:, kt, :], start=(kt == 0), stop=(kt == 7)),
                              ["wa", "sc"], ["pfm"])
                    V(lambda e, ch=ch: e.tensor_tensor(out=modfm[:, ch, :, :], in0=pfm[:],
                                                       in1=bc(bfm[:, ch * 8:(ch + 1) * 8].unsqueeze(2), [128, 8, 2]),
                                                       op=ALU.add), ["pfm", "bfm"], ["modfm"])
                else:
                    for nh in range(2):
                        for kt in range(8):
                            T(lambda e, nh=nh, kt=kt: e.matmul(prow[:, nh * 512:(nh + 1) * 512], lhsT=scb[:, kt, :],
                                                               rhs=wa[:, kt, nh * 512:(nh + 1) * 512],
                                                               start=(kt == 0), stop=(kt == 7)),
                              ["wa", "scb"], ["prow"])
                    V(lambda e, ch=ch: e.tensor_tensor(out=modrow[:, ch - 2, :], in0=prow[:],
                                                       in1=brow[:, (ch - 2) * D:(ch - 1) * D], op=ALU.add),
                      ["prow", "brow"], ["modrow"])
            V(lambda e: e.tensor_scalar_add(out=modfm[:, 1, :, :], in0=modfm[:, 1, :, :], scalar1=1.0), ["modfm"], ["modfm"])
            V(lambda e: e.tensor_scalar_add(out=modrow[:, 2, :], in0=modrow[:, 2, :], scalar1=1.0), ["modrow"], ["modrow"])
            V(lambda e: e.tensor_copy(out=fin3[:, 0, :], in_=modrow[:, 3, :]), ["modrow"], ["fin3"])
            V(lambda e: e.tensor_copy(out=fin3[:, 1:3, :], in_=lnrow[:, 2:4, :]), ["lnrow"], ["fin3"])
            P.barrier()

        with contextlib.ExitStack() as mst:
            U = sb(mst, "U", [128, 4, TOT], BF16)
            wst_ = contextlib.ExitStack()
            wS = sb(wst_, "wS", [128, 8, 512], BF16)
            wU = sb(wst_, "wU", [128, 8, 768], BF16)
            wV = sb(wst_, "wV", [128, 8, 768], BF16)
            wsT = sb(wst_, "wsT", [128, 6, 128], BF16)
            bsrow = sb(wst_, "bsrow", [128, 768])
            with contextlib.ExitStack() as st:
                stg = sb(st, "stg", [128, 8, 768])
                for (src, dst, n, nm) in ((w_in_s5, wS, 512, "wS"), (w_in_u, wU, 768, "wU"), (w_in_v, wV, 768, "wV")):
                    DS(lambda e, src=src, n=n: e.dma_start(out=stg[:, :, 0:n], in_=src.rearrange("(kt p) f -> p kt f", p=128)),
                       w=["stg"])
                    V(lambda e, dst=dst, n=n: e.tensor_copy(out=dst[:], in_=stg[:, :, 0:n]), ["stg"], [nm])
                DS(lambda e: e.dma_start(out=stg[:, 0:6, 0:128], in_=gm_wsT), w=["stg"])
                V(lambda e: e.tensor_copy(out=wsT[:], in_=stg[:, 0:6, 0:128]), ["stg"], ["wsT"])
                DS(lambda e: e.dma_start(out=bsrow[:], in_=bc(gm_bs_row, [128, 768])), w=["bsrow"])
                P.barrier()

            with contextlib.ExitStack() as st:
                xt = [sb(st, "xt%d" % i, [128, D]) for i in range(2)]
                xnA = [sb(st, "xn%d" % i, [128, D], BF16) for i in range(2)]
                xmTA = [sb(st, "xmT%d" % i, [128, 8, 128], BF16) for i in range(2)]
                statsA = [sb(st, "stats%d" % i, [128, 2, 6]) for i in range(2)]
                mvA = [sb(st, "mv%d" % i, [128, 2]) for i in range(2)]
                rstdA = [sb(st, "rstd%d" % i, [128, 1]) for i in range(2)]
                uTA = [sb(st, "uT%d" % i, [128, 6, 128]) for i in range(2)]
                vvA = [sb(st, "vv%d" % i, [128, 6, 128]) for i in range(2)]
                vcA = [sb(st, "vc%d" % i, [128, 6, 128]) for i in range(2)]
                vlnA = [sb(st, "vln%d" % i, [128, 6, 128], BF16) for i in range(2)]
                st6A = [sb(st, "st6%d" % i, [128, 6, 6]) for i in range(2)]
                mv6A = [sb(st, "mv6%d" % i, [128, 6, 2]) for i in range(2)]
                rs6A = [sb(st, "rs6%d" % i, [128, 6]) for i in range(2)]
                gmtA = [sb(st, "gmt%d" % i, [128, 6, 128]) for i in range(2)]
                gmb = [sb(st, "gmb%d" % i, [128, 6, 128], BF16) for i in range(2)]
                usb = [sb(st, "usb%d" % i, [128, 4, 128], BF16) for i in range(2)]
                pT = ps(st, "pT", [128, D], BF16)
                pS = ps(st, "pS", [128, 4, 128])
                pU = ps(st, "pU", [128, 8, 128])
                pV = ps(st, "pV", [128, D])
                pM = ps(st, "pM", [128, 8, 128])

                def stage_a(t):
                    i2 = t % 2
                    lat = t >= NTC
                    col = 0 if lat else 1
                    x_, xk = xt[i2], "xt%d" % i2
                    xn, xnk = xnA[i2], "xn%d" % i2
                    xmT, xmk = xmTA[i2], "xmT%d" % i2
                    stats, sk_ = statsA[i2], "stats%d" % i2
                    mv, mvk = mvA[i2], "mv%d" % i2
                    rstd, rk = rstdA[i2], "rstd%d" % i2
                    DS(lambda e: e.dma_start(out=x_[:], in_=xs[t * 128:(t + 1) * 128, :]), w=[xk])
                    for c in range(2):
                        V(lambda e, c=c: e.bn_stats(out=stats[:, c, :], in_=x_[:, c * 512:(c + 1) * 512]), [xk], [sk_])
                    V(lambda e: e.bn_aggr(out=mv[:], in_=stats[:].rearrange("p a b -> p (a b)")), [sk_], [mvk])
                    V(lambda e: e.tensor_scalar_add(out=rstd[:], in0=mv[:, 1:2], scalar1=EPS), [mvk], [rk])
                    A(lambda e: e.activation(out=rstd[:], in_=rstd[:], func=AF.Sqrt), [rk], [rk])
                    V(lambda e: e.reciprocal(out=rstd[:], in_=rstd[:]), [rk], [rk])
                    V(lambda e: e.tensor_scalar(out=xn[:], in0=x_[:], scalar1=mv[:, 0:1], scalar2=rstd[:, 0:1],
                                                op0=ALU.subtract, op1=ALU.mult), [xk, mvk, rk], [xnk])
                    for kt in range(8):
                        T(lambda e, kt=kt: e.transpose(out=pT[:, kt * 128:(kt + 1) * 128], in_=xn[:, kt * 128:(kt + 1) * 128],
                                                       identity=ident_bf[:]), [xnk, "ident_bf"], ["pT"])
                    for kt in range(8):
                        A(lambda e, kt=kt: e.activation(out=xmT[:, kt, :], in_=pT[:, kt * 128:(kt + 1) * 128],
                                                        func=AF.Identity, scale=modfm[:, 1, kt, col:col + 1],
                                                        bias=modfm[:, 0, kt, col:col + 1]),
                          ["pT", "modfm"], [xmk])

                def stage_b(t):
                    i2 = t % 2
                    lat = t >= NTC
                    xmT, xmk = xmTA[i2], "xmT%d" % i2
                    for ct_ in range(4):
                        for kt in range(8):
                            T(lambda e, ct_=ct_, kt=kt: e.matmul(pS[:, ct_, :], lhsT=wS[:, kt, ct_ * 128:(ct_ + 1) * 128],
                                                                 rhs=xmT[:, kt, :], start=(kt == 0), stop=(kt == 7)),
                              ["wS", xmk], ["pS"])
                    V(lambda e: e.tensor_copy(out=U[:, :, t * 128:(t + 1) * 128], in_=pS[:]), ["pS"], ["U"])
                    if not lat:
                        return
                    tl = t - NTC
                    uT, uk = uTA[i2], "uT%d" % i2
                    vv, vk = vvA[i2], "vv%d" % i2
                    vc, vck = vcA[i2], "vc%d" % i2
                    vln, vlk = vlnA[i2], "vln%d" % i2
                    st6, s6k = st6A[i2], "st6%d" % i2
                    mv6, m6k = mv6A[i2], "mv6%d" % i2
                    rs6, r6k = rs6A[i2], "rs6%d" % i2
                    gmt, gtk = gmtA[i2], "gmt%d" % i2
                    for ct_ in range(6):
                        for kt in range(8):
                            T(lambda e, ct_=ct_, kt=kt: e.matmul(pU[:, ct_, :], lhsT=wU[:, kt, ct_ * 128:(ct_ + 1) * 128],
                                                                 rhs=xmT[:, kt, :], start=(kt == 0), stop=(kt == 7)),
                              ["wU", xmk], ["pU"])
                    A(lambda e: e.activation(out=uT[:], in_=pU[:, 0:6, :], func=AF.Gelu_apprx_tanh), ["pU"], [uk])
                    for (c0, c1) in ((0, 512), (512, 768)):
                        for kt in range(8):
                            T(lambda e, c0=c0, c1=c1, kt=kt: e.matmul(pV[:, c0:c1], lhsT=xmT[:, kt, :], rhs=wV[:, kt, c0:c1],
                                                                      start=(kt == 0), stop=(kt == 7)),
                              ["wV", xmk], ["pV"])
                    A(lambda e: e.activation(out=vv[:].rearrange("p a b -> p (a b)"), in_=pV[:, 0:768], func=AF.Gelu_apprx_tanh),
                      ["pV"], [vk])
                    for g in range(6):
                        V(lambda e, g=g: e.bn_stats(out=st6[:, g, :], in_=vv[:, g, :]), [vk], [s6k])
                    for g in range(6):
                        V(lambda e, g=g: e.bn_aggr(out=mv6[:, g, :], in_=st6[:, g, :]), [s6k], [m6k])
                    V(lambda e: e.tensor_scalar_add(out=rs6[:], in0=mv6[:, :, 1], scalar1=EPS), [m6k], [r6k])
                    A(lambda e: e.activation(out=rs6[:], in_=rs6[:], func=AF.Sqrt), [r6k], [r6k])
                    V(lambda e: e.reciprocal(out=rs6[:], in_=rs6[:]), [r6k], [r6k])
                    V(lambda e: e.tensor_tensor(out=vc[:], in0=vv[:], in1=bc(mv6[:, :, 0:1], [128, 6, 128]), op=ALU.subtract),
                      [vk, m6k], [vck])
                    V(lambda e: e.tensor_tensor(out=vln[:], in0=vc[:], in1=bc(rs6[:].unsqueeze(2), [128, 6, 128]), op=ALU.mult),
                      [vck, r6k], [vlk])
                    for g in range(6):
                        T(lambda e, g=g: e.matmul(pM[:, g, :], lhsT=vln[:, g, :], rhs=wsT[:, g, :], start=True, stop=True),
                          [vlk, "wsT"], ["pM"])
                    G(lambda e: e.tensor_tensor(out=gmt[:], in0=bsrow[:].rearrange("p (a b) -> p a b", a=6), in1=bsrow[:].rearrange("p (a b) -> p a b", a=6),
                                                op=ALU.bypass), ["bsrow"], [gtk]) if False else None
                    V(lambda e: e.tensor_tensor(out=gmt[:], in0=pM[:, 0:6, :], in1=bsrow[:].rearrange("p (a b) -> p a b", a=6),
                                                op=ALU.add), ["pM", "bsrow"], [gtk])
                    gb = gmb[tl % 2]
                    gk = "gmb%d" % (tl % 2)
                    G(lambda e: e.tensor_tensor(out=gb[:], in0=gmt[:], in1=uT[:], op=ALU.mult), [gtk, uk], [gk])
                    DS(lambda e: e.dma_start(out=catT_d[4:10, :, tl * 128:(tl + 1) * 128].rearrange("a p t -> p a t"),
                                             in_=gb[:]), [gk], ["catT_gm"])

                stage_a(0)
                for t in range(NTC + NT):
                    la = record(stage_a, t + 1) if t + 1 < NTC + NT else []
                    lb = record(stage_b, t)
                    emit_zip(la, lb)
                P.barrier()
            wst_.close()

            with contextlib.ExitStack() as st:
                WB = sb(st, "WB", [128, 2, 2048], BF16)
                WC = sb(st, "WC", [128, 2, 2048], BF16)
                rho_pp = sb(st, "rho_pp", [128, 16])
                f_pp = sb(st, "f_pp", [128, 16])
                dsc = sb(st, "dsc", [128, 4])
                bglu = sb(st, "bglu", [128, 4])
                wglu = sb(st, "wglu", [128, 4, 512], BF16)
                for dr_ in range(4):
                  csl = slice(dr_ * 512, (dr_ + 1) * 512)
                  with contextlib.ExitStack() as s2:
                        r = {n: sb(s2, "r_" + n, [128, 512]) for n in
                             ("are", "aim", "stp", "rho", "f", "y", "y2", "sn", "cs", "x", "yv", "den", "cr", "ci", "t1", "t2", "bre", "bim")}
                        ri_ = sb(s2, "r_int", [128, 512], I32)
                        DS(lambda e: e.dma_start(out=r["are"][:], in_=bc(are_row[:, csl], [128, 512])), w=["are"])
                        DS(lambda e: e.dma_start(out=r["aim"][:], in_=bc(aim_row[:, csl], [128, 512])), w=["aim"])
                        DS(lambda e: e.dma_start(out=r["stp"][:], in_=bc(ls_row[:, csl], [128, 512])), w=["stp"])
                        DS(lambda e: e.dma_start(out=r["bre"][:], in_=WBre_raw[:, csl]), w=["bre"])
                        DS(lambda e: e.dma_start(out=r["bim"][:], in_=WBim_raw[:, csl]), w=["bim"])

                        def vt(o, a, b, op):
                            V(lambda e: e.tensor_tensor(out=r[o][:], in0=r[a][:], in1=r[b][:], op=op), [a, b], [o])

                        def frac(o, i):
                            V(lambda e: e.tensor_copy(out=ri_[:], in_=r[i][:]), [i], ["rint"])
                            V(lambda e: e.tensor_copy(out=r["t1"][:], in_=ri_[:]), ["rint"], ["t1"])
                            vt(o, i, "t1", ALU.subtract)

                        A(lambda e: e.activation(out=r["stp"][:], in_=r["stp"][:], func=AF.Exp), ["stp"], ["stp"])
                        vt("rho", "are", "stp", ALU.mult)
                        A(lambda e: e.activation(out=r["rho"][:], in_=r["rho"][:], func=AF.Exp), ["rho"], ["rho"])
                        vt("f", "aim", "stp", ALU.mult)
                        V(lambda e: e.tensor_scalar_mul(out=r["f"][:], in0=r["f"][:], scalar1=1.0 / (2 * math.pi)), ["f"], ["f"])
                        frac("y", "f")
                        A(lambda e: e.activation(out=r["sn"][:], in_=r["y"][:], func=AF.Sin, scale=TWO_PI), ["y"], ["sn"])
                        V(lambda e: e.tensor_scalar_add(out=r["y2"][:], in0=r["y"][:], scalar1=0.25), ["y"], ["y2"])
                        frac("y2", "y2")
                        A(lambda e: e.activation(out=r["cs"][:], in_=r["y2"][:], func=AF.Sin, scale=TWO_PI), ["y2"], ["cs"])
                        vt("x", "rho", "cs", ALU.mult)
                        V(lambda e: e.tensor_scalar_add(out=r["x"][:], in0=r["x"][:], scalar1=-1.0), ["x"], ["x"])
                        vt("yv", "rho", "sn", ALU.mult)
                        vt("den", "are", "are", ALU.mult)
                        vt("t2", "aim", "aim", ALU.mult)
                        vt("den", "den", "t2", ALU.add)
                        V(lambda e: e.reciprocal(out=r["den"][:], in_=r["den"][:]), ["den"], ["den"])
                        vt("cr", "x", "are", ALU.mult)
                        vt("t2", "yv", "aim", ALU.mult)
                        vt("cr", "cr", "t2", ALU.add)
                        vt("cr", "cr", "den", ALU.mult)
                        vt("ci", "yv", "are", ALU.mult)
                        vt("t2", "x", "aim", ALU.mult)
                        vt("ci", "ci", "t2", ALU.subtract)
                        vt("ci", "ci", "den", ALU.mult)
                        vt("t1", "cr", "bre", ALU.mult)
                        vt("t2", "ci", "bim", ALU.mult)
                        V(lambda e: e.tensor_tensor(out=WB[:, 0, csl], in0=r["t1"][:], in1=r["t2"][:], op=ALU.subtract), ["t1", "t2"], ["WB"])
                        vt("t1", "cr", "bim", ALU.mult)
                        vt("t2", "ci", "bre", ALU.mult)
                        V(lambda e: e.tensor_tensor(out=WB[:, 1, csl], in0=r["t1"][:], in1=r["t2"][:], op=ALU.add), ["t1", "t2"], ["WB"])
                        DS(lambda e: e.dma_start(out=r["bre"][:], in_=WCre_raw[:, csl]), w=["bre"])
                        DS(lambda e: e.dma_start(out=r["bim"][:], in_=WCim_raw[:, csl]), w=["bim"])
                        V(lambda e: e.tensor_copy(out=WC[:, 0, csl], in_=r["bre"][:]), ["bre"], ["WC"])
                        V(lambda e: e.tensor_scalar_mul(out=WC[:, 1, csl], in0=r["bim"][:], scalar1=-1.0), ["bim"], ["WC"])

                        P.barrier()
                with contextlib.ExitStack() as s2:
                    pa = sb(s2, "pa", [128, 16])
                    pb = sb(s2, "pb", [128, 16])
                    pc = sb(s2, "pc", [128, 16])
                    DS(lambda e: e.dma_start(out=pa[:], in_=are_pp), w=["pa"])
                    DS(lambda e: e.dma_start(out=pb[:], in_=aim_pp), w=["pb"])
                    DS(lambda e: e.dma_start(out=pc[:], in_=ls_pp), w=["pc"])
                    A(lambda e: e.activation(out=pc[:], in_=pc[:], func=AF.Exp), ["pc"], ["pc"])
                    V(lambda e: e.tensor_tensor(out=rho_pp[:], in0=pa[:], in1=pc[:], op=ALU.mult), ["pa", "pc"], ["rho_pp"])
                    A(lambda e: e.activation(out=rho_pp[:], in_=rho_pp[:], func=AF.Exp), ["rho_pp"], ["rho_pp"])
                    V(lambda e: e.tensor_tensor(out=f_pp[:], in0=pb[:], in1=pc[:], op=ALU.mult), ["pb", "pc"], ["f_pp"])
                    V(lambda e: e.tensor_scalar_mul(out=f_pp[:], in0=f_pp[:], scalar1=1.0 / (2 * math.pi)), ["f_pp"], ["f_pp"])
                    DS(lambda e: e.dma_start(out=dsc[:], in_=dpp), w=["dsc"])
                    DS(lambda e: e.dma_start(out=bglu[:], in_=b_glu_pp), w=["bglu"])
                    gst_ = sb(s2, "gst_", [128, 4, 512])
                    DS(lambda e: e.dma_start(out=gst_[:], in_=w_glu_pad.rearrange("(kt p) f -> p kt f", p=128)), w=["gst_"])
                    V(lambda e: e.tensor_copy(out=wglu[:], in_=gst_[:]), ["gst_"], ["wglu"])
                    P.barrier()

                L = SEG
                H = 512
                arg1 = sb(st, "arg", [128, L])
                arg = [arg1, arg1]
                rnd = sb(st, "rnd", [128, L])
                argi = [rnd, rnd]
                magic = sb(st, "magic", [128, 2])
                V(lambda e: e.memset(magic[:, 0:1], 12582912.0), w=["magic"])
                V(lambda e: e.memset(magic[:, 1:2], -12582912.0), w=["magic"])
                yv1 = sb(st, "yv", [128, L])
                yv = [yv1, yv1]
                s2 = arg
                sq = arg
                cs = [sb(st, "cs%d" % i, [128, L], BF16) for i in range(2)]
                sn = [sb(st, "sn%d" % i, [128, L], BF16) for i in range(2)]
                bre = [sb(st, "bre%d" % i, [128, L], BF16) for i in range(2)]
                bim = [sb(st, "bim%d" % i, [128, L], BF16) for i in range(2)]
                dre = sb(st, "dre", [128, L], BF16)
                dim_ = sb(st, "dim", [128, L], BF16)
                tA = sb(st, "tA", [128, L], BF16)
                tB = sb(st, "tB", [128, L], BF16)
                zre = sb(st, "zre", [128, L], BF16)
                zim = sb(st, "zim", [128, L], BF16)
                Sre1 = sb(st, "Sre", [128, L], BF16)
                Sim1 = sb(st, "Sim", [128, L], BF16)
                Sre = [Sre1, Sre1]
                Sim = [Sim1, Sim1]
                zst = sb(st, "zst", [128, 16, 2])
                ysb1 = sb(st, "ysb", [128, L])
                ysb = [ysb1, ysb1]
                yfl1 = sb(st, "yfl", [128, L])
                yfl = [yfl1, yfl1]
                gg = sb(st, "gg", [128, 4, L], BF16)
                sg1 = sb(st, "sg", [128, L])
                sg = [sg1, sg1]
                s5o1 = sb(st, "s5o", [128, L], BF16)
                s5o = [s5o1, s5o1]
                pB = [[ps(st, "pB%d%d" % (h_, ri), [128, H]) for ri in range(2)] for h_ in range(2)]
                pY = ps(st, "pY", [128, L])
                pG = ps(st, "pG", [128, L])
                V(lambda e: e.memset(zst[:], 0.0), w=["zst"])
                colctr = [0]

                plan = []
                for gp in range(8):
                    plan.append((0, gp, CTX, 0))
                for sgi in range(SEQ // L):
                    for kt in range(4):
                        for g2 in range(2):
                            plan.append((0, kt * 2 + g2, L, CTX + sgi * L))
                for gp in range(8):
                    plan.append((1, gp, CTX, 0))
                for sb_i in range(SEQ // L):
                    for kt in range(4):
                        for g2 in range(2):
                            plan.append((1, kt * 2 + g2, L, CTX + sb_i * L))

                def s5_tables(idx):
                    dr, gp, n, t0 = plan[idx]
                    ci_ = dr * 8 + gp
                    cb = idx % 2
                    arg_, argi_, yv_, s2_, sq_, cs_, sn_ = arg[cb], argi[cb], yv[cb], s2[cb], sq[cb], cs[cb], sn[cb]
                    k = lambda nm: nm if nm in ("arg", "yv", "Sre", "Sim") else "%s%d" % (nm, cb)
                    G(lambda e: e.tensor_scalar(out=arg_[:, 0:n], in0=iota_r[:, 0:n], scalar1=float(t0), scalar2=f_pp[:, ci_:ci_ + 1],
                                                op0=ALU.add, op1=ALU.mult), ["iota_r", "f_pp"], [k("arg")])
                    A(lambda e: e.activation(out=rnd[:, 0:n], in_=arg_[:, 0:n], func=AF.Identity, bias=magic[:, 0:1]), [k("arg"), "magic"], ["rnd"])
                    A(lambda e: e.activation(out=rnd[:, 0:n], in_=rnd[:, 0:n], func=AF.Identity, bias=magic[:, 1:2]), ["rnd", "magic"], ["rnd"])
                    G(lambda e: e.tensor_tensor(out=yv_[:, 0:n], in0=arg_[:, 0:n], in1=rnd[:, 0:n], op=ALU.subtract),
                      [k("arg"), "rnd"], [k("yv")])
                    A(lambda e: e.activation(out=sn_[:, 0:n], in_=yv_[:, 0:n], func=AF.Sin, scale=TWO_PI), [k("yv")], [k("sn")])
                    A(lambda e: e.activation(out=s2_[:, 0:n], in_=yv_[:, 0:n], func=AF.Sin, scale=TWO_PI / 2), [k("yv")], [k("arg")])
                    A(lambda e: e.activation(out=sq_[:, 0:n], in_=s2_[:, 0:n], func=AF.Square), [k("arg")], [k("arg")])
                    G(lambda e: e.tensor_scalar(out=cs_[:, 0:n], in0=sq_[:, 0:n], scalar1=-2.0, scalar2=1.0, op0=ALU.mult, op1=ALU.add),
                      [k("arg")], [k("cs")])

                def s5_col(dr, gp, u_lo, n, t0, rev, readout):
                    idx = colctr[0]
                    colctr[0] += 1
                    assert plan[idx] == (dr, gp, n, t0), (plan[idx], dr, gp, n, t0)
                    if idx == 0:
                        s5_tables(0)
                    ci_ = dr * 8 + gp
                    kt = gp // 2
                    r0 = 64 * (gp % 2)
                    wcol = slice(ci_ * 128, (ci_ + 1) * 128)
                    cb = idx % 2
                    cs_, sn_ = cs[cb], sn[cb]
                    bre_, bim_, Sre_, Sim_ = bre[cb], bim[cb], Sre[cb], Sim[cb]
                    k = lambda nm: nm if nm in ("arg", "yv", "Sre", "Sim") else "%s%d" % (nm, cb)
                    for hi_, c0 in enumerate(range(0, n, H)):
                        c1 = min(n, c0 + H)
                        for ri, dst, dk in ((0, bre_, k("bre")), (1, bim_, k("bim"))):
                            pb_ = pB[hi_][ri]
                            pk = "pB%d%d" % (hi_, ri)
                            T(lambda e, ri=ri, pb_=pb_, c0=c0, c1=c1: e.matmul(pb_[:, 0:c1 - c0], lhsT=WB[r0:r0 + 64, ri, wcol],
                                                                             rhs=U[r0:r0 + 64, kt, u_lo + c0:u_lo + c1],
                                                                             start=True, stop=True), ["WB", "U"], [pk])
                            A(lambda e, pb_=pb_, dst=dst, c0=c0, c1=c1: e.activation(out=dst[:, c0:c1], in_=pb_[:, 0:c1 - c0], func=AF.Identity),
                              [pk], [dk])

                    if idx + 1 < len(plan):
                        s5_tables(idx + 1)

                    def tvn(t_):
                        if not rev:
                            return t_[:, 0:n]
                        return t_[:, 0:n][:, ::-1]

                    def vtt(o, ok, a_, ak, b_, bk, op):
                        V(lambda e: e.tensor_tensor(out=o, in0=a_, in1=b_, op=op), [ak, bk], [ok])

                    vtt(tA[:, 0:n], "tA", bre_[:, 0:n], k("bre"), tvn(cs_), k("cs"), ALU.mult)
                    vtt(tB[:, 0:n], "tB", bim_[:, 0:n], k("bim"), tvn(sn_), k("sn"), ALU.mult)
                    vtt(dre[:, 0:n], "dre", tA[:, 0:n], "tA", tB[:, 0:n], "tB", ALU.add)
                    vtt(tA[:, 0:n], "tA", bim_[:, 0:n], k("bim"), tvn(cs_), k("cs"), ALU.mult)
                    vtt(tB[:, 0:n], "tB", bre_[:, 0:n], k("bre"), tvn(sn_), k("sn"), ALU.mult)
                    vtt(dim_[:, 0:n], "dim", tA[:, 0:n], "tA", tB[:, 0:n], "tB", ALU.subtract)
                    for (src, dst, ri) in ((dre, zre, 0), (dim_, zim, 1)):
                        V(lambda e, src=src, dst=dst, ri=ri: e.tensor_tensor_scan(
                            out=tvn(dst), data0=bc(rho_pp[:, ci_:ci_ + 1], [128, n]), data1=tvn(src),
                            initial=zst[:, ci_, ri:ri + 1], op0=ALU.mult, op1=ALU.add),
                          ["dre" if ri == 0 else "dim", "rho_pp", "zst"], ["zre" if ri == 0 else "zim"])
                    last = 0 if rev else n - 1
                    V(lambda e: e.tensor_copy(out=zst[:, ci_, 0:1], in_=zre[:, last:last + 1]), ["zre"], ["zst"])
                    V(lambda e: e.tensor_copy(out=zst[:, ci_, 1:2], in_=zim[:, last:last + 1]), ["zim"], ["zst"])
                    if not readout:
                        return
                    vtt(tA[:, 0:n], "tA", zre[:, 0:n], "zre", tvn(cs_), k("cs"), ALU.mult)
                    vtt(tB[:, 0:n], "tB", zim[:, 0:n], "zim", tvn(sn_), k("sn"), ALU.mult)
                    vtt(Sre_[:, 0:n], k("Sre"), tA[:, 0:n], "tA", tB[:, 0:n], "tB", ALU.subtract)
                    vtt(tA[:, 0:n], "tA", zre[:, 0:n], "zre", tvn(sn_), k("sn"), ALU.mult)
                    vtt(tB[:, 0:n], "tB", zim[:, 0:n], "zim", tvn(cs_), k("cs"), ALU.mult)
                    vtt(Sim_[:, 0:n], k("Sim"), tA[:, 0:n], "tA", tB[:, 0:n], "tB", ALU.add)
                    first = (gp % 2 == 0)
                    for c0 in range(0, n, 512):
                        for ri, S_, sk in ((0, Sre_, k("Sre")), (1, Sim_, k("Sim"))):
                            T(lambda e, ri=ri, S_=S_, c0=c0: e.matmul(pY[:, c0:c0 + 512], lhsT=WC[:, ri, wcol], rhs=S_[:, c0:c0 + 512],
                                                                     start=(first and ri == 0), stop=((not first) and ri == 1)),
                              ["WC", sk], ["pY"])

                for gp in range(8):
                    s5_col(0, gp, 0, CTX, 0, False, False)
                cnt_ = 0
                for sgi in range(SEQ // L):
                    for kt in range(4):
                        for g2 in range(2):
                            s5_col(0, kt * 2 + g2, CTX + sgi * L, L, CTX + sgi * L, False, True)
                        yb = ysb[cnt_ % 2]
                        yk = "ysb"
                        cnt_ += 1
                        A(lambda e, yb=yb: e.activation(out=yb[:], in_=pY[:], func=AF.Identity), ["pY"], [yk])
                        DS(lambda e, kt=kt, sgi=sgi, yb=yb: e.dma_start(out=yf_d[kt, :, sgi * L:(sgi + 1) * L], in_=yb[:]), [yk], ["yf"])
                for gp in range(8):
                    s5_col(1, gp, 0, CTX, 0, True, False)
                for sb_i in range(SEQ // L):
                    sgi = SEQ // L - 1 - sb_i
                    for kt in range(4):
                        yl = yfl[cnt_ % 2]
                        ylk = "yfl"
                        yb = ysb[cnt_ % 2]
                        yk = "ysb"
                        cnt_ += 1
                        DS(lambda e, kt=kt, sgi=sgi, yl=yl: e.dma_start(out=yl[:], in_=yf_d[kt, :, sgi * L:(sgi + 1) * L]), ["yf"], [ylk])
                        for g2 in range(2):
                            s5_col(1, kt * 2 + g2, CTX + sgi * L, L, CTX + sb_i * L, True, True)
                        V(lambda e, yb=yb, yl=yl: e.tensor_tensor(out=yb[:], in0=pY[:], in1=yl[:], op=ALU.add), ["pY", ylk], [yk])
                        V(lambda e, kt=kt, sgi=sgi, yb=yb: e.scalar_tensor_tensor(out=yb[:], in0=U[:, kt, CTX + sgi * L:CTX + (sgi + 1) * L],
                                                                                  scalar=dsc[:, kt:kt + 1], in1=yb[:],
                                                                                  op0=ALU.mult, op1=ALU.add), ["U", "dsc", yk], [yk])
                        A(lambda e, kt=kt, yb=yb: e.activation(out=gg[:, kt, :], in_=yb[:], func=AF.Gelu_apprx_tanh), [yk], ["gg"])
                    for mt in range(4):
                        sg_ = sg[mt % 2]
                        sgk = "sg"
                        so_ = s5o[mt % 2]
                        sok = "s5o"
                        for c0 in range(0, L, 512):
                            for kt in range(4):
                                T(lambda e, mt=mt, c0=c0, kt=kt: e.matmul(pG[:, c0:c0 + 512], lhsT=wglu[:, kt, mt * 128:(mt + 1) * 128],
                                                                         rhs=gg[:, kt, c0:c0 + 512], start=(kt == 0), stop=(kt == 3)),
                                  ["wglu", "gg"], ["pG"])
                        A(lambda e, mt=mt, sg_=sg_: e.activation(out=sg_[:], in_=pG[:], func=AF.Sigmoid, bias=bglu[:, mt:mt + 1]), ["pG", "bglu"], [sgk])
                        G(lambda e, mt=mt, sg_=sg_, so_=so_: e.tensor_tensor(out=so_[:], in0=sg_[:], in1=gg[:, mt, :], op=ALU.mult), [sgk, "gg"], [sok])
                        DS(lambda e, mt=mt, sgi=sgi, so_=so_: e.dma_start(out=catT_d[mt, :, sgi * L:(sgi + 1) * L], in_=so_[:]), [sok], ["catT_s5"])
                P.barrier()
        P.barrier()

        with contextlib.ExitStack() as st:
            wo = sb(st, "wo", [128, 10, D], BF16)
            wr = sb(st, "wr", [128, 8, 16], BF16)
            with contextlib.ExitStack() as s2:
                stg = sb(s2, "stg3", [128, 10, D])
                DS(lambda e: e.dma_start(out=stg[:], in_=w_out_pad.rearrange("(kt p) f -> p kt f", p=128)), w=["stg3"])
                V(lambda e: e.tensor_copy(out=wo[:], in_=stg[:]), ["stg3"], ["wo"])
                DS(lambda e: e.dma_start(out=stg[:, 0:8, 0:16], in_=w_router.rearrange("(kt p) f -> p kt f", p=128)), w=["stg3"])
                V(lambda e: e.tensor_copy(out=wr[:], in_=stg[:, 0:8, 0:16]), ["stg3"], ["wr"])
                P.barrier()
            cat = [sb(st, "cat%d" % i, [128, 10, 128], BF16) for i in range(2)]
            xt3 = [sb(st, "x3_%d" % i, [128, D]) for i in range(2)]
            tmA = [sb(st, "tm%d" % i, [128, D]) for i in range(2)]
            xrA = [sb(st, "xr%d" % i, [128, D]) for i in range(2)]
            xnwA = [sb(st, "xnw%d" % i, [128, D]) for i in range(2)]
            hhA = [sb(st, "hh%d" % i, [128, D]) for i in range(2)]
            hbA = [sb(st, "hb%d" % i, [128, D], BF16) for i in range(2)]
            hTA = [sb(st, "hT%d" % i, [128, 8, 128], BF16) for i in range(2)]
            statsA = [sb(st, "stats3_%d" % i, [128, 2, 6]) for i in range(4)]
            mvA = [sb(st, "mv3_%d" % i, [128, 2]) for i in range(4)]
            rstdA = [sb(st, "rstd3_%d" % i, [128, 1]) for i in range(4)]
            lgA = [sb(st, "lg%d" % i, [128, 16]) for i in range(2)]
            mxA = [sb(st, "mx%d" % i, [128, 1]) for i in range(2)]
            smA = [sb(st, "sm%d" % i, [128, 1]) for i in range(2)]
            pMxA = [ps(st, "pMx%d" % i, [128, D]) for i in range(2)]
            pT3A = [ps(st, "pT3%d" % i, [128, D], BF16) for i in range(2)]
            pLA = [ps(st, "pL%d" % i, [128, 16]) for i in range(2)]

            def lnorm(src, sk, dst, dk, si):
                stats, mv, rstd = statsA[si], mvA[si], rstdA[si]
                k1, k2, k3 = "stats3_%d" % si, "mv3_%d" % si, "rstd3_%d" % si
                for c in range(2):
                    V(lambda e, c=c: e.bn_stats(out=stats[:, c, :], in_=src[:, c * 512:(c + 1) * 512]), [sk], [k1])
                V(lambda e: e.bn_aggr(out=mv[:], in_=stats[:].rearrange("p a b -> p (a b)")), [k1], [k2])
                V(lambda e: e.tensor_scalar_add(out=rstd[:], in0=mv[:, 1:2], scalar1=EPS), [k2], [k3])
                A(lambda e: e.activation(out=rstd[:], in_=rstd[:], func=AF.Sqrt), [k3], [k3])
                V(lambda e: e.reciprocal(out=rstd[:], in_=rstd[:]), [k3], [k3])
                V(lambda e: e.tensor_scalar(out=dst[:], in0=src[:], scalar1=mv[:, 0:1], scalar2=rstd[:, 0:1],
                                            op0=ALU.subtract, op1=ALU.mult), [sk, k2, k3], [dk])

            def p3_loads(t):
                i2 = t % 2
                c_, ck = cat[i2], "cat%d" % i2
                x_, xk = xt3[i2], "x3_%d" % i2
                DS(lambda e: e.dma_start(out=c_[:], in_=catT_d[:, :, t * 128:(t + 1) * 128].rearrange("a p t -> p a t")), w=[ck])
                DS(lambda e: e.dma_start(out=x_[:], in_=xs[CTX + t * 128:CTX + (t + 1) * 128, :]), w=[xk])

            def p3_A(t):
                i2 = t % 2
                c_, ck = cat[i2], "cat%d" % i2
                x_, xk = xt3[i2], "x3_%d" % i2
                tm, tmk = tmA[i2], "tm%d" % i2
                xr, xrk = xrA[i2], "xr%d" % i2
                xnw, xnk = xnwA[i2], "xnw%d" % i2
                hh, hhk = hhA[i2], "hh%d" % i2
                hb, hbk = hbA[i2], "hb%d" % i2
                hT, hTk = hTA[i2], "hT%d" % i2
                lg, lgk = lgA[i2], "lg%d" % i2
                mx, mxk = mxA[i2], "mx%d" % i2
                sm, smk = smA[i2], "sm%d" % i2
                pMx, pMk = pMxA[i2], "pMx%d" % i2
                pT3, pTk = pT3A[i2], "pT3%d" % i2
                pL, pLk = pLA[i2], "pL%d" % i2
                for nh in range(2):
                    for kt in range(10):
                        T(lambda e, nh=nh, kt=kt, c_=c_, pMx=pMx: e.matmul(pMx[:, nh * 512:(nh + 1) * 512], lhsT=c_[:, kt, :],
                                                                           rhs=wo[:, kt, nh * 512:(nh + 1) * 512], start=(kt == 0), stop=(kt == 9)),
                          [ck, "wo"], [pMk])

            def p3_B1(t):
                i2 = t % 2
                c_, ck = cat[i2], "cat%d" % i2
                x_, xk = xt3[i2], "x3_%d" % i2
                tm, tmk = tmA[i2], "tm%d" % i2
                xr, xrk = xrA[i2], "xr%d" % i2
                xnw, xnk = xnwA[i2], "xnw%d" % i2
                hh, hhk = hhA[i2], "hh%d" % i2
                hb, hbk = hbA[i2], "hb%d" % i2
                hT, hTk = hTA[i2], "hT%d" % i2
                lg, lgk = lgA[i2], "lg%d" % i2
                mx, mxk = mxA[i2], "mx%d" % i2
                sm, smk = smA[i2], "sm%d" % i2
                pMx, pMk = pMxA[i2], "pMx%d" % i2
                pT3, pTk = pT3A[i2], "pT3%d" % i2
                pL, pLk = pLA[i2], "pL%d" % i2
                V(lambda e, tm=tm, pMx=pMx: e.tensor_tensor(out=tm[:], in0=pMx[:], in1=modrow[:, 0, :], op=ALU.mult), [pMk, "modrow"], [tmk])
                V(lambda e, x_=x_, xr=xr, tm=tm: e.scalar_tensor_tensor(out=xr[:], in0=x_[:], scalar=ALPHA, in1=tm[:], op0=ALU.mult, op1=ALU.add),
                  [xk, tmk], [xrk])
                lnorm(xr, xrk, tm, tmk, 2 * i2)
                G(lambda e, tm=tm: e.tensor_tensor(out=tm[:], in0=tm[:], in1=lnrow[:, 0, :], op=ALU.mult), [tmk, "lnrow"], [tmk])
                G(lambda e, tm=tm, xnw=xnw: e.tensor_tensor(out=xnw[:], in0=tm[:], in1=lnrow[:, 1, :], op=ALU.add), [tmk, "lnrow"], [xnk])
                DS(lambda e, t=t, xnw=xnw: e.dma_start(out=xnew_d[t * 128:(t + 1) * 128, :], in_=xnw[:]), [xnk], ["xnew_d"])

            def p3_B2(t):
                i2 = t % 2
                c_, ck = cat[i2], "cat%d" % i2
                x_, xk = xt3[i2], "x3_%d" % i2
                tm, tmk = tmA[i2], "tm%d" % i2
                xr, xrk = xrA[i2], "xr%d" % i2
                xnw, xnk = xnwA[i2], "xnw%d" % i2
                hh, hhk = hhA[i2], "hh%d" % i2
                hb, hbk = hbA[i2], "hb%d" % i2
                hT, hTk = hTA[i2], "hT%d" % i2
                lg, lgk = lgA[i2], "lg%d" % i2
                mx, mxk = mxA[i2], "mx%d" % i2
                sm, smk = smA[i2], "sm%d" % i2
                pMx, pMk = pMxA[i2], "pMx%d" % i2
                pT3, pTk = pT3A[i2], "pT3%d" % i2
                pL, pLk = pLA[i2], "pL%d" % i2
                lnorm(xnw, xnk, hh, hhk, 2 * i2 + 1)
                V(lambda e, hh=hh: e.tensor_tensor(out=hh[:], in0=hh[:], in1=modrow[:, 2, :], op=ALU.mult), [hhk, "modrow"], [hhk])
                V(lambda e, hh=hh, hb=hb: e.tensor_tensor(out=hb[:], in0=hh[:], in1=modrow[:, 1, :], op=ALU.add), [hhk, "modrow"], [hbk])
                DS(lambda e, t=t, hb=hb: e.dma_start(out=hrow_d[t * 128:(t + 1) * 128, :], in_=hb[:]), [hbk], ["hrow_d"])
                for kt in range(8):
                    T(lambda e, kt=kt, hb=hb, pT3=pT3: e.transpose(out=pT3[:, kt * 128:(kt + 1) * 128], in_=hb[:, kt * 128:(kt + 1) * 128],
                                                                   identity=ident_bf[:]), [hbk, "ident_bf"], [pTk])
                A(lambda e, hT=hT, pT3=pT3: e.activation(out=hT[:].rearrange("p a b -> p (a b)"), in_=pT3[:], func=AF.Identity), [pTk], [hTk])
                for kt in range(8):
                    T(lambda e, kt=kt, hT=hT, pL=pL: e.matmul(pL[:], lhsT=hT[:, kt, :], rhs=wr[:, kt, :], start=(kt == 0), stop=(kt == 7)),
                      [hTk, "wr"], [pLk])
                V(lambda e, mx=mx, pL=pL: e.tensor_reduce(out=mx[:], in_=pL[:], axis=AX.X, op=ALU.max), [pLk], [mxk])
                V(lambda e, mx=mx: e.tensor_scalar_mul(out=mx[:], in0=mx[:], scalar1=-1.0), [mxk], [mxk])
                A(lambda e, lg=lg, pL=pL, mx=mx: e.activation(out=lg[:], in_=pL[:], func=AF.Exp, bias=mx[:, 0:1]), [pLk, mxk], [lgk])
                V(lambda e, sm=sm, lg=lg: e.tensor_reduce(out=sm[:], in_=lg[:], axis=AX.X, op=ALU.add), [lgk], [smk])
                V(lambda e, sm=sm: e.reciprocal(out=sm[:], in_=sm[:]), [smk], [smk])
                V(lambda e, t=t, lg=lg, sm=sm: e.tensor_scalar_mul(out=aff[:, t, :], in0=lg[:], scalar1=sm[:, 0:1]), [lgk, smk], ["aff%d" % t])
                DS(lambda e, t=t: e.dma_start(out=aff_d[t * 128:(t + 1) * 128, :], in_=aff[:, t, :]), ["aff%d" % t], ["aff_d"])

            p3_loads(0)
            p3_loads(1)
            p3_A(0)
            emit_zip(record(p3_A, 1), record(p3_B1, 0))
            for t in range(NT):
                if t + 2 < NT:
                    p3_loads(t + 2)
                la = record(p3_A, t + 2) if t + 2 < NT else []
                lb1 = record(p3_B1, t + 1) if t + 1 < NT else []
                lb2 = record(p3_B2, t)
                emit_zip(la, lb1, lb2)
            P.barrier()

        if debug == 1:
            early.close()
            return nc

        with contextlib.ExitStack() as st:
            hs = sb(st, "hs", [128, 16])
            am = sb(st, "am", [128, NT, NE])
            lo = sb(st, "lo", [128, NE])
            hi = sb(st, "hi", [128, NE])
            mid = sb(st, "mid", [128, NE])
            cmp_ = sb(st, "cmp", [128, NT, NE])
            cnt = sb(st, "cnt", [128, NE])
            cntb = sb(st, "cntb", [128, NE], BF16)
            ge = sb(st, "ge", [128, NE])
            dl = sb(st, "dl", [128, NE])
            pC = ps(st, "pC", [128, NE])
            DS(lambda e: e.dma_start(out=hs[:], in_=half_sel), w=["hs"])
            V(lambda e: e.tensor_tensor(out=am[:], in0=aff[:, :, 0:8], in1=bc(hs[:, 0:8].unsqueeze(1), [128, NT, NE]), op=ALU.mult),
              ["aff", "hs"], ["am"])
            V(lambda e: e.tensor_tensor(out=cmp_[:], in0=aff[:, :, 8:16], in1=bc(hs[:, 8:16].unsqueeze(1), [128, NT, NE]), op=ALU.mult),
              ["aff", "hs"], ["cmp"])
            V(lambda e: e.tensor_tensor(out=am[:], in0=am[:], in1=cmp_[:], op=ALU.add), ["am", "cmp"], ["am"])
            V(lambda e: e.memset(lo[:], 0.0), w=["lo"])
            V(lambda e: e.memset(hi[:], 1.0), w=["hi"])
            for it in range(30):
                V(lambda e: e.tensor_tensor(out=mid[:], in0=lo[:], in1=hi[:], op=ALU.add), ["lo", "hi"], ["mid"])
                V(lambda e: e.tensor_scalar_mul(out=mid[:], in0=mid[:], scalar1=0.5), ["mid"], ["mid"])
                V(lambda e: e.tensor_tensor(out=cmp_[:], in0=am[:], in1=bc(mid[:].unsqueeze(1), [128, NT, NE]), op=ALU.is_ge),
                  ["am", "mid"], ["cmp"])
                V(lambda e: e.tensor_reduce(out=cnt[:], in_=cmp_[:].rearrange("p t e -> p e t"), axis=AX.X, op=ALU.add), ["cmp"], ["cnt"])
                V(lambda e: e.tensor_copy(out=cntb[:], in_=cnt[:]), ["cnt"], ["cntb"])
                T(lambda e: e.matmul(pC[:], lhsT=ones_bf[:], rhs=cntb[:], start=True, stop=True), ["ones", "cntb"], ["pC"])
                V(lambda e: e.tensor_single_scalar(out=ge[:], in_=pC[:], scalar=float(CAP), op=ALU.is_ge), ["pC"], ["ge"])
                V(lambda e: e.tensor_tensor(out=dl[:], in0=mid[:], in1=lo[:], op=ALU.subtract), ["mid", "lo"], ["dl"])
                V(lambda e: e.tensor_tensor(out=dl[:], in0=dl[:], in1=ge[:], op=ALU.mult), ["dl", "ge"], ["dl"])
                V(lambda e: e.tensor_tensor(out=lo[:], in0=lo[:], in1=dl[:], op=ALU.add), ["lo", "dl"], ["lo"])
                V(lambda e: e.tensor_tensor(out=dl[:], in0=hi[:], in1=mid[:], op=ALU.subtract), ["hi", "mid"], ["dl"])
                V(lambda e: e.tensor_tensor(out=dl[:], in0=dl[:], in1=ge[:], op=ALU.mult), ["dl", "ge"], ["dl"])
                V(lambda e: e.tensor_tensor(out=hi[:], in0=mid[:], in1=dl[:], op=ALU.add), ["mid", "dl"], ["hi"])
            mk = sb(st, "mk", [128, NE, NT], BF16)
            cin = sb(st, "cin", [128, NE * NT])
            tot = sb(st, "tot", [128, NE, NT])
            cend = sb(st, "cend", [128, NE, NT])
            pP = ps(st, "pP", [128, NE * NT])
            pTt = ps(st, "pTt", [128, NE * NT])
            V(lambda e: e.tensor_tensor(out=mk[:], in0=am[:].rearrange("p t e -> p e t"), in1=bc(lo[:].unsqueeze(2), [128, NE, NT]),
                                        op=ALU.is_ge), ["am", "lo"], ["mk"])
            T(lambda e: e.matmul(pP[:], lhsT=tri_bf[:], rhs=mk[:].rearrange("p e t -> p (e t)"), start=True, stop=True), ["tri", "mk"], ["pP"])
            T(lambda e: e.matmul(pTt[:], lhsT=ones_bf[:], rhs=mk[:].rearrange("p e t -> p (e t)"), start=True, stop=True), ["ones", "mk"], ["pTt"])
            V(lambda e: e.tensor_copy(out=cin[:], in_=pP[:]), ["pP"], ["cin"])
            V(lambda e: e.tensor_copy(out=tot[:].rearrange("p e t -> p (e t)"), in_=pTt[:]), ["pTt"], ["tot"])
            V(lambda e: e.tensor_copy(out=cend[:], in_=tot[:]), ["tot"], ["cend"])
            sh = 1
            tmpc = sb(st, "tmpc", [128, NE, NT])
            while sh < NT:
                V(lambda e: e.tensor_copy(out=tmpc[:], in_=cend[:]), ["cend"], ["tmpc"])
                V(lambda e, sh=sh: e.tensor_tensor(out=cend[:, :, sh:NT], in0=tmpc[:, :, sh:NT], in1=tmpc[:, :, 0:NT - sh], op=ALU.add),
                  ["tmpc"], ["cend"])
                sh *= 2
            cinT = sb(st, "cinT", [128, 4, 128])
            pX = ps(st, "pX", [128, 4, 128])
            for a in range(4):
                T(lambda e, a=a: e.transpose(out=pX[:, a, :], in_=cin[:, a * 128:(a + 1) * 128], identity=ident_f[:]), ["cin", "ident_f"], ["pX"])
            V(lambda e: e.tensor_copy(out=cinT[:], in_=pX[:]), ["pX"], ["cinT"])
            DS(lambda e: e.dma_start(out=cin_d.rearrange("(a p) t -> p a t", p=128), in_=cinT[:]), ["cinT"], ["cin_d"])
            spp = sb(st, "spp", [128, 8])
            DS(lambda e: e.dma_start(out=spp[:], in_=slot_pp_d), w=["spp"])
            le = sb(st, "le", [128, NE, 8, NT])
            tl_ = sb(st, "tl_", [128, NE, 8])
            cst = sb(st, "cst", [128, NE, 8])
            rr = sb(st, "rr", [128, NE, 8])
            rowi = sb(st, "rowi", [128, NE, 8], I32)
            rowf = sb(st, "rowf", [128, NE, 8])
            for e_ in range(NE):
                V(lambda e, e_=e_: e.tensor_tensor(out=le[:, e_, :, :], in0=bc(cend[:, e_, :].unsqueeze(1), [128, 8, NT]),
                                                   in1=bc(spp[:].unsqueeze(2), [128, 8, NT]), op=ALU.is_le), ["cend", "spp"], ["le"])
            V(lambda e: e.tensor_reduce(out=tl_[:].rearrange("p e j -> p (e j)"), in_=le[:].rearrange("p e j t -> p (e j) t"),
                                        axis=AX.X, op=ALU.add), ["le"], ["tl_"])
            for e_ in range(NE):
                V(lambda e, e_=e_: e.tensor_tensor(out=le[:, e_, :, :], in0=le[:, e_, :, :], in1=bc(tot[:, e_, :].unsqueeze(1), [128, 8, NT]),
                                                   op=ALU.mult), ["le", "tot"], ["le"])
            V(lambda e: e.tensor_reduce(out=cst[:].rearrange("p e j -> p (e j)"), in_=le[:].rearrange("p e j t -> p (e j) t"),
                                        axis=AX.X, op=ALU.add), ["le"], ["cst"])
            V(lambda e: e.tensor_scalar_min(out=tl_[:], in0=tl_[:], scalar1=float(NT - 1)), ["tl_"], ["tl_"])
            V(lambda e: e.tensor_tensor(out=rr[:], in0=bc(spp[:].unsqueeze(1), [128, NE, 8]), in1=cst[:], op=ALU.subtract), ["spp", "cst"], ["rr"])
            for e_ in range(NE):
                V(lambda e, e_=e_: e.tensor_scalar_add(out=rowf[:, e_, :], in0=tl_[:, e_, :], scalar1=float(e_ * NT)), ["tl_"], ["rowf"])
            V(lambda e: e.tensor_copy(out=rowi[:], in_=rowf[:]), ["rowf"], ["rowi"])
            crow = sb(st, "crow", [128, 128])
            cle = sb(st, "cle", [128, 128])
            tloc = sb(st, "tloc", [128, NE, 8])
            arow = sb(st, "arow", [128, 16])
            idf = sb(st, "idf", [128, NE, 8])
            V(lambda e: e.memset(tloc[:], 0.0), w=["tloc"])
            for e_ in range(NE):
                for j in range(8):
                    DG(lambda e, e_=e_, j=j: e.indirect_dma_start(out=crow[:], out_offset=None, in_=cin_d,
                                                                 in_offset=bass.IndirectOffsetOnAxis(ap=rowi[:, e_, j:j + 1], axis=0)),
                       ["cin_d", "rowi"], ["crow"])
                    V(lambda e, e_=e_, j=j: e.tensor_scalar(out=cle[:], in0=crow[:], scalar1=rr[:, e_, j:j + 1], scalar2=0.0,
                                                            op0=ALU.is_le, op1=ALU.add, accum_out=tloc[:, e_, j:j + 1]),
                      ["crow", "rr"], ["cle", "tloc"])
            V(lambda e: e.tensor_scalar_min(out=tloc[:], in0=tloc[:], scalar1=127.0), ["tloc"], ["tloc"])
            V(lambda e: e.scalar_tensor_tensor(out=idf[:], in0=tl_[:], scalar=128.0, in1=tloc[:], op0=ALU.mult, op1=ALU.add),
              ["tl_", "tloc"], ["idf"])
            V(lambda e: e.tensor_copy(out=idx_all[:], in_=idf[:]), ["idf"], ["idx_all"])
            for e_ in range(NE):
                for j in range(8):
                    DG(lambda e, e_=e_, j=j: e.indirect_dma_start(out=arow[:], out_offset=None, in_=aff_d,
                                                                 in_offset=bass.IndirectOffsetOnAxis(ap=idx_all[:, e_, j:j + 1], axis=0)),
                       ["aff_d", "idx_all"], ["arow"])
                    V(lambda e, e_=e_, j=j: e.tensor_tensor(out=cle[:, 0:16], in0=arow[:], in1=hs[:], op=ALU.mult), ["arow", "hs"], ["cle"])
                    V(lambda e, e_=e_, j=j: e.tensor_tensor(out=gate_all[:, e_, j:j + 1], in0=cle[:, e_:e_ + 1], in1=cle[:, 8 + e_:9 + e_],
                                                            op=ALU.add), ["cle"], ["gate_all"])
            P.barrier()

        if debug:
            DS(lambda e: e.dma_start(out=dbg_idx, in_=idx_all[:].rearrange("p a b -> p (a b)")), ["idx_all"], ["dbg_idx"])
            DS(lambda e: e.dma_start(out=dbg_gate, in_=gate_all[:].rearrange("p a b -> p (a b)")), ["gate_all"], ["dbg_gate"])
            P.barrier()
        if debug == 2:
            early.close()
            return nc

        early.close()
        with contextlib.ExitStack() as st:
            zt = sb(st, "zt", [128, D])
            V(lambda e: e.memset(zt[:], 0.0), w=["zt"])
            for t in range(NT):
                DS(lambda e, t=t: e.dma_start(out=moe_d[t * 128:(t + 1) * 128, :], in_=zt[:]), ["zt"], ["moe_d"])
            P.barrier()
        FB = 256
        NFB = DFF // FB
        with contextlib.ExitStack() as st:
            xgA = [sb(st, "xg%d" % i, [128, D], BF16) for i in range(2)]
            xgT = sb(st, "xgT", [128, 8, CAP], BF16)
            wdn = sb(st, "wdn", [128, NFT, D], BF16)
            hid = sb(st, "hid", [128, NFT, CAP], BF16)
            stg = [sb(st, "wstg%d" % i, [128, 2, 8, FB]) for i in range(2)]
            wgbA = [sb(st, "wgb%d" % i, [128, 8, FB], BF16) for i in range(2)]
            wubA = [sb(st, "wub%d" % i, [128, 8, FB], BF16) for i in range(2)]
            stgdA = [sb(st, "stgd%d" % i, [128, D]) for i in range(2)]
            slA = [sb(st, "sl%d" % i, [128, 512]) for i in range(2)]
            obA = [sb(st, "ob%d" % i, [128, D]) for i in range(2)]
            pTg = ps(st, "pTg", [128, D], BF16)
            pGU = [[ps(st, "pGU%d%d" % (h_, m_), [128, 512]) for m_ in range(2)] for h_ in range(2)]
            pO = ps(st, "pO", [128, D])
            gcnt = 0
            ocnt = 0
            for e_ in range(NE):
                for j in range(8):
                    xg, xgk = xgA[gcnt % 2], "xg%d" % (gcnt % 2)
                    gcnt += 1
                    DG(lambda e, e_=e_, j=j, xg=xg: e.indirect_dma_start(out=xg[:], out_offset=None, in_=hrow_d,
                                                                        in_offset=bass.IndirectOffsetOnAxis(ap=idx_all[:, e_, j:j + 1], axis=0)),
                       ["hrow_d", "idx_all"], [xgk])
                    for kt in range(8):
                        T(lambda e, kt=kt, xg=xg: e.transpose(out=pTg[:, kt * 128:(kt + 1) * 128], in_=xg[:, kt * 128:(kt + 1) * 128],
                                                              identity=ident_bf[:]), [xgk, "ident_bf"], ["pTg"])
                    V(lambda e, j=j: e.tensor_copy(out=xgT[:, :, j * 128:(j + 1) * 128], in_=pTg[:].rearrange("p (a b) -> p a b", a=8)),
                      ["pTg"], ["xgT"])
                for fb in range(NFB):
                    s_, sk = stg[fb % 2], "wstg%d" % (fb % 2)
                    wgb, wgk = wgbA[fb % 2], "wgb%d" % (fb % 2)
                    wub, wuk = wubA[fb % 2], "wub%d" % (fb % 2)
                    DS(lambda e, e_=e_, fb=fb, s_=s_: e.dma_start(out=s_[:, 0, :, :],
                                                                  in_=wg[e_].rearrange("(kt p) f -> p kt f", p=128)[:, :, fb * FB:(fb + 1) * FB]),
                       w=[sk + "g"])
                    DS(lambda e, e_=e_, fb=fb, s_=s_: e.dma_start(out=s_[:, 1, :, :],
                                                                  in_=wu[e_].rearrange("(kt p) f -> p kt f", p=128)[:, :, fb * FB:(fb + 1) * FB]),
                       w=[sk + "u"])
                    A(lambda e, s_=s_, wgb=wgb: e.activation(out=wgb[:], in_=s_[:, 0, :, :], func=AF.Identity), [sk + "g"], [wgk])
                    G(lambda e, s_=s_, wub=wub: e.tensor_copy(out=wub[:], in_=s_[:, 1, :, :]), [sk + "u"], [wuk])
                    if fb < 2 * 0 + NFB:
                        for q_ in range(2):
                            ft = fb * 2 + q_
                            sd_, sdk = stgdA[ft % 2], "stgd%d" % (ft % 2)
                            DS(lambda e, e_=e_, ft=ft, sd_=sd_: e.dma_start(out=sd_[:], in_=wd[e_, ft * 128:(ft + 1) * 128, :]), w=[sdk])
                            G(lambda e, ft=ft, sd_=sd_: e.tensor_copy(out=wdn[:, ft, :], in_=sd_[:]), [sdk], ["wdn"])
                    for fl in range(FB // 128):
                        ft = fb * (FB // 128) + fl
                        for h_ in range(2):
                            c0 = h_ * 512
                            pg, pu = pGU[h_][0], pGU[h_][1]
                            pgk, puk = "pGU%d0" % h_, "pGU%d1" % h_
                            sl, slk = slA[h_], "sl%d" % h_
                            for kt in range(8):
                                T(lambda e, kt=kt, c0=c0, fl=fl, pg=pg, wgb=wgb: e.matmul(pg[:], lhsT=wgb[:, kt, fl * 128:(fl + 1) * 128],
                                                                                        rhs=xgT[:, kt, c0:c0 + 512], start=(kt == 0), stop=(kt == 7)),
                                  [wgk, "xgT"], [pgk])
                            for kt in range(8):
                                T(lambda e, kt=kt, c0=c0, fl=fl, pu=pu, wub=wub: e.matmul(pu[:], lhsT=wub[:, kt, fl * 128:(fl + 1) * 128],
                                                                                        rhs=xgT[:, kt, c0:c0 + 512], start=(kt == 0), stop=(kt == 7)),
                                  [wuk, "xgT"], [puk])
                            A(lambda e, sl=sl, pg=pg: e.activation(out=sl[:], in_=pg[:], func=AF.Silu), [pgk], [slk])
                            V(lambda e, ft=ft, c0=c0, sl=sl, pu=pu: e.tensor_tensor(out=hid[:, ft, c0:c0 + 512], in0=sl[:], in1=pu[:], op=ALU.mult),
                              [slk, puk], ["hid"])
                for j in range(8):
                    ob, obk = obA[ocnt % 2], "ob%d" % (ocnt % 2)
                    ocnt += 1
                    for nh in range(2):
                        for ft in range(NFT):
                            T(lambda e, j=j, nh=nh, ft=ft: e.matmul(pO[:, nh * 512:(nh + 1) * 512], lhsT=hid[:, ft, j * 128:(j + 1) * 128],
                                                                    rhs=wdn[:, ft, nh * 512:(nh + 1) * 512], start=(ft == 0), stop=(ft == NFT - 1)),
                              ["hid", "wdn"], ["pO"])
                    V(lambda e, e_=e_, j=j, ob=ob: e.tensor_scalar_mul(out=ob[:], in0=pO[:], scalar1=gate_all[:, e_, j:j + 1]), ["pO", "gate_all"], [obk])
                    DG(lambda e, e_=e_, j=j, ob=ob: e.indirect_dma_start(out=moe_d, out_offset=bass.IndirectOffsetOnAxis(ap=idx_all[:, e_, j:j + 1], axis=0),
                                                                        in_=ob[:], in_offset=None, compute_op=ALU.add, oob_is_err=True),
                       [obk, "idx_all", "moe_d"], ["moe_d"])
            P.barrier()

        if debug == 3:
            return nc

        ccsem = gst.enter_context(nc.semaphore("ccsem"))
        CCH = 16
        crow_ = SEQ // CCH
        for cc_ in range(CCH):
            nc.gpsimd.collective_compute("AllReduce", ALU.add, replica_groups=[[0, 1], [2, 3], [4, 5], [6, 7]],
                                         ins=[moe_d[cc_ * crow_:(cc_ + 1) * crow_, :]],
                                         outs=[moe_r[cc_ * crow_:(cc_ + 1) * crow_, :]]).then_inc(ccsem)
            nc.gpsimd.wait_ge(ccsem, cc_ + 1)
        G(lambda e: e.memset(gate_all[:, 0, 0:1], 0.0), w=["gate_all"])
        P.barrier()
        with contextlib.ExitStack() as st:
            mt_ = [sb(st, "m6_%d" % i, [128, D]) for i in range(2)]
            xq = [sb(st, "x6_%d" % i, [128, D]) for i in range(2)]
            tm = sb(st, "tm6", [128, D])
            xr = sb(st, "xr6", [128, D])
            oo = [sb(st, "o6_%d" % i, [128, D]) for i in range(2)]
            stats = sb(st, "stats6", [128, 2, 6])
            mv = sb(st, "mv6_", [128, 2])
            rstd = sb(st, "rstd6", [128, 1])
            tki = sb(st, "tki", [128, NT // 2], I32)
            DS(lambda e: e.dma_start(out=tki[:], in_=tokidx_d), w=["tki"])
            for t in range(NT // 2):
                m_ = mt_[t % 2]
                mk_ = "m6_%d" % (t % 2)
                x_ = xq[t % 2]
                xk = "x6_%d" % (t % 2)
                o_ = oo[t % 2]
                ok = "o6_%d" % (t % 2)
                DG(lambda e, t=t, m_=m_: e.indirect_dma_start(out=m_[:], out_offset=None, in_=moe_r,
                                                             in_offset=bass.IndirectOffsetOnAxis(ap=tki[:, t:t + 1], axis=0)),
                   ["moe_r", "tki"], [mk_])
                DG(lambda e, t=t, x_=x_: e.indirect_dma_start(out=x_[:], out_offset=None, in_=xnew_d,
                                                             in_offset=bass.IndirectOffsetOnAxis(ap=tki[:, t:t + 1], axis=0)),
                   ["xnew_d", "tki"], [xk])
                V(lambda e, m_=m_: e.tensor_tensor(out=tm[:], in0=m_[:], in1=fin3[:, 0, :], op=ALU.mult), [mk_, "fin3"], ["tm6"])
                V(lambda e, x_=x_: e.scalar_tensor_tensor(out=xr[:], in0=x_[:], scalar=ALPHA, in1=tm[:], op0=ALU.mult, op1=ALU.add),
                  [xk, "tm6"], ["xr6"])
                for c in range(2):
                    V(lambda e, c=c: e.bn_stats(out=stats[:, c, :], in_=xr[:, c * 512:(c + 1) * 512]), ["xr6"], ["stats6"])
                V(lambda e: e.bn_aggr(out=mv[:], in_=stats[:].rearrange("p a b -> p (a b)")), ["stats6"], ["mv6_"])
                V(lambda e: e.tensor_scalar_add(out=rstd[:], in0=mv[:, 1:2], scalar1=EPS), ["mv6_"], ["rstd6"])
                A(lambda e: e.activation(out=rstd[:], in_=rstd[:], func=AF.Sqrt), ["rstd6"], ["rstd6"])
                V(lambda e: e.reciprocal(out=rstd[:], in_=rstd[:]), ["rstd6"], ["rstd6"])
                V(lambda e: e.tensor_scalar(out=tm[:], in0=xr[:], scalar1=mv[:, 0:1], scalar2=rstd[:, 0:1],
                                            op0=ALU.subtract, op1=ALU.mult), ["xr6", "mv6_", "rstd6"], ["tm6"])
                V(lambda e: e.tensor_tensor(out=tm[:], in0=tm[:], in1=fin3[:, 1, :], op=ALU.mult), ["tm6", "fin3"], ["tm6"])
                V(lambda e, o_=o_: e.tensor_tensor(out=o_[:], in0=tm[:], in1=fin3[:, 2, :], op=ALU.add), ["tm6", "fin3"], [ok])
                DS(lambda e, t=t, o_=o_: e.dma_start(out=out_d[t * 128:(t + 1) * 128, :], in_=o_[:]), [ok], ["out_d"])
            P.barrier()
    return nc


def _prep(inputs):
    f32 = np.float32
    g = {k: np.asarray(v) for k, v in inputs.items()}
    bf = ml_dtypes.bfloat16
    com = {}
    com["w_ada"] = np.ascontiguousarray(g["w_ada"][0], f32)
    ba = g["b_ada"][0]
    com["bada_fm"] = np.ascontiguousarray(ba[:2048].reshape(16, 128).T, f32)
    com["bada_row"] = np.ascontiguousarray(ba[2048:].reshape(1, 4096), f32)
    w_in = g["w_in"][0]
    ws5 = np.zeros((D, 4, 4, 32), f32)
    ws5[:, :, :, :16] = w_in[:, :256].reshape(D, 4, 4, 16)
    com["w_in_s5"] = ws5.reshape(D, 512)
    com["w_in_u"] = np.ascontiguousarray(w_in[:, 256:1024], f32)
    com["w_in_v"] = np.ascontiguousarray(w_in[:, 1024:1792], f32)
    com["gm_wsT"] = np.ascontiguousarray(g["gm_ws"][0].transpose(2, 0, 1), f32)
    com["gm_bs_row"] = np.ascontiguousarray(g["gm_bs"][0].reshape(1, 768), f32)
    are = g["s5_a_re"][0]; aim = g["s5_a_im"][0]; ls = g["s5_log_step"][0]
    com["are_row"] = np.ascontiguousarray(are.reshape(1, 2048), f32)
    com["aim_row"] = np.ascontiguousarray(aim.reshape(1, 2048), f32)
    com["ls_row"] = np.ascontiguousarray(np.repeat(ls.reshape(32), 64).reshape(1, 2048), f32)

    def pp(a):
        return np.ascontiguousarray(a.reshape(2, 8, 2, 64).transpose(2, 3, 0, 1).reshape(128, 16), f32)
    com["are_pp"] = pp(are)
    com["aim_pp"] = pp(aim)
    com["ls_pp"] = pp(np.repeat(ls[:, :, None], 64, axis=2))
    for nm, src in (("WBre_raw", g["s5_b_re"][0]), ("WBim_raw", g["s5_b_im"][0])):
        w = np.zeros((128, 2, 8, 128), f32)
        for gp in range(8):
            for g2 in range(2):
                gi = 2 * gp + g2
                r0 = 64 * (gp % 2) + 32 * g2
                w[r0:r0 + 16, :, gp, 64 * g2:64 * g2 + 64] = src[:, gi].transpose(2, 0, 1)
        com[nm] = w.reshape(128, 2048)
    for nm, src in (("WCre_raw", g["s5_c_re"][0]), ("WCim_raw", g["s5_c_im"][0])):
        w = np.zeros((128, 2, 8, 128), f32)
        for gp in range(8):
            for g2 in range(2):
                gi = 2 * gp + g2
                c0 = 64 * (gp % 2) + 32 * g2
                w[64 * g2:64 * g2 + 64, :, gp, c0:c0 + 16] = src[:, gi].transpose(2, 0, 1)
        com[nm] = w.reshape(128, 2048)

    def padpp(v):
        o = np.zeros((4, 4, 32), f32)
        o[:, :, :16] = v.reshape(4, 4, 16)
        return np.ascontiguousarray(o.reshape(4, 128).T, f32)
    com["dpp"] = padpp(g["s5_d"][0])
    com["b_glu_pp"] = padpp(g["s5_b_glu"][0].reshape(16, 16))
    wgl = np.zeros((4, 4, 32, 4, 4, 32), f32)
    wgl[:, :, :16, :, :, :16] = g["s5_w_glu"][0].reshape(4, 4, 16, 4, 4, 16)
    com["w_glu_pad"] = wgl.reshape(512, 512)
    wo = np.zeros((1280, D), f32)
    wo5 = np.zeros((4, 4, 32, D), f32)
    wo5[:, :, :16, :] = g["w_out"][0][:256].reshape(4, 4, 16, D)
    wo[:512] = wo5.reshape(512, D)
    wo[512:] = g["w_out"][0][256:]
    com["w_out_pad"] = wo
    for k in ("ln1_g", "ln1_b", "ln2_g", "ln2_b"):
        com[k] = np.ascontiguousarray(g[k][0].reshape(1, D), f32)
    com["w_router"] = np.ascontiguousarray(g["w_router"][0], f32)
    com["ident_bf"] = np.eye(128, dtype=f32).astype(bf)
    com["ident_f"] = np.eye(128, dtype=f32)
    com["tri"] = np.triu(np.ones((128, 128), f32)).astype(bf)
    com["ones"] = np.ones((128, 128), f32).astype(bf)
    com["iota_row"] = np.arange(1024, dtype=f32).reshape(1, 1024)
    com["slot_pp"] = (np.arange(128, dtype=f32)[:, None] + 128.0 * np.arange(8, dtype=f32)[None, :]).astype(f32)
    maps = []
    for c in range(8):
        b, half = c // 2, c % 2
        m = dict(com)
        m["xs"] = np.ascontiguousarray(np.concatenate([g["ctx"][b], g["x"][b]], axis=0), f32)
        cv = np.stack([g["c"][b], g["c_ctx"]], axis=1)
        m["cT"] = np.ascontiguousarray(cv.reshape(8, 128, 2).transpose(1, 0, 2), f32)
        es = slice(8 * half, 8 * half + 8)
        m["wg"] = np.ascontiguousarray(g["moe_w_gate"][0][es], f32)
        m["wu"] = np.ascontiguousarray(g["moe_w_up"][0][es], f32)
        m["wd"] = np.ascontiguousarray(g["moe_w_down"][0][es], f32)
        hs = np.zeros((128, 16), f32)
        hs[:, es] = 1.0
        m["half_sel"] = hs
        m["tokidx"] = (half * 4096 + np.arange(32, dtype=np.int32)[None, :] * 128 + np.arange(128, dtype=np.int32)[:, None]).astype(np.int32)
        maps.append(m)
    return maps


def kernel(**inputs):
    maps = _prep(inputs)
    nc = build()
    res = run_bass_kernel_spmd(nc, maps, core_ids=list(range(8)))
    out = np.zeros((4, SEQ, D), np.float32)
    for c in range(8):
        b, half = c // 2, c % 2
        out[b, half * 4096:(half + 1) * 4096] = res.results[c]["out"]
    return out
```

```python
import contextlib
import math
import numpy as np
import ml_dtypes
import concourse.bass as bass
import concourse.mybir as mybir
from concourse.bass_utils import run_bass_kernel_spmd

F32 = mybir.dt.float32
BF16 = mybir.dt.bfloat16
I32 = mybir.dt.int32
AF = mybir.ActivationFunctionType
ALU = mybir.AluOpType
AX = mybir.AxisListType

D = 1024
SEQ = 8192
CTX = 256
NT = SEQ // 128
NTC = CTX // 128
TOT = SEQ + CTX
DFF = 2816
NFT = DFF // 128
NE = 8
CAP = 1024
ALPHA = 2.0 ** 0.25
EPS = 1e-6
TWO_PI = 6.283185
SEG = 1024

ENGS = ("sync", "scalar", "vector", "gpsimd", "tensor")
DMA_K = 8


class Prog:
    def __init__(self, nc, stack):
        self.nc = nc
        self.eng = {"sync": nc.sync, "scalar": nc.scalar, "vector": nc.vector,
                    "gpsimd": nc.gpsimd, "tensor": nc.tensor}
        self.sems = {}
        for e in ENGS:
            self.sems["c" + e] = stack.enter_context(nc.semaphore("c" + e))
        for e in ("sync", "scalar", "gpsimd"):
            for i in range(DMA_K):
                n = "d%s%d" % (e, i)
                self.sems[n] = stack.enter_context(nc.semaphore(n))
        self.ccnt = {e: 0 for e in ENGS}
        self.dcnt = {e: 0 for e in ENGS}
        self.lastw = {}
        self.readers = {}
        self.known = {e: {} for e in ENGS}
        self.latest = {}

    def _need(self, eng, tok):
        if tok is None:
            return
        name, val = tok
        if name == "c" + eng and eng in ("tensor", "sync"):
            return
        if self.known[eng].get(name, 0) >= val:
            return
        self.known[eng][name] = val
        self.eng[eng].wait_ge(self.sems[name], val)

    def op(self, eng, fn, reads=(), writes=(), dma=False, sem_inc=None):
        for k in reads:
            self._need(eng, self.lastw.get(k))
        for k in writes:
            self._need(eng, self.lastw.get(k))
            for t in self.readers.get(k, ()):
                self._need(eng, t)
        if dma:
            i = self.dcnt[eng]
            self.dcnt[eng] += 1
            name = "d%s%d" % (eng, i % DMA_K)
            inc = 16 if sem_inc is None else sem_inc
            val = self.latest.get(name, 0) + inc
            if i >= DMA_K:
                self._need(eng, (name, self.latest.get(name, 0)))
        else:
            self.ccnt[eng] += 1
            name = "c" + eng
            val = self.ccnt[eng]
            inc = 1
        tok = (name, val)
        ins = fn(self.eng[eng])
        ins.then_inc(self.sems[name], inc)
        self.latest[name] = val
        for k in writes:
            self.lastw[k] = tok
            self.readers[k] = []
        for k in reads:
            if k not in writes:
                self.readers.setdefault(k, []).append(tok)
        return tok

    def barrier(self):
        toks = list(self.latest.items())
        for e in ENGS:
            for t in toks:
                self._need(e, t)
        self.lastw = {}
        self.readers = {}


def build(debug=0):
    nc = bass.Bass("TRN2", target_bir_lowering=False)

    def din(name, shape, dt=F32):
        return nc.dram_tensor(name, list(shape), dt, kind="ExternalInput").ap()

    def dscr(name, shape, dt=F32):
        return nc.dram_tensor(name, list(shape), dt).ap()

    xs = din("xs", [TOT, D])
    cT = din("cT", [128, 8, 2])
    w_ada = din("w_ada", [D, 6 * D])
    bada_fm = din("bada_fm", [128, 16])
    bada_row = din("bada_row", [1, 4 * D])
    w_in_s5 = din("w_in_s5", [D, 512])
    w_in_u = din("w_in_u", [D, 768])
    w_in_v = din("w_in_v", [D, 768])
    gm_wsT = din("gm_wsT", [128, 6, 128])
    gm_bs_row = din("gm_bs_row", [1, 768])
    are_row = din("are_row", [1, 2048])
    aim_row = din("aim_row", [1, 2048])
    ls_row = din("ls_row", [1, 2048])
    are_pp = din("are_pp", [128, 16])
    aim_pp = din("aim_pp", [128, 16])
    ls_pp = din("ls_pp", [128, 16])
    WBre_raw = din("WBre_raw", [128, 2048])
    WBim_raw = din("WBim_raw", [128, 2048])
    WCre_raw = din("WCre_raw", [128, 2048])
    WCim_raw = din("WCim_raw", [128, 2048])
    dpp = din("dpp", [128, 4])
    w_glu_pad = din("w_glu_pad", [512, 512])
    b_glu_pp = din("b_glu_pp", [128, 4])
    w_out_pad = din("w_out_pad", [1280, D])
    ln1_g = din("ln1_g", [1, D])
    ln1_b = din("ln1_b", [1, D])
    ln2_g = din("ln2_g", [1, D])
    ln2_b = din("ln2_b", [1, D])
    w_router = din("w_router", [D, 16])
    if debug in (0, 3):
        wg = din("wg", [NE, D, DFF])
        wu = din("wu", [NE, D, DFF])
        wd = din("wd", [NE, DFF, D])
    ident_bf_d = din("ident_bf", [128, 128], BF16)
    ident_f_d = din("ident_f", [128, 128])
    tri_d = din("tri", [128, 128], BF16)
    ones_d = din("ones", [128, 128], BF16)
    iota_row_d = din("iota_row", [1, 1024])
    slot_pp_d = din("slot_pp", [128, 8])
    half_sel = din("half_sel", [128, 16])
    tokidx_d = din("tokidx", [128, NT // 2], I32)

    out_d = nc.dram_tensor("out", [SEQ // 2, D], F32, kind="ExternalOutput").ap()
    okind = {"kind": "ExternalOutput"} if debug else {}
    xnew_d = nc.dram_tensor("xnew_s", [SEQ, D], F32, **okind).ap()
    hrow_d = nc.dram_tensor("hrow_s", [SEQ, D], BF16, **okind).ap()
    catT_d = dscr("catT_s", [10, 128, SEQ], BF16)
    yf_d = dscr("yf_s", [4, 128, SEQ], F32)
    aff_d = nc.dram_tensor("aff_s", [SEQ, 16], F32, **okind).ap()
    if debug:
        dbg_idx = nc.dram_tensor("dbg_idx", [128, NE * 8], I32, kind="ExternalOutput").ap()
        dbg_gate = nc.dram_tensor("dbg_gate", [128, NE * 8], F32, kind="ExternalOutput").ap()
    cin_d = dscr("cin_s", [NE * NT, 128], F32)
    moe_d = nc.dram_tensor("moe_s", [SEQ, D], F32, **({"kind": "ExternalOutput"} if debug == 3 else {})).ap()
    moe_r = dscr("moe_r", [SEQ, D], F32)

    with contextlib.ExitStack() as gst:
        P = Prog(nc, gst)

        _uid = [0]

        def sb(st, name, shape, dt=F32):
            _uid[0] += 1
            return st.enter_context(nc.sbuf_tensor("%s_%d" % (name, _uid[0]), list(shape), dt))

        def ps(st, name, shape, dt=F32):
            _uid[0] += 1
            return st.enter_context(nc.psum_tensor("%s_%d" % (name, _uid[0]), list(shape), dt))

        REC = [None]

        def _op(eng, fn, r, w, dma=False):
            if REC[0] is not None:
                REC[0].append((eng, fn, tuple(r), tuple(w), dma))
                return None
            return P.op(eng, fn, r, w, dma=dma)

        def record(f, *args):
            REC[0] = []
            f(*args)
            ops, REC[0] = REC[0], None
            return ops

        def emit_zip(*lists):
            lists = [l for l in lists if l]
            pos = [0] * len(lists)
            total = sum(len(l) for l in lists)
            for _ in range(total):
                bi, bv = -1, 2.0
                for i, l in enumerate(lists):
                    if pos[i] < len(l):
                        v = pos[i] / len(l)
                        if v < bv:
                            bi, bv = i, v
                eng, fn, r, w, dma = lists[bi][pos[bi]]
                pos[bi] += 1
                P.op(eng, fn, r, w, dma=dma)

        def V(fn, r=(), w=()):
            return _op("vector", fn, r, w)

        def A(fn, r=(), w=()):
            return _op("scalar", fn, r, w)

        def G(fn, r=(), w=()):
            return _op("gpsimd", fn, r, w)

        def T(fn, r=(), w=()):
            return _op("tensor", fn, r, w)

        def DS(fn, r=(), w=()):
            return _op("sync", fn, r, w, dma=True)

        def DG(fn, r=(), w=()):
            return _op("gpsimd", fn, r, w, dma=True)

        def bc(ap_, shape):
            return ap_.to_broadcast(list(shape))

        ident_bf = sb(gst, "ident_bf_t", [128, 128], BF16)
        ident_f = sb(gst, "ident_f_t", [128, 128])
        ones_bf = sb(gst, "ones_t", [128, 128], BF16)
        tri_bf = sb(gst, "tri_t", [128, 128], BF16)
        modfm = sb(gst, "modfm", [128, 2, 8, 2])
        fin3 = sb(gst, "fin3", [128, 3, D])
        idx_all = sb(gst, "idx_all", [128, NE, 8], I32)
        gate_all = sb(gst, "gate_all", [128, NE, 8])
        early = contextlib.ExitStack()
        iota_r = sb(early, "iota_r", [128, 1024])
        modrow = sb(early, "modrow", [128, 4, D])
        lnrow = sb(early, "lnrow", [128, 4, D])
        aff = sb(early, "aff", [128, NT, 16])
        DS(lambda e: e.dma_start(out=ident_bf[:], in_=ident_bf_d), w=["ident_bf"])
        DS(lambda e: e.dma_start(out=ident_f[:], in_=ident_f_d), w=["ident_f"])
        DS(lambda e: e.dma_start(out=ones_bf[:], in_=ones_d), w=["ones"])
        DS(lambda e: e.dma_start(out=tri_bf[:], in_=tri_d), w=["tri"])
        DS(lambda e: e.dma_start(out=iota_r[:], in_=bc(iota_row_d, [128, 1024])), w=["iota_r"])
        for i, a in enumerate((ln1_g, ln1_b, ln2_g, ln2_b)):
            DS(lambda e, i=i, a=a: e.dma_start(out=lnrow[:, i, :], in_=bc(a, [128, D])), w=["lnrow"])

        with contextlib.ExitStack() as st:
            ct = sb(st, "ct", [128, 8, 2])
            sc = sb(st, "sc", [128, 8, 2])
            scb = sb(st, "scb", [128, 8, 128])
            wa = sb(st, "wa", [128, 8, D])
            bfm = sb(st, "bfm", [128, 16])
            brow = sb(st, "brow", [128, 4 * D])
            pfm = ps(st, "pfm", [128, 8, 2])
            prow = ps(st, "prow", [128, D])
            DS(lambda e: e.dma_start(out=ct[:], in_=cT), w=["ct"])
            DS(lambda e: e.dma_start(out=bfm[:], in_=bada_fm), w=["bfm"])
            DS(lambda e: e.dma_start(out=brow[:], in_=bc(bada_row, [128, 4 * D])), w=["brow"])
            A(lambda e: e.activation(out=sc[:], in_=ct[:], func=AF.Silu), ["ct"], ["sc"])
            V(lambda e: e.tensor_copy(out=scb[:], in_=bc(sc[:, :, 0:1], [128, 8, 128])), ["sc"], ["scb"])
            wav = w_ada.rearrange("(kt p) f -> p kt f", p=128)
            for ch in range(6):
                DS(lambda e, ch=ch: e.dma_start(out=wa[:], in_=wav[:, :, ch * D:(ch + 1) * D]), w=["wa"])
                if ch < 2:
                    for ft in range(8):
                        for kt in range(8):
                            T(lambda e, ft=ft, kt=kt: e.matmul(pfm[:, ft, :], lhsT=wa[:, kt, ft * 128:(ft + 1) * 128],
                                                               rhs=sc[:, kt, :], start=(kt == 0), stop=(kt == 7)),
                              ["wa", "sc"], ["pfm"])
                    V(lambda e, ch=ch: e.tensor_tensor(out=modfm[:, ch, :, :], in0=pfm[:],
                                                       in1=bc(bfm[:, ch * 8:(ch + 1) * 8].unsqueeze(2), [128, 8, 2]),
                                                       op=ALU.add), ["pfm", "bfm"], ["modfm"])
                else:
                    for nh in range(2):
                        for kt in range(8):
                            T(lambda e, nh=nh, kt=kt: e.matmul(prow[:, nh * 512:(nh + 1) * 512], lhsT=scb[:, kt, :],
                                                               rhs=wa[:, kt, nh * 512:(nh + 1) * 512],
                                                               start=(kt == 0), stop=(kt == 7)),
                              ["wa", "scb"], ["prow"])
                    V(lambda e, ch=ch: e.tensor_tensor(out=modrow[:, ch - 2, :], in0=prow[:],
                                                       in1=brow[:, (ch - 2) * D:(ch - 1) * D], op=ALU.add),
                      ["prow", "brow"], ["modrow"])
            V(lambda e: e.tensor_scalar_add(out=modfm[:, 1, :, :], in0=modfm[:, 1, :, :], scalar1=1.0), ["modfm"], ["modfm"])
            V(lambda e: e.tensor_scalar_add(out=modrow[:, 2, :], in0=modrow[:, 2, :], scalar1=1.0), ["modrow"], ["modrow"])
            V(lambda e: e.tensor_copy(out=fin3[:, 0, :], in_=modrow[:, 3, :]), ["modrow"], ["fin3"])
            V(lambda e: e.tensor_copy(out=fin3[:, 1:3, :], in_=lnrow[:, 2:4, :]), ["lnrow"], ["fin3"])
            P.barrier()

        with contextlib.ExitStack() as mst:
            U = sb(mst, "U", [128, 4, TOT], BF16)
            wst_ = contextlib.ExitStack()
            wS = sb(wst_, "wS", [128, 8, 512], BF16)
            wU = sb(wst_, "wU", [128, 8, 768], BF16)
            wV = sb(wst_, "wV", [128, 8, 768], BF16)
            wsT = sb(wst_, "wsT", [128, 6, 128], BF16)
            bsrow = sb(wst_, "bsrow", [128, 768])
            with contextlib.ExitStack() as st:
                stg = sb(st, "stg", [128, 8, 768])
                for (src, dst, n, nm) in ((w_in_s5, wS, 512, "wS"), (w_in_u, wU, 768, "wU"), (w_in_v, wV, 768, "wV")):
                    DS(lambda e, src=src, n=n: e.dma_start(out=stg[:, :, 0:n], in_=src.rearrange("(kt p) f -> p kt f", p=128)),
                       w=["stg"])
                    V(lambda e, dst=dst, n=n: e.tensor_copy(out=dst[:], in_=stg[:, :, 0:n]), ["stg"], [nm])
                DS(lambda e: e.dma_start(out=stg[:, 0:6, 0:128], in_=gm_wsT), w=["stg"])
                V(lambda e: e.tensor_copy(out=wsT[:], in_=stg[:, 0:6, 0:128]), ["stg"], ["wsT"])
                DS(lambda e: e.dma_start(out=bsrow[:], in_=bc(gm_bs_row, [128, 768])), w=["bsrow"])
                P.barrier()

            with contextlib.ExitStack() as st:
                xt = [sb(st, "xt%d" % i, [128, D]) for i in range(2)]
                xnA = [sb(st, "xn%d" % i, [128, D], BF16) for i in range(2)]
                xmTA = [sb(st, "xmT%d" % i, [128, 8, 128], BF16) for i in range(2)]
                statsA = [sb(st, "stats%d" % i, [128, 2, 6]) for i in range(2)]
                mvA = [sb(st, "mv%d" % i, [128, 2]) for i in range(2)]
                rstdA = [sb(st, "rstd%d" % i, [128, 1]) for i in range(2)]
                uTA = [sb(st, "uT%d" % i, [128, 6, 128]) for i in range(2)]
                vvA = [sb(st, "vv%d" % i, [128, 6, 128]) for i in range(2)]
                vcA = [sb(st, "vc%d" % i, [128, 6, 128]) for i in range(2)]
                vlnA = [sb(st, "vln%d" % i, [128, 6, 128], BF16) for i in range(2)]
                st6A = [sb(st, "st6%d" % i, [128, 6, 6]) for i in range(2)]
                mv6A = [sb(st, "mv6%d" % i, [128, 6, 2]) for i in range(2)]
                rs6A = [sb(st, "rs6%d" % i, [128, 6]) for i in range(2)]
                gmtA = [sb(st, "gmt%d" % i, [128, 6, 128]) for i in range(2)]
                gmb = [sb(st, "gmb%d" % i, [128, 6, 128], BF16) for i in range(2)]
                usb = [sb(st, "usb%d" % i, [128, 4, 128], BF16) for i in range(2)]
                pT = ps(st, "pT", [128, D], BF16)
                pS = ps(st, "pS", [128, 4, 128])
                pU = ps(st, "pU", [128, 8, 128])
                pV = ps(st, "pV", [128, D])
                pM = ps(st, "pM", [128, 8, 128])

                def stage_a(t):
                    i2 = t % 2
                    lat = t >= NTC
                    col = 0 if lat else 1
                    x_, xk = xt[i2], "xt%d" % i2
                    xn, xnk = xnA[i2], "xn%d" % i2
                    xmT, xmk = xmTA[i2], "xmT%d" % i2
                    stats, sk_ = statsA[i2], "stats%d" % i2
                    mv, mvk = mvA[i2], "mv%d" % i2
                    rstd, rk = rstdA[i2], "rstd%d" % i2
                    DS(lambda e: e.dma_start(out=x_[:], in_=xs[t * 128:(t + 1) * 128, :]), w=[xk])
                    for c in range(2):
                        V(lambda e, c=c: e.bn_stats(out=stats[:, c, :], in_=x_[:, c * 512:(c + 1) * 512]), [xk], [sk_])
                    V(lambda e: e.bn_aggr(out=mv[:], in_=stats[:].rearrange("p a b -> p (a b)")), [sk_], [mvk])
                    V(lambda e: e.tensor_scalar_add(out=rstd[:], in0=mv[:, 1:2], scalar1=EPS), [mvk], [rk])
                    A(lambda e: e.activation(out=rstd[:], in_=rstd[:], func=AF.Sqrt), [rk], [rk])
                    V(lambda e: e.reciprocal(out=rstd[:], in_=rstd[:]), [rk], [rk])
                    V(lambda e: e.tensor_scalar(out=xn[:], in0=x_[:], scalar1=mv[:, 0:1], scalar2=rstd[:, 0:1],
                                                op0=ALU.subtract, op1=ALU.mult), [xk, mvk, rk], [xnk])
                    for kt in range(8):
                        T(lambda e, kt=kt: e.transpose(out=pT[:, kt * 128:(kt + 1) * 128], in_=xn[:, kt * 128:(kt + 1) * 128],
                                                       identity=ident_bf[:]), [xnk, "ident_bf"], ["pT"])
                    for kt in range(8):
                        A(lambda e, kt=kt: e.activation(out=xmT[:, kt, :], in_=pT[:, kt * 128:(kt + 1) * 128],
                                                        func=AF.Identity, scale=modfm[:, 1, kt, col:col + 1],
                                                        bias=modfm[:, 0, kt, col:col + 1]),
                          ["pT", "modfm"], [xmk])

                def stage_b(t):
                    i2 = t % 2
                    lat = t >= NTC
                    xmT, xmk = xmTA[i2], "xmT%d" % i2
                    for ct_ in range(4):
                        for kt in range(8):
                            T(lambda e, ct_=ct_, kt=kt: e.matmul(pS[:, ct_, :], lhsT=wS[:, kt, ct_ * 128:(ct_ + 1) * 128],
                                                                 rhs=xmT[:, kt, :], start=(kt == 0), stop=(kt == 7)),
                              ["wS", xmk], ["pS"])
                    V(lambda e: e.tensor_copy(out=U[:, :, t * 128:(t + 1) * 128], in_=pS[:]), ["pS"], ["U"])
                    if not lat:
                        return
                    tl = t - NTC
                    uT, uk = uTA[i2], "uT%d" % i2
                    vv, vk = vvA[i2], "vv%d" % i2
                    vc, vck = vcA[i2], "vc%d" % i2
                    vln, vlk = vlnA[i2], "vln%d" % i2
                    st6, s6k = st6A[i2], "st6%d" % i2
                    mv6, m6k = mv6A[i2], "mv6%d" % i2
                    rs6, r6k = rs6A[i2], "rs6%d" % i2
                    gmt, gtk = gmtA[i2], "gmt%d" % i2
                    for ct_ in range(6):
                        for kt in range(8):
                            T(lambda e, ct_=ct_, kt=kt: e.matmul(pU[:, ct_, :], lhsT=wU[:, kt, ct_ * 128:(ct_ + 1) * 128],
                                                                 rhs=xmT[:, kt, :], start=(kt == 0), stop=(kt == 7)),
                              ["wU", xmk], ["pU"])
                    A(lambda e: e.activation(out=uT[:], in_=pU[:, 0:6, :], func=AF.Gelu_apprx_tanh), ["pU"], [uk])
                    for (c0, c1) in ((0, 512), (512, 768)):
                        for kt in range(8):
                            T(lambda e, c0=c0, c1=c1, kt=kt: e.matmul(pV[:, c0:c1], lhsT=xmT[:, kt, :], rhs=wV[:, kt, c0:c1],
                                                                      start=(kt == 0), stop=(kt == 7)),
                              ["wV", xmk], ["pV"])
                    A(lambda e: e.activation(out=vv[:].rearrange("p a b -> p (a b)"), in_=pV[:, 0:768], func=AF.Gelu_apprx_tanh),
                      ["pV"], [vk])
                    for g in range(6):
                        V(lambda e, g=g: e.bn_stats(out=st6[:, g, :], in_=vv[:, g, :]), [vk], [s6k])
                    for g in range(6):
                        V(lambda e, g=g: e.bn_aggr(out=mv6[:, g, :], in_=st6[:, g, :]), [s6k], [m6k])
                    V(lambda e: e.tensor_scalar_add(out=rs6[:], in0=mv6[:, :, 1], scalar1=EPS), [m6k], [r6k])
                    A(lambda e: e.activation(out=rs6[:], in_=rs6[:], func=AF.Sqrt), [r6k], [r6k])
                    V(lambda e: e.reciprocal(out=rs6[:], in_=rs6[:]), [r6k], [r6k])
                    V(lambda e: e.tensor_tensor(out=vc[:], in0=vv[:], in1=bc(mv6[:, :, 0:1], [128, 6, 128]), op=ALU.subtract),
                      [vk, m6k], [vck])
                    V(lambda e: e.tensor_tensor(out=vln[:], in0=vc[:], in1=bc(rs6[:].unsqueeze(2), [128, 6, 128]), op=ALU.mult),
                      [vck, r6k], [vlk])
                    for g in range(6):
                        T(lambda e, g=g: e.matmul(pM[:, g, :], lhsT=vln[:, g, :], rhs=wsT[:, g, :], start=True, stop=True),
                          [vlk, "wsT"], ["pM"])
                    G(lambda e: e.tensor_tensor(out=gmt[:], in0=bsrow[:].rearrange("p (a b) -> p a b", a=6), in1=bsrow[:].rearrange("p (a b) -> p a b", a=6),
                                                op=ALU.bypass), ["bsrow"], [gtk]) if False else None
                    V(lambda e: e.tensor_tensor(out=gmt[:], in0=pM[:, 0:6, :], in1=bsrow[:].rearrange("p (a b) -> p a b", a=6),
                                                op=ALU.add), ["pM", "bsrow"], [gtk])
                    gb = gmb[tl % 2]
                    gk = "gmb%d" % (tl % 2)
                    G(lambda e: e.tensor_tensor(out=gb[:], in0=gmt[:], in1=uT[:], op=ALU.mult), [gtk, uk], [gk])
                    DS(lambda e: e.dma_start(out=catT_d[4:10, :, tl * 128:(tl + 1) * 128].rearrange("a p t -> p a t"),
                                             in_=gb[:]), [gk], ["catT_gm"])

                stage_a(0)
                for t in range(NTC + NT):
                    la = record(stage_a, t + 1) if t + 1 < NTC + NT else []
                    lb = record(stage_b, t)
                    emit_zip(la, lb)
                P.barrier()
            wst_.close()

            with contextlib.ExitStack() as st:
                WB = sb(st, "WB", [128, 2, 2048], BF16)
                WC = sb(st, "WC", [128, 2, 2048], BF16)
                rho_pp = sb(st, "rho_pp", [128, 16])
                f_pp = sb(st, "f_pp", [128, 16])
                dsc = sb(st, "dsc", [128, 4])
                bglu = sb(st, "bglu", [128, 4])
                wglu = sb(st, "wglu", [128, 4, 512], BF16)
                for dr_ in range(4):
                  csl = slice(dr_ * 512, (dr_ + 1) * 512)
                  with contextlib.ExitStack() as s2:
                        r = {n: sb(s2, "r_" + n, [128, 512]) for n in
                             ("are", "aim", "stp", "rho", "f", "y", "y2", "sn", "cs", "x", "yv", "den", "cr", "ci", "t1", "t2", "bre", "bim")}
                        ri_ = sb(s2, "r_int", [128, 512], I32)
                        DS(lambda e: e.dma_start(out=r["are"][:], in_=bc(are_row[:, csl], [128, 512])), w=["are"])
                        DS(lambda e: e.dma_start(out=r["aim"][:], in_=bc(aim_row[:, csl], [128, 512])), w=["aim"])
                        DS(lambda e: e.dma_start(out=r["stp"][:], in_=bc(ls_row[:, csl], [128, 512])), w=["stp"])
                        DS(lambda e: e.dma_start(out=r["bre"][:], in_=WBre_raw[:, csl]), w=["bre"])
                        DS(lambda e: e.dma_start(out=r["bim"][:], in_=WBim_raw[:, csl]), w=["bim"])

                        def vt(o, a, b, op):
                            V(lambda e: e.tensor_tensor(out=r[o][:], in0=r[a][:], in1=r[b][:], op=op), [a, b], [o])

                        def frac(o, i):
                            V(lambda e: e.tensor_copy(out=ri_[:], in_=r[i][:]), [i], ["rint"])
                            V(lambda e: e.tensor_copy(out=r["t1"][:], in_=ri_[:]), ["rint"], ["t1"])
                            vt(o, i, "t1", ALU.subtract)

                        A(lambda e: e.activation(out=r["stp"][:], in_=r["stp"][:], func=AF.Exp), ["stp"], ["stp"])
                        vt("rho", "are", "stp", ALU.mult)
                        A(lambda e: e.activation(out=r["rho"][:], in_=r["rho"][:], func=AF.Exp), ["rho"], ["rho"])
                        vt("f", "aim", "stp", ALU.mult)
                        V(lambda e: e.tensor_scalar_mul(out=r["f"][:], in0=r["f"][:], scalar1=1.0 / (2 * math.pi)), ["f"], ["f"])
                        frac("y", "f")
                        A(lambda e: e.activation(out=r["sn"][:], in_=r["y"][:], func=AF.Sin, scale=TWO_PI), ["y"], ["sn"])
                        V(lambda e: e.tensor_scalar_add(out=r["y2"][:], in0=r["y"][:], scalar1=0.25), ["y"], ["y2"])
                        frac("y2", "y2")
                        A(lambda e: e.activation(out=r["cs"][:], in_=r["y2"][:], func=AF.Sin, scale=TWO_PI), ["y2"], ["cs"])
                        vt("x", "rho", "cs", ALU.mult)
                        V(lambda e: e.tensor_scalar_add(out=r["x"][:], in0=r["x"][:], scalar1=-1.0), ["x"], ["x"])
                        vt("yv", "rho", "sn", ALU.mult)
                        vt("den", "are", "are", ALU.mult)
                        vt("t2", "aim", "aim", ALU.mult)
                        vt("den", "den", "t2", ALU.add)
                        V(lambda e: e.reciprocal(out=r["den"][:], in_=r["den"][:]), ["den"], ["den"])
                        vt("cr", "x", "are", ALU.mult)
                        vt("t2", "yv", "aim", ALU.mult)
                        vt("cr", "cr", "t2", ALU.add)
                        vt("cr", "cr", "den", ALU.mult)
                        vt("ci", "yv", "are", ALU.mult)
                        vt("t2", "x", "aim", ALU.mult)
                        vt("ci", "ci", "t2", ALU.subtract)
                        vt("ci", "ci", "den", ALU.mult)
                        vt("t1", "cr", "bre", ALU.mult)
                        vt("t2", "ci", "bim", ALU.mult)
                        V(lambda e: e.tensor_tensor(out=WB[:, 0, csl], in0=r["t1"][:], in1=r["t2"][:], op=ALU.subtract), ["t1", "t2"], ["WB"])
                        vt("t1", "cr", "bim", ALU.mult)
                        vt("t2", "ci", "bre", ALU.mult)
                        V(lambda e: e.tensor_tensor(out=WB[:, 1, csl], in0=r["t1"][:], in1=r["t2"][:], op=ALU.add), ["t1", "t2"], ["WB"])
                        DS(lambda e: e.dma_start(out=r["bre"][:], in_=WCre_raw[:, csl]), w=["bre"])
                        DS(lambda e: e.dma_start(out=r["bim"][:], in_=WCim_raw[:, csl]), w=["bim"])
                        V(lambda e: e.tensor_copy(out=WC[:, 0, csl], in_=r["bre"][:]), ["bre"], ["WC"])
                        V(lambda e: e.tensor_scalar_mul(out=WC[:, 1, csl], in0=r["bim"][:], scalar1=-1.0), ["bim"], ["WC"])

                        P.barrier()
                with contextlib.ExitStack() as s2:
                    pa = sb(s2, "pa", [128, 16])
                    pb = sb(s2, "pb", [128, 16])
                    pc = sb(s2, "pc", [128, 16])
                    DS(lambda e: e.dma_start(out=pa[:], in_=are_pp), w=["pa"])
                    DS(lambda e: e.dma_start(out=pb[:], in_=aim_pp), w=["pb"])
                    DS(lambda e: e.dma_start(out=pc[:], in_=ls_pp), w=["pc"])
                    A(lambda e: e.activation(out=pc[:], in_=pc[:], func=AF.Exp), ["pc"], ["pc"])
                    V(lambda e: e.tensor_tensor(out=rho_pp[:], in0=pa[:], in1=pc[:], op=ALU.mult), ["pa", "pc"], ["rho_pp"])
                    A(lambda e: e.activation(out=rho_pp[:], in_=rho_pp[:], func=AF.Exp), ["rho_pp"], ["rho_pp"])
                    V(lambda e: e.tensor_tensor(out=f_pp[:], in0=pb[:], in1=pc[:], op=ALU.mult), ["pb", "pc"], ["f_pp"])
                    V(lambda e: e.tensor_scalar_mul(out=f_pp[:], in0=f_pp[:], scalar1=1.0 / (2 * math.pi)), ["f_pp"], ["f_pp"])
                    DS(lambda e: e.dma_start(out=dsc[:], in_=dpp), w=["dsc"])
                    DS(lambda e: e.dma_start(out=bglu[:], in_=b_glu_pp), w=["bglu"])
                    gst_ = sb(s2, "gst_", [128, 4, 512])
                    DS(lambda e: e.dma_start(out=gst_[:], in_=w_glu_pad.rearrange("(kt p) f -> p kt f", p=128)), w=["gst_"])
                    V(lambda e: e.tensor_copy(out=wglu[:], in_=gst_[:]), ["gst_"], ["wglu"])
                    P.barrier()

                L = SEG
                H = 512
                arg1 = sb(st, "arg", [128, L])
                arg = [arg1, arg1]
                rnd = sb(st, "rnd", [128, L])
                argi = [rnd, rnd]
                magic = sb(st, "magic", [128, 2])
                V(lambda e: e.memset(magic[:, 0:1], 12582912.0), w=["magic"])
                V(lambda e: e.memset(magic[:, 1:2], -12582912.0), w=["magic"])
                yv1 = sb(st, "yv", [128, L])
                yv = [yv1, yv1]
                s2 = arg
                sq = arg
                cs = [sb(st, "cs%d" % i, [128, L], BF16) for i in range(2)]
                sn = [sb(st, "sn%d" % i, [128, L], BF16) for i in range(2)]
                bre = [sb(st, "bre%d" % i, [128, L], BF16) for i in range(2)]
                bim = [sb(st, "bim%d" % i, [128, L], BF16) for i in range(2)]
                dre = sb(st, "dre", [128, L], BF16)
                dim_ = sb(st, "dim", [128, L], BF16)
                tA = sb(st, "tA", [128, L], BF16)
                tB = sb(st, "tB", [128, L], BF16)
                zre = sb(st, "zre", [128, L], BF16)
                zim = sb(st, "zim", [128, L], BF16)
                Sre1 = sb(st, "Sre", [128, L], BF16)
                Sim1 = sb(st, "Sim", [128, L], BF16)
                Sre = [Sre1, Sre1]
                Sim = [Sim1, Sim1]
                zst = sb(st, "zst", [128, 16, 2])
                ysb1 = sb(st, "ysb", [128, L])
                ysb = [ysb1, ysb1]
                yfl1 = sb(st, "yfl", [128, L])
                yfl = [yfl1, yfl1]
                gg = sb(st, "gg", [128, 4, L], BF16)
                sg1 = sb(st, "sg", [128, L])
                sg = [sg1, sg1]
                s5o1 = sb(st, "s5o", [128, L], BF16)
                s5o = [s5o1, s5o1]
                pB = [[ps(st, "pB%d%d" % (h_, ri), [128, H]) for ri in range(2)] for h_ in range(2)]
                pY = ps(st, "pY", [128, L])
                pG = ps(st, "pG", [128, L])
                V(lambda e: e.memset(zst[:], 0.0), w=["zst"])
                colctr = [0]

                plan = []
                ulo = []
                for gp in range(8):
                    plan.append((0, gp, CTX, 0)); ulo.append(0)
                for sgi in range(SEQ // L):
                    for kt in range(4):
                        for g2 in range(2):
                            plan.append((0, kt * 2 + g2, L, CTX + sgi * L)); ulo.append(CTX + sgi * L)
                for gp in range(8):
                    plan.append((1, gp, CTX, 0)); ulo.append(0)
                for sb_i in range(SEQ // L):
                    for kt in range(4):
                        for g2 in range(2):
                            plan.append((1, kt * 2 + g2, L, CTX + sb_i * L)); ulo.append(CTX + (SEQ // L - 1 - sb_i) * L)

                def s5_bu(idx):
                    dr, gp, n, t0 = plan[idx]
                    u_lo = ulo[idx]
                    ci_ = dr * 8 + gp
                    kt = gp // 2
                    r0 = 64 * (gp % 2)
                    wcol = slice(ci_ * 128, (ci_ + 1) * 128)
                    cb = idx % 2
                    for hi_, c0 in enumerate(range(0, n, H)):
                        c1 = min(n, c0 + H)
                        for ri, dst, dk in ((0, bre[cb], "bre%d" % cb), (1, bim[cb], "bim%d" % cb)):
                            pb_ = pB[hi_][ri]
                            pk = "pB%d%d" % (hi_, ri)
                            T(lambda e, ri=ri, pb_=pb_, c0=c0, c1=c1: e.matmul(pb_[:, 0:c1 - c0], lhsT=WB[r0:r0 + 64, ri, wcol],
                                                                             rhs=U[r0:r0 + 64, kt, u_lo + c0:u_lo + c1],
                                                                             start=True, stop=True), ["WB", "U"], [pk])
                            A(lambda e, pb_=pb_, dst=dst, c0=c0, c1=c1: e.activation(out=dst[:, c0:c1], in_=pb_[:, 0:c1 - c0], func=AF.Identity),
                              [pk], [dk])

                def s5_tables(idx):
                    dr, gp, n, t0 = plan[idx]
                    ci_ = dr * 8 + gp
                    cb = idx % 2
                    arg_, argi_, yv_, s2_, sq_, cs_, sn_ = arg[cb], argi[cb], yv[cb], s2[cb], sq[cb], cs[cb], sn[cb]
                    k = lambda nm: nm if nm in ("arg", "yv", "Sre", "Sim") else "%s%d" % (nm, cb)
                    G(lambda e: e.tensor_scalar(out=arg_[:, 0:n], in0=iota_r[:, 0:n], scalar1=float(t0), scalar2=f_pp[:, ci_:ci_ + 1],
                                                op0=ALU.add, op1=ALU.mult), ["iota_r", "f_pp"], [k("arg")])
                    A(lambda e: e.activation(out=rnd[:, 0:n], in_=arg_[:, 0:n], func=AF.Identity, bias=magic[:, 0:1]), [k("arg"), "magic"], ["rnd"])
                    A(lambda e: e.activation(out=rnd[:, 0:n], in_=rnd[:, 0:n], func=AF.Identity, bias=magic[:, 1:2]), ["rnd", "magic"], ["rnd"])
                    G(lambda e: e.tensor_tensor(out=yv_[:, 0:n], in0=arg_[:, 0:n], in1=rnd[:, 0:n], op=ALU.subtract),
                      [k("arg"), "rnd"], [k("yv")])
                    A(lambda e: e.activation(out=sn_[:, 0:n], in_=yv_[:, 0:n], func=AF.Sin, scale=TWO_PI), [k("yv")], [k("sn")])
                    A(lambda e: e.activation(out=s2_[:, 0:n], in_=yv_[:, 0:n], func=AF.Sin, scale=TWO_PI / 2), [k("yv")], [k("arg")])
                    A(lambda e: e.activation(out=sq_[:, 0:n], in_=s2_[:, 0:n], func=AF.Square), [k("arg")], [k("arg")])
                    G(lambda e: e.tensor_scalar(out=cs_[:, 0:n], in0=sq_[:, 0:n], scalar1=-2.0, scalar2=1.0, op0=ALU.mult, op1=ALU.add),
                      [k("arg")], [k("cs")])

                def s5_col(dr, gp, u_lo, n, t0, rev, readout):
                    idx = colctr[0]
                    colctr[0] += 1
                    assert plan[idx] == (dr, gp, n, t0), (plan[idx], dr, gp, n, t0)
                    if idx == 0:
                        s5_tables(0)
                        s5_bu(0)
                    ci_ = dr * 8 + gp
                    kt = gp // 2
                    r0 = 64 * (gp % 2)
                    wcol = slice(ci_ * 128, (ci_ + 1) * 128)
                    cb = idx % 2
                    cs_, sn_ = cs[cb], sn[cb]
                    bre_, bim_, Sre_, Sim_ = bre[cb], bim[cb], Sre[cb], Sim[cb]
                    k = lambda nm: nm if nm in ("arg", "yv", "Sre", "Sim") else "%s%d" % (nm, cb)
                    assert ulo[idx] == u_lo
                    if idx + 1 < len(plan):
                        s5_tables(idx + 1)
                        s5_bu(idx + 1)

                    def tvn(t_):
                        if not rev:
                            return t_[:, 0:n]
                        return t_[:, 0:n][:, ::-1]

                    def vtt(o, ok, a_, ak, b_, bk, op):
                        V(lambda e: e.tensor_tensor(out=o, in0=a_, in1=b_, op=op), [ak, bk], [ok])

                    vtt(tA[:, 0:n], "tA", bre_[:, 0:n], k("bre"), tvn(cs_), k("cs"), ALU.mult)
                    vtt(tB[:, 0:n], "tB", bim_[:, 0:n], k("bim"), tvn(sn_), k("sn"), ALU.mult)
                    vtt(dre[:, 0:n], "dre", tA[:, 0:n], "tA", tB[:, 0:n], "tB", ALU.add)
                    vtt(tA[:, 0:n], "tA", bim_[:, 0:n], k("bim"), tvn(cs_), k("cs"), ALU.mult)
                    vtt(tB[:, 0:n], "tB", bre_[:, 0:n], k("bre"), tvn(sn_), k("sn"), ALU.mult)
                    vtt(dim_[:, 0:n], "dim", tA[:, 0:n], "tA", tB[:, 0:n], "tB", ALU.subtract)
                    for (src, dst, ri) in ((dre, zre, 0), (dim_, zim, 1)):
                        V(lambda e, src=src, dst=dst, ri=ri: e.tensor_tensor_scan(
                            out=tvn(dst), data0=bc(rho_pp[:, ci_:ci_ + 1], [128, n]), data1=tvn(src),
                            initial=zst[:, ci_, ri:ri + 1], op0=ALU.mult, op1=ALU.add),
                          ["dre" if ri == 0 else "dim", "rho_pp", "zst"], ["zre" if ri == 0 else "zim"])
                    last = 0 if rev else n - 1
                    V(lambda e: e.tensor_copy(out=zst[:, ci_, 0:1], in_=zre[:, last:last + 1]), ["zre"], ["zst"])
                    V(lambda e: e.tensor_copy(out=zst[:, ci_, 1:2], in_=zim[:, last:last + 1]), ["zim"], ["zst"])
                    if not readout:
                        return
                    vtt(tA[:, 0:n], "tA", zre[:, 0:n], "zre", tvn(cs_), k("cs"), ALU.mult)
                    vtt(tB[:, 0:n], "tB", zim[:, 0:n], "zim", tvn(sn_), k("sn"), ALU.mult)
                    vtt(Sre_[:, 0:n], k("Sre"), tA[:, 0:n], "tA", tB[:, 0:n], "tB", ALU.subtract)
                    vtt(tA[:, 0:n], "tA", zre[:, 0:n], "zre", tvn(sn_), k("sn"), ALU.mult)
                    vtt(tB[:, 0:n], "tB", zim[:, 0:n], "zim", tvn(cs_), k("cs"), ALU.mult)
                    vtt(Sim_[:, 0:n], k("Sim"), tA[:, 0:n], "tA", tB[:, 0:n], "tB", ALU.add)
                    first = (gp % 2 == 0)
                    for c0 in range(0, n, 512):
                        for ri, S_, sk in ((0, Sre_, k("Sre")), (1, Sim_, k("Sim"))):
                            T(lambda e, ri=ri, S_=S_, c0=c0: e.matmul(pY[:, c0:c0 + 512], lhsT=WC[:, ri, wcol], rhs=S_[:, c0:c0 + 512],
                                                                     start=(first and ri == 0), stop=((not first) and ri == 1)),
                              ["WC", sk], ["pY"])

                for gp in range(8):
                    s5_col(0, gp, 0, CTX, 0, False, False)
                cnt_ = 0
                for sgi in range(SEQ // L):
                    for kt in range(4):
                        for g2 in range(2):
                            s5_col(0, kt * 2 + g2, CTX + sgi * L, L, CTX + sgi * L, False, True)
                        yb = ysb[cnt_ % 2]
                        yk = "ysb"
                        cnt_ += 1
                        A(lambda e, yb=yb: e.activation(out=yb[:], in_=pY[:], func=AF.Identity), ["pY"], [yk])
                        DS(lambda e, kt=kt, sgi=sgi, yb=yb: e.dma_start(out=yf_d[kt, :, sgi * L:(sgi + 1) * L], in_=yb[:]), [yk], ["yf"])
                for gp in range(8):
                    s5_col(1, gp, 0, CTX, 0, True, False)
                for sb_i in range(SEQ // L):
                    sgi = SEQ // L - 1 - sb_i
                    for kt in range(4):
                        yl = yfl[cnt_ % 2]
                        ylk = "yfl"
                        yb = ysb[cnt_ % 2]
                        yk = "ysb"
                        cnt_ += 1
                        DS(lambda e, kt=kt, sgi=sgi, yl=yl: e.dma_start(out=yl[:], in_=yf_d[kt, :, sgi * L:(sgi + 1) * L]), ["yf"], [ylk])
                        for g2 in range(2):
                            s5_col(1, kt * 2 + g2, CTX + sgi * L, L, CTX + sb_i * L, True, True)
                        V(lambda e, yb=yb, yl=yl: e.tensor_tensor(out=yb[:], in0=pY[:], in1=yl[:], op=ALU.add), ["pY", ylk], [yk])
                        V(lambda e, kt=kt, sgi=sgi, yb=yb: e.scalar_tensor_tensor(out=yb[:], in0=U[:, kt, CTX + sgi * L:CTX + (sgi + 1) * L],
                                                                                  scalar=dsc[:, kt:kt + 1], in1=yb[:],
                                                                                  op0=ALU.mult, op1=ALU.add), ["U", "dsc", yk], [yk])
                        A(lambda e, kt=kt, yb=yb: e.activation(out=gg[:, kt, :], in_=yb[:], func=AF.Gelu_apprx_tanh), [yk], ["gg"])
                    for mt in range(4):
                        sg_ = sg[mt % 2]
                        sgk = "sg"
                        so_ = s5o[mt % 2]
                        sok = "s5o"
                        for c0 in range(0, L, 512):
                            for kt in range(4):
                                T(lambda e, mt=mt, c0=c0, kt=kt: e.matmul(pG[:, c0:c0 + 512], lhsT=wglu[:, kt, mt * 128:(mt + 1) * 128],
                                                                         rhs=gg[:, kt, c0:c0 + 512], start=(kt == 0), stop=(kt == 3)),
                                  ["wglu", "gg"], ["pG"])
                        A(lambda e, mt=mt, sg_=sg_: e.activation(out=sg_[:], in_=pG[:], func=AF.Sigmoid, bias=bglu[:, mt:mt + 1]), ["pG", "bglu"], [sgk])
                        G(lambda e, mt=mt, sg_=sg_, so_=so_: e.tensor_tensor(out=so_[:], in0=sg_[:], in1=gg[:, mt, :], op=ALU.mult), [sgk, "gg"], [sok])
                        DS(lambda e, mt=mt, sgi=sgi, so_=so_: e.dma_start(out=catT_d[mt, :, sgi * L:(sgi + 1) * L], in_=so_[:]), [sok], ["catT_s5"])
                P.barrier()
        P.barrier()

        with contextlib.ExitStack() as st:
            wo = sb(st, "wo", [128, 10, D], BF16)
            wr = sb(st, "wr", [128, 8, 16], BF16)
            with contextlib.ExitStack() as s2:
                stg = sb(s2, "stg3", [128, 10, D])
                DS(lambda e: e.dma_start(out=stg[:], in_=w_out_pad.rearrange("(kt p) f -> p kt f", p=128)), w=["stg3"])
                V(lambda e: e.tensor_copy(out=wo[:], in_=stg[:]), ["stg3"], ["wo"])
                DS(lambda e: e.dma_start(out=stg[:, 0:8, 0:16], in_=w_router.rearrange("(kt p) f -> p kt f", p=128)), w=["stg3"])
                V(lambda e: e.tensor_copy(out=wr[:], in_=stg[:, 0:8, 0:16]), ["stg3"], ["wr"])
                P.barrier()
            cat = [sb(st, "cat%d" % i, [128, 10, 128], BF16) for i in range(2)]
            xt3 = [sb(st, "x3_%d" % i, [128, D]) for i in range(2)]
            tmA = [sb(st, "tm%d" % i, [128, D]) for i in range(2)]
            xrA = [sb(st, "xr%d" % i, [128, D]) for i in range(2)]
            xnwA = [sb(st, "xnw%d" % i, [128, D]) for i in range(2)]
            hhA = [sb(st, "hh%d" % i, [128, D]) for i in range(2)]
            hbA = [sb(st, "hb%d" % i, [128, D], BF16) for i in range(2)]
            hTA = [sb(st, "hT%d" % i, [128, 8, 128], BF16) for i in range(2)]
            statsA = [sb(st, "stats3_%d" % i, [128, 2, 6]) for i in range(4)]
            mvA = [sb(st, "mv3_%d" % i, [128, 2]) for i in range(4)]
            rstdA = [sb(st, "rstd3_%d" % i, [128, 1]) for i in range(4)]
            lgA = [sb(st, "lg%d" % i, [128, 16]) for i in range(2)]
            mxA = [sb(st, "mx%d" % i, [128, 1]) for i in range(2)]
            smA = [sb(st, "sm%d" % i, [128, 1]) for i in range(2)]
            pMxA = [ps(st, "pMx%d" % i, [128, D]) for i in range(2)]
            pT3A = [ps(st, "pT3%d" % i, [128, D], BF16) for i in range(2)]
            pLA = [ps(st, "pL%d" % i, [128, 16]) for i in range(2)]

            def lnorm(src, sk, dst, dk, si):
                stats, mv, rstd = statsA[si], mvA[si], rstdA[si]
                k1, k2, k3 = "stats3_%d" % si, "mv3_%d" % si, "rstd3_%d" % si
                for c in range(2):
                    V(lambda e, c=c: e.bn_stats(out=stats[:, c, :], in_=src[:, c * 512:(c + 1) * 512]), [sk], [k1])
                V(lambda e: e.bn_aggr(out=mv[:], in_=stats[:].rearrange("p a b -> p (a b)")), [k1], [k2])
                V(lambda e: e.tensor_scalar_add(out=rstd[:], in0=mv[:, 1:2], scalar1=EPS), [k2], [k3])
                A(lambda e: e.activation(out=rstd[:], in_=rstd[:], func=AF.Sqrt), [k3], [k3])
                V(lambda e: e.reciprocal(out=rstd[:], in_=rstd[:]), [k3], [k3])
                V(lambda e: e.tensor_scalar(out=dst[:], in0=src[:], scalar1=mv[:, 0:1], scalar2=rstd[:, 0:1],
                                            op0=ALU.subtract, op1=ALU.mult), [sk, k2, k3], [dk])

            def p3_loads(t):
                i2 = t % 2
                c_, ck = cat[i2], "cat%d" % i2
                x_, xk = xt3[i2], "x3_%d" % i2
                DS(lambda e: e.dma_start(out=c_[:], in_=catT_d[:, :, t * 128:(t + 1) * 128].rearrange("a p t -> p a t")), w=[ck])
                DS(lambda e: e.dma_start(out=x_[:], in_=xs[CTX + t * 128:CTX + (t + 1) * 128, :]), w=[xk])

            def p3_A(t):
                i2 = t % 2
                c_, ck = cat[i2], "cat%d" % i2
                x_, xk = xt3[i2], "x3_%d" % i2
                tm, tmk = tmA[i2], "tm%d" % i2
                xr, xrk = xrA[i2], "xr%d" % i2
                xnw, xnk = xnwA[i2], "xnw%d" % i2
                hh, hhk = hhA[i2], "hh%d" % i2
                hb, hbk = hbA[i2], "hb%d" % i2
                hT, hTk = hTA[i2], "hT%d" % i2
                lg, lgk = lgA[i2], "lg%d" % i2
                mx, mxk = mxA[i2], "mx%d" % i2
                sm, smk = smA[i2], "sm%d" % i2
                pMx, pMk = pMxA[i2], "pMx%d" % i2
                pT3, pTk = pT3A[i2], "pT3%d" % i2
                pL, pLk = pLA[i2], "pL%d" % i2
                for nh in range(2):
                    for kt in range(10):
                        T(lambda e, nh=nh, kt=kt, c_=c_, pMx=pMx: e.matmul(pMx[:, nh * 512:(nh + 1) * 512], lhsT=c_[:, kt, :],
                                                                           rhs=wo[:, kt, nh * 512:(nh + 1) * 512], start=(kt == 0), stop=(kt == 9)),
                          [ck, "wo"], [pMk])

            def p3_B1(t):
                i2 = t % 2
                c_, ck = cat[i2], "cat%d" % i2
                x_, xk = xt3[i2], "x3_%d" % i2
                tm, tmk = tmA[i2], "tm%d" % i2
                xr, xrk = xrA[i2], "xr%d" % i2
                xnw, xnk = xnwA[i2], "xnw%d" % i2
                hh, hhk = hhA[i2], "hh%d" % i2
                hb, hbk = hbA[i2], "hb%d" % i2
                hT, hTk = hTA[i2], "hT%d" % i2
                lg, lgk = lgA[i2], "lg%d" % i2
                mx, mxk = mxA[i2], "mx%d" % i2
                sm, smk = smA[i2], "sm%d" % i2
                pMx, pMk = pMxA[i2], "pMx%d" % i2
                pT3, pTk = pT3A[i2], "pT3%d" % i2
                pL, pLk = pLA[i2], "pL%d" % i2
                V(lambda e, tm=tm, pMx=pMx: e.tensor_tensor(out=tm[:], in0=pMx[:], in1=modrow[:, 0, :], op=ALU.mult), [pMk, "modrow"], [tmk])
                V(lambda e, x_=x_, xr=xr, tm=tm: e.scalar_tensor_tensor(out=xr[:], in0=x_[:], scalar=ALPHA, in1=tm[:], op0=ALU.mult, op1=ALU.add),
                  [xk, tmk], [xrk])
                lnorm(xr, xrk, tm, tmk, 2 * i2)
                G(lambda e, tm=tm: e.tensor_tensor(out=tm[:], in0=tm[:], in1=lnrow[:, 0, :], op=ALU.mult), [tmk, "lnrow"], [tmk])
                G(lambda e, tm=tm, xnw=xnw: e.tensor_tensor(out=xnw[:], in0=tm[:], in1=lnrow[:, 1, :], op=ALU.add), [tmk, "lnrow"], [xnk])
                DS(lambda e, t=t, xnw=xnw: e.dma_start(out=xnew_d[t * 128:(t + 1) * 128, :], in_=xnw[:]), [xnk], ["xnew_d"])

            def p3_B2(t):
                i2 = t % 2
                c_, ck = cat[i2], "cat%d" % i2
                x_, xk = xt3[i2], "x3_%d" % i2
                tm, tmk = tmA[i2], "tm%d" % i2
                xr, xrk = xrA[i2], "xr%d" % i2
                xnw, xnk = xnwA[i2], "xnw%d" % i2
                hh, hhk = hhA[i2], "hh%d" % i2
                hb, hbk = hbA[i2], "hb%d" % i2
                hT, hTk = hTA[i2], "hT%d" % i2
                lg, lgk = lgA[i2], "lg%d" % i2
                mx, mxk = mxA[i2], "mx%d" % i2
                sm, smk = smA[i2], "sm%d" % i2
                pMx, pMk = pMxA[i2], "pMx%d" % i2
                pT3, pTk = pT3A[i2], "pT3%d" % i2
                pL, pLk = pLA[i2], "pL%d" % i2
                lnorm(xnw, xnk, hh, hhk, 2 * i2 + 1)
                V(lambda e, hh=hh: e.tensor_tensor(out=hh[:], in0=hh[:], in1=modrow[:, 2, :], op=ALU.mult), [hhk, "modrow"], [hhk])
                V(lambda e, hh=hh, hb=hb: e.tensor_tensor(out=hb[:], in0=hh[:], in1=modrow[:, 1, :], op=ALU.add), [hhk, "modrow"], [hbk])
                DS(lambda e, t=t, hb=hb: e.dma_start(out=hrow_d[t * 128:(t + 1) * 128, :], in_=hb[:]), [hbk], ["hrow_d"])
                for kt in range(8):
                    T(lambda e, kt=kt, hb=hb, pT3=pT3: e.transpose(out=pT3[:, kt * 128:(kt + 1) * 128], in_=hb[:, kt * 128:(kt + 1) * 128],
                                                                   identity=ident_bf[:]), [hbk, "ident_bf"], [pTk])
                A(lambda e, hT=hT, pT3=pT3: e.activation(out=hT[:].rearrange("p a b -> p (a b)"), in_=pT3[:], func=AF.Identity), [pTk], [hTk])
                for kt in range(8):
                    T(lambda e, kt=kt, hT=hT, pL=pL: e.matmul(pL[:], lhsT=hT[:, kt, :], rhs=wr[:, kt, :], start=(kt == 0), stop=(kt == 7)),
                      [hTk, "wr"], [pLk])
                V(lambda e, mx=mx, pL=pL: e.tensor_reduce(out=mx[:], in_=pL[:], axis=AX.X, op=ALU.max), [pLk], [mxk])
                V(lambda e, mx=mx: e.tensor_scalar_mul(out=mx[:], in0=mx[:], scalar1=-1.0), [mxk], [mxk])
                A(lambda e, lg=lg, pL=pL, mx=mx: e.activation(out=lg[:], in_=pL[:], func=AF.Exp, bias=mx[:, 0:1]), [pLk, mxk], [lgk])
                V(lambda e, sm=sm, lg=lg: e.tensor_reduce(out=sm[:], in_=lg[:], axis=AX.X, op=ALU.add), [lgk], [smk])
                V(lambda e, sm=sm: e.reciprocal(out=sm[:], in_=sm[:]), [smk], [smk])
                V(lambda e, t=t, lg=lg, sm=sm: e.tensor_scalar_mul(out=aff[:, t, :], in0=lg[:], scalar1=sm[:, 0:1]), [lgk, smk], ["aff%d" % t])
                DS(lambda e, t=t: e.dma_start(out=aff_d[t * 128:(t + 1) * 128, :], in_=aff[:, t, :]), ["aff%d" % t], ["aff_d"])

            p3_loads(0)
            p3_loads(1)
            p3_A(0)
            emit_zip(record(p3_A, 1), record(p3_B1, 0))
            for t in range(NT):
                if t + 2 < NT:
                    p3_loads(t + 2)
                la = record(p3_A, t + 2) if t + 2 < NT else []
                lb1 = record(p3_B1, t + 1) if t + 1 < NT else []
                lb2 = record(p3_B2, t)
                emit_zip(la, lb1, lb2)
            P.barrier()

        if debug == 1:
            early.close()
            return nc

        with contextlib.ExitStack() as st:
            hs = sb(st, "hs", [128, 16])
            am = sb(st, "am", [128, NT, NE])
            lo = sb(st, "lo", [128, NE])
            hi = sb(st, "hi", [128, NE])
            mid = sb(st, "mid", [128, NE])
            cmp_ = sb(st, "cmp", [128, NT, NE])
            cnt = sb(st, "cnt", [128, NE])
            cntb = sb(st, "cntb", [128, NE], BF16)
            ge = sb(st, "ge", [128, NE])
            dl = sb(st, "dl", [128, NE])
            pC = ps(st, "pC", [128, NE])
            DS(lambda e: e.dma_start(out=hs[:], in_=half_sel), w=["hs"])
            V(lambda e: e.tensor_tensor(out=am[:], in0=aff[:, :, 0:8], in1=bc(hs[:, 0:8].unsqueeze(1), [128, NT, NE]), op=ALU.mult),
              ["aff", "hs"], ["am"])
            V(lambda e: e.tensor_tensor(out=cmp_[:], in0=aff[:, :, 8:16], in1=bc(hs[:, 8:16].unsqueeze(1), [128, NT, NE]), op=ALU.mult),
              ["aff", "hs"], ["cmp"])
            V(lambda e: e.tensor_tensor(out=am[:], in0=am[:], in1=cmp_[:], op=ALU.add), ["am", "cmp"], ["am"])
            V(lambda e: e.memset(lo[:], 0.0), w=["lo"])
            V(lambda e: e.memset(hi[:], 1.0), w=["hi"])
            for it in range(30):
                V(lambda e: e.tensor_tensor(out=mid[:], in0=lo[:], in1=hi[:], op=ALU.add), ["lo", "hi"], ["mid"])
                V(lambda e: e.tensor_scalar_mul(out=mid[:], in0=mid[:], scalar1=0.5), ["mid"], ["mid"])
                V(lambda e: e.tensor_tensor(out=cmp_[:], in0=am[:], in1=bc(mid[:].unsqueeze(1), [128, NT, NE]), op=ALU.is_ge),
                  ["am", "mid"], ["cmp"])
                V(lambda e: e.tensor_reduce(out=cnt[:], in_=cmp_[:].rearrange("p t e -> p e t"), axis=AX.X, op=ALU.add), ["cmp"], ["cnt"])
                V(lambda e: e.tensor_copy(out=cntb[:], in_=cnt[:]), ["cnt"], ["cntb"])
                T(lambda e: e.matmul(pC[:], lhsT=ones_bf[:], rhs=cntb[:], start=True, stop=True), ["ones", "cntb"], ["pC"])
                V(lambda e: e.tensor_single_scalar(out=ge[:], in_=pC[:], scalar=float(CAP), op=ALU.is_ge), ["pC"], ["ge"])
                V(lambda e: e.tensor_tensor(out=dl[:], in0=mid[:], in1=lo[:], op=ALU.subtract), ["mid", "lo"], ["dl"])
                V(lambda e: e.tensor_tensor(out=dl[:], in0=dl[:], in1=ge[:], op=ALU.mult), ["dl", "ge"], ["dl"])
                V(lambda e: e.tensor_tensor(out=lo[:], in0=lo[:], in1=dl[:], op=ALU.add), ["lo", "dl"], ["lo"])
                V(lambda e: e.tensor_tensor(out=dl[:], in0=hi[:], in1=mid[:], op=ALU.subtract), ["hi", "mid"], ["dl"])
                V(lambda e: e.tensor_tensor(out=dl[:], in0=dl[:], in1=ge[:], op=ALU.mult), ["dl", "ge"], ["dl"])
                V(lambda e: e.tensor_tensor(out=hi[:], in0=mid[:], in1=dl[:], op=ALU.add), ["mid", "dl"], ["hi"])
            mk = sb(st, "mk", [128, NE, NT], BF16)
            cin = sb(st, "cin", [128, NE * NT])
            tot = sb(st, "tot", [128, NE, NT])
            cend = sb(st, "cend", [128, NE, NT])
            pP = ps(st, "pP", [128, NE * NT])
            pTt = ps(st, "pTt", [128, NE * NT])
            V(lambda e: e.tensor_tensor(out=mk[:], in0=am[:].rearrange("p t e -> p e t"), in1=bc(lo[:].unsqueeze(2), [128, NE, NT]),
                                        op=ALU.is_ge), ["am", "lo"], ["mk"])
            T(lambda e: e.matmul(pP[:], lhsT=tri_bf[:], rhs=mk[:].rearrange("p e t -> p (e t)"), start=True, stop=True), ["tri", "mk"], ["pP"])
            T(lambda e: e.matmul(pTt[:], lhsT=ones_bf[:], rhs=mk[:].rearrange("p e t -> p (e t)"), start=True, stop=True), ["ones", "mk"], ["pTt"])
            V(lambda e: e.tensor_copy(out=cin[:], in_=pP[:]), ["pP"], ["cin"])
            V(lambda e: e.tensor_copy(out=tot[:].rearrange("p e t -> p (e t)"), in_=pTt[:]), ["pTt"], ["tot"])
            V(lambda e: e.tensor_copy(out=cend[:], in_=tot[:]), ["tot"], ["cend"])
            sh = 1
            tmpc = sb(st, "tmpc", [128, NE, NT])
            while sh < NT:
                V(lambda e: e.tensor_copy(out=tmpc[:], in_=cend[:]), ["cend"], ["tmpc"])
                V(lambda e, sh=sh: e.tensor_tensor(out=cend[:, :, sh:NT], in0=tmpc[:, :, sh:NT], in1=tmpc[:, :, 0:NT - sh], op=ALU.add),
                  ["tmpc"], ["cend"])
                sh *= 2
            cinT = sb(st, "cinT", [128, 4, 128])
            pX = ps(st, "pX", [128, 4, 128])
            for a in range(4):
                T(lambda e, a=a: e.transpose(out=pX[:, a, :], in_=cin[:, a * 128:(a + 1) * 128], identity=ident_f[:]), ["cin", "ident_f"], ["pX"])
            V(lambda e: e.tensor_copy(out=cinT[:], in_=pX[:]), ["pX"], ["cinT"])
            DS(lambda e: e.dma_start(out=cin_d.rearrange("(a p) t -> p a t", p=128), in_=cinT[:]), ["cinT"], ["cin_d"])
            spp = sb(st, "spp", [128, 8])
            DS(lambda e: e.dma_start(out=spp[:], in_=slot_pp_d), w=["spp"])
            le = sb(st, "le", [128, NE, 8, NT])
            tl_ = sb(st, "tl_", [128, NE, 8])
            cst = sb(st, "cst", [128, NE, 8])
            rr = sb(st, "rr", [128, NE, 8])
            rowi = sb(st, "rowi", [128, NE, 8], I32)
            rowf = sb(st, "rowf", [128, NE, 8])
            for e_ in range(NE):
                V(lambda e, e_=e_: e.tensor_tensor(out=le[:, e_, :, :], in0=bc(cend[:, e_, :].unsqueeze(1), [128, 8, NT]),
                                                   in1=bc(spp[:].unsqueeze(2), [128, 8, NT]), op=ALU.is_le), ["cend", "spp"], ["le"])
            V(lambda e: e.tensor_reduce(out=tl_[:].rearrange("p e j -> p (e j)"), in_=le[:].rearrange("p e j t -> p (e j) t"),
                                        axis=AX.X, op=ALU.add), ["le"], ["tl_"])
            for e_ in range(NE):
                V(lambda e, e_=e_: e.tensor_tensor(out=le[:, e_, :, :], in0=le[:, e_, :, :], in1=bc(tot[:, e_, :].unsqueeze(1), [128, 8, NT]),
                                                   op=ALU.mult), ["le", "tot"], ["le"])
            V(lambda e: e.tensor_reduce(out=cst[:].rearrange("p e j -> p (e j)"), in_=le[:].rearrange("p e j t -> p (e j) t"),
                                        axis=AX.X, op=ALU.add), ["le"], ["cst"])
            V(lambda e: e.tensor_scalar_min(out=tl_[:], in0=tl_[:], scalar1=float(NT - 1)), ["tl_"], ["tl_"])
            V(lambda e: e.tensor_tensor(out=rr[:], in0=bc(spp[:].unsqueeze(1), [128, NE, 8]), in1=cst[:], op=ALU.subtract), ["spp", "cst"], ["rr"])
            for e_ in range(NE):
                V(lambda e, e_=e_: e.tensor_scalar_add(out=rowf[:, e_, :], in0=tl_[:, e_, :], scalar1=float(e_ * NT)), ["tl_"], ["rowf"])
            V(lambda e: e.tensor_copy(out=rowi[:], in_=rowf[:]), ["rowf"], ["rowi"])
            crowA = [sb(st, "crow%d" % i, [128, 128]) for i in range(4)]
            cleA = [sb(st, "cle%d" % i, [128, 128]) for i in range(4)]
            cle = cleA[0]
            tloc = sb(st, "tloc", [128, NE, 8])
            arowA = [sb(st, "arow%d" % i, [128, 16]) for i in range(4)]
            cl2A = [sb(st, "cl2%d" % i, [128, 16]) for i in range(4)]
            idf = sb(st, "idf", [128, NE, 8])
            V(lambda e: e.memset(tloc[:], 0.0), w=["tloc"])
            for e_ in range(NE):
                for j in range(8):
                    q4 = (e_ * 8 + j) % 4
                    crow, crk = crowA[q4], "crow%d" % q4
                    cle_, clk = cleA[q4], "cle%d" % q4
                    DG(lambda e, e_=e_, j=j, crow=crow: e.indirect_dma_start(out=crow[:], out_offset=None, in_=cin_d,
                                                                            in_offset=bass.IndirectOffsetOnAxis(ap=rowi[:, e_, j:j + 1], axis=0)),
                       ["cin_d", "rowi"], [crk])
                    V(lambda e, e_=e_, j=j, crow=crow, cle_=cle_: e.tensor_scalar(out=cle_[:], in0=crow[:], scalar1=rr[:, e_, j:j + 1], scalar2=0.0,
                                                                                  op0=ALU.is_le, op1=ALU.add, accum_out=tloc[:, e_, j:j + 1]),
                      [crk, "rr", "tloc"], [clk, "tloc%d_%d" % (e_, j)])
            V(lambda e: e.tensor_scalar_min(out=tloc[:], in0=tloc[:], scalar1=127.0),
              ["tloc"] + ["tloc%d_%d" % (a_, b_) for a_ in range(NE) for b_ in range(8)], ["tloc"])
            V(lambda e: e.scalar_tensor_tensor(out=idf[:], in0=tl_[:], scalar=128.0, in1=tloc[:], op0=ALU.mult, op1=ALU.add),
              ["tl_", "tloc"] + ["tloc%d_%d" % (a_, b_) for a_ in range(NE) for b_ in range(8)], ["idf"])
            V(lambda e: e.tensor_copy(out=idx_all[:], in_=idf[:]), ["idf"], ["idx_all"])
            for e_ in range(NE):
                for j in range(8):
                    q4 = (e_ * 8 + j) % 4
                    arow, ark = arowA[q4], "arow%d" % q4
                    cl2, c2k = cl2A[q4], "cl2%d" % q4
                    DG(lambda e, e_=e_, j=j, arow=arow: e.indirect_dma_start(out=arow[:], out_offset=None, in_=aff_d,
                                                                            in_offset=bass.IndirectOffsetOnAxis(ap=idx_all[:, e_, j:j + 1], axis=0)),
                       ["aff_d", "idx_all"], [ark])
                    V(lambda e, arow=arow, cl2=cl2: e.tensor_tensor(out=cl2[:], in0=arow[:], in1=hs[:], op=ALU.mult), [ark, "hs"], [c2k])
                    V(lambda e, e_=e_, j=j, cl2=cl2: e.tensor_tensor(out=gate_all[:, e_, j:j + 1], in0=cl2[:, e_:e_ + 1], in1=cl2[:, 8 + e_:9 + e_],
                                                                     op=ALU.add), [c2k], ["gate_all%d_%d" % (e_, j)])
            P.barrier()

        if debug:
            DS(lambda e: e.dma_start(out=dbg_idx, in_=idx_all[:].rearrange("p a b -> p (a b)")), ["idx_all"], ["dbg_idx"])
            DS(lambda e: e.dma_start(out=dbg_gate, in_=gate_all[:].rearrange("p a b -> p (a b)")), ["gate_all"], ["dbg_gate"])
            P.barrier()
        if debug == 2:
            early.close()
            return nc

        early.close()
        with contextlib.ExitStack() as st:
            zt = sb(st, "zt", [128, D])
            V(lambda e: e.memset(zt[:], 0.0), w=["zt"])
            for t in range(NT):
                DS(lambda e, t=t: e.dma_start(out=moe_d[t * 128:(t + 1) * 128, :], in_=zt[:]), ["zt"], ["moe_d"])
            P.barrier()
        FB = 256
        NFB = DFF // FB
        with contextlib.ExitStack() as st:
            xgA = [sb(st, "xg%d" % i, [128, D], BF16) for i in range(2)]
            xgT = sb(st, "xgT", [128, 8, CAP], BF16)
            wdn = sb(st, "wdn", [128, NFT, D], BF16)
            hid = sb(st, "hid", [128, NFT, CAP], BF16)
            stg = [sb(st, "wstg%d" % i, [128, 2, 8, FB]) for i in range(2)]
            wgbA = [sb(st, "wgb%d" % i, [128, 8, FB], BF16) for i in range(2)]
            wubA = [sb(st, "wub%d" % i, [128, 8, FB], BF16) for i in range(2)]
            stgdA = [sb(st, "stgd%d" % i, [128, D]) for i in range(2)]
            slA = [sb(st, "sl%d" % i, [128, 512]) for i in range(2)]
            obA = [sb(st, "ob%d" % i, [128, D]) for i in range(2)]
            pTg = ps(st, "pTg", [128, D], BF16)
            pGU = [[ps(st, "pGU%d%d" % (h_, m_), [128, 512]) for m_ in range(2)] for h_ in range(2)]
            pO = ps(st, "pO", [128, D])
            gcnt = 0
            ocnt = 0
            for e_ in range(NE):
                for j in range(8):
                    xg, xgk = xgA[gcnt % 2], "xg%d" % (gcnt % 2)
                    gcnt += 1
                    DG(lambda e, e_=e_, j=j, xg=xg: e.indirect_dma_start(out=xg[:], out_offset=None, in_=hrow_d,
                                                                        in_offset=bass.IndirectOffsetOnAxis(ap=idx_all[:, e_, j:j + 1], axis=0)),
                       ["hrow_d", "idx_all"], [xgk])
                    for kt in range(8):
                        T(lambda e, kt=kt, xg=xg: e.transpose(out=pTg[:, kt * 128:(kt + 1) * 128], in_=xg[:, kt * 128:(kt + 1) * 128],
                                                              identity=ident_bf[:]), [xgk, "ident_bf"], ["pTg"])
                    V(lambda e, j=j: e.tensor_copy(out=xgT[:, :, j * 128:(j + 1) * 128], in_=pTg[:].rearrange("p (a b) -> p a b", a=8)),
                      ["pTg"], ["xgT"])
                for fb in range(NFB):
                    s_, sk = stg[fb % 2], "wstg%d" % (fb % 2)
                    wgb, wgk = wgbA[fb % 2], "wgb%d" % (fb % 2)
                    wub, wuk = wubA[fb % 2], "wub%d" % (fb % 2)
                    DS(lambda e, e_=e_, fb=fb, s_=s_: e.dma_start(out=s_[:, 0, :, :],
                                                                  in_=wg[e_].rearrange("(kt p) f -> p kt f", p=128)[:, :, fb * FB:(fb + 1) * FB]),
                       w=[sk + "g"])
                    DS(lambda e, e_=e_, fb=fb, s_=s_: e.dma_start(out=s_[:, 1, :, :],
                                                                  in_=wu[e_].rearrange("(kt p) f -> p kt f", p=128)[:, :, fb * FB:(fb + 1) * FB]),
                       w=[sk + "u"])
                    A(lambda e, s_=s_, wgb=wgb: e.activation(out=wgb[:], in_=s_[:, 0, :, :], func=AF.Identity), [sk + "g"], [wgk])
                    G(lambda e, s_=s_, wub=wub: e.tensor_copy(out=wub[:], in_=s_[:, 1, :, :]), [sk + "u"], [wuk])
                    if fb < 2 * 0 + NFB:
                        for q_ in range(2):
                            ft = fb * 2 + q_
                            sd_, sdk = stgdA[ft % 2], "stgd%d" % (ft % 2)
                            DS(lambda e, e_=e_, ft=ft, sd_=sd_: e.dma_start(out=sd_[:], in_=wd[e_, ft * 128:(ft + 1) * 128, :]), w=[sdk])
                            G(lambda e, ft=ft, sd_=sd_: e.tensor_copy(out=wdn[:, ft, :], in_=sd_[:]), [sdk], ["wdn"])
                    for fl in range(FB // 128):
                        ft = fb * (FB // 128) + fl
                        for h_ in range(2):
                            c0 = h_ * 512
                            pg, pu = pGU[h_][0], pGU[h_][1]
                            pgk, puk = "pGU%d0" % h_, "pGU%d1" % h_
                            sl, slk = slA[h_], "sl%d" % h_
                            for kt in range(8):
                                T(lambda e, kt=kt, c0=c0, fl=fl, pg=pg, wgb=wgb: e.matmul(pg[:], lhsT=wgb[:, kt, fl * 128:(fl + 1) * 128],
                                                                                        rhs=xgT[:, kt, c0:c0 + 512], start=(kt == 0), stop=(kt == 7)),
                                  [wgk, "xgT"], [pgk])
                            for kt in range(8):
                                T(lambda e, kt=kt, c0=c0, fl=fl, pu=pu, wub=wub: e.matmul(pu[:], lhsT=wub[:, kt, fl * 128:(fl + 1) * 128],
                                                                                        rhs=xgT[:, kt, c0:c0 + 512], start=(kt == 0), stop=(kt == 7)),
                                  [wuk, "xgT"], [puk])
                            A(lambda e, sl=sl, pg=pg: e.activation(out=sl[:], in_=pg[:], func=AF.Silu), [pgk], [slk])
                            V(lambda e, ft=ft, c0=c0, sl=sl, pu=pu: e.tensor_tensor(out=hid[:, ft, c0:c0 + 512], in0=sl[:], in1=pu[:], op=ALU.mult),
                              [slk, puk], ["hid"])
                for j in range(8):
                    ob, obk = obA[ocnt % 2], "ob%d" % (ocnt % 2)
                    ocnt += 1
                    for nh in range(2):
                        for ft in range(NFT):
                            T(lambda e, j=j, nh=nh, ft=ft: e.matmul(pO[:, nh * 512:(nh + 1) * 512], lhsT=hid[:, ft, j * 128:(j + 1) * 128],
                                                                    rhs=wdn[:, ft, nh * 512:(nh + 1) * 512], start=(ft == 0), stop=(ft == NFT - 1)),
                              ["hid", "wdn"], ["pO"])
                    V(lambda e, e_=e_, j=j, ob=ob: e.tensor_scalar_mul(out=ob[:], in0=pO[:], scalar1=gate_all[:, e_, j:j + 1]), ["pO", "gate_all"], [obk])
                    DG(lambda e, e_=e_, j=j, ob=ob: e.indirect_dma_start(out=moe_d, out_offset=bass.IndirectOffsetOnAxis(ap=idx_all[:, e_, j:j + 1], axis=0),
                                                                        in_=ob[:], in_offset=None, compute_op=ALU.add, oob_is_err=True),
                       [obk, "idx_all", "moe_d"], ["moe_d"])
            P.barrier()

        if debug == 3:
            return nc

        ccsem = gst.enter_context(nc.semaphore("ccsem"))
        CCH = 16
        crow_ = SEQ // CCH
        for cc_ in range(CCH):
            nc.gpsimd.collective_compute("AllReduce", ALU.add, replica_groups=[[0, 1], [2, 3], [4, 5], [6, 7]],
                                         ins=[moe_d[cc_ * crow_:(cc_ + 1) * crow_, :]],
                                         outs=[moe_r[cc_ * crow_:(cc_ + 1) * crow_, :]]).then_inc(ccsem)
            nc.gpsimd.wait_ge(ccsem, cc_ + 1)
        G(lambda e: e.memset(gate_all[:, 0, 0:1], 0.0), w=["gate_all"])
        P.barrier()
        with contextlib.ExitStack() as st:
            mt_ = [sb(st, "m6_%d" % i, [128, D]) for i in range(2)]
            xq = [sb(st, "x6_%d" % i, [128, D]) for i in range(2)]
            tm = sb(st, "tm6", [128, D])
            xr = sb(st, "xr6", [128, D])
            oo = [sb(st, "o6_%d" % i, [128, D]) for i in range(2)]
            stats = sb(st, "stats6", [128, 2, 6])
            mv = sb(st, "mv6_", [128, 2])
            rstd = sb(st, "rstd6", [128, 1])
            tki = sb(st, "tki", [128, NT // 2], I32)
            DS(lambda e: e.dma_start(out=tki[:], in_=tokidx_d), w=["tki"])
            for t in range(NT // 2):
                m_ = mt_[t % 2]
                mk_ = "m6_%d" % (t % 2)
                x_ = xq[t % 2]
                xk = "x6_%d" % (t % 2)
                o_ = oo[t % 2]
                ok = "o6_%d" % (t % 2)
                DG(lambda e, t=t, m_=m_: e.indirect_dma_start(out=m_[:], out_offset=None, in_=moe_r,
                                                             in_offset=bass.IndirectOffsetOnAxis(ap=tki[:, t:t + 1], axis=0)),
                   ["moe_r", "tki"], [mk_])
                DG(lambda e, t=t, x_=x_: e.indirect_dma_start(out=x_[:], out_offset=None, in_=xnew_d,
                                                             in_offset=bass.IndirectOffsetOnAxis(ap=tki[:, t:t + 1], axis=0)),
                   ["xnew_d", "tki"], [xk])
                V(lambda e, m_=m_: e.tensor_tensor(out=tm[:], in0=m_[:], in1=fin3[:, 0, :], op=ALU.mult), [mk_, "fin3"], ["tm6"])
                V(lambda e, x_=x_: e.scalar_tensor_tensor(out=xr[:], in0=x_[:], scalar=ALPHA, in1=tm[:], op0=ALU.mult, op1=ALU.add),
                  [xk, "tm6"], ["xr6"])
                for c in range(2):
                    V(lambda e, c=c: e.bn_stats(out=stats[:, c, :], in_=xr[:, c * 512:(c + 1) * 512]), ["xr6"], ["stats6"])
                V(lambda e: e.bn_aggr(out=mv[:], in_=stats[:].rearrange("p a b -> p (a b)")), ["stats6"], ["mv6_"])
                V(lambda e: e.tensor_scalar_add(out=rstd[:], in0=mv[:, 1:2], scalar1=EPS), ["mv6_"], ["rstd6"])
                A(lambda e: e.activation(out=rstd[:], in_=rstd[:], func=AF.Sqrt), ["rstd6"], ["rstd6"])
                V(lambda e: e.reciprocal(out=rstd[:], in_=rstd[:]), ["rstd6"], ["rstd6"])
                V(lambda e: e.tensor_scalar(out=tm[:], in0=xr[:], scalar1=mv[:, 0:1], scalar2=rstd[:, 0:1],
                                            op0=ALU.subtract, op1=ALU.mult), ["xr6", "mv6_", "rstd6"], ["tm6"])
                V(lambda e: e.tensor_tensor(out=tm[:], in0=tm[:], in1=fin3[:, 1, :], op=ALU.mult), ["tm6", "fin3"], ["tm6"])
                V(lambda e, o_=o_: e.tensor_tensor(out=o_[:], in0=tm[:], in1=fin3[:, 2, :], op=ALU.add), ["tm6", "fin3"], [ok])
                DS(lambda e, t=t, o_=o_: e.dma_start(out=out_d[t * 128:(t + 1) * 128, :], in_=o_[:]), [ok], ["out_d"])
            P.barrier()
    return nc


def _prep(inputs):
    f32 = np.float32
    g = {k: np.asarray(v) for k, v in inputs.items()}
    bf = ml_dtypes.bfloat16
    com = {}
    com["w_ada"] = np.ascontiguousarray(g["w_ada"][0], f32)
    ba = g["b_ada"][0]
    com["bada_fm"] = np.ascontiguousarray(ba[:2048].reshape(16, 128).T, f32)
    com["bada_row"] = np.ascontiguousarray(ba[2048:].reshape(1, 4096), f32)
    w_in = g["w_in"][0]
    ws5 = np.zeros((D, 4, 4, 32), f32)
    ws5[:, :, :, :16] = w_in[:, :256].reshape(D, 4, 4, 16)
    com["w_in_s5"] = ws5.reshape(D, 512)
    com["w_in_u"] = np.ascontiguousarray(w_in[:, 256:1024], f32)
    com["w_in_v"] = np.ascontiguousarray(w_in[:, 1024:1792], f32)
    com["gm_wsT"] = np.ascontiguousarray(g["gm_ws"][0].transpose(2, 0, 1), f32)
    com["gm_bs_row"] = np.ascontiguousarray(g["gm_bs"][0].reshape(1, 768), f32)
    are = g["s5_a_re"][0]; aim = g["s5_a_im"][0]; ls = g["s5_log_step"][0]
    com["are_row"] = np.ascontiguousarray(are.reshape(1, 2048), f32)
    com["aim_row"] = np.ascontiguousarray(aim.reshape(1, 2048), f32)
    com["ls_row"] = np.ascontiguousarray(np.repeat(ls.reshape(32), 64).reshape(1, 2048), f32)

    def pp(a):
        return np.ascontiguousarray(a.reshape(2, 8, 2, 64).transpose(2, 3, 0, 1).reshape(128, 16), f32)
    com["are_pp"] = pp(are)
    com["aim_pp"] = pp(aim)
    com["ls_pp"] = pp(np.repeat(ls[:, :, None], 64, axis=2))
    for nm, src in (("WBre_raw", g["s5_b_re"][0]), ("WBim_raw", g["s5_b_im"][0])):
        w = np.zeros((128, 2, 8, 128), f32)
        for gp in range(8):
            for g2 in range(2):
                gi = 2 * gp + g2
                r0 = 64 * (gp % 2) + 32 * g2
                w[r0:r0 + 16, :, gp, 64 * g2:64 * g2 + 64] = src[:, gi].transpose(2, 0, 1)
        com[nm] = w.reshape(128, 2048)
    for nm, src in (("WCre_raw", g["s5_c_re"][0]), ("WCim_raw", g["s5_c_im"][0])):
        w = np.zeros((128, 2, 8, 128), f32)
        for gp in range(8):
            for g2 in range(2):
                gi = 2 * gp + g2
                c0 = 64 * (gp % 2) + 32 * g2
                w[64 * g2:64 * g2 + 64, :, gp, c0:c0 + 16] = src[:, gi].transpose(2, 0, 1)
        com[nm] = w.reshape(128, 2048)

    def padpp(v):
        o = np.zeros((4, 4, 32), f32)
        o[:, :, :16] = v.reshape(4, 4, 16)
        return np.ascontiguousarray(o.reshape(4, 128).T, f32)
    com["dpp"] = padpp(g["s5_d"][0])
    com["b_glu_pp"] = padpp(g["s5_b_glu"][0].reshape(16, 16))
    wgl = np.zeros((4, 4, 32, 4, 4, 32), f32)
    wgl[:, :, :16, :, :, :16] = g["s5_w_glu"][0].reshape(4, 4, 16, 4, 4, 16)
    com["w_glu_pad"] = wgl.reshape(512, 512)
    wo = np.zeros((1280, D), f32)
    wo5 = np.zeros((4, 4, 32, D), f32)
    wo5[:, :, :16, :] = g["w_out"][0][:256].reshape(4, 4, 16, D)
    wo[:512] = wo5.reshape(512, D)
    wo[512:] = g["w_out"][0][256:]
    com["w_out_pad"] = wo
    for k in ("ln1_g", "ln1_b", "ln2_g", "ln2_b"):
        com[k] = np.ascontiguousarray(g[k][0].reshape(1, D), f32)
    com["w_router"] = np.ascontiguousarray(g["w_router"][0], f32)
    com["ident_bf"] = np.eye(128, dtype=f32).astype(bf)
    com["ident_f"] = np.eye(128, dtype=f32)
    com["tri"] = np.triu(np.ones((128, 128), f32)).astype(bf)
    com["ones"] = np.ones((128, 128), f32).astype(bf)
    com["iota_row"] = np.arange(1024, dtype=f32).reshape(1, 1024)
    com["slot_pp"] = (np.arange(128, dtype=f32)[:, None] + 128.0 * np.arange(8, dtype=f32)[None, :]).astype(f32)
    maps = []
    for c in range(8):
        b, half = c // 2, c % 2
        m = dict(com)
        m["xs"] = np.ascontiguousarray(np.concatenate([g["ctx"][b], g["x"][b]], axis=0), f32)
        cv = np.stack([g["c"][b], g["c_ctx"]], axis=1)
        m["cT"] = np.ascontiguousarray(cv.reshape(8, 128, 2).transpose(1, 0, 2), f32)
        es = slice(8 * half, 8 * half + 8)
        m["wg"] = np.ascontiguousarray(g["moe_w_gate"][0][es], f32)
        m["wu"] = np.ascontiguousarray(g["moe_w_up"][0][es], f32)
        m["wd"] = np.ascontiguousarray(g["moe_w_down"][0][es], f32)
        hs = np.zeros((128, 16), f32)
        hs[:, es] = 1.0
        m["half_sel"] = hs
        m["tokidx"] = (half * 4096 + np.arange(32, dtype=np.int32)[None, :] * 128 + np.arange(128, dtype=np.int32)[:, None]).astype(np.int32)
        maps.append(m)
    return maps


def kernel(**inputs):
    maps = _prep(inputs)
    nc = build()
    res = run_bass_kernel_spmd(nc, maps, core_ids=list(range(8)))
    out = np.zeros((4, SEQ, D), np.float32)
    for c in range(8):
        b, half = c // 2, c % 2
        out[b, half * 4096:(half + 1) * 4096] = res.results[c]["out"]
    return out
```

```python
import contextlib
import math
import numpy as np
import ml_dtypes
import concourse.bass as bass
import concourse.mybir as mybir
from concourse.bass_utils import run_bass_kernel_spmd

F32 = mybir.dt.float32
BF16 = mybir.dt.bfloat16
I32 = mybir.dt.int32
AF = mybir.ActivationFunctionType
ALU = mybir.AluOpType
AX = mybir.AxisListType

D = 1024
SEQ = 8192
CTX = 256
NT = SEQ // 128
NTC = CTX // 128
TOT = SEQ + CTX
DFF = 2816
NFT = DFF // 128
NE = 8
CAP = 1024
ALPHA = 2.0 ** 0.25
EPS = 1e-6
TWO_PI = 6.283185
SEG = 1024

ENGS = ("sync", "scalar", "vector", "gpsimd", "tensor")
DMA_K = 8


class Prog:
    def __init__(self, nc, stack):
        self.nc = nc
        self.eng = {"sync": nc.sync, "scalar": nc.scalar, "vector": nc.vector,
                    "gpsimd": nc.gpsimd, "tensor": nc.tensor}
        self.sems = {}
        for e in ENGS:
            self.sems["c" + e] = stack.enter_context(nc.semaphore("c" + e))
        for e in ("sync", "scalar", "gpsimd"):
            for i in range(DMA_K):
                n = "d%s%d" % (e, i)
                self.sems[n] = stack.enter_context(nc.semaphore(n))
        self.ccnt = {e: 0 for e in ENGS}
        self.dcnt = {e: 0 for e in ENGS}
        self.lastw = {}
        self.readers = {}
        self.known = {e: {} for e in ENGS}
        self.latest = {}

    def _need(self, eng, tok):
        if tok is None:
            return
        name, val = tok
        if name == "c" + eng and eng in ("tensor", "sync"):
            return
        if self.known[eng].get(name, 0) >= val:
            return
        self.known[eng][name] = val
        self.eng[eng].wait_ge(self.sems[name], val)

    def op(self, eng, fn, reads=(), writes=(), dma=False, sem_inc=None):
        for k in reads:
            self._need(eng, self.lastw.get(k))
        for k in writes:
            self._need(eng, self.lastw.get(k))
            for t in self.readers.get(k, ()):
                self._need(eng, t)
        if dma:
            i = self.dcnt[eng]
            self.dcnt[eng] += 1
            name = "d%s%d" % (eng, i % DMA_K)
            inc = 16 if sem_inc is None else sem_inc
            val = self.latest.get(name, 0) + inc
            if i >= DMA_K:
                self._need(eng, (name, self.latest.get(name, 0)))
        else:
            self.ccnt[eng] += 1
            name = "c" + eng
            val = self.ccnt[eng]
            inc = 1
        tok = (name, val)
        ins = fn(self.eng[eng])
        ins.then_inc(self.sems[name], inc)
        self.latest[name] = val
        for k in writes:
            self.lastw[k] = tok
            self.readers[k] = []
        for k in reads:
            if k not in writes:
                self.readers.setdefault(k, []).append(tok)
        return tok

    def barrier(self):
        toks = list(self.latest.items())
        for e in ENGS:
            for t in toks:
                self._need(e, t)
        self.lastw = {}
        self.readers = {}


def build(debug=0):
    nc = bass.Bass("TRN2", target_bir_lowering=False)

    def din(name, shape, dt=F32):
        return nc.dram_tensor(name, list(shape), dt, kind="ExternalInput").ap()

    def dscr(name, shape, dt=F32):
        return nc.dram_tensor(name, list(shape), dt).ap()

    xs = din("xs", [TOT, D])
    cT = din("cT", [128, 8, 2])
    w_ada = din("w_ada", [D, 6 * D])
    bada_fm = din("bada_fm", [128, 16])
    bada_row = din("bada_row", [1, 4 * D])
    w_in_s5 = din("w_in_s5", [D, 512])
    w_in_u = din("w_in_u", [D, 768])
    w_in_v = din("w_in_v", [D, 768])
    gm_wsT = din("gm_wsT", [128, 6, 128])
    gm_bs_row = din("gm_bs_row", [1, 768])
    are_row = din("are_row", [1, 2048])
    aim_row = din("aim_row", [1, 2048])
    ls_row = din("ls_row", [1, 2048])
    are_pp = din("are_pp", [128, 16])
    aim_pp = din("aim_pp", [128, 16])
    ls_pp = din("ls_pp", [128, 16])
    WBre_raw = din("WBre_raw", [128, 2048])
    WBim_raw = din("WBim_raw", [128, 2048])
    WCre_raw = din("WCre_raw", [128, 2048])
    WCim_raw = din("WCim_raw", [128, 2048])
    dpp = din("dpp", [128, 4])
    w_glu_pad = din("w_glu_pad", [512, 512])
    b_glu_pp = din("b_glu_pp", [128, 4])
    w_out_pad = din("w_out_pad", [1280, D])
    ln1_g = din("ln1_g", [1, D])
    ln1_b = din("ln1_b", [1, D])
    ln2_g = din("ln2_g", [1, D])
    ln2_b = din("ln2_b", [1, D])
    w_router = din("w_router", [D, 16])
    if debug in (0, 3):
        wg = din("wg", [NE, D, DFF])
        wu = din("wu", [NE, D, DFF])
        wd = din("wd", [NE, DFF, D])
    ident_bf_d = din("ident_bf", [128, 128], BF16)
    ident_f_d = din("ident_f", [128, 128])
    tri_d = din("tri", [128, 128], BF16)
    ones_d = din("ones", [128, 128], BF16)
    iota_row_d = din("iota_row", [1, 1024])
    slot_pp_d = din("slot_pp", [128, 8])
    half_sel = din("half_sel", [128, 16])
    tokidx_d = din("tokidx", [128, NT // 2], I32)

    out_d = nc.dram_tensor("out", [SEQ // 2, D], F32, kind="ExternalOutput").ap()
    okind = {"kind": "ExternalOutput"} if debug else {}
    xnew_d = nc.dram_tensor("xnew_s", [SEQ, D], F32, **okind).ap()
    hrow_d = nc.dram_tensor("hrow_s", [SEQ, D], BF16, **okind).ap()
    catT_d = dscr("catT_s", [10, 128, SEQ], BF16)
    yf_d = dscr("yf_s", [4, 128, SEQ], F32)
    aff_d = nc.dram_tensor("aff_s", [SEQ, 16], F32, **okind).ap()
    if debug:
        dbg_idx = nc.dram_tensor("dbg_idx", [128, NE * 8], I32, kind="ExternalOutput").ap()
        dbg_gate = nc.dram_tensor("dbg_gate", [128, NE * 8], F32, kind="ExternalOutput").ap()
    cin_d = dscr("cin_s", [NE * NT, 128], F32)
    moe_d = nc.dram_tensor("moe_s", [SEQ, D], F32, **({"kind": "ExternalOutput"} if debug == 3 else {})).ap()
    moe_r = dscr("moe_r", [SEQ, D], F32)

    with contextlib.ExitStack() as gst:
        P = Prog(nc, gst)

        _uid = [0]

        def sb(st, name, shape, dt=F32):
            _uid[0] += 1
            return st.enter_context(nc.sbuf_tensor("%s_%d" % (name, _uid[0]), list(shape), dt))

        def ps(st, name, shape, dt=F32):
            _uid[0] += 1
            return st.enter_context(nc.psum_tensor("%s_%d" % (name, _uid[0]), list(shape), dt))

        REC = [None]

        def _op(eng, fn, r, w, dma=False):
            if REC[0] is not None:
                REC[0].append((eng, fn, tuple(r), tuple(w), dma))
                return None
            return P.op(eng, fn, r, w, dma=dma)

        def record(f, *args):
            REC[0] = []
            f(*args)
            ops, REC[0] = REC[0], None
            return ops

        def emit_zip(*lists):
            lists = [l for l in lists if l]
            pos = [0] * len(lists)
            total = sum(len(l) for l in lists)
            for _ in range(total):
                bi, bv = -1, 2.0
                for i, l in enumerate(lists):
                    if pos[i] < len(l):
                        v = pos[i] / len(l)
                        if v < bv:
                            bi, bv = i, v
                eng, fn, r, w, dma = lists[bi][pos[bi]]
                pos[bi] += 1
                P.op(eng, fn, r, w, dma=dma)

        def V(fn, r=(), w=()):
            return _op("vector", fn, r, w)

        def A(fn, r=(), w=()):
            return _op("scalar", fn, r, w)

        def G(fn, r=(), w=()):
            return _op("gpsimd", fn, r, w)

        def T(fn, r=(), w=()):
            return _op("tensor", fn, r, w)

        def DS(fn, r=(), w=()):
            return _op("sync", fn, r, w, dma=True)

        def DG(fn, r=(), w=()):
            return _op("gpsimd", fn, r, w, dma=True)

        def bc(ap_, shape):
            return ap_.to_broadcast(list(shape))

        ident_bf = sb(gst, "ident_bf_t", [128, 128], BF16)
        ident_f = sb(gst, "ident_f_t", [128, 128])
        ones_bf = sb(gst, "ones_t", [128, 128], BF16)
        tri_bf = sb(gst, "tri_t", [128, 128], BF16)
        modfm = sb(gst, "modfm", [128, 2, 8, 2])
        fin3 = sb(gst, "fin3", [128, 3, D])
        idx_all = sb(gst, "idx_all", [128, NE, 8], I32)
        gate_all = sb(gst, "gate_all", [128, NE, 8])
        early = contextlib.ExitStack()
        iota_r = sb(early, "iota_r", [128, 1024])
        modrow = sb(early, "modrow", [128, 4, D])
        lnrow = sb(early, "lnrow", [128, 4, D])
        aff = sb(early, "aff", [128, NT, 16])
        DS(lambda e: e.dma_start(out=ident_bf[:], in_=ident_bf_d), w=["ident_bf"])
        DS(lambda e: e.dma_start(out=ident_f[:], in_=ident_f_d), w=["ident_f"])
        DS(lambda e: e.dma_start(out=ones_bf[:], in_=ones_d), w=["ones"])
        DS(lambda e: e.dma_start(out=tri_bf[:], in_=tri_d), w=["tri"])
        DS(lambda e: e.dma_start(out=iota_r[:], in_=bc(iota_row_d, [128, 1024])), w=["iota_r"])
        for i, a in enumerate((ln1_g, ln1_b, ln2_g, ln2_b)):
            DS(lambda e, i=i, a=a: e.dma_start(out=lnrow[:, i, :], in_=bc(a, [128, D])), w=["lnrow"])

        with contextlib.ExitStack() as st:
            ct = sb(st, "ct", [128, 8, 2])
            sc = sb(st, "sc", [128, 8, 2])
            scb = sb(st, "scb", [128, 8, 128])
            wa = sb(st, "wa", [128, 8, D])
            bfm = sb(st, "bfm", [128, 16])
            brow = sb(st, "brow", [128, 4 * D])
            pfm = ps(st, "pfm", [128, 8, 2])
            prow = ps(st, "prow", [128, D])
            DS(lambda e: e.dma_start(out=ct[:], in_=cT), w=["ct"])
            DS(lambda e: e.dma_start(out=bfm[:], in_=bada_fm), w=["bfm"])
            DS(lambda e: e.dma_start(out=brow[:], in_=bc(bada_row, [128, 4 * D])), w=["brow"])
            A(lambda e: e.activation(out=sc[:], in_=ct[:], func=AF.Silu), ["ct"], ["sc"])
            V(lambda e: e.tensor_copy(out=scb[:], in_=bc(sc[:, :, 0:1], [128, 8, 128])), ["sc"], ["scb"])
            wav = w_ada.rearrange("(kt p) f -> p kt f", p=128)
            for ch in range(6):
                DS(lambda e, ch=ch: e.dma_start(out=wa[:], in_=wav[:, :, ch * D:(ch + 1) * D]), w=["wa"])
                if ch < 2:
                    for ft in range(8):
                        for kt in range(8):
                            T(lambda e, ft=ft, kt=kt: e.matmul(pfm[:, ft, :], lhsT=wa[:, kt, ft * 128:(ft + 1) * 128],
                                                               rhs=sc[:, kt, :], start=(kt == 0), stop=(kt == 7)),
                              ["wa", "sc"], ["pfm"])
                    V(lambda e, ch=ch: e.tensor_tensor(out=modfm[:, ch, :, :], in0=pfm[:],
                                                       in1=bc(bfm[:, ch * 8:(ch + 1) * 8].unsqueeze(2), [128, 8, 2]),
                                                       op=ALU.add), ["pfm", "bfm"], ["modfm"])
                else:
                    for nh in range(2):
                        for kt in range(8):
                            T(lambda e, nh=nh, kt=kt: e.matmul(prow[:, nh * 512:(nh + 1) * 512], lhsT=scb[:, kt, :],
                                                               rhs=wa[:, kt, nh * 512:(nh + 1) * 512],
                                                               start=(kt == 0), stop=(kt == 7)),
                              ["wa", "scb"], ["prow"])
                    V(lambda e, ch=ch: e.tensor_tensor(out=modrow[:, ch - 2, :], in0=prow[:],
                                                       in1=brow[:, (ch - 2) * D:(ch - 1) * D], op=ALU.add),
                      ["prow", "brow"], ["modrow"])
            V(lambda e: e.tensor_scalar_add(out=modfm[:, 1, :, :], in0=modfm[:, 1, :, :], scalar1=1.0), ["modfm"], ["modfm"])
            V(lambda e: e.tensor_scalar_add(out=modrow[:, 2, :], in0=modrow[:, 2, :], scalar1=1.0), ["modrow"], ["modrow"])
            V(lambda e: e.tensor_copy(out=fin3[:, 0, :], in_=modrow[:, 3, :]), ["modrow"], ["fin3"])
            V(lambda e: e.tensor_copy(out=fin3[:, 1:3, :], in_=lnrow[:, 2:4, :]), ["lnrow"], ["fin3"])
            P.barrier()

        with contextlib.ExitStack() as mst:
            U = sb(mst, "U", [128, 4, TOT], BF16)
            wst_ = contextlib.ExitStack()
            wS = sb(wst_, "wS", [128, 8, 512], BF16)
            wU = sb(wst_, "wU", [128, 8, 768], BF16)
            wV = sb(wst_, "wV", [128, 8, 768], BF16)
            wsT = sb(wst_, "wsT", [128, 6, 128], BF16)
            bsrow = sb(wst_, "bsrow", [128, 768])
            with contextlib.ExitStack() as st:
                stg = sb(st, "stg", [128, 8, 768])
                for (src, dst, n, nm) in ((w_in_s5, wS, 512, "wS"), (w_in_u, wU, 768, "wU"), (w_in_v, wV, 768, "wV")):
                    DS(lambda e, src=src, n=n: e.dma_start(out=stg[:, :, 0:n], in_=src.rearrange("(kt p) f -> p kt f", p=128)),
                       w=["stg"])
                    V(lambda e, dst=dst, n=n: e.tensor_copy(out=dst[:], in_=stg[:, :, 0:n]), ["stg"], [nm])
                DS(lambda e: e.dma_start(out=stg[:, 0:6, 0:128], in_=gm_wsT), w=["stg"])
                V(lambda e: e.tensor_copy(out=wsT[:], in_=stg[:, 0:6, 0:128]), ["stg"], ["wsT"])
                DS(lambda e: e.dma_start(out=bsrow[:], in_=bc(gm_bs_row, [128, 768])), w=["bsrow"])
                P.barrier()

            with contextlib.ExitStack() as st:
                xt = [sb(st, "xt%d" % i, [128, D]) for i in range(2)]
                xnA = [sb(st, "xn%d" % i, [128, D], BF16) for i in range(2)]
                xmTA = [sb(st, "xmT%d" % i, [128, 8, 128], BF16) for i in range(2)]
                statsA = [sb(st, "stats%d" % i, [128, 2, 6]) for i in range(2)]
                mvA = [sb(st, "mv%d" % i, [128, 2]) for i in range(2)]
                rstdA = [sb(st, "rstd%d" % i, [128, 1]) for i in range(2)]
                uTA = [sb(st, "uT%d" % i, [128, 6, 128]) for i in range(2)]
                vvA = [sb(st, "vv%d" % i, [128, 6, 128]) for i in range(2)]
                vcA = [sb(st, "vc%d" % i, [128, 6, 128]) for i in range(2)]
                vlnA = [sb(st, "vln%d" % i, [128, 6, 128], BF16) for i in range(2)]
                st6A = [sb(st, "st6%d" % i, [128, 6, 6]) for i in range(2)]
                mv6A = [sb(st, "mv6%d" % i, [128, 6, 2]) for i in range(2)]
                rs6A = [sb(st, "rs6%d" % i, [128, 6]) for i in range(2)]
                gmtA = [sb(st, "gmt%d" % i, [128, 6, 128]) for i in range(2)]
                gmb = [sb(st, "gmb%d" % i, [128, 6, 128], BF16) for i in range(2)]
                usb = [sb(st, "usb%d" % i, [128, 4, 128], BF16) for i in range(2)]
                pT = ps(st, "pT", [128, D], BF16)
                pS = ps(st, "pS", [128, 4, 128])
                pU = ps(st, "pU", [128, 8, 128])
                pV = ps(st, "pV", [128, D])
                pM = ps(st, "pM", [128, 8, 128])

                def stage_a(t):
                    i2 = t % 2
                    lat = t >= NTC
                    col = 0 if lat else 1
                    x_, xk = xt[i2], "xt%d" % i2
                    xn, xnk = xnA[i2], "xn%d" % i2
                    xmT, xmk = xmTA[i2], "xmT%d" % i2
                    stats, sk_ = statsA[i2], "stats%d" % i2
                    mv, mvk = mvA[i2], "mv%d" % i2
                    rstd, rk = rstdA[i2], "rstd%d" % i2
                    DS(lambda e: e.dma_start(out=x_[:], in_=xs[t * 128:(t + 1) * 128, :]), w=[xk])
                    for c in range(2):
                        V(lambda e, c=c: e.bn_stats(out=stats[:, c, :], in_=x_[:, c * 512:(c + 1) * 512]), [xk], [sk_])
                    V(lambda e: e.bn_aggr(out=mv[:], in_=stats[:].rearrange("p a b -> p (a b)")), [sk_], [mvk])
                    V(lambda e: e.tensor_scalar_add(out=rstd[:], in0=mv[:, 1:2], scalar1=EPS), [mvk], [rk])
                    A(lambda e: e.activation(out=rstd[:], in_=rstd[:], func=AF.Sqrt), [rk], [rk])
                    V(lambda e: e.reciprocal(out=rstd[:], in_=rstd[:]), [rk], [rk])
                    V(lambda e: e.tensor_scalar(out=xn[:], in0=x_[:], scalar1=mv[:, 0:1], scalar2=rstd[:, 0:1],
                                                op0=ALU.subtract, op1=ALU.mult), [xk, mvk, rk], [xnk])
                    for kt in range(8):
                        T(lambda e, kt=kt: e.transpose(out=pT[:, kt * 128:(kt + 1) * 128], in_=xn[:, kt * 128:(kt + 1) * 128],
                                                       identity=ident_bf[:]), [xnk, "ident_bf"], ["pT"])
                    for kt in range(8):
                        A(lambda e, kt=kt: e.activation(out=xmT[:, kt, :], in_=pT[:, kt * 128:(kt + 1) * 128],
                                                        func=AF.Identity, scale=modfm[:, 1, kt, col:col + 1],
                                                        bias=modfm[:, 0, kt, col:col + 1]),
                          ["pT", "modfm"], [xmk])

                def stage_b(t):
                    i2 = t % 2
                    lat = t >= NTC
                    xmT, xmk = xmTA[i2], "xmT%d" % i2
                    for ct_ in range(4):
                        for kt in range(8):
                            T(lambda e, ct_=ct_, kt=kt: e.matmul(pS[:, ct_, :], lhsT=wS[:, kt, ct_ * 128:(ct_ + 1) * 128],
                                                                 rhs=xmT[:, kt, :], start=(kt == 0), stop=(kt == 7)),
                              ["wS", xmk], ["pS"])
                    V(lambda e: e.tensor_copy(out=U[:, :, t * 128:(t + 1) * 128], in_=pS[:]), ["pS"], ["U"])
                    if not lat:
                        return
                    tl = t - NTC
                    uT, uk = uTA[i2], "uT%d" % i2
                    vv, vk = vvA[i2], "vv%d" % i2
                    vc, vck = vcA[i2], "vc%d" % i2
                    vln, vlk = vlnA[i2], "vln%d" % i2
                    st6, s6k = st6A[i2], "st6%d" % i2
                    mv6, m6k = mv6A[i2], "mv6%d" % i2
                    rs6, r6k = rs6A[i2], "rs6%d" % i2
                    gmt, gtk = gmtA[i2], "gmt%d" % i2
                    for ct_ in range(6):
                        for kt in range(8):
                            T(lambda e, ct_=ct_, kt=kt: e.matmul(pU[:, ct_, :], lhsT=wU[:, kt, ct_ * 128:(ct_ + 1) * 128],
                                                                 rhs=xmT[:, kt, :], start=(kt == 0), stop=(kt == 7)),
                              ["wU", xmk], ["pU"])
                    A(lambda e: e.activation(out=uT[:], in_=pU[:, 0:6, :], func=AF.Gelu_apprx_tanh), ["pU"], [uk])
                    for (c0, c1) in ((0, 512), (512, 768)):
                        for kt in range(8):
                            T(lambda e, c0=c0, c1=c1, kt=kt: e.matmul(pV[:, c0:c1], lhsT=xmT[:, kt, :], rhs=wV[:, kt, c0:c1],
                                                                      start=(kt == 0), stop=(kt == 7)),
                              ["wV", xmk], ["pV"])
                    A(lambda e: e.activation(out=vv[:].rearrange("p a b -> p (a b)"), in_=pV[:, 0:768], func=AF.Gelu_apprx_tanh),
                      ["pV"], [vk])
                    for g in range(6):
                        V(lambda e, g=g: e.bn_stats(out=st6[:, g, :], in_=vv[:, g, :]), [vk], [s6k])
                    for g in range(6):
                        V(lambda e, g=g: e.bn_aggr(out=mv6[:, g, :], in_=st6[:, g, :]), [s6k], [m6k])
                    V(lambda e: e.tensor_scalar_add(out=rs6[:], in0=mv6[:, :, 1], scalar1=EPS), [m6k], [r6k])
                    A(lambda e: e.activation(out=rs6[:], in_=rs6[:], func=AF.Sqrt), [r6k], [r6k])
                    V(lambda e: e.reciprocal(out=rs6[:], in_=rs6[:]), [r6k], [r6k])
                    V(lambda e: e.tensor_tensor(out=vc[:], in0=vv[:], in1=bc(mv6[:, :, 0:1], [128, 6, 128]), op=ALU.subtract),
                      [vk, m6k], [vck])
                    V(lambda e: e.tensor_tensor(out=vln[:], in0=vc[:], in1=bc(rs6[:].unsqueeze(2), [128, 6, 128]), op=ALU.mult),
                      [vck, r6k], [vlk])
                    for g in range(6):
                        T(lambda e, g=g: e.matmul(pM[:, g, :], lhsT=vln[:, g, :], rhs=wsT[:, g, :], start=True, stop=True),
                          [vlk, "wsT"], ["pM"])
                    G(lambda e: e.tensor_tensor(out=gmt[:], in0=bsrow[:].rearrange("p (a b) -> p a b", a=6), in1=bsrow[:].rearrange("p (a b) -> p a b", a=6),
                                                op=ALU.bypass), ["bsrow"], [gtk]) if False else None
                    V(lambda e: e.tensor_tensor(out=gmt[:], in0=pM[:, 0:6, :], in1=bsrow[:].rearrange("p (a b) -> p a b", a=6),
                                                op=ALU.add), ["pM", "bsrow"], [gtk])
                    gb = gmb[tl % 2]
                    gk = "gmb%d" % (tl % 2)
                    G(lambda e: e.tensor_tensor(out=gb[:], in0=gmt[:], in1=uT[:], op=ALU.mult), [gtk, uk], [gk])
                    DS(lambda e: e.dma_start(out=catT_d[4:10, :, tl * 128:(tl + 1) * 128].rearrange("a p t -> p a t"),
                                             in_=gb[:]), [gk], ["catT_gm"])

                stage_a(0)
                for t in range(NTC + NT):
                    la = record(stage_a, t + 1) if t + 1 < NTC + NT else []
                    lb = record(stage_b, t)
                    emit_zip(la, lb)
                P.barrier()
            wst_.close()

            with contextlib.ExitStack() as st:
                WB = sb(st, "WB", [128, 2, 2048], BF16)
                WC = sb(st, "WC", [128, 2, 2048], BF16)
                rho_pp = sb(st, "rho_pp", [128, 16])
                f_pp = sb(st, "f_pp", [128, 16])
                dsc = sb(st, "dsc", [128, 4])
                bglu = sb(st, "bglu", [128, 4])
                wglu = sb(st, "wglu", [128, 4, 512], BF16)
                for dr_ in range(4):
                  csl = slice(dr_ * 512, (dr_ + 1) * 512)
                  with contextlib.ExitStack() as s2:
                        r = {n: sb(s2, "r_" + n, [128, 512]) for n in
                             ("are", "aim", "stp", "rho", "f", "y", "y2", "sn", "cs", "x", "yv", "den", "cr", "ci", "t1", "t2", "bre", "bim")}
                        ri_ = sb(s2, "r_int", [128, 512], I32)
                        DS(lambda e: e.dma_start(out=r["are"][:], in_=bc(are_row[:, csl], [128, 512])), w=["are"])
                        DS(lambda e: e.dma_start(out=r["aim"][:], in_=bc(aim_row[:, csl], [128, 512])), w=["aim"])
                        DS(lambda e: e.dma_start(out=r["stp"][:], in_=bc(ls_row[:, csl], [128, 512])), w=["stp"])
                        DS(lambda e: e.dma_start(out=r["bre"][:], in_=WBre_raw[:, csl]), w=["bre"])
                        DS(lambda e: e.dma_start(out=r["bim"][:], in_=WBim_raw[:, csl]), w=["bim"])

                        def vt(o, a, b, op):
                            V(lambda e: e.tensor_tensor(out=r[o][:], in0=r[a][:], in1=r[b][:], op=op), [a, b], [o])

                        def frac(o, i):
                            V(lambda e: e.tensor_copy(out=ri_[:], in_=r[i][:]), [i], ["rint"])
                            V(lambda e: e.tensor_copy(out=r["t1"][:], in_=ri_[:]), ["rint"], ["t1"])
                            vt(o, i, "t1", ALU.subtract)

                        A(lambda e: e.activation(out=r["stp"][:], in_=r["stp"][:], func=AF.Exp), ["stp"], ["stp"])
                        vt("rho", "are", "stp", ALU.mult)
                        A(lambda e: e.activation(out=r["rho"][:], in_=r["rho"][:], func=AF.Exp), ["rho"], ["rho"])
                        vt("f", "aim", "stp", ALU.mult)
                        V(lambda e: e.tensor_scalar_mul(out=r["f"][:], in0=r["f"][:], scalar1=1.0 / (2 * math.pi)), ["f"], ["f"])
                        frac("y", "f")
                        A(lambda e: e.activation(out=r["sn"][:], in_=r["y"][:], func=AF.Sin, scale=TWO_PI), ["y"], ["sn"])
                        V(lambda e: e.tensor_scalar_add(out=r["y2"][:], in0=r["y"][:], scalar1=0.25), ["y"], ["y2"])
                        frac("y2", "y2")
                        A(lambda e: e.activation(out=r["cs"][:], in_=r["y2"][:], func=AF.Sin, scale=TWO_PI), ["y2"], ["cs"])
                        vt("x", "rho", "cs", ALU.mult)
                        V(lambda e: e.tensor_scalar_add(out=r["x"][:], in0=r["x"][:], scalar1=-1.0), ["x"], ["x"])
                        vt("yv", "rho", "sn", ALU.mult)
                        vt("den", "are", "are", ALU.mult)
                        vt("t2", "aim", "aim", ALU.mult)
                        vt("den", "den", "t2", ALU.add)
                        V(lambda e: e.reciprocal(out=r["den"][:], in_=r["den"][:]), ["den"], ["den"])
                        vt("cr", "x", "are", ALU.mult)
                        vt("t2", "yv", "aim", ALU.mult)
                        vt("cr", "cr", "t2", ALU.add)
                        vt("cr", "cr", "den", ALU.mult)
                        vt("ci", "yv", "are", ALU.mult)
                        vt("t2", "x", "aim", ALU.mult)
                        vt("ci", "ci", "t2", ALU.subtract)
                        vt("ci", "ci", "den", ALU.mult)
                        vt("t1", "cr", "bre", ALU.mult)
                        vt("t2", "ci", "bim", ALU.mult)
                        V(lambda e: e.tensor_tensor(out=WB[:, 0, csl], in0=r["t1"][:], in1=r["t2"][:], op=ALU.subtract), ["t1", "t2"], ["WB"])
                        vt("t1", "cr", "bim", ALU.mult)
                        vt("t2", "ci", "bre", ALU.mult)
                        V(lambda e: e.tensor_tensor(out=WB[:, 1, csl], in0=r["t1"][:], in1=r["t2"][:], op=ALU.add), ["t1", "t2"], ["WB"])
                        DS(lambda e: e.dma_start(out=r["bre"][:], in_=WCre_raw[:, csl]), w=["bre"])
                        DS(lambda e: e.dma_start(out=r["bim"][:], in_=WCim_raw[:, csl]), w=["bim"])
                        V(lambda e: e.tensor_copy(out=WC[:, 0, csl], in_=r["bre"][:]), ["bre"], ["WC"])
                        V(lambda e: e.tensor_scalar_mul(out=WC[:, 1, csl], in0=r["bim"][:], scalar1=-1.0), ["bim"], ["WC"])

                        P.barrier()
                with contextlib.ExitStack() as s2:
                    pa = sb(s2, "pa", [128, 16])
                    pb = sb(s2, "pb", [128, 16])
                    pc = sb(s2, "pc", [128, 16])
                    DS(lambda e: e.dma_start(out=pa[:], in_=are_pp), w=["pa"])
                    DS(lambda e: e.dma_start(out=pb[:], in_=aim_pp), w=["pb"])
                    DS(lambda e: e.dma_start(out=pc[:], in_=ls_pp), w=["pc"])
                    A(lambda e: e.activation(out=pc[:], in_=pc[:], func=AF.Exp), ["pc"], ["pc"])
                    V(lambda e: e.tensor_tensor(out=rho_pp[:], in0=pa[:], in1=pc[:], op=ALU.mult), ["pa", "pc"], ["rho_pp"])
                    A(lambda e: e.activation(out=rho_pp[:], in_=rho_pp[:], func=AF.Exp), ["rho_pp"], ["rho_pp"])
                    V(lambda e: e.tensor_tensor(out=f_pp[:], in0=pb[:], in1=pc[:], op=ALU.mult), ["pb", "pc"], ["f_pp"])
                    V(lambda e: e.tensor_scalar_mul(out=f_pp[:], in0=f_pp[:], scalar1=1.0 / (2 * math.pi)), ["f_pp"], ["f_pp"])
                    DS(lambda e: e.dma_start(out=dsc[:], in_=dpp), w=["dsc"])
                    DS(lambda e: e.dma_start(out=bglu[:], in_=b_glu_pp), w=["bglu"])
                    gst_ = sb(s2, "gst_", [128, 4, 512])
                    DS(lambda e: e.dma_start(out=gst_[:], in_=w_glu_pad.rearrange("(kt p) f -> p kt f", p=128)), w=["gst_"])
                    V(lambda e: e.tensor_copy(out=wglu[:], in_=gst_[:]), ["gst_"], ["wglu"])
                    P.barrier()

                L = SEG
                H = 512
                arg1 = sb(st, "arg", [128, L])
                arg = [arg1, arg1]
                rnd = sb(st, "rnd", [128, L])
                argi = [rnd, rnd]
                magic = sb(st, "magic", [128, 2])
                V(lambda e: e.memset(magic[:, 0:1], 12582912.0), w=["magic"])
                V(lambda e: e.memset(magic[:, 1:2], -12582912.0), w=["magic"])
                yv1 = sb(st, "yv", [128, L])
                yv = [yv1, yv1]
                s2 = arg
                sq = arg
                cs = [sb(st, "cs%d" % i, [128, L], BF16) for i in range(2)]
                sn = [sb(st, "sn%d" % i, [128, L], BF16) for i in range(2)]
                bre = [sb(st, "bre%d" % i, [128, L], BF16) for i in range(2)]
                bim = [sb(st, "bim%d" % i, [128, L], BF16) for i in range(2)]
                dre = sb(st, "dre", [128, L], BF16)
                dim_ = sb(st, "dim", [128, L], BF16)
                tA = sb(st, "tA", [128, L], BF16)
                tB = sb(st, "tB", [128, L], BF16)
                zre = sb(st, "zre", [128, L], BF16)
                zim = sb(st, "zim", [128, L], BF16)
                Sre1 = sb(st, "Sre", [128, L], BF16)
                Sim1 = sb(st, "Sim", [128, L], BF16)
                Sre = [Sre1, Sre1]
                Sim = [Sim1, Sim1]
                zst = sb(st, "zst", [128, 16, 2])
                ysb1 = sb(st, "ysb", [128, L])
                ysb = [ysb1, ysb1]
                yfl1 = sb(st, "yfl", [128, L])
                yfl = [yfl1, yfl1]
                gg = sb(st, "gg", [128, 4, L], BF16)
                sg1 = sb(st, "sg", [128, L])
                sg = [sg1, sg1]
                s5o1 = sb(st, "s5o", [128, L], BF16)
                s5o = [s5o1, s5o1]
                pB = [[ps(st, "pB%d%d" % (h_, ri), [128, H]) for ri in range(2)] for h_ in range(2)]
                pY = ps(st, "pY", [128, L])
                pG = ps(st, "pG", [128, L])
                V(lambda e: e.memset(zst[:], 0.0), w=["zst"])
                V(lambda e: e.memset(ysb1[:], 0.0), w=["ysb"])
                for t in range(NT):
                    DS(lambda e, t=t: e.dma_start(out=moe_d[t * 128:(t + 1) * 128, :], in_=ysb1[:]), ["ysb"], ["moe_d"])
                colctr = [0]

                plan = []
                ulo = []
                for gp in range(8):
                    plan.append((0, gp, CTX, 0)); ulo.append(0)
                for sgi in range(SEQ // L):
                    for kt in range(4):
                        for g2 in range(2):
                            plan.append((0, kt * 2 + g2, L, CTX + sgi * L)); ulo.append(CTX + sgi * L)
                for gp in range(8):
                    plan.append((1, gp, CTX, 0)); ulo.append(0)
                for sb_i in range(SEQ // L):
                    for kt in range(4):
                        for g2 in range(2):
                            plan.append((1, kt * 2 + g2, L, CTX + sb_i * L)); ulo.append(CTX + (SEQ // L - 1 - sb_i) * L)

                def s5_bu(idx):
                    dr, gp, n, t0 = plan[idx]
                    u_lo = ulo[idx]
                    ci_ = dr * 8 + gp
                    kt = gp // 2
                    r0 = 64 * (gp % 2)
                    wcol = slice(ci_ * 128, (ci_ + 1) * 128)
                    cb = idx % 2
                    for hi_, c0 in enumerate(range(0, n, H)):
                        c1 = min(n, c0 + H)
                        for ri, dst, dk in ((0, bre[cb], "bre%d" % cb), (1, bim[cb], "bim%d" % cb)):
                            pb_ = pB[hi_][ri]
                            pk = "pB%d%d" % (hi_, ri)
                            T(lambda e, ri=ri, pb_=pb_, c0=c0, c1=c1: e.matmul(pb_[:, 0:c1 - c0], lhsT=WB[r0:r0 + 64, ri, wcol],
                                                                             rhs=U[r0:r0 + 64, kt, u_lo + c0:u_lo + c1],
                                                                             start=True, stop=True), ["WB", "U"], [pk])
                            A(lambda e, pb_=pb_, dst=dst, c0=c0, c1=c1: e.activation(out=dst[:, c0:c1], in_=pb_[:, 0:c1 - c0], func=AF.Identity),
                              [pk], [dk])

                def s5_tables(idx):
                    dr, gp, n, t0 = plan[idx]
                    ci_ = dr * 8 + gp
                    cb = idx % 2
                    arg_, argi_, yv_, s2_, sq_, cs_, sn_ = arg[cb], argi[cb], yv[cb], s2[cb], sq[cb], cs[cb], sn[cb]
                    k = lambda nm: nm if nm in ("arg", "yv", "Sre", "Sim") else "%s%d" % (nm, cb)
                    G(lambda e: e.tensor_scalar(out=arg_[:, 0:n], in0=iota_r[:, 0:n], scalar1=float(t0), scalar2=f_pp[:, ci_:ci_ + 1],
                                                op0=ALU.add, op1=ALU.mult), ["iota_r", "f_pp"], [k("arg")])
                    A(lambda e: e.activation(out=rnd[:, 0:n], in_=arg_[:, 0:n], func=AF.Identity, bias=magic[:, 0:1]), [k("arg"), "magic"], ["rnd"])
                    A(lambda e: e.activation(out=rnd[:, 0:n], in_=rnd[:, 0:n], func=AF.Identity, bias=magic[:, 1:2]), ["rnd", "magic"], ["rnd"])
                    G(lambda e: e.tensor_tensor(out=yv_[:, 0:n], in0=arg_[:, 0:n], in1=rnd[:, 0:n], op=ALU.subtract),
                      [k("arg"), "rnd"], [k("yv")])
                    A(lambda e: e.activation(out=sn_[:, 0:n], in_=yv_[:, 0:n], func=AF.Sin, scale=TWO_PI), [k("yv")], [k("sn")])
                    A(lambda e: e.activation(out=s2_[:, 0:n], in_=yv_[:, 0:n], func=AF.Sin, scale=TWO_PI / 2), [k("yv")], [k("arg")])
                    A(lambda e: e.activation(out=sq_[:, 0:n], in_=s2_[:, 0:n], func=AF.Square), [k("arg")], [k("arg")])
                    G(lambda e: e.tensor_scalar(out=cs_[:, 0:n], in0=sq_[:, 0:n], scalar1=-2.0, scalar2=1.0, op0=ALU.mult, op1=ALU.add),
                      [k("arg")], [k("cs")])

                def s5_col(dr, gp, u_lo, n, t0, rev, readout):
                    idx = colctr[0]
                    colctr[0] += 1
                    assert plan[idx] == (dr, gp, n, t0), (plan[idx], dr, gp, n, t0)
                    if idx == 0:
                        s5_tables(0)
                        s5_bu(0)
                    ci_ = dr * 8 + gp
                    kt = gp // 2
                    r0 = 64 * (gp % 2)
                    wcol = slice(ci_ * 128, (ci_ + 1) * 128)
                    cb = idx % 2
                    cs_, sn_ = cs[cb], sn[cb]
                    bre_, bim_, Sre_, Sim_ = bre[cb], bim[cb], Sre[cb], Sim[cb]
                    k = lambda nm: nm if nm in ("arg", "yv", "Sre", "Sim") else "%s%d" % (nm, cb)
                    assert ulo[idx] == u_lo
                    if idx + 1 < len(plan):
                        s5_tables(idx + 1)
                        s5_bu(idx + 1)

                    def tvn(t_):
                        if not rev:
                            return t_[:, 0:n]
                        return t_[:, 0:n][:, ::-1]

                    def vtt(o, ok, a_, ak, b_, bk, op):
                        V(lambda e: e.tensor_tensor(out=o, in0=a_, in1=b_, op=op), [ak, bk], [ok])

                    vtt(tA[:, 0:n], "tA", bre_[:, 0:n], k("bre"), tvn(cs_), k("cs"), ALU.mult)
                    vtt(tB[:, 0:n], "tB", bim_[:, 0:n], k("bim"), tvn(sn_), k("sn"), ALU.mult)
                    vtt(dre[:, 0:n], "dre", tA[:, 0:n], "tA", tB[:, 0:n], "tB", ALU.add)
                    vtt(tA[:, 0:n], "tA", bim_[:, 0:n], k("bim"), tvn(cs_), k("cs"), ALU.mult)
                    vtt(tB[:, 0:n], "tB", bre_[:, 0:n], k("bre"), tvn(sn_), k("sn"), ALU.mult)
                    vtt(dim_[:, 0:n], "dim", tA[:, 0:n], "tA", tB[:, 0:n], "tB", ALU.subtract)
                    for (src, dst, ri) in ((dre, zre, 0), (dim_, zim, 1)):
                        V(lambda e, src=src, dst=dst, ri=ri: e.tensor_tensor_scan(
                            out=tvn(dst), data0=bc(rho_pp[:, ci_:ci_ + 1], [128, n]), data1=tvn(src),
                            initial=zst[:, ci_, ri:ri + 1], op0=ALU.mult, op1=ALU.add),
                          ["dre" if ri == 0 else "dim", "rho_pp", "zst"], ["zre" if ri == 0 else "zim"])
                    last = 0 if rev else n - 1
                    V(lambda e: e.tensor_copy(out=zst[:, ci_, 0:1], in_=zre[:, last:last + 1]), ["zre"], ["zst"])
                    V(lambda e: e.tensor_copy(out=zst[:, ci_, 1:2], in_=zim[:, last:last + 1]), ["zim"], ["zst"])
                    if not readout:
                        return
                    vtt(tA[:, 0:n], "tA", zre[:, 0:n], "zre", tvn(cs_), k("cs"), ALU.mult)
                    vtt(tB[:, 0:n], "tB", zim[:, 0:n], "zim", tvn(sn_), k("sn"), ALU.mult)
                    vtt(Sre_[:, 0:n], k("Sre"), tA[:, 0:n], "tA", tB[:, 0:n], "tB", ALU.subtract)
                    vtt(tA[:, 0:n], "tA", zre[:, 0:n], "zre", tvn(sn_), k("sn"), ALU.mult)
                    vtt(tB[:, 0:n], "tB", zim[:, 0:n], "zim", tvn(cs_), k("cs"), ALU.mult)
                    vtt(Sim_[:, 0:n], k("Sim"), tA[:, 0:n], "tA", tB[:, 0:n], "tB", ALU.add)
                    first = (gp % 2 == 0)
                    for c0 in range(0, n, 512):
                        for ri, S_, sk in ((0, Sre_, k("Sre")), (1, Sim_, k("Sim"))):
                            T(lambda e, ri=ri, S_=S_, c0=c0: e.matmul(pY[:, c0:c0 + 512], lhsT=WC[:, ri, wcol], rhs=S_[:, c0:c0 + 512],
                                                                     start=(first and ri == 0), stop=((not first) and ri == 1)),
                              ["WC", sk], ["pY"])

                for gp in range(8):
                    s5_col(0, gp, 0, CTX, 0, False, False)
                cnt_ = 0
                for sgi in range(SEQ // L):
                    for kt in range(4):
                        for g2 in range(2):
                            s5_col(0, kt * 2 + g2, CTX + sgi * L, L, CTX + sgi * L, False, True)
                        yb = ysb[cnt_ % 2]
                        yk = "ysb"
                        cnt_ += 1
                        A(lambda e, yb=yb: e.activation(out=yb[:], in_=pY[:], func=AF.Identity), ["pY"], [yk])
                        DS(lambda e, kt=kt, sgi=sgi, yb=yb: e.dma_start(out=yf_d[kt, :, sgi * L:(sgi + 1) * L], in_=yb[:]), [yk], ["yf"])
                for gp in range(8):
                    s5_col(1, gp, 0, CTX, 0, True, False)
                for sb_i in range(SEQ // L):
                    sgi = SEQ // L - 1 - sb_i
                    for kt in range(4):
                        yl = yfl[cnt_ % 2]
                        ylk = "yfl"
                        yb = ysb[cnt_ % 2]
                        yk = "ysb"
                        cnt_ += 1
                        DS(lambda e, kt=kt, sgi=sgi, yl=yl: e.dma_start(out=yl[:], in_=yf_d[kt, :, sgi * L:(sgi + 1) * L]), ["yf"], [ylk])
                        for g2 in range(2):
                            s5_col(1, kt * 2 + g2, CTX + sgi * L, L, CTX + sb_i * L, True, True)
                        V(lambda e, yb=yb, yl=yl: e.tensor_tensor(out=yb[:], in0=pY[:], in1=yl[:], op=ALU.add), ["pY", ylk], [yk])
                        V(lambda e, kt=kt, sgi=sgi, yb=yb: e.scalar_tensor_tensor(out=yb[:], in0=U[:, kt, CTX + sgi * L:CTX + (sgi + 1) * L],
                                                                                  scalar=dsc[:, kt:kt + 1], in1=yb[:],
                                                                                  op0=ALU.mult, op1=ALU.add), ["U", "dsc", yk], [yk])
                        A(lambda e, kt=kt, yb=yb: e.activation(out=gg[:, kt, :], in_=yb[:], func=AF.Gelu_apprx_tanh), [yk], ["gg"])
                    for mt in range(4):
                        sg_ = sg[mt % 2]
                        sgk = "sg"
                        so_ = s5o[mt % 2]
                        sok = "s5o"
                        for c0 in range(0, L, 512):
                            for kt in range(4):
                                T(lambda e, mt=mt, c0=c0, kt=kt: e.matmul(pG[:, c0:c0 + 512], lhsT=wglu[:, kt, mt * 128:(mt + 1) * 128],
                                                                         rhs=gg[:, kt, c0:c0 + 512], start=(kt == 0), stop=(kt == 3)),
                                  ["wglu", "gg"], ["pG"])
                        A(lambda e, mt=mt, sg_=sg_: e.activation(out=sg_[:], in_=pG[:], func=AF.Sigmoid, bias=bglu[:, mt:mt + 1]), ["pG", "bglu"], [sgk])
                        G(lambda e, mt=mt, sg_=sg_, so_=so_: e.tensor_tensor(out=so_[:], in0=sg_[:], in1=gg[:, mt, :], op=ALU.mult), [sgk, "gg"], [sok])
                        DS(lambda e, mt=mt, sgi=sgi, so_=so_: e.dma_start(out=catT_d[mt, :, sgi * L:(sgi + 1) * L], in_=so_[:]), [sok], ["catT_s5"])
                P.barrier()
        P.barrier()

        with contextlib.ExitStack() as st:
            wo = sb(st, "wo", [128, 10, D], BF16)
            wr = sb(st, "wr", [128, 8, 16], BF16)
            with contextlib.ExitStack() as s2:
                stg = sb(s2, "stg3", [128, 10, D])
                DS(lambda e: e.dma_start(out=stg[:], in_=w_out_pad.rearrange("(kt p) f -> p kt f", p=128)), w=["stg3"])
                V(lambda e: e.tensor_copy(out=wo[:], in_=stg[:]), ["stg3"], ["wo"])
                DS(lambda e: e.dma_start(out=stg[:, 0:8, 0:16], in_=w_router.rearrange("(kt p) f -> p kt f", p=128)), w=["stg3"])
                V(lambda e: e.tensor_copy(out=wr[:], in_=stg[:, 0:8, 0:16]), ["stg3"], ["wr"])
                P.barrier()
            cat = [sb(st, "cat%d" % i, [128, 10, 128], BF16) for i in range(2)]
            xt3 = [sb(st, "x3_%d" % i, [128, D]) for i in range(2)]
            tmA = [sb(st, "tm%d" % i, [128, D]) for i in range(2)]
            xrA = [sb(st, "xr%d" % i, [128, D]) for i in range(2)]
            xnwA = [sb(st, "xnw%d" % i, [128, D]) for i in range(2)]
            hhA = [sb(st, "hh%d" % i, [128, D]) for i in range(2)]
            hbA = [sb(st, "hb%d" % i, [128, D], BF16) for i in range(2)]
            hTA = [sb(st, "hT%d" % i, [128, 8, 128], BF16) for i in range(2)]
            statsA = [sb(st, "stats3_%d" % i, [128, 2, 6]) for i in range(4)]
            mvA = [sb(st, "mv3_%d" % i, [128, 2]) for i in range(4)]
            rstdA = [sb(st, "rstd3_%d" % i, [128, 1]) for i in range(4)]
            lgA = [sb(st, "lg%d" % i, [128, 16]) for i in range(2)]
            mxA = [sb(st, "mx%d" % i, [128, 1]) for i in range(2)]
            smA = [sb(st, "sm%d" % i, [128, 1]) for i in range(2)]
            pMxA = [ps(st, "pMx%d" % i, [128, D]) for i in range(2)]
            pT3A = [ps(st, "pT3%d" % i, [128, D], BF16) for i in range(2)]
            pLA = [ps(st, "pL%d" % i, [128, 16]) for i in range(2)]

            def lnorm(src, sk, dst, dk, si):
                stats, mv, rstd = statsA[si], mvA[si], rstdA[si]
                k1, k2, k3 = "stats3_%d" % si, "mv3_%d" % si, "rstd3_%d" % si
                for c in range(2):
                    V(lambda e, c=c: e.bn_stats(out=stats[:, c, :], in_=src[:, c * 512:(c + 1) * 512]), [sk], [k1])
                V(lambda e: e.bn_aggr(out=mv[:], in_=stats[:].rearrange("p a b -> p (a b)")), [k1], [k2])
                V(lambda e: e.tensor_scalar_add(out=rstd[:], in0=mv[:, 1:2], scalar1=EPS), [k2], [k3])
                A(lambda e: e.activation(out=rstd[:], in_=rstd[:], func=AF.Sqrt), [k3], [k3])
                V(lambda e: e.reciprocal(out=rstd[:], in_=rstd[:]), [k3], [k3])
                V(lambda e: e.tensor_scalar(out=dst[:], in0=src[:], scalar1=mv[:, 0:1], scalar2=rstd[:, 0:1],
                                            op0=ALU.subtract, op1=ALU.mult), [sk, k2, k3], [dk])

            def p3_loads(t):
                i2 = t % 2
                c_, ck = cat[i2], "cat%d" % i2
                x_, xk = xt3[i2], "x3_%d" % i2
                DS(lambda e: e.dma_start(out=c_[:], in_=catT_d[:, :, t * 128:(t + 1) * 128].rearrange("a p t -> p a t")), w=[ck])
                DS(lambda e: e.dma_start(out=x_[:], in_=xs[CTX + t * 128:CTX + (t + 1) * 128, :]), w=[xk])

            def p3_A(t):
                i2 = t % 2
                c_, ck = cat[i2], "cat%d" % i2
                x_, xk = xt3[i2], "x3_%d" % i2
                tm, tmk = tmA[i2], "tm%d" % i2
                xr, xrk = xrA[i2], "xr%d" % i2
                xnw, xnk = xnwA[i2], "xnw%d" % i2
                hh, hhk = hhA[i2], "hh%d" % i2
                hb, hbk = hbA[i2], "hb%d" % i2
                hT, hTk = hTA[i2], "hT%d" % i2
                lg, lgk = lgA[i2], "lg%d" % i2
                mx, mxk = mxA[i2], "mx%d" % i2
                sm, smk = smA[i2], "sm%d" % i2
                pMx, pMk = pMxA[i2], "pMx%d" % i2
                pT3, pTk = pT3A[i2], "pT3%d" % i2
                pL, pLk = pLA[i2], "pL%d" % i2
                for nh in range(2):
                    for kt in range(10):
                        T(lambda e, nh=nh, kt=kt, c_=c_, pMx=pMx: e.matmul(pMx[:, nh * 512:(nh + 1) * 512], lhsT=c_[:, kt, :],
                                                                           rhs=wo[:, kt, nh * 512:(nh + 1) * 512], start=(kt == 0), stop=(kt == 9)),
                          [ck, "wo"], [pMk])

            def p3_B1(t):
                i2 = t % 2
                c_, ck = cat[i2], "cat%d" % i2
                x_, xk = xt3[i2], "x3_%d" % i2
                tm, tmk = tmA[i2], "tm%d" % i2
                xr, xrk = xrA[i2], "xr%d" % i2
                xnw, xnk = xnwA[i2], "xnw%d" % i2
                hh, hhk = hhA[i2], "hh%d" % i2
                hb, hbk = hbA[i2], "hb%d" % i2
                hT, hTk = hTA[i2], "hT%d" % i2
                lg, lgk = lgA[i2], "lg%d" % i2
                mx, mxk = mxA[i2], "mx%d" % i2
                sm, smk = smA[i2], "sm%d" % i2
                pMx, pMk = pMxA[i2], "pMx%d" % i2
                pT3, pTk = pT3A[i2], "pT3%d" % i2
                pL, pLk = pLA[i2], "pL%d" % i2
                V(lambda e, tm=tm, pMx=pMx: e.tensor_tensor(out=tm[:], in0=pMx[:], in1=modrow[:, 0, :], op=ALU.mult), [pMk, "modrow"], [tmk])
                V(lambda e, x_=x_, xr=xr, tm=tm: e.scalar_tensor_tensor(out=xr[:], in0=x_[:], scalar=ALPHA, in1=tm[:], op0=ALU.mult, op1=ALU.add),
                  [xk, tmk], [xrk])
                lnorm(xr, xrk, tm, tmk, 2 * i2)
                G(lambda e, tm=tm: e.tensor_tensor(out=tm[:], in0=tm[:], in1=lnrow[:, 0, :], op=ALU.mult), [tmk, "lnrow"], [tmk])
                G(lambda e, tm=tm, xnw=xnw: e.tensor_tensor(out=xnw[:], in0=tm[:], in1=lnrow[:, 1, :], op=ALU.add), [tmk, "lnrow"], [xnk])
                DS(lambda e, t=t, xnw=xnw: e.dma_start(out=xnew_d[t * 128:(t + 1) * 128, :], in_=xnw[:]), [xnk], ["xnew_d"])

            def p3_B2(t):
                i2 = t % 2
                c_, ck = cat[i2], "cat%d" % i2
                x_, xk = xt3[i2], "x3_%d" % i2
                tm, tmk = tmA[i2], "tm%d" % i2
                xr, xrk = xrA[i2], "xr%d" % i2
                xnw, xnk = xnwA[i2], "xnw%d" % i2
                hh, hhk = hhA[i2], "hh%d" % i2
                hb, hbk = hbA[i2], "hb%d" % i2
                hT, hTk = hTA[i2], "hT%d" % i2
                lg, lgk = lgA[i2], "lg%d" % i2
                mx, mxk = mxA[i2], "mx%d" % i2
                sm, smk = smA[i2], "sm%d" % i2
                pMx, pMk = pMxA[i2], "pMx%d" % i2
                pT3, pTk = pT3A[i2], "pT3%d" % i2
                pL, pLk = pLA[i2], "pL%d" % i2
                lnorm(xnw, xnk, hh, hhk, 2 * i2 + 1)
                V(lambda e, hh=hh: e.tensor_tensor(out=hh[:], in0=hh[:], in1=modrow[:, 2, :], op=ALU.mult), [hhk, "modrow"], [hhk])
                V(lambda e, hh=hh, hb=hb: e.tensor_tensor(out=hb[:], in0=hh[:], in1=modrow[:, 1, :], op=ALU.add), [hhk, "modrow"], [hbk])
                DS(lambda e, t=t, hb=hb: e.dma_start(out=hrow_d[t * 128:(t + 1) * 128, :], in_=hb[:]), [hbk], ["hrow_d"])
                for kt in range(8):
                    T(lambda e, kt=kt, hb=hb, pT3=pT3: e.transpose(out=pT3[:, kt * 128:(kt + 1) * 128], in_=hb[:, kt * 128:(kt + 1) * 128],
                                                                   identity=ident_bf[:]), [hbk, "ident_bf"], [pTk])
                A(lambda e, hT=hT, pT3=pT3: e.activation(out=hT[:].rearrange("p a b -> p (a b)"), in_=pT3[:], func=AF.Identity), [pTk], [hTk])
                for kt in range(8):
                    T(lambda e, kt=kt, hT=hT, pL=pL: e.matmul(pL[:], lhsT=hT[:, kt, :], rhs=wr[:, kt, :], start=(kt == 0), stop=(kt == 7)),
                      [hTk, "wr"], [pLk])
                V(lambda e, mx=mx, pL=pL: e.tensor_reduce(out=mx[:], in_=pL[:], axis=AX.X, op=ALU.max), [pLk], [mxk])
                V(lambda e, mx=mx: e.tensor_scalar_mul(out=mx[:], in0=mx[:], scalar1=-1.0), [mxk], [mxk])
                A(lambda e, lg=lg, pL=pL, mx=mx: e.activation(out=lg[:], in_=pL[:], func=AF.Exp, bias=mx[:, 0:1]), [pLk, mxk], [lgk])
                V(lambda e, sm=sm, lg=lg: e.tensor_reduce(out=sm[:], in_=lg[:], axis=AX.X, op=ALU.add), [lgk], [smk])
                V(lambda e, sm=sm: e.reciprocal(out=sm[:], in_=sm[:]), [smk], [smk])
                V(lambda e, t=t, lg=lg, sm=sm: e.tensor_scalar_mul(out=aff[:, t, :], in0=lg[:], scalar1=sm[:, 0:1]), [lgk, smk], ["aff%d" % t])
                DS(lambda e, t=t: e.dma_start(out=aff_d[t * 128:(t + 1) * 128, :], in_=aff[:, t, :]), ["aff%d" % t], ["aff_d"])

            p3_loads(0)
            p3_loads(1)
            p3_A(0)
            emit_zip(record(p3_A, 1), record(p3_B1, 0))
            for t in range(NT):
                if t + 2 < NT:
                    p3_loads(t + 2)
                la = record(p3_A, t + 2) if t + 2 < NT else []
                lb1 = record(p3_B1, t + 1) if t + 1 < NT else []
                lb2 = record(p3_B2, t)
                emit_zip(la, lb1, lb2)
            P.barrier()

        if debug == 1:
            early.close()
            return nc

        with contextlib.ExitStack() as st:
            hs = sb(st, "hs", [128, 16])
            am = sb(st, "am", [128, NT, NE])
            lo = sb(st, "lo", [128, NE])
            hi = sb(st, "hi", [128, NE])
            mid = sb(st, "mid", [128, NE])
            cmp_ = sb(st, "cmp", [128, NT, NE])
            cnt = sb(st, "cnt", [128, NE])
            cntb = sb(st, "cntb", [128, NE], BF16)
            ge = sb(st, "ge", [128, NE])
            dl = sb(st, "dl", [128, NE])
            pC = ps(st, "pC", [128, NE])
            DS(lambda e: e.dma_start(out=hs[:], in_=half_sel), w=["hs"])
            V(lambda e: e.tensor_tensor(out=am[:], in0=aff[:, :, 0:8], in1=bc(hs[:, 0:8].unsqueeze(1), [128, NT, NE]), op=ALU.mult),
              ["aff", "hs"], ["am"])
            V(lambda e: e.tensor_tensor(out=cmp_[:], in0=aff[:, :, 8:16], in1=bc(hs[:, 8:16].unsqueeze(1), [128, NT, NE]), op=ALU.mult),
              ["aff", "hs"], ["cmp"])
            V(lambda e: e.tensor_tensor(out=am[:], in0=am[:], in1=cmp_[:], op=ALU.add), ["am", "cmp"], ["am"])
            V(lambda e: e.memset(lo[:], 0.0), w=["lo"])
            V(lambda e: e.memset(hi[:], 1.0), w=["hi"])
            for it in range(30):
                V(lambda e: e.tensor_tensor(out=mid[:], in0=lo[:], in1=hi[:], op=ALU.add), ["lo", "hi"], ["mid"])
                V(lambda e: e.tensor_scalar_mul(out=mid[:], in0=mid[:], scalar1=0.5), ["mid"], ["mid"])
                V(lambda e: e.tensor_tensor(out=cmp_[:], in0=am[:], in1=bc(mid[:].unsqueeze(1), [128, NT, NE]), op=ALU.is_ge),
                  ["am", "mid"], ["cmp"])
                V(lambda e: e.tensor_reduce(out=cnt[:], in_=cmp_[:].rearrange("p t e -> p e t"), axis=AX.X, op=ALU.add), ["cmp"], ["cnt"])
                V(lambda e: e.tensor_copy(out=cntb[:], in_=cnt[:]), ["cnt"], ["cntb"])
                T(lambda e: e.matmul(pC[:], lhsT=ones_bf[:], rhs=cntb[:], start=True, stop=True), ["ones", "cntb"], ["pC"])
                V(lambda e: e.tensor_single_scalar(out=ge[:], in_=pC[:], scalar=float(CAP), op=ALU.is_ge), ["pC"], ["ge"])
                V(lambda e: e.tensor_tensor(out=dl[:], in0=mid[:], in1=lo[:], op=ALU.subtract), ["mid", "lo"], ["dl"])
                V(lambda e: e.tensor_tensor(out=dl[:], in0=dl[:], in1=ge[:], op=ALU.mult), ["dl", "ge"], ["dl"])
                V(lambda e: e.tensor_tensor(out=lo[:], in0=lo[:], in1=dl[:], op=ALU.add), ["lo", "dl"], ["lo"])
                V(lambda e: e.tensor_tensor(out=dl[:], in0=hi[:], in1=mid[:], op=ALU.subtract), ["hi", "mid"], ["dl"])
                V(lambda e: e.tensor_tensor(out=dl[:], in0=dl[:], in1=ge[:], op=ALU.mult), ["dl", "ge"], ["dl"])
                V(lambda e: e.tensor_tensor(out=hi[:], in0=mid[:], in1=dl[:], op=ALU.add), ["mid", "dl"], ["hi"])
            mk = sb(st, "mk", [128, NE, NT], BF16)
            cin = sb(st, "cin", [128, NE * NT])
            tot = sb(st, "tot", [128, NE, NT])
            cend = sb(st, "cend", [128, NE, NT])
            pP = ps(st, "pP", [128, NE * NT])
            pTt = ps(st, "pTt", [128, NE * NT])
            V(lambda e: e.tensor_tensor(out=mk[:], in0=am[:].rearrange("p t e -> p e t"), in1=bc(lo[:].unsqueeze(2), [128, NE, NT]),
                                        op=ALU.is_ge), ["am", "lo"], ["mk"])
            T(lambda e: e.matmul(pP[:], lhsT=tri_bf[:], rhs=mk[:].rearrange("p e t -> p (e t)"), start=True, stop=True), ["tri", "mk"], ["pP"])
            T(lambda e: e.matmul(pTt[:], lhsT=ones_bf[:], rhs=mk[:].rearrange("p e t -> p (e t)"), start=True, stop=True), ["ones", "mk"], ["pTt"])
            V(lambda e: e.tensor_copy(out=cin[:], in_=pP[:]), ["pP"], ["cin"])
            V(lambda e: e.tensor_copy(out=tot[:].rearrange("p e t -> p (e t)"), in_=pTt[:]), ["pTt"], ["tot"])
            V(lambda e: e.tensor_copy(out=cend[:], in_=tot[:]), ["tot"], ["cend"])
            sh = 1
            tmpc = sb(st, "tmpc", [128, NE, NT])
            while sh < NT:
                V(lambda e: e.tensor_copy(out=tmpc[:], in_=cend[:]), ["cend"], ["tmpc"])
                V(lambda e, sh=sh: e.tensor_tensor(out=cend[:, :, sh:NT], in0=tmpc[:, :, sh:NT], in1=tmpc[:, :, 0:NT - sh], op=ALU.add),
                  ["tmpc"], ["cend"])
                sh *= 2
            cinT = sb(st, "cinT", [128, 4, 128])
            pX = ps(st, "pX", [128, 4, 128])
            for a in range(4):
                T(lambda e, a=a: e.transpose(out=pX[:, a, :], in_=cin[:, a * 128:(a + 1) * 128], identity=ident_f[:]), ["cin", "ident_f"], ["pX"])
            V(lambda e: e.tensor_copy(out=cinT[:], in_=pX[:]), ["pX"], ["cinT"])
            DS(lambda e: e.dma_start(out=cin_d.rearrange("(a p) t -> p a t", p=128), in_=cinT[:]), ["cinT"], ["cin_d"])
            spp = sb(st, "spp", [128, 8])
            DS(lambda e: e.dma_start(out=spp[:], in_=slot_pp_d), w=["spp"])
            le = sb(st, "le", [128, NE, 8, NT])
            tl_ = sb(st, "tl_", [128, NE, 8])
            cst = sb(st, "cst", [128, NE, 8])
            rr = sb(st, "rr", [128, NE, 8])
            rowi = sb(st, "rowi", [128, NE, 8], I32)
            rowf = sb(st, "rowf", [128, NE, 8])
            for e_ in range(NE):
                V(lambda e, e_=e_: e.tensor_tensor(out=le[:, e_, :, :], in0=bc(cend[:, e_, :].unsqueeze(1), [128, 8, NT]),
                                                   in1=bc(spp[:].unsqueeze(2), [128, 8, NT]), op=ALU.is_le), ["cend", "spp"], ["le"])
            V(lambda e: e.tensor_reduce(out=tl_[:].rearrange("p e j -> p (e j)"), in_=le[:].rearrange("p e j t -> p (e j) t"),
                                        axis=AX.X, op=ALU.add), ["le"], ["tl_"])
            for e_ in range(NE):
                V(lambda e, e_=e_: e.tensor_tensor(out=le[:, e_, :, :], in0=le[:, e_, :, :], in1=bc(tot[:, e_, :].unsqueeze(1), [128, 8, NT]),
                                                   op=ALU.mult), ["le", "tot"], ["le"])
            V(lambda e: e.tensor_reduce(out=cst[:].rearrange("p e j -> p (e j)"), in_=le[:].rearrange("p e j t -> p (e j) t"),
                                        axis=AX.X, op=ALU.add), ["le"], ["cst"])
            V(lambda e: e.tensor_scalar_min(out=tl_[:], in0=tl_[:], scalar1=float(NT - 1)), ["tl_"], ["tl_"])
            V(lambda e: e.tensor_tensor(out=rr[:], in0=bc(spp[:].unsqueeze(1), [128, NE, 8]), in1=cst[:], op=ALU.subtract), ["spp", "cst"], ["rr"])
            for e_ in range(NE):
                V(lambda e, e_=e_: e.tensor_scalar_add(out=rowf[:, e_, :], in0=tl_[:, e_, :], scalar1=float(e_ * NT)), ["tl_"], ["rowf"])
            V(lambda e: e.tensor_copy(out=rowi[:], in_=rowf[:]), ["rowf"], ["rowi"])
            crowA = [sb(st, "crow%d" % i, [128, 128]) for i in range(4)]
            cleA = [sb(st, "cle%d" % i, [128, 128]) for i in range(4)]
            cle = cleA[0]
            tloc = sb(st, "tloc", [128, NE, 8])
            arowA = [sb(st, "arow%d" % i, [128, 16]) for i in range(4)]
            cl2A = [sb(st, "cl2%d" % i, [128, 16]) for i in range(4)]
            idf = sb(st, "idf", [128, NE, 8])
            V(lambda e: e.memset(tloc[:], 0.0), w=["tloc"])
            for e_ in range(NE):
                for j in range(8):
                    q4 = (e_ * 8 + j) % 4
                    crow, crk = crowA[q4], "crow%d" % q4
                    cle_, clk = cleA[q4], "cle%d" % q4
                    DG(lambda e, e_=e_, j=j, crow=crow: e.indirect_dma_start(out=crow[:], out_offset=None, in_=cin_d,
                                                                            in_offset=bass.IndirectOffsetOnAxis(ap=rowi[:, e_, j:j + 1], axis=0)),
                       ["cin_d", "rowi"], [crk])
                    V(lambda e, e_=e_, j=j, crow=crow, cle_=cle_: e.tensor_scalar(out=cle_[:], in0=crow[:], scalar1=rr[:, e_, j:j + 1], scalar2=0.0,
                                                                                  op0=ALU.is_le, op1=ALU.add, accum_out=tloc[:, e_, j:j + 1]),
                      [crk, "rr", "tloc"], [clk, "tloc%d_%d" % (e_, j)])
            V(lambda e: e.tensor_scalar_min(out=tloc[:], in0=tloc[:], scalar1=127.0),
              ["tloc"] + ["tloc%d_%d" % (a_, b_) for a_ in range(NE) for b_ in range(8)], ["tloc"])
            V(lambda e: e.scalar_tensor_tensor(out=idf[:], in0=tl_[:], scalar=128.0, in1=tloc[:], op0=ALU.mult, op1=ALU.add),
              ["tl_", "tloc"] + ["tloc%d_%d" % (a_, b_) for a_ in range(NE) for b_ in range(8)], ["idf"])
            V(lambda e: e.tensor_copy(out=idx_all[:], in_=idf[:]), ["idf"], ["idx_all"])
            for e_ in range(NE):
                for j in range(8):
                    q4 = (e_ * 8 + j) % 4
                    arow, ark = arowA[q4], "arow%d" % q4
                    cl2, c2k = cl2A[q4], "cl2%d" % q4
                    DG(lambda e, e_=e_, j=j, arow=arow: e.indirect_dma_start(out=arow[:], out_offset=None, in_=aff_d,
                                                                            in_offset=bass.IndirectOffsetOnAxis(ap=idx_all[:, e_, j:j + 1], axis=0)),
                       ["aff_d", "idx_all"], [ark])
                    V(lambda e, arow=arow, cl2=cl2: e.tensor_tensor(out=cl2[:], in0=arow[:], in1=hs[:], op=ALU.mult), [ark, "hs"], [c2k])
                    V(lambda e, e_=e_, j=j, cl2=cl2: e.tensor_tensor(out=gate_all[:, e_, j:j + 1], in0=cl2[:, e_:e_ + 1], in1=cl2[:, 8 + e_:9 + e_],
                                                                     op=ALU.add), [c2k], ["gate_all%d_%d" % (e_, j)])
            P.barrier()

        if debug:
            DS(lambda e: e.dma_start(out=dbg_idx, in_=idx_all[:].rearrange("p a b -> p (a b)")), ["idx_all"], ["dbg_idx"])
            DS(lambda e: e.dma_start(out=dbg_gate, in_=gate_all[:].rearrange("p a b -> p (a b)")), ["gate_all"], ["dbg_gate"])
            P.barrier()
        if debug == 2:
            early.close()
            return nc

        early.close()
        FB = 256
        NFB = DFF // FB
        with contextlib.ExitStack() as st:
            xgA = [sb(st, "xg%d" % i, [128, D], BF16) for i in range(2)]
            xgT = sb(st, "xgT", [128, 8, CAP], BF16)
            wdn = sb(st, "wdn", [128, NFT, D], BF16)
            hid = sb(st, "hid", [128, NFT, CAP], BF16)
            stg = [sb(st, "wstg%d" % i, [128, 2, 8, FB]) for i in range(2)]
            wgbA = [sb(st, "wgb%d" % i, [128, 8, FB], BF16) for i in range(2)]
            wubA = [sb(st, "wub%d" % i, [128, 8, FB], BF16) for i in range(2)]
            stgdA = [sb(st, "stgd%d" % i, [128, D]) for i in range(2)]
            slA = [sb(st, "sl%d" % i, [128, 512]) for i in range(2)]
            obA = [sb(st, "ob%d" % i, [128, D]) for i in range(2)]
            pTg = ps(st, "pTg", [128, D], BF16)
            pGU = [[ps(st, "pGU%d%d" % (h_, m_), [128, 512]) for m_ in range(2)] for h_ in range(2)]
            pO = ps(st, "pO", [128, D])
            gcnt = 0
            ocnt = 0
            for e_ in range(NE):
                for j in range(8):
                    xg, xgk = xgA[gcnt % 2], "xg%d" % (gcnt % 2)
                    gcnt += 1
                    DG(lambda e, e_=e_, j=j, xg=xg: e.indirect_dma_start(out=xg[:], out_offset=None, in_=hrow_d,
                                                                        in_offset=bass.IndirectOffsetOnAxis(ap=idx_all[:, e_, j:j + 1], axis=0)),
                       ["hrow_d", "idx_all"], [xgk])
                    for kt in range(8):
                        T(lambda e, kt=kt, xg=xg: e.transpose(out=pTg[:, kt * 128:(kt + 1) * 128], in_=xg[:, kt * 128:(kt + 1) * 128],
                                                              identity=ident_bf[:]), [xgk, "ident_bf"], ["pTg"])
                    V(lambda e, j=j: e.tensor_copy(out=xgT[:, :, j * 128:(j + 1) * 128], in_=pTg[:].rearrange("p (a b) -> p a b", a=8)),
                      ["pTg"], ["xgT"])
                for fb in range(NFB):
                    s_, sk = stg[fb % 2], "wstg%d" % (fb % 2)
                    wgb, wgk = wgbA[fb % 2], "wgb%d" % (fb % 2)
                    wub, wuk = wubA[fb % 2], "wub%d" % (fb % 2)
                    DS(lambda e, e_=e_, fb=fb, s_=s_: e.dma_start(out=s_[:, 0, :, :],
                                                                  in_=wg[e_].rearrange("(kt p) f -> p kt f", p=128)[:, :, fb * FB:(fb + 1) * FB]),
                       w=[sk + "g"])
                    DS(lambda e, e_=e_, fb=fb, s_=s_: e.dma_start(out=s_[:, 1, :, :],
                                                                  in_=wu[e_].rearrange("(kt p) f -> p kt f", p=128)[:, :, fb * FB:(fb + 1) * FB]),
                       w=[sk + "u"])
                    A(lambda e, s_=s_, wgb=wgb: e.activation(out=wgb[:], in_=s_[:, 0, :, :], func=AF.Identity), [sk + "g"], [wgk])
                    G(lambda e, s_=s_, wub=wub: e.tensor_copy(out=wub[:], in_=s_[:, 1, :, :]), [sk + "u"], [wuk])
                    if fb < 2 * 0 + NFB:
                        for q_ in range(2):
                            ft = fb * 2 + q_
                            sd_, sdk = stgdA[ft % 2], "stgd%d" % (ft % 2)
                            DS(lambda e, e_=e_, ft=ft, sd_=sd_: e.dma_start(out=sd_[:], in_=wd[e_, ft * 128:(ft + 1) * 128, :]), w=[sdk])
                            G(lambda e, ft=ft, sd_=sd_: e.tensor_copy(out=wdn[:, ft, :], in_=sd_[:]), [sdk], ["wdn"])
                    for fl in range(FB // 128):
                        ft = fb * (FB // 128) + fl
                        for h_ in range(2):
                            c0 = h_ * 512
                            pg, pu = pGU[h_][0], pGU[h_][1]
                            pgk, puk = "pGU%d0" % h_, "pGU%d1" % h_
                            sl, slk = slA[h_], "sl%d" % h_
                            for kt in range(8):
                                T(lambda e, kt=kt, c0=c0, fl=fl, pg=pg, wgb=wgb: e.matmul(pg[:], lhsT=wgb[:, kt, fl * 128:(fl + 1) * 128],
                                                                                        rhs=xgT[:, kt, c0:c0 + 512], start=(kt == 0), stop=(kt == 7)),
                                  [wgk, "xgT"], [pgk])
                            for kt in range(8):
                                T(lambda e, kt=kt, c0=c0, fl=fl, pu=pu, wub=wub: e.matmul(pu[:], lhsT=wub[:, kt, fl * 128:(fl + 1) * 128],
                                                                                        rhs=xgT[:, kt, c0:c0 + 512], start=(kt == 0), stop=(kt == 7)),
                                  [wuk, "xgT"], [puk])
                            A(lambda e, sl=sl, pg=pg: e.activation(out=sl[:], in_=pg[:], func=AF.Silu), [pgk], [slk])
                            V(lambda e, ft=ft, c0=c0, sl=sl, pu=pu: e.tensor_tensor(out=hid[:, ft, c0:c0 + 512], in0=sl[:], in1=pu[:], op=ALU.mult),
                              [slk, puk], ["hid"])
                for j in range(8):
                    ob, obk = obA[ocnt % 2], "ob%d" % (ocnt % 2)
                    ocnt += 1
                    for nh in range(2):
                        for ft in range(NFT):
                            T(lambda e, j=j, nh=nh, ft=ft: e.matmul(pO[:, nh * 512:(nh + 1) * 512], lhsT=hid[:, ft, j * 128:(j + 1) * 128],
                                                                    rhs=wdn[:, ft, nh * 512:(nh + 1) * 512], start=(ft == 0), stop=(ft == NFT - 1)),
                              ["hid", "wdn"], ["pO"])
                    V(lambda e, e_=e_, j=j, ob=ob: e.tensor_scalar_mul(out=ob[:], in0=pO[:], scalar1=gate_all[:, e_, j:j + 1]), ["pO", "gate_all"], [obk])
                    DG(lambda e, e_=e_, j=j, ob=ob: e.indirect_dma_start(out=moe_d, out_offset=bass.IndirectOffsetOnAxis(ap=idx_all[:, e_, j:j + 1], axis=0),
                                                                        in_=ob[:], in_offset=None, compute_op=ALU.add, oob_is_err=True),
                       [obk, "idx_all", "moe_d"], ["moe_d"])
            P.barrier()

        if debug == 3:
            return nc

        ccsem = gst.enter_context(nc.semaphore("ccsem"))
        CCH = 16
        crow_ = SEQ // CCH
        for cc_ in range(CCH):
            nc.gpsimd.collective_compute("AllReduce", ALU.add, replica_groups=[[0, 1], [2, 3], [4, 5], [6, 7]],
                                         ins=[moe_d[cc_ * crow_:(cc_ + 1) * crow_, :]],
                                         outs=[moe_r[cc_ * crow_:(cc_ + 1) * crow_, :]]).then_inc(ccsem)
            nc.gpsimd.wait_ge(ccsem, cc_ + 1)
        G(lambda e: e.memset(gate_all[:, 0, 0:1], 0.0), w=["gate_all"])
        P.barrier()
        with contextlib.ExitStack() as st:
            mt_ = [sb(st, "m6_%d" % i, [128, D]) for i in range(2)]
            xq = [sb(st, "x6_%d" % i, [128, D]) for i in range(2)]
            tm = sb(st, "tm6", [128, D])
            xr = sb(st, "xr6", [128, D])
            oo = [sb(st, "o6_%d" % i, [128, D]) for i in range(2)]
            stats = sb(st, "stats6", [128, 2, 6])
            mv = sb(st, "mv6_", [128, 2])
            rstd = sb(st, "rstd6", [128, 1])
            tki = sb(st, "tki", [128, NT // 2], I32)
            DS(lambda e: e.dma_start(out=tki[:], in_=tokidx_d), w=["tki"])
            for t in range(NT // 2):
                m_ = mt_[t % 2]
                mk_ = "m6_%d" % (t % 2)
                x_ = xq[t % 2]
                xk = "x6_%d" % (t % 2)
                o_ = oo[t % 2]
                ok = "o6_%d" % (t % 2)
                DG(lambda e, t=t, m_=m_: e.indirect_dma_start(out=m_[:], out_offset=None, in_=moe_r,
                                                             in_offset=bass.IndirectOffsetOnAxis(ap=tki[:, t:t + 1], axis=0)),
                   ["moe_r", "tki"], [mk_])
                DG(lambda e, t=t, x_=x_: e.indirect_dma_start(out=x_[:], out_offset=None, in_=xnew_d,
                                                             in_offset=bass.IndirectOffsetOnAxis(ap=tki[:, t:t + 1], axis=0)),
                   ["xnew_d", "tki"], [xk])
                V(lambda e, m_=m_: e.tensor_tensor(out=tm[:], in0=m_[:], in1=fin3[:, 0, :], op=ALU.mult), [mk_, "fin3"], ["tm6"])
                V(lambda e, x_=x_: e.scalar_tensor_tensor(out=xr[:], in0=x_[:], scalar=ALPHA, in1=tm[:], op0=ALU.mult, op1=ALU.add),
                  [xk, "tm6"], ["xr6"])
                for c in range(2):
                    V(lambda e, c=c: e.bn_stats(out=stats[:, c, :], in_=xr[:, c * 512:(c + 1) * 512]), ["xr6"], ["stats6"])
                V(lambda e: e.bn_aggr(out=mv[:], in_=stats[:].rearrange("p a b -> p (a b)")), ["stats6"], ["mv6_"])
                V(lambda e: e.tensor_scalar_add(out=rstd[:], in0=mv[:, 1:2], scalar1=EPS), ["mv6_"], ["rstd6"])
                A(lambda e: e.activation(out=rstd[:], in_=rstd[:], func=AF.Sqrt), ["rstd6"], ["rstd6"])
                V(lambda e: e.reciprocal(out=rstd[:], in_=rstd[:]), ["rstd6"], ["rstd6"])
                V(lambda e: e.tensor_scalar(out=tm[:], in0=xr[:], scalar1=mv[:, 0:1], scalar2=rstd[:, 0:1],
                                            op0=ALU.subtract, op1=ALU.mult), ["xr6", "mv6_", "rstd6"], ["tm6"])
                V(lambda e: e.tensor_tensor(out=tm[:], in0=tm[:], in1=fin3[:, 1, :], op=ALU.mult), ["tm6", "fin3"], ["tm6"])
                V(lambda e, o_=o_: e.tensor_tensor(out=o_[:], in0=tm[:], in1=fin3[:, 2, :], op=ALU.add), ["tm6", "fin3"], [ok])
                DS(lambda e, t=t, o_=o_: e.dma_start(out=out_d[t * 128:(t + 1) * 128, :], in_=o_[:]), [ok], ["out_d"])
            P.barrier()
    return nc


def _prep(inputs):
    f32 = np.float32
    g = {k: np.asarray(v) for k, v in inputs.items()}
    bf = ml_dtypes.bfloat16
    com = {}
    com["w_ada"] = np.ascontiguousarray(g["w_ada"][0], f32)
    ba = g["b_ada"][0]
    com["bada_fm"] = np.ascontiguousarray(ba[:2048].reshape(16, 128).T, f32)
    com["bada_row"] = np.ascontiguousarray(ba[2048:].reshape(1, 4096), f32)
    w_in = g["w_in"][0]
    ws5 = np.zeros((D, 4, 4, 32), f32)
    ws5[:, :, :, :16] = w_in[:, :256].reshape(D, 4, 4, 16)
    com["w_in_s5"] = ws5.reshape(D, 512)
    com["w_in_u"] = np.ascontiguousarray(w_in[:, 256:1024], f32)
    com["w_in_v"] = np.ascontiguousarray(w_in[:, 1024:1792], f32)
    com["gm_wsT"] = np.ascontiguousarray(g["gm_ws"][0].transpose(2, 0, 1), f32)
    com["gm_bs_row"] = np.ascontiguousarray(g["gm_bs"][0].reshape(1, 768), f32)
    are = g["s5_a_re"][0]; aim = g["s5_a_im"][0]; ls = g["s5_log_step"][0]
    com["are_row"] = np.ascontiguousarray(are.reshape(1, 2048), f32)
    com["aim_row"] = np.ascontiguousarray(aim.reshape(1, 2048), f32)
    com["ls_row"] = np.ascontiguousarray(np.repeat(ls.reshape(32), 64).reshape(1, 2048), f32)

    def pp(a):
        return np.ascontiguousarray(a.reshape(2, 8, 2, 64).transpose(2, 3, 0, 1).reshape(128, 16), f32)
    com["are_pp"] = pp(are)
    com["aim_pp"] = pp(aim)
    com["ls_pp"] = pp(np.repeat(ls[:, :, None], 64, axis=2))
    for nm, src in (("WBre_raw", g["s5_b_re"][0]), ("WBim_raw", g["s5_b_im"][0])):
        w = np.zeros((128, 2, 8, 128), f32)
        for gp in range(8):
            for g2 in range(2):
                gi = 2 * gp + g2
                r0 = 64 * (gp % 2) + 32 * g2
                w[r0:r0 + 16, :, gp, 64 * g2:64 * g2 + 64] = src[:, gi].transpose(2, 0, 1)
        com[nm] = w.reshape(128, 2048)
    for nm, src in (("WCre_raw", g["s5_c_re"][0]), ("WCim_raw", g["s5_c_im"][0])):
        w = np.zeros((128, 2, 8, 128), f32)
        for gp in range(8):
            for g2 in range(2):
                gi = 2 * gp + g2
                c0 = 64 * (gp % 2) + 32 * g2
                w[64 * g2:64 * g2 + 64, :, gp, c0:c0 + 16] = src[:, gi].transpose(2, 0, 1)
        com[nm] = w.reshape(128, 2048)

    def padpp(v):
        o = np.zeros((4, 4, 32), f32)
        o[:, :, :16] = v.reshape(4, 4, 16)
        return np.ascontiguousarray(o.reshape(4, 128).T, f32)
    com["dpp"] = padpp(g["s5_d"][0])
    com["b_glu_pp"] = padpp(g["s5_b_glu"][0].reshape(16, 16))
    wgl = np.zeros((4, 4, 32, 4, 4, 32), f32)
    wgl[:, :, :16, :, :, :16] = g["s5_w_glu"][0].reshape(4, 4, 16, 4, 4, 16)
    com["w_glu_pad"] = wgl.reshape(512, 512)
    wo = np.zeros((1280, D), f32)
    wo5 = np.zeros((4, 4, 32, D), f32)
    wo5[:, :, :16, :] = g["w_out"][0][:256].reshape(4, 4, 16, D)
    wo[:512] = wo5.reshape(512, D)
    wo[512:] = g["w_out"][0][256:]
    com["w_out_pad"] = wo
    for k in ("ln1_g", "ln1_b", "ln2_g", "ln2_b"):
        com[k] = np.ascontiguousarray(g[k][0].reshape(1, D), f32)
    com["w_router"] = np.ascontiguousarray(g["w_router"][0], f32)
    com["ident_bf"] = np.eye(128, dtype=f32).astype(bf)
    com["ident_f"] = np.eye(128, dtype=f32)
    com["tri"] = np.triu(np.ones((128, 128), f32)).astype(bf)
    com["ones"] = np.ones((128, 128), f32).astype(bf)
    com["iota_row"] = np.arange(1024, dtype=f32).reshape(1, 1024)
    com["slot_pp"] = (np.arange(128, dtype=f32)[:, None] + 128.0 * np.arange(8, dtype=f32)[None, :]).astype(f32)
    maps = []
    for c in range(8):
        b, half = c // 2, c % 2
        m = dict(com)
        m["xs"] = np.ascontiguousarray(np.concatenate([g["ctx"][b], g["x"][b]], axis=0), f32)
        cv = np.stack([g["c"][b], g["c_ctx"]], axis=1)
        m["cT"] = np.ascontiguousarray(cv.reshape(8, 128, 2).transpose(1, 0, 2), f32)
        es = slice(8 * half, 8 * half + 8)
        m["wg"] = np.ascontiguousarray(g["moe_w_gate"][0][es], f32)
        m["wu"] = np.ascontiguousarray(g["moe_w_up"][0][es], f32)
        m["wd"] = np.ascontiguousarray(g["moe_w_down"][0][es], f32)
        hs = np.zeros((128, 16), f32)
        hs[:, es] = 1.0
        m["half_sel"] = hs
        m["tokidx"] = (half * 4096 + np.arange(32, dtype=np.int32)[None, :] * 128 + np.arange(128, dtype=np.int32)[:, None]).astype(np.int32)
        maps.append(m)
    return maps


def kernel(**inputs):
    maps = _prep(inputs)
    nc = build()
    res = run_bass_kernel_spmd(nc, maps, core_ids=list(range(8)))
    out = np.zeros((4, SEQ, D), np.float32)
    for c in range(8):
        b, half = c // 2, c % 2
        out[b, half * 4096:(half + 1) * 4096] = res.results[c]["out"]
    return out
```
